# Optimizing a Trainium2 kernel written in Bass

```python
import math
import jax, jax.numpy as jnp
from jax import lax
import numpy as np

D_MODEL = 1024
BATCH = 16
SEQ = 2048
DEPTH = 2
DEC_BATCH = 8
DEC_SEQ = 16
PAST_LEN = 1024

CHUNK = 64
N_EVEN = (DEPTH + 1) // 2
N_ODD = DEPTH // 2
S5_WIDTH = D_MODEL // 2
S5_GROUP = 16
S5_GROUPS = S5_WIDTH // S5_GROUP
S5_STATE = 64
GDN_HEADS = 4
GDN_DK = 128
GDN_DV = 128
GDN_CONV = 4
GDN_QKV = GDN_HEADS * (2 * GDN_DK + GDN_DV)
RET_HEADS = 4
RET_DK = D_MODEL // RET_HEADS
RET_DV = 2 * D_MODEL // RET_HEADS
ROPE_BASE = 10000.0
N_EXPERTS = 32
TOP_K = 4
D_FF = D_MODEL
SWIGLU_LIMIT = 7.0
SWIGLU_ALPHA = 1.702
EXPERT_BLOCK = 256
PLE_DIM = 256
DEEPNORM_ALPHA = (2 * DEPTH) ** 0.25
DEEPNORM_BETA = (8 * DEPTH) ** -0.25
LN_EPS = 1e-5
NORM_EPS = 1e-6
EVEN_IN = S5_WIDTH + GDN_QKV + GDN_HEADS * GDN_DV + 2 * GDN_HEADS
EVEN_MIX = S5_WIDTH + GDN_HEADS * GDN_DV
ODD_IN = 2 * RET_HEADS * RET_DK + 2 * RET_HEADS * RET_DV

kernel_name = 'hybrid_s5_gdn_retention_moe_stream_step'

F32 = jnp.float32


def layer_norm(x, g, b):
    xf = x.astype(F32)
    mu = jnp.mean(xf, -1, keepdims=True)
    var = jnp.mean(jnp.square(xf - mu), -1, keepdims=True)
    return (xf - mu) * lax.rsqrt(var + LN_EPS) * g + b


def l2norm(x):
    return x * lax.rsqrt(jnp.sum(x * x, -1, keepdims=True) + NORM_EPS)


def to_chunks(x, lc):
    return x.reshape(x.shape[0], x.shape[1] // lc, lc, *x.shape[2:])


def complex_affine(e1, e2):
    a1r, a1i, b1r, b1i = e1
    a2r, a2i, b2r, b2i = e2
    return (a2r * a1r - a2i * a1i, a2r * a1i + a2i * a1r,
            a2r * b1r - a2i * b1i + b2r, a2r * b1i + a2i * b1r + b2i)


def s5_scan(u, h0_re, h0_im, a_re, a_im, log_dt, b_re, b_im, c_re, c_im, d_skip, lc):
    bsz, L, G, N = u.shape
    dt = jnp.exp(log_dt)[:, None]
    lr, li = a_re * dt, a_im * dt
    mag = jnp.exp(lr)
    ab_re, ab_im = mag * jnp.cos(li), mag * jnp.sin(li)
    den = a_re * a_re + a_im * a_im
    cf_re = ((ab_re - 1.0) * a_re + ab_im * a_im) / den
    cf_im = (ab_im * a_re - (ab_re - 1.0) * a_im) / den
    bb_re = cf_re[..., None] * b_re - cf_im[..., None] * b_im
    bb_im = cf_re[..., None] * b_im + cf_im[..., None] * b_re
    t = jnp.arange(1, lc + 1, dtype=F32)[:, None, None]
    pw_mag = jnp.exp(lr[None] * t)
    pw_re, pw_im = pw_mag * jnp.cos(li[None] * t), pw_mag * jnp.sin(li[None] * t)
    uc = jnp.swapaxes(to_chunks(u, lc), 0, 1)

    def step(carry, uk):
        h_re, h_im = carry
        bu_re = jnp.einsum('blgn,gpn->blgp', uk, bb_re)
        bu_im = jnp.einsum('blgn,gpn->blgp', uk, bb_im)
        a_full_re = jnp.broadcast_to(ab_re, bu_re.shape)
        a_full_im = jnp.broadcast_to(ab_im, bu_re.shape)
        _, _, s_re, s_im = lax.associative_scan(complex_affine, (a_full_re, a_full_im, bu_re, bu_im), axis=1)
        s_re = s_re + pw_re * h_re[:, None] - pw_im * h_im[:, None]
        s_im = s_im + pw_re * h_im[:, None] + pw_im * h_re[:, None]
        y = jnp.einsum('gnp,blgp->blgn', c_re, s_re) - jnp.einsum('gnp,blgp->blgn', c_im, s_im)
        return (s_re[:, -1], s_im[:, -1]), y

    (h_re, h_im), ys = lax.scan(step, (h0_re, h0_im), uc)
    y = jnp.swapaxes(ys, 0, 1).reshape(bsz, L, G, N) + d_skip.reshape(G, N) * u
    return y, h_re, h_im


def causal_conv(x, ctx, w):
    xp = jnp.concatenate([ctx, x], axis=1)
    C = x.shape[-1]
    y = lax.conv_general_dilated(xp, w[:, None, :].astype(xp.dtype), window_strides=(1,), padding='VALID',
                                 dimension_numbers=('NWC', 'WIO', 'NWC'), feature_group_count=C)
    return y, xp[:, -(GDN_CONV - 1):]


def gated_delta(q, k, v, beta, g, s0, lc):
    bsz, L, H, dv = v.shape
    ch = lambda t: jnp.moveaxis(to_chunks(t, lc), 3, 2)
    qc, kc, vc, bc, gc = ch(q), ch(k), ch(v), ch(beta), ch(g)
    G = jnp.cumsum(gc, axis=-1)
    diff = G[..., :, None] - G[..., None, :]
    idx = jnp.arange(lc)
    incl = idx[:, None] >= idx[None, :]
    strict = idx[:, None] > idx[None, :]
    dec = jnp.where(incl, jnp.exp(jnp.where(incl, diff, 0.0)), 0.0)
    kb = kc * bc[..., None]
    tri = jnp.eye(lc, dtype=F32) + jnp.where(strict, jnp.einsum('bchid,bchjd->bchij', kb, kc) * dec, 0.0)
    rhs = jnp.concatenate([vc * bc[..., None], kb * jnp.exp(G)[..., None]], axis=-1)
    sol = lax.linalg.triangular_solve(tri, rhs, left_side=True, lower=True, unit_diagonal=True)
    u0, w = sol[..., :dv], sol[..., dv:]
    attn = jnp.einsum('bchid,bchjd->bchij', qc, kc) * dec
    q_dec = qc * jnp.exp(G)[..., None]
    g_last = G[..., -1]
    k_dec = kc * jnp.exp(g_last[..., None] - G)[..., None]
    xs = tuple(jnp.moveaxis(t, 1, 0) for t in (u0, w, attn, q_dec, k_dec, jnp.exp(g_last)))

    def step(S, inp):
        u0k, wk, ak, qk, kk, dk = inp
        u = u0k - jnp.einsum('bhik,bhkv->bhiv', wk, S)
        o = jnp.einsum('bhik,bhkv->bhiv', qk, S) + jnp.einsum('bhij,bhjv->bhiv', ak, u)
        S = dk[..., None, None] * S + jnp.einsum('bhik,bhiv->bhkv', kk, u)
        return S, o

    S, os_ = lax.scan(step, s0, xs)
    o = jnp.transpose(os_, (1, 0, 3, 2, 4)).reshape(bsz, L, H, dv)
    return o, S


def rotary(x, pos):
    d = x.shape[-1]
    freq = 1.0 / (ROPE_BASE ** jnp.linspace(0.0, 1.0, d // 2, dtype=F32))
    ang = pos[:, None] * freq[None]
    cos, sin = jnp.cos(ang)[None, :, None, :], jnp.sin(ang)[None, :, None, :]
    x2 = x.reshape(*x.shape[:-1], d // 2, 2)
    x0, x1 = x2[..., 0], x2[..., 1]
    return jnp.stack([x0 * cos - x1 * sin, x0 * sin + x1 * cos], axis=-1).reshape(x.shape)


def retention(q, k, v, r0, lc):
    bsz, L, H, dv = v.shape
    log_g = jnp.log(1.0 - 2.0 ** (-5.0 - jnp.arange(H, dtype=F32)))
    idx = jnp.arange(lc, dtype=F32)
    dec_intra = jnp.exp(log_g[:, None, None] * jnp.abs(idx[:, None] - idx[None, :]))
    q_scale = jnp.exp(log_g[:, None] * (idx + 1.0))
    k_scale = jnp.exp(log_g[:, None] * (lc - 1.0 - idx))
    c_dec = jnp.exp(log_g * lc)
    ch = lambda t: jnp.transpose(to_chunks(t, lc), (1, 0, 3, 2, 4))

    def step(R, inp):
        qk, kk, vk = inp
        s = jnp.einsum('bhid,bhjd->bhij', qk, kk) * dec_intra
        o = jnp.einsum('bhij,bhjv->bhiv', s, vk) + jnp.einsum('bhid,bhdv->bhiv', qk * q_scale[..., None], R)
        R = c_dec[:, None, None] * R + jnp.einsum('bhjd,bhjv->bhdv', kk * k_scale[..., None], vk)
        return R, o

    R, os_ = lax.scan(step, r0, (ch(q), ch(k), ch(v)))
    o = jnp.transpose(os_, (1, 0, 3, 2, 4)).reshape(bsz, L, H, dv)
    return o, R


def even_mixer(x, h_re, h_im, s_gdn, conv_ctx, lc, w_in, a_re, a_im, log_dt, b_re, b_im, c_re, c_im,
               d_skip, w_glu, b_glu, conv_w, a_log, dt_bias, norm_w, w_out):
    bsz, L, _ = x.shape
    proj = x @ w_in
    o1 = S5_WIDTH
    o2 = o1 + GDN_QKV
    o3 = o2 + GDN_HEADS * GDN_DV
    o4 = o3 + GDN_HEADS
    u, qkv, z, b_raw, a_raw = proj[..., :o1], proj[..., o1:o2], proj[..., o2:o3], proj[..., o3:o4], proj[..., o4:]
    yA, h_re, h_im = s5_scan(u.reshape(bsz, L, S5_GROUPS, S5_GROUP), h_re, h_im, a_re, a_im, log_dt,
                             b_re, b_im, c_re, c_im, d_skip, lc)
    yA = jax.nn.gelu(yA.reshape(bsz, L, S5_WIDTH))
    yA = yA * jax.nn.sigmoid(yA @ w_glu + b_glu)
    qkv, conv_new = causal_conv(qkv, conv_ctx, conv_w)
    qkv = jax.nn.silu(qkv)
    nq = GDN_HEADS * GDN_DK
    q = l2norm(qkv[..., :nq].reshape(bsz, L, GDN_HEADS, GDN_DK)) * (GDN_DK ** -0.5)
    k = l2norm(qkv[..., nq:2 * nq].reshape(bsz, L, GDN_HEADS, GDN_DK))
    v = qkv[..., 2 * nq:].reshape(bsz, L, GDN_HEADS, GDN_DV)
    beta = jax.nn.sigmoid(b_raw)
    g = -jnp.exp(a_log) * jax.nn.softplus(a_raw + dt_bias)
    o, S = gated_delta(q, k, v, beta, g, s_gdn, lc)
    o = o * lax.rsqrt(jnp.mean(o * o, -1, keepdims=True) + NORM_EPS) * norm_w
    o = o * jax.nn.silu(z.reshape(bsz, L, GDN_HEADS, GDN_DV))
    yB = o.reshape(bsz, L, GDN_HEADS * GDN_DV)
    out = jnp.concatenate([yA, yB], axis=-1) @ w_out
    return out, h_re, h_im, S, conv_new


def odd_mixer(x, r0, pos, lc, w_in, w_out):
    bsz, L, _ = x.shape
    proj = x @ w_in
    nk = RET_HEADS * RET_DK
    nv = RET_HEADS * RET_DV
    q = rotary(proj[..., :nk].reshape(bsz, L, RET_HEADS, RET_DK), pos)
    k = rotary(proj[..., nk:2 * nk].reshape(bsz, L, RET_HEADS, RET_DK), pos) * (RET_DK ** -0.5)
    v = proj[..., 2 * nk:2 * nk + nv].reshape(bsz, L, RET_HEADS, RET_DV)
    gate = proj[..., 2 * nk + nv:]
    o, R = retention(q, k, v, r0, lc)
    mu = jnp.mean(o, -1, keepdims=True)
    var = jnp.mean(jnp.square(o - mu), -1, keepdims=True)
    o = ((o - mu) * lax.rsqrt(var + LN_EPS)).reshape(bsz, L, nv)
    return (jax.nn.silu(gate) * o) @ w_out, R


def moe(x, router_w, router_b, w1, b1, w2, b2):
    bsz, L, D = x.shape
    xt = x.reshape(-1, D)
    T = xt.shape[0]
    logits = (xt @ router_w + router_b).astype(F32)
    top_v, top_i = lax.top_k(logits, TOP_K)
    gates = jax.nn.softmax(top_v, axis=-1)
    flat_e = top_i.reshape(-1)
    flat_g = gates.reshape(-1)
    order = jnp.argsort(flat_e)
    se = flat_e[order]
    counts = jnp.zeros((N_EXPERTS,), jnp.int32).at[flat_e].add(1)
    padded = (counts + EXPERT_BLOCK - 1) // EXPERT_BLOCK * EXPERT_BLOCK
    pend = jnp.cumsum(padded)
    pstart = pend - padded
    start = jnp.cumsum(counts) - counts
    dest = pstart[se] + (jnp.arange(T * TOP_K, dtype=jnp.int32) - start[se])
    n_pad = -(-(T * TOP_K + N_EXPERTS * (EXPERT_BLOCK - 1)) // EXPERT_BLOCK) * EXPERT_BLOCK
    n_blk = n_pad // EXPERT_BLOCK
    src = jnp.full((n_pad,), T, jnp.int32).at[dest].set((order // TOP_K).astype(jnp.int32))
    gate_pad = jnp.zeros((n_pad,), F32).at[dest].set(flat_g[order])
    blk_start = jnp.arange(n_blk, dtype=jnp.int32) * EXPERT_BLOCK
    blk_e = jnp.minimum(jnp.searchsorted(pend, blk_start, side='right'), N_EXPERTS - 1)
    x_ext = jnp.concatenate([xt, jnp.zeros((1, D), xt.dtype)], axis=0)

    def expert_block(args):
        rows, e = args
        h = x_ext[rows] @ w1[e] + b1[e]
        glu = jnp.minimum(h[:, :D_FF], SWIGLU_LIMIT)
        lin = jnp.clip(h[:, D_FF:], -SWIGLU_LIMIT, SWIGLU_LIMIT)
        return (glu * jax.nn.sigmoid(SWIGLU_ALPHA * glu) * (lin + 1.0)) @ w2[e] + b2[e]

    y_pad = lax.map(expert_block, (src.reshape(n_blk, EXPERT_BLOCK), blk_e)).reshape(n_pad, D)
    y = jax.ops.segment_sum(y_pad * gate_pad[:, None], src, num_segments=T + 1)[:T]
    return y.reshape(bsz, L, D)


def setup_inputs(seed: int = 0) -> dict:
    key = jax.random.key(seed)
    ks = iter(jax.random.split(key, 64))
    nrm = lambda shape, scale: scale * jax.random.normal(next(ks), shape, F32)
    uni = lambda shape, lo, hi: jax.random.uniform(next(ks), shape, F32, lo, hi)
    n = jnp.arange(S5_STATE, dtype=F32)
    gdn_dt = jnp.exp(uni((N_EVEN, GDN_HEADS), math.log(1e-3), math.log(1e-1)))
    return {
        'x_prompt': nrm((BATCH, SEQ, D_MODEL), 1.0),
        'x_sample': nrm((DEC_BATCH, DEC_SEQ, D_MODEL), 1.0),
        'state_s5_re': nrm((N_EVEN, DEC_BATCH, S5_GROUPS, S5_STATE), 0.1),
        'state_s5_im': nrm((N_EVEN, DEC_BATCH, S5_GROUPS, S5_STATE), 0.1),
        'state_gdn': nrm((N_EVEN, DEC_BATCH, GDN_HEADS, GDN_DK, GDN_DV), 0.1),
        'state_gdn_conv': nrm((N_EVEN, DEC_BATCH, GDN_CONV - 1, GDN_QKV), 1.0),
        'state_ret': nrm((N_ODD, DEC_BATCH, RET_HEADS, RET_DK, RET_DV), 0.3),
        'p_prompt': nrm((DEPTH, BATCH, SEQ, PLE_DIM), 1.0),
        'p_sample': nrm((DEPTH, DEC_BATCH, DEC_SEQ, PLE_DIM), 1.0),
        'w_in_even': nrm((N_EVEN, D_MODEL, EVEN_IN), D_MODEL ** -0.5),
        's5_a_re': -0.5 + nrm((N_EVEN, S5_GROUPS, S5_STATE), 0.01),
        's5_a_im': math.pi * n + nrm((N_EVEN, S5_GROUPS, S5_STATE), 0.01),
        's5_log_dt': uni((N_EVEN, S5_GROUPS), math.log(1e-3), math.log(1e-1)),
        's5_b_re': nrm((N_EVEN, S5_GROUPS, S5_STATE, S5_GROUP), (2.0 * S5_GROUP) ** -0.5),
        's5_b_im': nrm((N_EVEN, S5_GROUPS, S5_STATE, S5_GROUP), (2.0 * S5_GROUP) ** -0.5),
        's5_c_re': nrm((N_EVEN, S5_GROUPS, S5_GROUP, S5_STATE), (2.0 * S5_STATE) ** -0.5),
        's5_c_im': nrm((N_EVEN, S5_GROUPS, S5_GROUP, S5_STATE), (2.0 * S5_STATE) ** -0.5),
        's5_d': nrm((N_EVEN, S5_WIDTH), 1.0),
        's5_w_glu': nrm((N_EVEN, S5_WIDTH, S5_WIDTH), S5_WIDTH ** -0.5),
        's5_b_glu': nrm((N_EVEN, S5_WIDTH), 0.01),
        'gdn_conv_w': nrm((N_EVEN, GDN_CONV, GDN_QKV), GDN_CONV ** -0.5),
        'gdn_a_log': jnp.log(uni((N_EVEN, GDN_HEADS), 1.0, 16.0)),
        'gdn_dt_bias': jnp.log(jnp.expm1(gdn_dt)),
        'gdn_norm_w': 1.0 + nrm((N_EVEN, GDN_DV), 0.01),
        'w_out_even': nrm((N_EVEN, EVEN_MIX, D_MODEL), EVEN_MIX ** -0.5 * DEEPNORM_BETA),
        'w_in_odd': nrm((N_ODD, D_MODEL, ODD_IN), D_MODEL ** -0.5),
        'w_out_odd': nrm((N_ODD, RET_HEADS * RET_DV, D_MODEL), (RET_HEADS * RET_DV) ** -0.5 * DEEPNORM_BETA),
        'ln1_g': 1.0 + nrm((DEPTH, D_MODEL), 0.01),
        'ln1_b': nrm((DEPTH, D_MODEL), 0.01),
        'ln2_g': 1.0 + nrm((DEPTH, D_MODEL), 0.01),
        'ln2_b': nrm((DEPTH, D_MODEL), 0.01),
        'router_w': nrm((DEPTH, D_MODEL, N_EXPERTS), D_MODEL ** -0.5),
        'router_b': nrm((DEPTH, N_EXPERTS), 0.01),
        'moe_w1': nrm((DEPTH, N_EXPERTS, D_MODEL, 2 * D_FF), D_MODEL ** -0.5),
        'moe_b1': nrm((DEPTH, N_EXPERTS, 2 * D_FF), 0.01),
        'moe_w2': nrm((DEPTH, N_EXPERTS, D_FF, D_MODEL), D_FF ** -0.5 * DEEPNORM_BETA),
        'moe_b2': nrm((DEPTH, N_EXPERTS, D_MODEL), 0.01),
        'ple_w': nrm((DEPTH, PLE_DIM, D_MODEL), PLE_DIM ** -0.5),
        'ple_gate_w': nrm((DEPTH, D_MODEL, D_MODEL), D_MODEL ** -0.5),
    }


def reference(x_prompt, x_sample, state_s5_re, state_s5_im, state_gdn, state_gdn_conv, state_ret,
              p_prompt, p_sample, w_in_even, s5_a_re, s5_a_im, s5_log_dt, s5_b_re, s5_b_im, s5_c_re,
              s5_c_im, s5_d, s5_w_glu, s5_b_glu, gdn_conv_w, gdn_a_log, gdn_dt_bias, gdn_norm_w,
              w_out_even, w_in_odd, w_out_odd, ln1_g, ln1_b, ln2_g, ln2_b, router_w, router_b,
              moe_w1, moe_b1, moe_w2, moe_b2, ple_w, ple_gate_w):

    def run_group(x, p, s5_re, s5_im, gdn_s, conv_s, ret_s, pos0):
        x = x.astype(F32)
        L = x.shape[1]
        lc = L if L <= CHUNK else CHUNK
        pos = pos0 + jnp.arange(L, dtype=F32)
        new_re, new_im, new_gdn, new_conv, new_ret = [], [], [], [], []
        for i in range(DEPTH):
            j = i // 2
            if i % 2 == 0:
                mix, hr, hi, S, cv = even_mixer(
                    x, s5_re[j].astype(F32), s5_im[j].astype(F32), gdn_s[j].astype(F32),
                    conv_s[j].astype(F32), lc, w_in_even[j], s5_a_re[j], s5_a_im[j], s5_log_dt[j],
                    s5_b_re[j], s5_b_im[j], s5_c_re[j], s5_c_im[j], s5_d[j], s5_w_glu[j], s5_b_glu[j],
                    gdn_conv_w[j], gdn_a_log[j], gdn_dt_bias[j], gdn_norm_w[j], w_out_even[j])
                new_re.append(hr)
                new_im.append(hi)
                new_gdn.append(S)
                new_conv.append(cv)
            else:
                mix, R = odd_mixer(x, ret_s[j].astype(F32), pos, lc, w_in_odd[j], w_out_odd[j])
                new_ret.append(R)
            x = layer_norm(DEEPNORM_ALPHA * x + mix, ln1_g[i], ln1_b[i])
            ff = moe(x, router_w[i], router_b[i], moe_w1[i], moe_b1[i], moe_w2[i], moe_b2[i])
            x = layer_norm(DEEPNORM_ALPHA * x + ff, ln2_g[i], ln2_b[i])
            x = x + (p[i].astype(F32) @ ple_w[i]) * jax.nn.sigmoid(x @ ple_gate_w[i])
        return x, jnp.stack(new_re), jnp.stack(new_im), jnp.stack(new_gdn), jnp.stack(new_conv), jnp.stack(new_ret)

    bp = x_prompt.shape[0]
    zeros = lambda *s: jnp.zeros(s, F32)
    y_p, p_re, p_im, p_gdn, p_conv, p_ret = run_group(
        x_prompt, p_prompt,
        zeros(N_EVEN, bp, S5_GROUPS, S5_STATE), zeros(N_EVEN, bp, S5_GROUPS, S5_STATE),
        zeros(N_EVEN, bp, GDN_HEADS, GDN_DK, GDN_DV), zeros(N_EVEN, bp, GDN_CONV - 1, GDN_QKV),
        zeros(N_ODD, bp, RET_HEADS, RET_DK, RET_DV), 0.0)
    y_s, s_re, s_im, s_gdn, s_conv, s_ret = run_group(
        x_sample, p_sample, state_s5_re, state_s5_im, state_gdn, state_gdn_conv, state_ret, float(PAST_LEN))
    dp = x_prompt.dtype
    return (y_p.astype(dp), y_s.astype(x_sample.dtype),
            p_re.astype(dp), p_im.astype(dp), p_gdn.astype(dp), p_conv.astype(dp), p_ret.astype(dp),
            s_re.astype(state_s5_re.dtype), s_im.astype(state_s5_im.dtype), s_gdn.astype(state_gdn.dtype),
            s_conv.astype(state_gdn_conv.dtype), s_ret.astype(state_ret.dtype))
```

```python
from contextlib import ExitStack
import math
import os
CUT = int(os.environ.get('KCUT', '99'))
CUT2 = int(os.environ.get('KCUT2', '99'))
import numpy as np
import concourse.bass as bass
import concourse.mybir as mybir
from concourse.bass_utils import run_bass_kernel_spmd

F32 = mybir.dt.float32
BF16 = mybir.dt.bfloat16
I32 = mybir.dt.int32
U32 = mybir.dt.uint32
ALU = mybir.AluOpType
AF = mybir.ActivationFunctionType

ENGS = ("pe", "dve", "act", "pool", "sp")
SEM_CHUNK = 30000

D = 1024
NE = 32
TOPK = 4
ALPHA = 4.0 ** 0.25
LN_EPS = 1e-5
NORM_EPS = 1e-6
EVEN_IN = 2568
ODD_IN = 6144


class Instr:
    __slots__ = ("eng", "fn", "waits", "is_dma", "sig", "key", "val", "idx", "clock", "sval")

    def __init__(self, eng, fn, is_dma):
        self.eng = eng
        self.fn = fn
        self.is_dma = is_dma
        self.waits = []
        self.sig = False
        self.key = None
        self.val = 0
        self.idx = 0
        self.clock = None
        self.sval = None


class Trk:
    def __init__(self, name=""):
        self.name = name
        self.ent = {}

    def _conf(self, k):
        if k == "*":
            return list(self.ent.values())
        out = []
        e = self.ent.get(k)
        if e is not None:
            out.append(e)
        e = self.ent.get("*")
        if e is not None:
            out.append(e)
        return out


class Buf:
    def __init__(self, h, name):
        self.h = h
        self.trk = Trk(name)

    def __getitem__(self, k):
        return self.h[k]


def alias(ap, parent, name="alias"):
    b = Buf(ap, name)
    b.trk = parent.trk
    return b


class Prog:
    def __init__(self, nc, sb_words):
        self.nc = nc
        self.q = {e: [] for e in ENGS}
        self.clock = {e: {} for e in ENGS}
        self.es = ExitStack()
        self.dma_sems = {}
        self.dma_rr = {e: 0 for e in ENGS}
        self.n_dma_sems = 8
        self.pending = {e: [] for e in ENGS}
        self.big = self.es.enter_context(nc.sbuf_tensor("big", [128, sb_words], F32))
        self.sb_words = sb_words
        self.top = 0
        self.psn = 0

    def sb(self, name, shape, dtype=F32):
        isz = 2 if dtype == BF16 else 4
        n = 1
        for s in shape[1:]:
            n *= s
        words = (n * isz + 3) // 4
        off = self.top
        self.top += words
        assert self.top <= self.sb_words, (name, self.top, self.sb_words)
        v = self.big[0:shape[0], off:off + words]
        if dtype != F32:
            v = v.bitcast(dtype)
        if dtype == BF16 and n % 2 == 1:
            v = v[:, 0:n]
        if len(shape) == 3:
            v = v.rearrange("p (a b) -> p a b", a=shape[1])
        elif len(shape) == 4:
            v = v.rearrange("p (a b c) -> p a b c", a=shape[1], b=shape[2])
        return Buf(v, name)

    def mark(self):
        return self.top

    def release(self, m):
        self.barrier()
        self.top = m

    def ps(self, name):
        t = self.es.enter_context(self.nc.psum_tensor(name, [128, 512], F32))
        b = Buf(t, name)
        b.trk.excl = True
        return b

    def barrier(self):
        lasts = []
        for e in ENGS:
            if self.q[e]:
                for ins in reversed(self.q[e]):
                    if not ins.is_dma:
                        lasts.append(ins)
                        break
        for q, lst in self.dma_sems.items():
            for s in lst:
                if s[2] is not None:
                    lasts.append(s[2])
        for e in ENGS:
            self.pending[e] = list(lasts)

    def _norm(self, lst):
        out = []
        for x in lst:
            if isinstance(x, tuple):
                t, k = x
            else:
                t, k = x, "*"
            trk = t if isinstance(t, Trk) else t.trk
            out.append((trk, k))
        return out

    def _add_dep(self, ins, prod):
        if prod is None or prod is ins:
            return
        eng = ins.eng
        if prod.eng == "pe" and eng == "pe" and not prod.is_dma:
            return
        clk = self.clock[eng]
        if clk.get(prod.key, -1) >= prod.val:
            return
        prod.sig = True
        ins.waits.append(prod)
        new = dict(clk)
        new[prod.key] = prod.val
        if prod.clock:
            for k, v in prod.clock.items():
                if new.get(k, -1) < v:
                    new[k] = v
        self.clock[eng] = new

    def _record(self, ins, reads, writes):
        if self.pending[ins.eng]:
            for pr in self.pending[ins.eng]:
                self._add_dep(ins, pr)
            self.pending[ins.eng] = []
        reads = self._norm(reads)
        writes = self._norm(writes)
        excl = [x for x in reads if getattr(x[0], "excl", False)]
        if excl:
            reads = [x for x in reads if not getattr(x[0], "excl", False)]
            writes = writes + [x for x in excl if x not in writes]
        for trk, k in reads:
            for e in trk._conf(k):
                self._add_dep(ins, e[0])
        for trk, k in writes:
            for e in trk._conf(k):
                self._add_dep(ins, e[0])
                for r in e[1]:
                    self._add_dep(ins, r)
        for trk, k in reads:
            e = trk.ent.get(k)
            if e is None:
                e = trk.ent[k] = [None, []]
            e[1].append(ins)
        for trk, k in writes:
            if k == "*":
                trk.ent.clear()
            trk.ent[k] = [ins, []]
        ins.clock = self.clock[ins.eng]
        self.q[ins.eng].append(ins)

    def op(self, eng, fn, r=(), w=()):
        ins = Instr(eng, fn, False)
        ins.idx = len(self.q[eng])
        ins.key = eng
        ins.val = ins.idx
        self._record(ins, r, w)
        return ins

    def V(self, eng, meth, *args, r=(), w=(), **kw):
        return self.op(eng, lambda e: getattr(e, meth)(*args, **kw), r=r, w=w)

    def dma(self, eng, fn, r=(), w=()):
        ins = Instr(eng, fn, True)
        ins.idx = len(self.q[eng])
        sems = self.dma_sems.setdefault(eng, [])
        if len(sems) < self.n_dma_sems:
            s = [f"dq_{eng}_{len(sems)}", 0, None]
            sems.append(s)
        else:
            s = sems[self.dma_rr[eng] % self.n_dma_sems]
        self.dma_rr[eng] += 1
        if s[2] is not None:
            self._add_dep(ins, s[2])
        s[1] += 1
        s[2] = ins
        ins.key = s[0]
        ins.val = s[1]
        ins.sig = True
        self._record(ins, r, w)
        return ins

    def DM(self, eng, out, in_, r=(), w=(), **kw):
        return self.dma(eng, lambda e: e.dma_start(out=out, in_=in_, **kw), r=r, w=w)

    def finalize(self):
        nc = self.nc
        sem_names = set()
        for e in ENGS:
            cnt = 0
            for ins in self.q[e]:
                if ins.is_dma:
                    sem_names.add(ins.key)
                elif ins.sig:
                    ep, v = divmod(cnt, SEM_CHUNK)
                    ins.sval = (f"e_{e}_{ep}", v + 1)
                    sem_names.add(ins.sval[0])
                    cnt += 1
        sems = {}
        for n in sorted(sem_names):
            sems[n] = self.es.enter_context(nc.semaphore(n))

        def semval(prod):
            if prod.is_dma:
                return sems[prod.key], prod.val * 16
            return sems[prod.sval[0]], prod.sval[1]

        def run(e, eng_name):
            for ins in self.q[eng_name]:
                best = {}
                for pr in ins.waits:
                    s, v = semval(pr)
                    k = id(s)
                    if k not in best or best[k][1] < v:
                        best[k] = (s, v)
                ws = list(best.values())
                attach = None
                if ws and eng_name != "pe":
                    attach = ws.pop()
                for s, v in ws:
                    e.wait_ge(s, v)
                bi = ins.fn(e)
                if attach is not None:
                    bi._wait_ge(attach[0], attach[1])
                if ins.is_dma:
                    bi.then_inc(sems[ins.key], 16)
                elif ins.sig:
                    bi.then_inc(sems[ins.sval[0]], 1)

        block = self.es.enter_context(nc.Block())

        @block.tensor
        def _(e):
            run(e, "pe")

        @block.vector
        def _(e):
            run(e, "dve")

        @block.scalar
        def _(e):
            run(e, "act")

        @block.gpsimd
        def _(e):
            run(e, "pool")

        @block.sync
        def _(e):
            run(e, "sp")
            for q, lst in self.dma_sems.items():
                for s in lst:
                    if s[1] > 0:
                        e.wait_ge(sems[s[0]], s[1] * 16)

        self.es.close()
        return nc


def host_consts():
    c = {}
    c["identf"] = np.eye(128, dtype=np.float32)
    jj = np.arange(128)[:, None]
    ii = np.arange(128)[None, :]
    c["uincl"] = (jj <= ii).astype(np.float32)
    c["ustrict"] = (jj < ii).astype(np.float32)
    c["ones"] = np.ones((128, 128), np.float32)
    c["iota_e"] = np.tile(np.arange(32, dtype=np.float32)[None, :], (128, 1))
    c["tau"] = np.tile(np.arange(1, 129, dtype=np.float32)[None, :], (128, 1))
    c["pidx"] = np.arange(128, dtype=np.float32)[:, None].copy()
    log_g = np.log(1.0 - 2.0 ** (-5.0 - np.arange(4, dtype=np.float32))).astype(np.float32)
    M = np.zeros((4, 128, 128), np.float32)
    for h in range(4):
        m = np.exp(log_g[h] * np.abs(ii - jj).astype(np.float32))
        m = np.where((jj >= 64) & (ii < 64), 0.0, m)
        M[h] = m
    c["retmask"] = np.ascontiguousarray(M.transpose(1, 0, 2)).astype(np.float32)
    qs = np.zeros((128, 4, 128), np.float32)
    for h in range(4):
        qs[:, h, :] = np.exp(log_g[h] * (np.arange(128, dtype=np.float32) + 1.0))[None, :]
    c["retqs"] = qs
    ks = np.zeros((128, 8), np.float32)
    for h in range(4):
        ks[:, h] = np.exp(log_g[h] * (127.0 - np.arange(128, dtype=np.float32)))
        ks[:, 4 + h] = np.exp(log_g[h] * (15.0 - np.arange(128, dtype=np.float32)))
    c["retks"] = ks * np.float32(256 ** -0.5)
    c["retcdec"] = [[float(np.exp(log_g[h] * 128.0)) for h in range(4)],
                    [float(np.exp(log_g[h] * 16.0)) for h in range(4)]]
    freq = (1.0 / (10000.0 ** np.linspace(0.0, 1.0, 128, dtype=np.float32))).astype(np.float32)
    pos = np.arange(2048, dtype=np.float32)
    ang = (pos[None, :] * freq[:, None]).astype(np.float32)
    c["rcos"] = np.cos(ang).astype(np.float32)
    c["rsin"] = np.sin(ang).astype(np.float32)
    return c


CONST_SHAPES = {"identf": [128, 128], "uincl": [128, 128], "ustrict": [128, 128], "ones": [128, 128],
                "iota_e": [128, 32], "tau": [128, 128], "pidx": [128, 1], "retmask": [128, 4, 128],
                "retqs": [128, 4, 128], "retks": [128, 8], "rcos": [128, 2048], "rsin": [128, 2048]}


def build(NPS, L, C, stages=("A0", "M0", "C0", "A1", "M1", "C1"), dbg=()):
    nc = bass.Bass("TRN2", target_bir_lowering=False)
    NSEQ = NPS + 1
    NTOK = NPS * L + 16
    TPS = L // 128
    tiles = []
    for s in range(NPS):
        for t in range(TPS):
            tiles.append(dict(row0=s * L + t * 128, nt=128, seq=s, first=(t == 0), last=(t == TPS - 1),
                              pos0=t * 128, ti=len(tiles)))
    tiles.append(dict(row0=NPS * L, nt=16, seq=NPS, first=True, last=True, pos0=1024, ti=len(tiles)))
    NT = len(tiles)
    NROWP = NT * 128
    CT = C // 128
    TRASH = NE * C
    cgs = []
    c0 = 0
    while c0 < C:
        cgs.append((c0, min(C, c0 + 512)))
        c0 += 512

    def din(name, shape, dt=F32):
        return nc.dram_tensor(name, list(shape), dt, kind="ExternalInput").ap()

    def dout(name, shape, dt=F32):
        return nc.dram_tensor(name, list(shape), dt, kind="ExternalOutput").ap()

    def dint(name, shape, dt=F32):
        return nc.dram_tensor(name, list(shape), dt, kind="Internal").ap()

    xin = din("xin", [NTOK, D])
    pin = din("pin", [2, NTOK, 256])
    st_s5re = din("st_s5re", [128, 16])
    st_s5im = din("st_s5im", [128, 16])
    st_gdn = din("st_gdn", [4, 128, 128])
    st_conv = din("st_conv", [128, 12, 3])
    st_ret = din("st_ret", [4, 256, 512])
    w_in_even = din("w_in_even", [D, EVEN_IN])
    s5_are = din("s5_are", [128, 16])
    s5_aim = din("s5_aim", [128, 16])
    s5_ldt = din("s5_ldt", [128, 16])
    s5_bst = din("s5_bst", [2, 16, 128, 128])
    s5_cst = din("s5_cst", [2, 16, 128, 128])
    s5_d = din("s5_d", [128, 4])
    s5_wglu = din("s5_wglu", [512, 512])
    s5_bglu = din("s5_bglu", [128, 4])
    gdn_convw = din("gdn_convw", [128, 12, 4])
    gdn_alog = din("gdn_alog", [1, 4])
    gdn_dtb = din("gdn_dtb", [1, 4])
    gdn_normw = din("gdn_normw", [1, 128])
    w_out_even = din("w_out_even", [D, D])
    w_in_odd = din("w_in_odd", [D, ODD_IN])
    w_out_odd = din("w_out_odd", [2048, D])
    ln1_g = din("ln1_g", [2, D])
    ln1_b = din("ln1_b", [2, D])
    ln2_g = din("ln2_g", [2, D])
    ln2_b = din("ln2_b", [2, D])
    router_w = din("router_w", [2, D, NE])
    router_b = din("router_b", [2, NE])
    moe_w1 = din("moe_w1", [2, NE, D, 2 * D])
    moe_b1 = din("moe_b1", [2, NE, 128, 16])
    moe_w2 = din("moe_w2", [2, NE, D, D])
    moe_b2 = din("moe_b2", [2, NE, D])
    ple_w = din("ple_w", [2, 256, D])
    ple_gw = din("ple_gw", [2, D, D])
    cst = {k: din("c_" + k, v) for k, v in CONST_SHAPES.items()}

    y_out = dout("y_out", [NTOK, D])
    o_s5re = dout("o_s5re", [NSEQ, 128, 16])
    o_s5im = dout("o_s5im", [NSEQ, 128, 16])
    o_gdn = dout("o_gdn", [NSEQ, 4, 128, 128])
    o_conv = dout("o_conv", [NSEQ, 128, 12, 3])
    o_ret = dout("o_ret", [NSEQ, 4, 256, 512])
    dbg_out = {k: dout("dbg_" + k, [NTOK, D]) for k in dbg if k.startswith("x")}
    taps = {}

    def tap(name, ap, ti, npart, width, dt=F32):
        if ("t_" + name) not in dbg:
            return
        if name not in taps:
            taps[name] = dout("tap_" + name, [NT, 128, width], dt)
        p.DM("sp", taps[name][ti, 0:npart, :], ap, r=[tapsrc[0]], w=[T_out])

    tapsrc = [None]

    x1s = dint("x1s", [NROWP, D])
    x3s = dint("x3s", [NROWP, D])
    xs = dint("xs", [NE * C + 128, D], BF16)
    ys = dint("ys", [NE * C + 128, D])

    p = Prog(nc, 52900)
    DR = Trk("dram_in")
    T_x1s, T_x3s, T_xs, T_ys, T_out = Trk("x1s"), Trk("x3s"), Trk("xs"), Trk("ys"), Trk("out")
    PS = [p.ps(f"ps{i}") for i in range(8)]

    def psbf(b, n):
        return PS[b][:, 0:n // 2].bitcast(BF16)

    identf = p.sb("identf", [128, 128])
    identb = p.sb("identb", [128, 128], BF16)
    uincl = p.sb("uincl", [128, 128])
    ustr_b = p.sb("ustr_b", [128, 128], BF16)
    ones_f = p.sb("ones_f", [128, 128])
    ones_b = p.sb("ones_b", [128, 128], BF16)
    iota_e = p.sb("iota_e", [128, 32])
    pidx = p.sb("pidx", [128, 1])
    gates_all = p.sb("gates_all", [128, NT, 4])
    slots_all = p.sb("slots_all", [128, NT, 4], I32)
    tmpc = p.sb("tmpc", [128, 128])
    for nm, b in (("identf", identf), ("uincl", uincl), ("ones", ones_f), ("iota_e", iota_e), ("pidx", pidx)):
        p.DM("sp", b[:], cst[nm], r=[DR], w=[b])
    p.DM("sp", tmpc[:], cst["ustrict"], r=[DR], w=[tmpc])
    p.V("dve", "tensor_copy", ustr_b[:], tmpc[:], r=[tmpc], w=[ustr_b])
    p.V("dve", "tensor_copy", identb[:], identf[:], r=[identf], w=[identb])
    p.V("dve", "tensor_copy", ones_b[:], ones_f[:], r=[ones_f], w=[ones_b])

    rr = {"ev": 0}

    def evac(out_ap, in_ap, r, w):
        rr["ev"] += 1
        if rr["ev"] % 2:
            p.V("act", "activation", out_ap, in_ap, AF.Copy, r=r, w=w)
        else:
            p.V("dve", "tensor_copy", out_ap, in_ap, r=r, w=w)

    def bcast_load(buf, src_row):
        p.DM("sp", buf[:], src_row.partition_broadcast(128), r=[DR], w=[buf])

    def load_w_bf16(buf, src, kc):
        n = src.shape[1]
        nch = (n + 2047) // 2048
        step = (n + nch - 1) // nch
        v = src.rearrange("(kc q) n -> q kc n", q=128)
        for c0 in range(0, n, step):
            c1 = min(n, c0 + step)
            p.DM("pool", buf[:, :, c0:c1], v[:, :, c0:c1], r=[DR], w=[(buf, c0)] if nch > 1 else [buf])

    def layernorm(eng_h, h, nt, gt, bt, out, scr6, scr2):
        p.V("dve", "bn_stats", scr6[0:nt, 0, :], h[0:nt, 0:512], r=[h], w=[(scr6, 0)])
        p.V("dve", "bn_stats", scr6[0:nt, 1, :], h[0:nt, 512:1024], r=[h], w=[(scr6, 1)])
        p.V("dve", "bn_aggr", scr2[0:nt, 0:2], scr6[0:nt, :, :].rearrange("p a b -> p (a b)"), r=[scr6], w=[scr2])
        p.V("act", "activation", scr2[0:nt, 2:3], scr2[0:nt, 1:2], AF.Sqrt, bias=epsln[0:nt, 0:1], r=[scr2, epsln], w=[(scr2, "s")])
        p.V("dve", "reciprocal", scr2[0:nt, 3:4], scr2[0:nt, 2:3], r=[(scr2, "s")], w=[(scr2, "r")])
        p.V("dve", "tensor_scalar", out[0:nt, :], h[0:nt, :], scr2[0:nt, 0:1], scr2[0:nt, 3:4], ALU.subtract, ALU.mult,
            r=[h, scr2, (scr2, "r")], w=[out])
        p.V("pool", "tensor_tensor", out[0:nt, :], out[0:nt, :], gt[0:nt, :], ALU.mult, r=[out, gt], w=[out])
        p.V("pool", "tensor_tensor", out[0:nt, :], out[0:nt, :], bt[0:nt, :], ALU.add, r=[out, bt], w=[out])

    epsln = p.sb("epsln", [128, 2])
    p.V("dve", "memset", epsln[:, 0:1], LN_EPS, w=[(epsln, 0)])
    p.V("dve", "memset", epsln[:, 1:2], NORM_EPS, w=[(epsln, 1)])

    def transpose_to(dstT, src_bf, nt, nblk, bank):
        for b0 in range(0, nblk, 8):
            nb = min(8, nblk - b0)
            for b in range(nb):
                p.V("pe", "transpose", psbf(bank, 1024)[:, b * 128:b * 128 + nt], src_bf[0:nt, (b0 + b) * 128:(b0 + b + 1) * 128],
                    identb[0:nt, 0:nt], r=[src_bf, identb], w=[PS[bank]])
            evac(dstT[:, b0:b0 + nb, 0:nt], psbf(bank, 1024).rearrange("p (a b) -> p a b", a=8)[:, 0:nb, 0:nt],
                 r=[PS[bank]], w=[dstT])

    def phaseA_tail(li, tl, xt, mixps, lnw, rw, rb, work):
        nt, ti = tl["nt"], tl["ti"]
        h, x1, xrow, x1T, lg, scr6, scr2, small, Mb, tot = work
        p.V("dve", "scalar_tensor_tensor", h[0:nt, 0:512], xt[0:nt, 0:512], ALPHA, PS[mixps[0]][0:nt, :], ALU.mult, ALU.add,
            r=[xt, PS[mixps[0]]], w=[(h, 0)])
        p.V("dve", "scalar_tensor_tensor", h[0:nt, 512:1024], xt[0:nt, 512:1024], ALPHA, PS[mixps[1]][0:nt, :], ALU.mult, ALU.add,
            r=[xt, PS[mixps[1]]], w=[(h, 1)])
        layernorm("dve", h, nt, lnw[0], lnw[1], x1, scr6, scr2)
        p.DM("sp", x1s[ti * 128:ti * 128 + nt, :], x1[0:nt, :], r=[x1], w=[(T_x1s, ti)])
        if ("x1_%d" % li) in dbg_out:
            p.DM("sp", dbg_out["x1_%d" % li][tl["row0"]:tl["row0"] + nt, :], x1[0:nt, :], r=[x1], w=[T_out])
        p.V("act", "activation", xrow[0:nt, :], x1[0:nt, :], AF.Copy, r=[x1], w=[xrow])
        for half in range(2):
            for b in range(4):
                kc = half * 4 + b
                p.V("pe", "transpose", PS[6][:, b * 128:b * 128 + nt], x1[0:nt, kc * 128:(kc + 1) * 128], identf[0:nt, 0:nt],
                    r=[x1, identf], w=[PS[6]])
            evac(x1T[:, half * 4:half * 4 + 4, 0:nt], PS[6][:, :].rearrange("p (a b) -> p a b", a=4)[:, :, 0:nt], r=[PS[6]], w=[x1T])
        for kc in range(8):
            p.V("pe", "matmul", PS[7][0:nt, 0:32], x1T[:, kc, 0:nt], rw[:, kc, :], start=(kc == 0), stop=(kc == 7),
                r=[x1T, rw], w=[PS[7]])
        p.V("dve", "tensor_tensor", lg[0:nt, :], PS[7][0:nt, 0:32], rb[0:nt, :], ALU.add, r=[PS[7], rb], w=[lg])
        top, ti8, nt0, ex, gs, ef, rk, sl, ov, tmp32, rnk = small
        p.V("dve", "max", top[0:nt, :], lg[0:nt, :], r=[lg], w=[top])
        p.V("dve", "max_index", ti8[0:nt, :], top[0:nt, :], lg[0:nt, :], r=[lg, top], w=[ti8])
        p.V("dve", "tensor_scalar", nt0[0:nt, :], top[0:nt, 0:1], -1.0, None, ALU.mult, r=[top], w=[nt0])
        p.V("act", "activation", ex[0:nt, :], top[0:nt, 0:4], AF.Exp, bias=nt0[0:nt, 0:1], r=[top, nt0], w=[ex])
        p.V("dve", "reduce_sum", gs[0:nt, 0:1], ex[0:nt, :], mybir.AxisListType.X, r=[ex], w=[gs])
        p.V("dve", "reciprocal", gs[0:nt, 1:2], gs[0:nt, 0:1], r=[gs], w=[(gs, "r")])
        p.V("dve", "tensor_scalar", gates_all[0:nt, ti, :], ex[0:nt, :], gs[0:nt, 1:2], None, ALU.mult, r=[ex, (gs, "r")], w=[(gates_all, ti)])
        p.V("pool", "memset", Mb[:], 0.0, w=[Mb])
        p.V("dve", "tensor_scalar", Mb[0:nt, :], lg[0:nt, :], top[0:nt, 3:4], None, ALU.is_ge, r=[lg, top], w=[Mb])
        p.V("pe", "matmul", PS[7][:, 64:96], ustr_b[:, :], Mb[:, :], start=True, stop=True, r=[ustr_b, Mb], w=[PS[7]])
        p.V("pe", "matmul", PS[7][:, 96:128], ones_b[:, :], Mb[:, :], start=True, stop=True, r=[ones_b, Mb], w=[PS[7]])
        p.V("dve", "tensor_tensor", rnk[:, :], PS[7][:, 64:96], tot[:, :], ALU.add, r=[PS[7], tot], w=[rnk])
        p.V("dve", "tensor_tensor", tot[:, :], PS[7][:, 96:128], tot[:, :], ALU.add, r=[PS[7], tot], w=[tot])
        p.V("dve", "tensor_copy", ef[0:nt, :], ti8[0:nt, 0:4], r=[ti8], w=[ef])
        for k in range(4):
            p.V("dve", "scalar_tensor_tensor", tmp32[0:nt, :], iota_e[0:nt, :], ef[0:nt, k:k + 1], rnk[0:nt, :], ALU.is_equal, ALU.mult,
                accum_out=rk[0:nt, k:k + 1], r=[iota_e, ef, rnk], w=[tmp32, (rk, k)])
        p.V("dve", "scalar_tensor_tensor", sl[0:nt, :], ef[0:nt, :], float(C), rk[0:nt, :], ALU.mult, ALU.add, r=[ef, rk], w=[sl])
        p.V("dve", "tensor_scalar", ov[0:nt, :], rk[0:nt, :], float(C), None, ALU.is_ge, r=[rk], w=[ov])
        p.V("dve", "tensor_scalar", tmp32[0:nt, 0:4], sl[0:nt, :], -1.0, pidx[0:nt, 0:1], ALU.mult, ALU.add, r=[sl, pidx], w=[tmp32])
        p.V("dve", "tensor_scalar", tmp32[0:nt, 0:4], tmp32[0:nt, 0:4], float(TRASH), None, ALU.add, r=[tmp32], w=[tmp32])
        p.V("dve", "tensor_tensor", tmp32[0:nt, 0:4], tmp32[0:nt, 0:4], ov[0:nt, :], ALU.mult, r=[tmp32, ov], w=[tmp32])
        p.V("dve", "tensor_tensor", sl[0:nt, :], sl[0:nt, :], tmp32[0:nt, 0:4], ALU.add, r=[sl, tmp32], w=[sl])
        if nt < 128:
            p.V("dve", "tensor_scalar", tmp32[:, 0:4], pidx[:, 0:1].to_broadcast([128, 4]), float(TRASH), None, ALU.add, r=[pidx], w=[tmp32])
            p.V("dve", "tensor_copy", slots_all[:, ti, :], tmp32[:, 0:4], r=[tmp32], w=[(slots_all, ti)])
        p.V("dve", "tensor_copy", slots_all[0:nt, ti, :], sl[0:nt, :], r=[sl], w=[(slots_all, ti)])
        for k in range(4):
            p.dma("pool", lambda e, k=k, ti=ti: e.indirect_dma_start(
                out=xs[:, :], out_offset=bass.IndirectOffsetOnAxis(ap=slots_all[:, ti, k:k + 1], axis=0),
                in_=xrow[:, :], in_offset=None), r=[xrow, (slots_all, ti), (T_xs, "*")], w=[])

    def alloc_tail_work(h=None, x1=None, x1T=None):
        if h is None:
            h = p.sb("h", [128, D])
        if x1 is None:
            x1 = p.sb("x1", [128, D])
        xrow = p.sb("xrow", [128, D], BF16)
        if x1T is None:
            x1T = p.sb("x1T", [128, 8, 128])
        lg = p.sb("lg", [128, 32])
        scr6 = p.sb("scr6", [128, 2, 6])
        scr2 = p.sb("scr2", [128, 4])
        small = (p.sb("top", [128, 8]), p.sb("ti8", [128, 8], U32), p.sb("nt0", [128, 1]), p.sb("ex", [128, 4]),
                 p.sb("gs", [128, 2]), p.sb("ef", [128, 4]), p.sb("rk", [128, 4]), p.sb("sl", [128, 4]),
                 p.sb("ov", [128, 4]), p.sb("tmp32", [128, 32]), p.sb("rnk", [128, 32]))
        Mb = p.sb("Mb", [128, 32], BF16)
        tot = p.sb("tot", [128, 32])
        p.V("dve", "memset", tot[:], 0.0, w=[tot])
        p.V("pool", "memset", xrow[:, :], 0.0, w=[xrow])
        return (h, x1, xrow, x1T, lg, scr6, scr2, small, Mb, tot)

    def load_ln_router(li):
        g1 = p.sb("ln1g", [128, D]); b1 = p.sb("ln1b", [128, D])
        bcast_load(g1, ln1_g[li:li + 1, :]); bcast_load(b1, ln1_b[li:li + 1, :])
        rw = p.sb("rw", [128, 8, NE])
        p.DM("sp", rw[:], router_w[li].rearrange("(kc q) n -> q kc n", q=128), r=[DR], w=[rw])
        rb = p.sb("rb", [128, NE])
        bcast_load(rb, router_b[li:li + 1, :])
        return (g1, b1), rw, rb

    def phaseA0():
        m0 = p.mark()
        win = p.sb("win", [128, 8, EVEN_IN], BF16)
        load_w_bf16(win, w_in_even, 8)
        wout = p.sb("wout", [128, 8, D], BF16)
        load_w_bf16(wout, w_out_even, 8)
        wglu = p.sb("wglu", [128, 4, 512], BF16)
        load_w_bf16(wglu, s5_wglu, 4)
        bst = p.sb("bst", [128, 2, 16, 128], BF16)
        cstt = p.sb("cstt", [128, 2, 16, 128], BF16)
        for ri in range(2):
            p.DM("pool", bst[:, ri, :, :], s5_bst[ri].rearrange("b k m -> k b m"), r=[DR], w=[(bst, ri)])
            p.DM("pool", cstt[:, ri, :, :], s5_cst[ri].rearrange("b k m -> k b m"), r=[DR], w=[(cstt, ri)])
        lnw, rw, rb = load_ln_router(0)
        are = p.sb("are", [128, 16]); aim = p.sb("aim", [128, 16]); ldt = p.sb("ldt", [128, 16])
        for b, s in ((are, s5_are), (aim, s5_aim), (ldt, s5_ldt)):
            p.DM("sp", b[:], s, r=[DR], w=[b])
        dsk = p.sb("dsk", [128, 4]); bgl = p.sb("bgl", [128, 4])
        p.DM("sp", dsk[:], s5_d, r=[DR], w=[dsk])
        p.DM("sp", bgl[:], s5_bglu, r=[DR], w=[bgl])
        tau = p.sb("tau", [128, 128])
        p.DM("sp", tau[:], cst["tau"], r=[DR], w=[tau])
        lam = p.sb("lam", [128, 16]); li_ = p.sb("li", [128, 16]); dtt = p.sb("dtt", [128, 16])
        p.V("act", "activation", dtt[:], ldt[:], AF.Exp, r=[ldt], w=[dtt])
        p.V("dve", "tensor_tensor", li_[:], aim[:], dtt[:], ALU.mult, r=[aim, dtt], w=[li_])
        p.V("dve", "tensor_tensor", lam[:], are[:], dtt[:], ALU.mult, r=[are, dtt], w=[lam])
        p.V("act", "activation", lam[:], lam[:], AF.Exp, r=[lam], w=[lam])
        cosT = p.sb("cosT", [128, 16, 128]); sinT = p.sb("sinT", [128, 16, 128])
        crT = p.sb("crT", [128, 16, 128]); ciT = p.sb("ciT", [128, 16, 128])
        ang = crT
        kq = ciT
        gsc = p.sb("gsc", [128, 2, 16, 128])
        ki = Buf(gsc[:, 0, :, :].bitcast(I32), "ki")
        ki.trk = gsc.trk
        TWO_PI = 2.0 * math.pi

        def sin_of(dst, shift):
            p.V("dve", "tensor_tensor", ang[:], li_[:, :].unsqueeze(2).to_broadcast([128, 16, 128]),
                tau[:, :].unsqueeze(1).to_broadcast([128, 16, 128]), ALU.mult, r=[li_, tau], w=[ang])
            if shift != 0.0:
                p.V("dve", "tensor_scalar", ang[:], ang[:], shift, None, ALU.add, r=[ang], w=[ang])
            p.V("dve", "tensor_scalar", kq[:], ang[:], 1.0 / TWO_PI, None, ALU.mult, r=[ang], w=[kq])
            p.V("dve", "tensor_copy", ki[:], kq[:], r=[kq], w=[ki])
            p.V("dve", "tensor_copy", kq[:], ki[:], r=[ki], w=[kq])
            p.V("dve", "scalar_tensor_tensor", ang[:], kq[:], -TWO_PI, ang[:], ALU.mult, ALU.add, r=[kq, ang], w=[ang])
            p.V("dve", "tensor_scalar", kq[:], ang[:], math.pi, TWO_PI, ALU.is_gt, ALU.mult, r=[ang], w=[kq])
            p.V("dve", "tensor_tensor", ang[:], ang[:], kq[:], ALU.subtract, r=[ang, kq], w=[ang])
            p.V("dve", "tensor_scalar", kq[:], ang[:], -math.pi, TWO_PI, ALU.is_lt, ALU.mult, r=[ang], w=[kq])
            p.V("dve", "tensor_tensor", ang[:], ang[:], kq[:], ALU.add, r=[ang, kq], w=[ang])
            p.V("dve", "tensor_scalar", ang[:], ang[:], math.pi, -math.pi, ALU.min, ALU.max, r=[ang], w=[ang])
            p.V("act", "activation", dst[:], ang[:], AF.Sin, r=[ang], w=[dst])

        sin_of(sinT, 0.0)
        sin_of(cosT, math.pi / 2)
        sm = p.sb("s5sm", [128, 8, 16])
        abre, abim, den, t1, t2, cfre, cfim, t3 = [sm[:, i, :] for i in range(8)]
        S = [sm]
        p.V("dve", "tensor_tensor", abre, lam[:], cosT[:, :, 0], ALU.mult, r=[lam, cosT], w=S)
        p.V("dve", "tensor_tensor", abim, lam[:], sinT[:, :, 0], ALU.mult, r=[lam, sinT], w=S)
        p.V("dve", "tensor_scalar", abre, abre, -1.0, None, ALU.add, r=S, w=S)
        p.V("dve", "tensor_tensor", t1, are[:], are[:], ALU.mult, r=[are], w=S)
        p.V("dve", "tensor_tensor", t2, aim[:], aim[:], ALU.mult, r=[aim], w=S)
        p.V("dve", "tensor_tensor", den, t1, t2, ALU.add, r=S, w=S)
        p.V("dve", "reciprocal", den, den, r=S, w=S)
        p.V("dve", "tensor_tensor", t1, abre, are[:], ALU.mult, r=S + [are], w=S)
        p.V("dve", "tensor_tensor", t2, abim, aim[:], ALU.mult, r=S + [aim], w=S)
        p.V("dve", "tensor_tensor", cfre, t1, t2, ALU.add, r=S, w=S)
        p.V("dve", "tensor_tensor", cfre, cfre, den, ALU.mult, r=S, w=S)
        p.V("dve", "tensor_tensor", t1, abim, are[:], ALU.mult, r=S + [are], w=S)
        p.V("dve", "tensor_tensor", t2, abre, aim[:], ALU.mult, r=S + [aim], w=S)
        p.V("dve", "tensor_tensor", cfim, t1, t2, ALU.subtract, r=S, w=S)
        p.V("dve", "tensor_tensor", cfim, cfim, den, ALU.mult, r=S, w=S)
        sc3 = Buf(gsc[:, 1, :, :], "sc3")
        sc3.trk = gsc.trk
        bc = lambda a: a.unsqueeze(2).to_broadcast([128, 16, 128])
        p.V("dve", "tensor_tensor", crT[:], cosT[:], bc(cfre), ALU.mult, r=[cosT] + S, w=[crT])
        p.V("dve", "tensor_tensor", sc3[:], sinT[:], bc(cfim), ALU.mult, r=[sinT] + S, w=[sc3])
        p.V("dve", "tensor_tensor", crT[:], crT[:], sc3[:], ALU.add, r=[crT, sc3], w=[crT])
        p.V("dve", "tensor_tensor", ciT[:], cosT[:], bc(cfim), ALU.mult, r=[cosT] + S, w=[ciT])
        p.V("dve", "tensor_tensor", sc3[:], sinT[:], bc(cfre), ALU.mult, r=[sinT] + S, w=[sc3])
        p.V("dve", "tensor_tensor", ciT[:], ciT[:], sc3[:], ALU.subtract, r=[ciT, sc3], w=[ciT])
        wc = p.sb("wc", [128, 12, 4])
        p.DM("sp", wc[:], gdn_convw, r=[DR], w=[wc])
        alog = p.sb("alog", [128, 4]); dtb = p.sb("dtb", [128, 4]); nrmw = p.sb("nrmw", [128, 128])
        bcast_load(alog, gdn_alog); bcast_load(dtb, gdn_dtb); bcast_load(nrmw, gdn_normw)
        p.V("act", "activation", alog[:], alog[:], AF.Exp, r=[alog], w=[alog])
        Hre = p.sb("Hre", [128, 16]); Him = p.sb("Him", [128, 16])
        Sg = p.sb("Sg", [128, 4, 128])
        ctx3 = p.sb("ctx3", [128, 12, 3])
        xt_one = p.sb("xt0", [128, D])
        xts = [xt_one, xt_one]
        xb = p.sb("xb", [128, D], BF16)
        xT = p.sb("xT", [128, 8, 128], BF16)
        uTf = p.sb("uTf", [128, 4, 128]); uTb = p.sb("uTb", [128, 4, 128], BF16)
        cb = p.sb("cb", [128, 12, 131])
        cacc = p.sb("cacc", [128, 12, 128])
        g8 = p.sb("g8", [128, 16, 128])
        ctmp = Buf(g8[:, 0:12, :], "ctmp"); ctmp.trk = g8.trk
        ztok = p.sb("ztok", [128, 8])
        rbuf = p.sb("rbuf", [128, 2, 8, 128]); rtmp = p.sb("rtmp", [128, 2, 8, 128])
        hbf = p.sb("hbf", [128, 2, 16, 128], BF16)
        hl = p.sb("hl", [128, 4, 16])
        yA = Buf(rtmp[:, 0, 0:4, :], "yA"); yA.trk = rtmp.trk
        ysq = Buf(rtmp[:, 0, 4:8, :], "ysq"); ysq.trk = rtmp.trk
        gaf = Buf(rtmp[:, 1, 0:4, :], "gaf"); gaf.trk = rtmp.trk
        gab = p.sb("gab", [128, 4, 128], BF16)
        mixT = p.sb("mixT", [128, 8, 128], BF16)
        qkn = Buf(g8[:, 8:16, :], "qkn"); qkn.trk = g8.trk
        sq8 = Buf(g8[:, 0:8, :], "sq8"); sq8.trk = g8.trk
        kvtok = alias(rbuf[:, 0, :, :], rbuf, "kvtok")
        gd = p.sb("gd", [128, 16])
        gd2 = p.sb("gd2", [128, 16])
        _gt = [alias(gsc[:, 0, i, :], gsc, "gt%d" % i) for i in range(16)]
        gbc, dec, erow, attn, attnT, rv, rk_, nwT, ub, qdT, kd, Yt = _gt[0:12]
        Mm = [_gt[12], _gt[13]]
        MT = [_gt[14], _gt[15]]
        yB = p.sb("yB", [128, 512], BF16); osb = alias(gsc[:, 1, 0, :], gsc, "osb"); ssq = p.sb("ssq", [128, 4])
        zs = p.sb("zs", [128, 512], BF16)
        x1a = alias(g8[:, 0:8, :].rearrange("p a b -> p (a b)"), g8, "x1a")
        x1Ta = alias(cacc[:, 0:8, :], cacc, "x1Ta")
        work = alloc_tail_work(h=xt_one, x1=x1a, x1T=x1Ta)
        print("A0 sbuf words", p.top)

        def load_x(tl, buf):
            if tl["nt"] < 128:
                p.V("pool", "memset", buf[:], 0.0, w=[buf])
            p.DM("sp", buf[0:tl["nt"], :], xin[tl["row0"]:tl["row0"] + tl["nt"], :], r=[DR], w=[buf])

        for tl in tiles:
            nt, ti, sq = tl["nt"], tl["ti"], tl["seq"]
            xt = xts[ti % 2]
            load_x(tl, xt)
            if tl["first"]:
                if sq < NPS:
                    p.V("pool", "memset", Hre[:], 0.0, w=[Hre]); p.V("pool", "memset", Him[:], 0.0, w=[Him])
                    p.V("pool", "memset", Sg[:], 0.0, w=[Sg]); p.V("pool", "memset", ctx3[:], 0.0, w=[ctx3])
                else:
                    p.DM("sp", Hre[:], st_s5re, r=[DR], w=[Hre])
                    p.DM("sp", Him[:], st_s5im, r=[DR], w=[Him])
                    p.DM("sp", Sg[:], st_gdn.rearrange("h k v -> k h v"), r=[DR], w=[Sg])
                    p.DM("sp", ctx3[:], st_conv, r=[DR], w=[ctx3])
            if CUT <= 1:
                continue
            p.V("act", "activation", xb[0:nt, :], xt[0:nt, :], AF.Copy, r=[xt], w=[xb])
            transpose_to(xT, xb, nt, 8, 0)
            tapsrc[0] = xt; tap("xt", xt[0:nt, :], ti, nt, D)
            tapsrc[0] = xb; tap("xb", xb[0:nt, :], ti, nt, D, BF16)
            tapsrc[0] = xT; tap("xT", xT[:, :, :].rearrange("p a b -> p (a b)"), ti, 128, 1024, BF16)
            for ob in range(4):
                for kc in range(8):
                    p.V("pe", "matmul", PS[1][:, ob * 128:ob * 128 + nt], win[:, kc, ob * 128:(ob + 1) * 128], xT[:, kc, 0:nt],
                        start=(kc == 0), stop=(kc == 7), r=[win, xT], w=[PS[1]])
            ps1v = PS[1][:, :].rearrange("p (a b) -> p a b", a=4)[:, :, 0:nt]
            p.V("act", "activation", uTf[:, :, 0:nt], ps1v, AF.Copy, r=[PS[1]], w=[uTf])
            p.V("dve", "tensor_copy", uTb[:, :, 0:nt], ps1v, r=[PS[1]], w=[uTb])
            p.V("pool", "tensor_copy", cb[:, :, 0:3], ctx3[:, :, :], r=[ctx3], w=[(cb, "c")])
            for g4 in range(3):
                bank = 2 + (g4 % 2)
                for b in range(4):
                    blk = g4 * 4 + b
                    for kc in range(8):
                        p.V("pe", "matmul", PS[bank][:, b * 128:b * 128 + nt], win[:, kc, 512 + blk * 128:512 + (blk + 1) * 128],
                            xT[:, kc, 0:nt], start=(kc == 0), stop=(kc == 7), r=[win, xT], w=[PS[bank]])
                evac(cb[:, g4 * 4:g4 * 4 + 4, 3:3 + nt], PS[bank][:, :].rearrange("p (a b) -> p a b", a=4)[:, :, 0:nt],
                     r=[PS[bank]], w=[(cb, g4)])
            tapsrc[0] = cb; tap("cb", cb[:, :, :].rearrange("p a b -> p (a b)"), ti, 128, 12 * 131)
            tapsrc[0] = uTf; tap("uTf", uTf[:, :, :].rearrange("p a b -> p (a b)"), ti, 128, 512)
            for kc in range(8):
                p.V("pe", "matmul", PS[4][0:nt, 0:512], xT[:, kc, 0:nt], win[:, kc, 2048:2560], start=(kc == 0), stop=(kc == 7),
                    r=[xT, win], w=[PS[4]])
            for kc in range(8):
                p.V("pe", "matmul", PS[5][0:nt, 0:8], xT[:, kc, 0:nt], win[:, kc, 2560:2568], start=(kc == 0), stop=(kc == 7),
                    r=[xT, win], w=[PS[5]])
            p.V("act", "activation", zs[0:nt, :], PS[4][0:nt, 0:512], AF.Silu, r=[PS[4]], w=[zs])
            p.V("dve", "tensor_copy", ztok[0:nt, 0:8], PS[5][0:nt, 0:8], r=[PS[5]], w=[ztok])
            if CUT <= 2:
                continue
            for hf in range(2):
                for ri in range(2):
                    for b8 in range(8):
                        blk = hf * 8 + b8
                        bank = 4 + ri * 2 + (b8 // 4)
                        p.V("pe", "matmul", PS[bank][:, (b8 % 4) * 128:(b8 % 4) * 128 + nt], bst[:, ri, blk, :], uTb[:, blk // 4, 0:nt],
                            start=True, stop=True, r=[bst, uTb], w=[PS[bank]])
                for q4 in range(2):
                    bre = PS[4 + q4][:, :].rearrange("p (a b) -> p a b", a=4)[:, :, 0:nt]
                    bim = PS[6 + q4][:, :].rearrange("p (a b) -> p a b", a=4)[:, :, 0:nt]
                    bs = slice(hf * 8 + q4 * 4, hf * 8 + q4 * 4 + 4)
                    o4 = slice(q4 * 4, q4 * 4 + 4)
                    p.V("dve", "tensor_tensor", rbuf[:, 0, o4, 0:nt], bre, crT[:, bs, 0:nt], ALU.mult, r=[PS[4 + q4], crT], w=[(rbuf, 0)])
                    p.V("dve", "tensor_tensor", rtmp[:, 0, o4, 0:nt], bim, ciT[:, bs, 0:nt], ALU.mult, r=[PS[6 + q4], ciT], w=[(rtmp, 0)])
                    p.V("dve", "tensor_tensor", rbuf[:, 1, o4, 0:nt], bre, ciT[:, bs, 0:nt], ALU.mult, r=[PS[4 + q4], ciT], w=[(rbuf, 1)])
                    p.V("dve", "tensor_tensor", rtmp[:, 1, o4, 0:nt], bim, crT[:, bs, 0:nt], ALU.mult, r=[PS[6 + q4], crT], w=[(rtmp, 1)])
                p.V("pool", "tensor_tensor", rbuf[:, 0, :, 0:nt], rbuf[:, 0, :, 0:nt], rtmp[:, 0, :, 0:nt], ALU.subtract, r=[(rbuf, 0), (rtmp, 0)], w=[(rbuf, 0)])
                p.V("pool", "tensor_tensor", rbuf[:, 1, :, 0:nt], rbuf[:, 1, :, 0:nt], rtmp[:, 1, :, 0:nt], ALU.add, r=[(rbuf, 1), (rtmp, 1)], w=[(rbuf, 1)])
                for b8 in range(8):
                    blk = hf * 8 + b8
                    p.V("dve", "tensor_tensor_scan", gsc[:, 0, blk, 0:nt], lam[:, blk:blk + 1].to_broadcast([128, nt]), rbuf[:, 0, b8, 0:nt],
                        Hre[:, blk:blk + 1], ALU.mult, ALU.add, r=[lam, (rbuf, 0), Hre], w=[(gsc, (0, blk))])
                    p.V("dve", "tensor_tensor_scan", gsc[:, 1, blk, 0:nt], lam[:, blk:blk + 1].to_broadcast([128, nt]), rbuf[:, 1, b8, 0:nt],
                        Him[:, blk:blk + 1], ALU.mult, ALU.add, r=[lam, (rbuf, 1), Him], w=[(gsc, (1, blk))])
            t_a, t_b = rbuf[:, :, :, :].rearrange("p a b c -> p (a b) c"), rtmp[:, :, :, :].rearrange("p a b c -> p (a b) c")
            p.V("pool", "tensor_tensor", t_a[:, :, 0:nt], gsc[:, 0, :, 0:nt], cosT[:, :, 0:nt], ALU.mult, r=[gsc, cosT], w=[rbuf])
            p.V("pool", "tensor_tensor", t_b[:, :, 0:nt], gsc[:, 1, :, 0:nt], sinT[:, :, 0:nt], ALU.mult, r=[gsc, sinT], w=[rtmp])
            p.V("dve", "tensor_tensor", hbf[:, 0, :, 0:nt], t_a[:, :, 0:nt], t_b[:, :, 0:nt], ALU.subtract, r=[rbuf, rtmp], w=[(hbf, 0)])
            lc = nt - 1
            p.V("dve", "tensor_tensor", hl[:, 0, :], gsc[:, 0, :, lc], cosT[:, :, lc], ALU.mult, r=[gsc, cosT], w=[(hl, 0)])
            p.V("dve", "tensor_tensor", hl[:, 1, :], gsc[:, 1, :, lc], sinT[:, :, lc], ALU.mult, r=[gsc, sinT], w=[(hl, 1)])
            p.V("dve", "tensor_tensor", hl[:, 2, :], gsc[:, 0, :, lc], sinT[:, :, lc], ALU.mult, r=[gsc, sinT], w=[(hl, 2)])
            p.V("dve", "tensor_tensor", hl[:, 3, :], gsc[:, 1, :, lc], cosT[:, :, lc], ALU.mult, r=[gsc, cosT], w=[(hl, 3)])
            p.V("pool", "tensor_tensor", t_a[:, :, 0:nt], gsc[:, 0, :, 0:nt], sinT[:, :, 0:nt], ALU.mult, r=[gsc, sinT, (hbf, 0)], w=[rbuf])
            p.V("pool", "tensor_tensor", t_b[:, :, 0:nt], gsc[:, 1, :, 0:nt], cosT[:, :, 0:nt], ALU.mult, r=[gsc, cosT, (hbf, 0)], w=[rtmp])
            p.V("dve", "scalar_tensor_tensor", hbf[:, 1, :, 0:nt], t_a[:, :, 0:nt], -1.0, t_b[:, :, 0:nt], ALU.mult, ALU.subtract,
                r=[rbuf, rtmp], w=[(hbf, 1)])
            p.V("dve", "tensor_tensor", Hre[:], hl[:, 0, :], hl[:, 1, :], ALU.subtract, r=[hl], w=[Hre])
            p.V("dve", "tensor_tensor", Him[:], hl[:, 2, :], hl[:, 3, :], ALU.add, r=[hl], w=[Him])
            if tl["last"]:
                p.DM("sp", o_s5re[sq], Hre[:], r=[Hre], w=[T_out])
                p.DM("sp", o_s5im[sq], Him[:], r=[Him], w=[T_out])
            for ob in range(4):
                n = 0
                for b4 in range(4):
                    blk = ob * 4 + b4
                    for ri in range(2):
                        p.V("pe", "matmul", PS[1][:, ob * 128:ob * 128 + nt], cstt[:, ri, blk, :], hbf[:, ri, blk, 0:nt],
                            start=(n == 0), stop=(n == 7), r=[cstt, hbf], w=[PS[1]])
                        n += 1
            for ob in range(4):
                p.V("dve", "scalar_tensor_tensor", yA[:, ob, 0:nt], uTf[:, ob, 0:nt], dsk[:, ob:ob + 1], PS[1][:, ob * 128:ob * 128 + nt],
                    ALU.mult, ALU.add, r=[uTf, dsk, PS[1]], w=[yA])
            cg = math.sqrt(2.0 / math.pi)
            p.V("act", "activation", ysq[:, :, 0:nt], yA[:, :, 0:nt], AF.Square, r=[yA], w=[ysq])
            p.V("dve", "tensor_scalar", ysq[:, :, 0:nt], ysq[:, :, 0:nt], 2.0 * cg * 0.044715, 2.0 * cg, ALU.mult, ALU.add, r=[ysq], w=[ysq])
            p.V("dve", "tensor_tensor", ysq[:, :, 0:nt], ysq[:, :, 0:nt], yA[:, :, 0:nt], ALU.mult, r=[ysq, yA], w=[ysq])
            p.V("act", "activation", ysq[:, :, 0:nt], ysq[:, :, 0:nt], AF.Sigmoid, r=[ysq], w=[ysq])
            p.V("dve", "tensor_tensor", gaf[:, :, 0:nt], ysq[:, :, 0:nt], yA[:, :, 0:nt], ALU.mult, r=[ysq, yA], w=[gaf])
            p.V("act", "activation", gab[:, :, 0:nt], gaf[:, :, 0:nt], AF.Copy, r=[gaf], w=[gab])
            for ob in range(4):
                for kc in range(4):
                    p.V("pe", "matmul", PS[0][:, ob * 128:ob * 128 + nt], wglu[:, kc, ob * 128:(ob + 1) * 128], gab[:, kc, 0:nt],
                        start=(kc == 0), stop=(kc == 3), r=[wglu, gab], w=[PS[0]])
            for ob in range(4):
                p.V("act", "activation", ysq[:, ob, 0:nt], PS[0][:, ob * 128:ob * 128 + nt], AF.Sigmoid, bias=bgl[:, ob:ob + 1],
                    r=[PS[0], bgl], w=[ysq])
            p.V("dve", "tensor_tensor", mixT[:, 0:4, 0:nt], ysq[:, :, 0:nt], gaf[:, :, 0:nt], ALU.mult, r=[ysq, gaf], w=[(mixT, "a")])
            if CUT <= 3:
                continue
            for j in range(4):
                wj = wc[:, :, j:j + 1].to_broadcast([128, 12, nt])
                if j == 0:
                    p.V("dve", "tensor_tensor", cacc[:, :, 0:nt], cb[:, :, 0:nt], wj, ALU.mult, r=[cb, wc], w=[cacc])
                else:
                    p.V("pool", "tensor_tensor", ctmp[:, :, 0:nt], cb[:, :, j:j + nt], wj, ALU.mult, r=[cb, wc], w=[ctmp])
                    p.V("dve", "tensor_tensor", cacc[:, :, 0:nt], cacc[:, :, 0:nt], ctmp[:, :, 0:nt], ALU.add, r=[cacc, ctmp], w=[cacc])
            p.V("pool", "tensor_copy", ctx3[:, :, :], cb[:, :, nt:nt + 3], r=[cb], w=[ctx3])
            if tl["last"]:
                p.DM("sp", o_conv[sq], ctx3[:], r=[ctx3], w=[T_out])
            p.V("act", "activation", cacc[:, :, 0:nt], cacc[:, :, 0:nt], AF.Silu, r=[cacc], w=[cacc])
            p.V("act", "activation", sq8[:, :, 0:nt], cacc[:, 0:8, 0:nt], AF.Square, r=[cacc], w=[sq8])
            for hb in range(2):
                for b in range(4):
                    p.V("pe", "matmul", PS[2 + hb][:, b * 128:b * 128 + nt], ones_f[:, :], sq8[:, hb * 4 + b, 0:nt], start=True, stop=True,
                        r=[ones_f, sq8], w=[PS[2 + hb]])
            for hb in range(2):
                v = PS[2 + hb][:, :].rearrange("p (a b) -> p a b", a=4)[:, :, 0:nt]
                p.V("act", "activation", sq8[:, hb * 4:hb * 4 + 4, 0:nt], v, AF.Sqrt, bias=epsln[:, 1:2], r=[PS[2 + hb], epsln], w=[(sq8, hb)])
            p.V("dve", "reciprocal", sq8[:, :, 0:nt], sq8[:, :, 0:nt], r=[sq8], w=[sq8])
            p.V("dve", "scalar_tensor_tensor", qkn[:, 0:4, 0:nt], cacc[:, 0:4, 0:nt], 128.0 ** -0.5, sq8[:, 0:4, 0:nt], ALU.mult, ALU.mult,
                r=[cacc, sq8], w=[(qkn, "q")])
            p.V("dve", "tensor_tensor", qkn[:, 4:8, 0:nt], cacc[:, 4:8, 0:nt], sq8[:, 4:8, 0:nt], ALU.mult, r=[cacc, sq8], w=[(qkn, "k")])
            for b in range(4):
                p.V("pe", "transpose", PS[2][0:nt, b * 128:(b + 1) * 128], qkn[:, 4 + b, 0:nt], identf[:, :], r=[qkn, identf], w=[PS[2]])
                p.V("pe", "transpose", PS[3][0:nt, b * 128:(b + 1) * 128], cacc[:, 8 + b, 0:nt], identf[:, :], r=[cacc, identf], w=[PS[3]])
            evac(kvtok[0:nt, 0:4, :], PS[2][0:nt, :].rearrange("p (a b) -> p a b", a=4), r=[PS[2]], w=[kvtok])
            evac(kvtok[0:nt, 4:8, :], PS[3][0:nt, :].rearrange("p (a b) -> p a b", a=4), r=[PS[3]], w=[kvtok])
            if CUT2 <= 1:
                continue
            p.V("act", "activation", gd[0:nt, 0:4], ztok[0:nt, 0:4], AF.Sigmoid, r=[ztok], w=[(gd, "b")])
            p.V("dve", "tensor_scalar", gd[0:nt, 4:8], gd[0:nt, 0:4], -1.0, None, ALU.mult, r=[(gd, "b")], w=[(gd, "nb")])
            p.V("dve", "tensor_tensor", gd[0:nt, 8:12], ztok[0:nt, 4:8], dtb[0:nt, :], ALU.add, r=[ztok, dtb], w=[(gd, "g")])
            p.V("act", "activation", gd[0:nt, 8:12], gd[0:nt, 8:12], AF.Exp, r=[(gd, "g")], w=[(gd, "g")])
            p.V("act", "activation", gd[0:nt, 8:12], gd[0:nt, 8:12], AF.Ln, bias=1.0, r=[(gd, "g")], w=[(gd, "g")])
            p.V("dve", "scalar_tensor_tensor", gd[0:nt, 8:12], gd[0:nt, 8:12], -1.0, alog[0:nt, :], ALU.mult, ALU.mult, r=[(gd, "g"), alog], w=[(gd, "g")])
            p.V("pe", "matmul", PS[0][0:nt, 0:4], uincl[0:nt, 0:nt], gd[0:nt, 8:12], start=True, stop=True, r=[uincl, (gd, "g")], w=[PS[0]])
            p.V("dve", "tensor_copy", gd[0:nt, 12:16], PS[0][0:nt, 0:4], r=[PS[0]], w=[(gd, "G")])
            p.V("act", "activation", gd2[0:nt, 0:4], gd[0:nt, 12:16], AF.Exp, r=[(gd, "G")], w=[(gd2, "e")])
            p.V("dve", "tensor_tensor", gd2[0:nt, 4:8], gd2[0:nt, 0:4], gd[0:nt, 0:4], ALU.mult, r=[(gd2, "e"), (gd, "b")], w=[(gd2, "be")])
            if CUT2 <= 2:
                continue
            for hd in range(4):
                kT = qkn[:, 4 + hd, 0:nt]
                qT = qkn[:, hd, 0:nt]
                p.V("dve", "tensor_scalar", gbc[0:nt, :], ones_f[0:nt, :], gd[0:nt, 8 + hd:9 + hd], None, ALU.mult, r=[ones_f, (gd, "g")], w=[gbc])
                p.V("pe", "matmul", PS[0][:, 128:128 + nt], gbc[0:nt, :], uincl[0:nt, 0:nt], start=True, stop=True, r=[gbc, uincl], w=[PS[0]])
                p.V("pe", "matmul", PS[0][0:nt, 256:256 + nt], kT, kT, start=True, stop=True, r=[(qkn, "k")], w=[PS[0]])
                p.V("pe", "matmul", PS[0][0:nt, 384:384 + nt], qT, kT, start=True, stop=True, r=[(qkn, "q"), (qkn, "k")], w=[PS[0]])
                grow = PS[0][0:nt, 128:128 + nt]
                p.V("act", "activation", dec[0:nt, 0:nt], grow, AF.Exp, bias=gd[0:nt, 12 + hd:13 + hd], scale=-1.0, r=[PS[0], (gd, "G")], w=[dec])
                p.V("act", "activation", erow[:, 0:nt], PS[0][:, 128:128 + nt], AF.Exp, r=[PS[0]], w=[erow])
                p.V("pool", "affine_select", dec[0:nt, 0:nt], dec[0:nt, 0:nt], [[-1, nt]], ALU.is_ge, 0.0, base=0, channel_multiplier=1, r=[dec], w=[dec])
                p.V("dve", "scalar_tensor_tensor", Mm[0][0:nt, 0:nt], PS[0][0:nt, 256:256 + nt], gd[0:nt, 4 + hd:5 + hd], dec[0:nt, 0:nt], ALU.mult, ALU.mult,
                    r=[PS[0], (gd, "nb"), dec], w=[Mm[0]])
                p.V("pool", "affine_select", Mm[0][0:nt, 0:nt], Mm[0][0:nt, 0:nt], [[-1, nt]], ALU.is_gt, 0.0, base=0, channel_multiplier=1, r=[Mm[0]], w=[Mm[0]])
                p.V("dve", "tensor_tensor", attn[0:nt, 0:nt], PS[0][0:nt, 384:384 + nt], dec[0:nt, 0:nt], ALU.mult, r=[PS[0], dec], w=[attn])
                p.V("pe", "transpose", PS[1][0:nt, 0:nt], Mm[0][0:nt, 0:nt], identf[0:nt, 0:nt], r=[Mm[0], identf], w=[PS[1]])
                p.V("pe", "transpose", PS[1][0:nt, 128:128 + nt], attn[0:nt, 0:nt], identf[0:nt, 0:nt], r=[attn, identf], w=[PS[1]])
                p.V("act", "activation", MT[0][0:nt, 0:nt], PS[1][0:nt, 0:nt], AF.Copy, r=[PS[1]], w=[MT[0]])
                p.V("dve", "tensor_tensor", Yt[0:nt, 0:nt], PS[1][0:nt, 0:nt], identf[0:nt, 0:nt], ALU.add, r=[PS[1], identf], w=[Yt])
                p.V("act", "activation", attnT[0:nt, 0:nt], PS[1][0:nt, 128:128 + nt], AF.Copy, r=[PS[1]], w=[attnT])
                if CUT2 <= 3:
                    continue
                nlev = 6 if nt == 128 else 3
                for lv in range(nlev):
                    a, b_ = lv % 2, (lv + 1) % 2
                    bank = 6 + (lv % 2)
                    p.V("pe", "matmul", PS[bank][0:nt, 0:nt], MT[a][0:nt, 0:nt], Mm[a][0:nt, 0:nt], start=True, stop=True, r=[MT[a], Mm[a]], w=[PS[bank]])
                    if lv < nlev - 1:
                        p.V("pe", "matmul", PS[bank][0:nt, 128:128 + nt], Mm[a][0:nt, 0:nt], MT[a][0:nt, 0:nt], start=True, stop=True, r=[MT[a], Mm[a]], w=[PS[bank]])
                    p.V("act", "activation", Mm[b_][0:nt, 0:nt], PS[bank][0:nt, 0:nt], AF.Copy, r=[PS[bank]], w=[Mm[b_]])
                    if lv < nlev - 1:
                        p.V("dve", "tensor_copy", MT[b_][0:nt, 0:nt], PS[bank][0:nt, 128:128 + nt], r=[PS[bank]], w=[MT[b_]])
                    p.V("pe", "matmul", PS[bank][0:nt, 256:256 + nt], Mm[b_][0:nt, 0:nt], Yt[0:nt, 0:nt], start=True, stop=True, r=[Mm[b_], Yt], w=[PS[bank]])
                    p.V("dve", "tensor_tensor", Yt[0:nt, 0:nt], Yt[0:nt, 0:nt], PS[bank][0:nt, 256:256 + nt], ALU.add, r=[Yt, PS[bank]], w=[Yt])
                if CUT2 <= 4:
                    continue
                p.V("dve", "tensor_scalar", rv[0:nt, :], kvtok[0:nt, 4 + hd, :], gd[0:nt, hd:hd + 1], None, ALU.mult, r=[kvtok, (gd, "b")], w=[rv])
                p.V("dve", "tensor_scalar", rk_[0:nt, :], kvtok[0:nt, hd, :], gd2[0:nt, 4 + hd:5 + hd], None, ALU.mult, r=[kvtok, (gd2, "be")], w=[rk_])
                p.V("pe", "matmul", PS[1][:, 256:256 + nt], rk_[0:nt, :], Yt[0:nt, 0:nt], start=True, stop=True, r=[rk_, Yt], w=[PS[1]])
                p.V("dve", "tensor_scalar", nwT[:, 0:nt], PS[1][:, 256:256 + nt], -1.0, None, ALU.mult, r=[PS[1]], w=[nwT])
                p.V("pe", "matmul", PS[5][0:nt, 0:128], Yt[0:nt, 0:nt], rv[0:nt, :], start=True, stop=False, r=[Yt, rv], w=[PS[5]])
                p.V("pe", "matmul", PS[5][0:nt, 0:128], nwT[:, 0:nt], Sg[:, hd, :], start=False, stop=True, r=[nwT, Sg], w=[PS[5]])
                p.V("act", "activation", ub[0:nt, :], PS[5][0:nt, 0:128], AF.Copy, r=[PS[5]], w=[ub])
                p.V("dve", "tensor_tensor", qdT[:, 0:nt], qT, erow[:, 0:nt], ALU.mult, r=[(qkn, "q"), erow], w=[qdT])
                p.V("pe", "matmul", PS[5][0:nt, 128:256], qdT[:, 0:nt], Sg[:, hd, :], start=True, stop=False, r=[qdT, Sg], w=[PS[5]])
                p.V("pe", "matmul", PS[5][0:nt, 128:256], attnT[0:nt, 0:nt], ub[0:nt, :], start=False, stop=True, r=[attnT, ub], w=[PS[5]])
                if CUT2 <= 5:
                    continue
                p.V("dve", "tensor_copy", gd2[:, 12:13], PS[0][:, 128 + nt - 1:128 + nt], r=[PS[0]], w=[(gd2, "gl")])
                p.V("act", "activation", gd2[0:nt, 8 + hd:9 + hd], gd[0:nt, 12 + hd:13 + hd], AF.Exp, bias=gd2[0:nt, 12:13], scale=-1.0,
                    r=[(gd, "G"), (gd2, "gl")], w=[(gd2, ("kd", hd))])
                p.V("dve", "tensor_scalar", kd[0:nt, :], kvtok[0:nt, hd, :], gd2[0:nt, 8 + hd:9 + hd], None, ALU.mult, r=[kvtok, (gd2, ("kd", hd))], w=[kd])
                p.V("pe", "matmul", PS[5][:, 256:384], kd[0:nt, :], ub[0:nt, :], start=True, stop=True, r=[kd, ub], w=[PS[5]])
                p.V("act", "activation", gd2[:, 13:14], gd2[:, 12:13], AF.Exp, r=[(gd2, "gl")], w=[(gd2, "egl")])
                p.V("dve", "scalar_tensor_tensor", Sg[:, hd, :], Sg[:, hd, :], gd2[:, 13:14], PS[5][:, 256:384], ALU.mult, ALU.add,
                    r=[Sg, (gd2, "egl"), PS[5]], w=[Sg])
                if CUT2 <= 6:
                    continue
                p.V("act", "activation", osb[0:nt, :], PS[5][0:nt, 128:256], AF.Square, r=[PS[5]], w=[osb])
                p.V("dve", "reduce_sum", ssq[0:nt, hd:hd + 1], osb[0:nt, :], mybir.AxisListType.X, r=[osb], w=[(ssq, hd)])
                p.V("act", "activation", ssq[0:nt, hd:hd + 1], ssq[0:nt, hd:hd + 1], AF.Sqrt, bias=epsln[0:nt, 1:2], scale=1.0 / 128.0, r=[(ssq, hd), epsln], w=[(ssq, hd)])
                p.V("dve", "reciprocal", ssq[0:nt, hd:hd + 1], ssq[0:nt, hd:hd + 1], r=[(ssq, hd)], w=[(ssq, hd)])
                if CUT2 <= 7:
                    continue
                p.V("dve", "scalar_tensor_tensor", osb[0:nt, :], PS[5][0:nt, 128:256], ssq[0:nt, hd:hd + 1], nrmw[0:nt, :], ALU.mult, ALU.mult,
                    r=[PS[5], (ssq, hd), nrmw], w=[osb])
                p.V("dve", "tensor_tensor", yB[0:nt, hd * 128:(hd + 1) * 128], osb[0:nt, :], zs[0:nt, hd * 128:(hd + 1) * 128], ALU.mult, r=[osb, zs], w=[(yB, hd)])
            if tl["last"]:
                p.DM("sp", o_gdn[sq].rearrange("h k v -> k h v"), Sg[:], r=[Sg], w=[T_out])
            if CUT <= 4:
                continue
            for b in range(4):
                p.V("pe", "transpose", psbf(2, 1024)[:, b * 128:b * 128 + nt], yB[0:nt, b * 128:(b + 1) * 128], identb[0:nt, 0:nt], r=[yB, identb], w=[PS[2]])
            evac(mixT[:, 4:8, 0:nt], psbf(2, 1024).rearrange("p (a b) -> p a b", a=8)[:, 0:4, 0:nt], r=[PS[2]], w=[(mixT, "b")])
            for half in range(2):
                for kc in range(8):
                    p.V("pe", "matmul", PS[2 + half][0:nt, :], mixT[:, kc, 0:nt], wout[:, kc, half * 512:(half + 1) * 512], start=(kc == 0), stop=(kc == 7),
                        r=[mixT, wout], w=[PS[2 + half]])
            if CUT <= 5:
                continue
            phaseA_tail(0, tl, xt, (2, 3), lnw, rw, rb, work)
        p.release(m0)

    def phaseA1():
        m0 = p.mark()
        win = p.sb("win1", [128, 8, ODD_IN], BF16)
        load_w_bf16(win, w_in_odd, 8)
        wout = p.sb("wout1", [128, 16, D], BF16)
        load_w_bf16(wout, w_out_odd, 16)
        lnw, rw, rb = load_ln_router(1)
        retmask = p.sb("retmask", [128, 4, 128]); retqs = p.sb("retqs", [128, 4, 128]); retks = p.sb("retks", [128, 8])
        for b, nm in ((retmask, "retmask"), (retqs, "retqs"), (retks, "retks")):
            p.DM("sp", b[:], cst[nm], r=[DR], w=[b])
        Rf = p.sb("Rf", [128, 4, 2, 512])
        xt_one = p.sb("xt0", [128, D])
        xts = [xt_one, xt_one]
        xb = p.sb("xb", [128, D], BF16)
        xT = p.sb("xT", [128, 8, 128], BF16)
        cs = p.sb("cs", [128, 2, 128])
        qkT = p.sb("qkT", [128, 16, 128], BF16)
        qsT = p.sb("qsT", [128, 2, 128])
        rt1 = p.sb("rt1", [128, 128]); rt2 = p.sb("rt2", [128, 128])
        ktok = p.sb("ktok", [128, 8, 128], BF16)
        vtok = p.sb("vtok", [128, 2048], BF16); gsil = p.sb("gsil", [128, 2048], BF16)
        sT = p.sb("sT", [128, 128], BF16)
        og = vtok
        ogT = p.sb("ogT", [128, 16, 128], BF16)
        onrm = p.sb("onrm", [128, 512]); st6 = p.sb("st6", [128, 6]); st2 = p.sb("st2", [128, 4])
        work = alloc_tail_work(h=xt_one)

        def load_x(tl, buf):
            if tl["nt"] < 128:
                p.V("pool", "memset", buf[:], 0.0, w=[buf])
            p.DM("sp", buf[0:tl["nt"], :], x3s[tl["ti"] * 128:tl["ti"] * 128 + tl["nt"], :], r=[(T_x3s, tl["ti"])], w=[buf])

        for tl in tiles:
            nt, ti, sq = tl["nt"], tl["ti"], tl["seq"]
            Lc = 0 if nt == 128 else 1
            xt = xts[ti % 2]
            load_x(tl, xt)
            if tl["first"]:
                if sq < NPS:
                    p.V("pool", "memset", Rf[:], 0.0, w=[Rf])
                else:
                    for h in range(4):
                        for par in range(2):
                            p.DM("sp", Rf[:, h, par, :], st_ret[h].rearrange("(q two) v -> q two v", two=2)[:, par, :], r=[DR], w=[(Rf, (h, par))])
            p.DM("sp", cs[:, 0, 0:nt], cst["rcos"][:, tl["pos0"]:tl["pos0"] + nt], r=[DR], w=[(cs, 0)])
            p.DM("sp", cs[:, 1, 0:nt], cst["rsin"][:, tl["pos0"]:tl["pos0"] + nt], r=[DR], w=[(cs, 1)])
            p.V("act", "activation", xb[0:nt, :], xt[0:nt, :], AF.Copy, r=[xt], w=[xb])
            transpose_to(xT, xb, nt, 8, 0)
            for qk in range(2):
                for h in range(4):
                    bank = 1 + ((qk * 4 + h) % 2)
                    for par in range(2):
                        col0 = qk * 1024 + h * 256 + par * 128
                        for kc in range(8):
                            p.V("pe", "matmul", PS[bank][:, par * 128:par * 128 + nt], win[:, kc, col0:col0 + 128], xT[:, kc, 0:nt],
                                start=(kc == 0), stop=(kc == 7), r=[win, xT], w=[PS[bank]])
                    x0 = PS[bank][:, 0:nt]
                    x1_ = PS[bank][:, 128:128 + nt]
                    blk = qk * 8 + h * 2
                    p.V("dve", "tensor_tensor", rt1[:, 0:nt], x0, cs[:, 0, 0:nt], ALU.mult, r=[PS[bank], cs], w=[rt1])
                    p.V("dve", "tensor_tensor", rt2[:, 0:nt], x1_, cs[:, 1, 0:nt], ALU.mult, r=[PS[bank], cs], w=[rt2])
                    p.V("pool", "tensor_tensor", qkT[:, blk, 0:nt], rt1[:, 0:nt], rt2[:, 0:nt], ALU.subtract, r=[rt1, rt2], w=[(qkT, blk)])
                    p.V("dve", "tensor_tensor", rt1[:, 0:nt], x0, cs[:, 1, 0:nt], ALU.mult, r=[PS[bank], cs, (qkT, blk)], w=[rt1])
                    p.V("dve", "tensor_tensor", rt2[:, 0:nt], x1_, cs[:, 0, 0:nt], ALU.mult, r=[PS[bank], cs, (qkT, blk)], w=[rt2])
                    p.V("pool", "tensor_tensor", qkT[:, blk + 1, 0:nt], rt1[:, 0:nt], rt2[:, 0:nt], ALU.add, r=[rt1, rt2], w=[(qkT, blk + 1)])
            for cg in range(8):
                bank = 3 + (cg % 2)
                for kc in range(8):
                    p.V("pe", "matmul", PS[bank][0:nt, :], xT[:, kc, 0:nt], win[:, kc, 2048 + cg * 512:2048 + (cg + 1) * 512], start=(kc == 0), stop=(kc == 7),
                        r=[xT, win], w=[PS[bank]])
                if cg < 4:
                    p.V("dve", "tensor_copy", vtok[0:nt, cg * 512:(cg + 1) * 512], PS[bank][0:nt, :], r=[PS[bank]], w=[(vtok, cg)])
                else:
                    p.V("act", "activation", gsil[0:nt, (cg - 4) * 512:(cg - 3) * 512], PS[bank][0:nt, :], AF.Silu, r=[PS[bank]], w=[(gsil, cg - 4)])
            for b in range(8):
                p.V("pe", "transpose", psbf(5, 1024)[0:nt, b * 128:(b + 1) * 128], qkT[:, 8 + b, 0:nt], identb[:, :], r=[(qkT, 8 + b), identb], w=[PS[5]])
            for h in range(4):
                p.V("dve", "tensor_scalar", ktok[0:nt, 2 * h:2 * h + 2, :], psbf(5, 1024).rearrange("p (a b) -> p a b", a=8)[0:nt, 2 * h:2 * h + 2, :],
                    retks[0:nt, Lc * 4 + h:Lc * 4 + h + 1], None, ALU.mult, r=[PS[5], retks], w=[(ktok, h)])
            for h in range(4):
                for dc in range(2):
                    p.V("pe", "matmul", PS[6][0:nt, 0:nt], qkT[:, 8 + 2 * h + dc, 0:nt], qkT[:, 2 * h + dc, 0:nt], start=(dc == 0), stop=(dc == 1),
                        r=[(qkT, 8 + 2 * h + dc), (qkT, 2 * h + dc)], w=[PS[6]])
                p.V("dve", "scalar_tensor_tensor", sT[0:nt, 0:nt], PS[6][0:nt, 0:nt], 256.0 ** -0.5, retmask[0:nt, h, 0:nt], ALU.mult, ALU.mult,
                    r=[PS[6], retmask], w=[sT])
                for dc in range(2):
                    p.V("pool", "tensor_tensor", qsT[:, dc, 0:nt], qkT[:, 2 * h + dc, 0:nt], retqs[:, h, 0:nt], ALU.mult, r=[(qkT, 2 * h + dc), retqs], w=[(qsT, dc)])
                p.V("pe", "matmul", PS[7][0:nt, :], sT[0:nt, 0:nt], vtok[0:nt, h * 512:(h + 1) * 512], start=True, stop=False, r=[sT, (vtok, h)], w=[PS[7]])
                for dc in range(2):
                    p.V("pe", "matmul", PS[7][0:nt, :], qsT[:, dc, 0:nt], Rf[:, h, dc, :], start=False, stop=(dc == 1), r=[(qsT, dc), Rf], w=[PS[7]])
                cdec = cst_host["retcdec"][Lc][h]
                for dc in range(2):
                    bank = 1 + dc
                    p.V("pe", "matmul", PS[bank][:, :], ktok[0:nt, 2 * h + dc, :], vtok[0:nt, h * 512:(h + 1) * 512], start=True, stop=True,
                        r=[(ktok, h), (vtok, h)], w=[PS[bank]])
                    p.V("dve", "scalar_tensor_tensor", Rf[:, h, dc, :], Rf[:, h, dc, :], cdec, PS[bank][:, :], ALU.mult, ALU.add, r=[Rf, PS[bank]], w=[Rf])
                p.V("dve", "bn_stats", st6[0:nt, :], PS[7][0:nt, :], r=[PS[7]], w=[st6])
                p.V("dve", "bn_aggr", st2[0:nt, 0:2], st6[0:nt, :], r=[st6], w=[st2])
                p.V("act", "activation", st2[0:nt, 2:3], st2[0:nt, 1:2], AF.Sqrt, bias=epsln[0:nt, 0:1], r=[st2, epsln], w=[(st2, "s")])
                p.V("dve", "reciprocal", st2[0:nt, 3:4], st2[0:nt, 2:3], r=[(st2, "s")], w=[(st2, "r")])
                p.V("dve", "tensor_scalar", onrm[0:nt, :], PS[7][0:nt, :], st2[0:nt, 0:1], st2[0:nt, 3:4], ALU.subtract, ALU.mult, r=[PS[7], st2, (st2, "r")], w=[onrm])
                p.V("pool", "tensor_tensor", og[0:nt, h * 512:(h + 1) * 512], onrm[0:nt, :], gsil[0:nt, h * 512:(h + 1) * 512], ALU.mult, r=[onrm, (gsil, h)], w=[(vtok, h)])
            if tl["last"]:
                for h in range(4):
                    for par in range(2):
                        p.DM("sp", o_ret[sq, h].rearrange("(q two) v -> q two v", two=2)[:, par, :], Rf[:, h, par, :], r=[Rf], w=[T_out])
            transpose_to(ogT, og, nt, 16, 5)
            for half in range(2):
                for kc in range(16):
                    p.V("pe", "matmul", PS[2 + half][0:nt, :], ogT[:, kc, 0:nt], wout[:, kc, half * 512:(half + 1) * 512], start=(kc == 0), stop=(kc == 15),
                        r=[ogT, wout], w=[PS[2 + half]])
            phaseA_tail(1, tl, xt, (2, 3), lnw, rw, rb, work)
        p.release(m0)

    def phaseM(li):
        m0 = p.mark()
        w1b = [p.sb("w1b0", [128, 8, 2 * D], BF16), p.sb("w1b1", [128, 8, 2 * D], BF16)]
        w2b = [p.sb("w2b0", [128, 8, D], BF16), p.sb("w2b1", [128, 8, D], BF16)]
        b1t = [p.sb("b1t0", [128, 16]), p.sb("b1t1", [128, 16])]
        b2f = [p.sb("b2f0", [1, D]), p.sb("b2f1", [1, D])]
        b2b = p.sb("b2b", [1, D], BF16)
        xg = p.sb("xg", [128, CT, D], BF16)
        xgT = p.sb("xgT", [128, 8, C], BF16)
        glu = p.sb("glu", [128, C]); sig = p.sb("sig", [128, C]); lin = p.sb("lin", [128, C])
        actT = p.sb("actT", [128, 8, C], BF16)
        yo = [p.sb("yo0", [128, D]), p.sb("yo1", [128, D])]

        def load_w(e):
            s = e % 2
            load_w_bf16(w1b[s], moe_w1[li, e], 8)
            load_w_bf16(w2b[s], moe_w2[li, e], 8)
            p.DM("sp", b1t[s][:], moe_b1[li, e], r=[DR], w=[b1t[s]])
            p.DM("sp", b2f[s][:], moe_b2[li, e:e + 1, :], r=[DR], w=[b2f[s]])

        load_w(0)
        for e in range(NE):
            s = e % 2
            if e + 1 < NE:
                load_w(e + 1)
            p.DM("sp", xg[:], xs[e * C:(e + 1) * C, :].rearrange("(ct q) d -> q ct d", q=128), r=[(T_xs, "*")], w=[xg, (T_xs, e)])
            p.V("act", "activation", b2b[:], b2f[s][:], AF.Copy, r=[b2f[s]], w=[b2b])
            for ct in range(CT):
                bank = 4 + (ct % 2)
                for kc in range(8):
                    p.V("pe", "transpose", psbf(bank, 1024)[:, kc * 128:(kc + 1) * 128], xg[:, ct, kc * 128:(kc + 1) * 128], identb[:, :],
                        r=[xg, identb], w=[PS[bank]])
                evac(xgT[:, :, ct * 128:(ct + 1) * 128], psbf(bank, 1024).rearrange("p (a b) -> p a b", a=8), r=[PS[bank]], w=[(xgT, ct)])
            for i in range(8):
                for part in range(2):
                    fc = i + part * 8
                    banks = [(0, 1), (2, 3)][(i * 2 + part) % 2]
                    for gi, (ca, cb_) in enumerate(cgs):
                        for kc in range(8):
                            p.V("pe", "matmul", PS[banks[gi]][:, 0:cb_ - ca], w1b[s][:, kc, fc * 128:(fc + 1) * 128], xgT[:, kc, ca:cb_],
                                start=(kc == 0), stop=(kc == 7), r=[w1b[s], xgT], w=[PS[banks[gi]]])
                    for gi, (ca, cb_) in enumerate(cgs):
                        src = PS[banks[gi]][:, 0:cb_ - ca]
                        if part == 0:
                            p.V("dve", "tensor_scalar", glu[:, ca:cb_], src, b1t[s][:, fc:fc + 1], 7.0, ALU.add, ALU.min, r=[PS[banks[gi]], b1t[s]], w=[(glu, gi)])
                        else:
                            p.V("dve", "tensor_scalar", lin[:, ca:cb_], src, b1t[s][:, fc:fc + 1], 7.0, ALU.add, ALU.min, r=[PS[banks[gi]], b1t[s]], w=[(lin, gi)])
                    if part == 0:
                        p.V("act", "activation", sig[:, :], glu[:, :], AF.Sigmoid, scale=1.702, r=[glu], w=[sig])
                        p.V("pool", "tensor_tensor", glu[:, :], glu[:, :], sig[:, :], ALU.mult, r=[glu, sig], w=[glu])
                    else:
                        p.V("dve", "tensor_scalar", lin[:, :], lin[:, :], -7.0, 1.0, ALU.max, ALU.add, r=[lin], w=[lin])
                        p.V("pool", "tensor_tensor", actT[:, i, :], glu[:, :], lin[:, :], ALU.mult, r=[glu, lin], w=[(actT, i)])
            for ct in range(CT):
                yb = yo[ct % 2]
                for half in range(2):
                    bank = 4 + (ct % 2) * 2 + half
                    for fc in range(8):
                        p.V("pe", "matmul", PS[bank][:, :], actT[:, fc, ct * 128:(ct + 1) * 128], w2b[s][:, fc, half * 512:(half + 1) * 512],
                            start=(fc == 0), stop=False, r=[actT, w2b[s]], w=[PS[bank]])
                    p.V("pe", "matmul", PS[bank][:, :], ones_b[0:1, :], b2b[0:1, half * 512:(half + 1) * 512], start=False, stop=True,
                        r=[ones_b, b2b], w=[PS[bank]])
                    evac(yb[:, half * 512:(half + 1) * 512], PS[bank][:, :], r=[PS[bank]], w=[(yb, half)])
                p.DM("sp", ys[e * C + ct * 128:e * C + (ct + 1) * 128, :], yb[:, :], r=[yb], w=[(T_ys, (e, ct))])
        p.release(m0)

    def phaseC(li):
        m0 = p.mark()
        wg = p.sb("wg", [128, 8, D], BF16)
        load_w_bf16(wg, ple_gw[li], 8)
        wp = p.sb("wp", [128, 2, D], BF16)
        load_w_bf16(wp, ple_w[li], 2)
        g2 = p.sb("ln2g", [128, D]); b2 = p.sb("ln2b", [128, D])
        bcast_load(g2, ln2_g[li:li + 1, :]); bcast_load(b2, ln2_b[li:li + 1, :])
        rows = [[p.sb(f"row{s}{k}", [128, D]) for k in range(4)] for s in range(2)]
        x1t = [p.sb("x1t0", [128, D]), p.sb("x1t1", [128, D])]
        pt = [p.sb("pt0", [128, 256]), p.sb("pt1", [128, 256])]
        ff = p.sb("ff", [128, D]); x2 = p.sb("x2", [128, D]); x2b = p.sb("x2b", [128, D], BF16)
        x2T = p.sb("x2T", [128, 8, 128], BF16)
        pb = p.sb("pb", [128, 256], BF16); pT = p.sb("pT", [128, 2, 128], BF16)
        gt = p.sb("gt", [128, D]); x3 = p.sb("x3", [128, D])
        scr6 = p.sb("scr6", [128, 2, 6]); scr2 = p.sb("scr2", [128, 4])

        def loads(tl):
            s = tl["ti"] % 2
            ti, nt = tl["ti"], tl["nt"]
            for k in range(4):
                p.dma("pool", lambda e, k=k, ti=ti, s=s: e.indirect_dma_start(
                    out=rows[s][k][:, :], out_offset=None, in_=ys[:, :],
                    in_offset=bass.IndirectOffsetOnAxis(ap=slots_all[:, ti, k:k + 1], axis=0)),
                    r=[(T_ys, "*"), (slots_all, ti)], w=[rows[s][k]])
            p.DM("sp", x1t[s][0:nt, :], x1s[ti * 128:ti * 128 + nt, :], r=[(T_x1s, ti)], w=[x1t[s]])
            p.DM("sp", pt[s][0:nt, :], pin[li, tl["row0"]:tl["row0"] + nt, :], r=[DR], w=[pt[s]])

        loads(tiles[0])
        for tl in tiles:
            nt, ti = tl["nt"], tl["ti"]
            s = ti % 2
            if ti + 1 < NT:
                loads(tiles[ti + 1])
            p.V("dve", "tensor_scalar", ff[0:nt, :], rows[s][0][0:nt, :], gates_all[0:nt, ti, 0:1], None, ALU.mult, r=[rows[s][0], (gates_all, ti)], w=[ff])
            for k in range(1, 4):
                eng = "dve"
                p.V(eng, "scalar_tensor_tensor", ff[0:nt, :], rows[s][k][0:nt, :], gates_all[0:nt, ti, k:k + 1], ff[0:nt, :], ALU.mult, ALU.add,
                    r=[rows[s][k], (gates_all, ti), ff], w=[ff])
            p.V("dve", "scalar_tensor_tensor", ff[0:nt, :], x1t[s][0:nt, :], ALPHA, ff[0:nt, :], ALU.mult, ALU.add, r=[x1t[s], ff], w=[ff])
            layernorm("dve", ff, nt, g2, b2, x2, scr6, scr2)
            p.V("act", "activation", x2b[0:nt, :], x2[0:nt, :], AF.Copy, r=[x2], w=[x2b])
            transpose_to(x2T, x2b, nt, 8, 0)
            p.V("act", "activation", pb[0:nt, :], pt[s][0:nt, :], AF.Copy, r=[pt[s]], w=[pb])
            transpose_to(pT, pb, nt, 2, 1)
            for half in range(2):
                for kc in range(8):
                    p.V("pe", "matmul", PS[2 + half][0:nt, :], x2T[:, kc, 0:nt], wg[:, kc, half * 512:(half + 1) * 512], start=(kc == 0), stop=(kc == 7),
                        r=[x2T, wg], w=[PS[2 + half]])
                for kc in range(2):
                    p.V("pe", "matmul", PS[4 + half][0:nt, :], pT[:, kc, 0:nt], wp[:, kc, half * 512:(half + 1) * 512], start=(kc == 0), stop=(kc == 1),
                        r=[pT, wp], w=[PS[4 + half]])
                hs = slice(half * 512, (half + 1) * 512)
                p.V("act", "activation", gt[0:nt, hs], PS[2 + half][0:nt, :], AF.Sigmoid, r=[PS[2 + half]], w=[(gt, half)])
                p.V("dve", "tensor_tensor", gt[0:nt, hs], gt[0:nt, hs], PS[4 + half][0:nt, :], ALU.mult, r=[(gt, half), PS[4 + half]], w=[(gt, half)])
                p.V("pool", "tensor_tensor", x3[0:nt, hs], gt[0:nt, hs], x2[0:nt, hs], ALU.add, r=[(gt, half), x2], w=[(x3, half)])
            if li == 0:
                p.DM("sp", x3s[ti * 128:ti * 128 + nt, :], x3[0:nt, :], r=[x3], w=[(T_x3s, ti)])
                if "x3_0" in dbg_out:
                    p.DM("sp", dbg_out["x3_0"][tl["row0"]:tl["row0"] + nt, :], x3[0:nt, :], r=[x3], w=[T_out])
            else:
                p.DM("sp", y_out[tl["row0"]:tl["row0"] + nt, :], x3[0:nt, :], r=[x3], w=[T_out])
        p.release(m0)

    cst_host = host_consts()
    for st in stages:
        if st == "A0":
            phaseA0()
        elif st == "A1":
            phaseA1()
        elif st[0] == "M":
            phaseM(int(st[1]))
        elif st[0] == "C":
            phaseC(int(st[1]))
    p.finalize()
    return nc


def prep_shared(inp):
    f = lambda a: np.ascontiguousarray(np.asarray(a, dtype=np.float32))
    sh = {}
    sh["w_in_even"] = f(inp["w_in_even"][0])
    qb = lambda v, nb: np.ascontiguousarray(np.asarray(v, dtype=np.float32).reshape(nb, 128).T)
    sh["s5_are"] = qb(inp["s5_a_re"][0].reshape(-1), 16)
    sh["s5_aim"] = qb(inp["s5_a_im"][0].reshape(-1), 16)
    sh["s5_ldt"] = qb(np.repeat(np.asarray(inp["s5_log_dt"][0]), 64), 16)
    bst = np.zeros((2, 16, 128, 128), np.float32)
    cstt = np.zeros((2, 16, 128, 128), np.float32)
    for ri, (bsrc, csrc) in enumerate(((inp["s5_b_re"][0], inp["s5_c_re"][0]), (inp["s5_b_im"][0], inp["s5_c_im"][0]))):
        bsrc = np.asarray(bsrc)
        csrc = np.asarray(csrc)
        for g in range(32):
            blk = g // 2
            m0 = (g % 2) * 64
            k0 = (g % 8) * 16
            bst[ri, blk, k0:k0 + 16, m0:m0 + 64] = bsrc[g].T
            cstt[ri, blk, m0:m0 + 64, k0:k0 + 16] = csrc[g].T
    sh["s5_bst"] = bst
    sh["s5_cst"] = cstt
    sh["s5_d"] = qb(inp["s5_d"][0], 4)
    sh["s5_wglu"] = f(inp["s5_w_glu"][0])
    sh["s5_bglu"] = qb(inp["s5_b_glu"][0], 4)
    sh["gdn_convw"] = np.ascontiguousarray(np.asarray(inp["gdn_conv_w"][0], dtype=np.float32).reshape(4, 12, 128).transpose(2, 1, 0))
    sh["gdn_alog"] = f(inp["gdn_a_log"][0].reshape(1, 4))
    sh["gdn_dtb"] = f(inp["gdn_dt_bias"][0].reshape(1, 4))
    sh["gdn_normw"] = f(inp["gdn_norm_w"][0].reshape(1, 128))
    sh["w_out_even"] = f(inp["w_out_even"][0])
    wio = np.asarray(inp["w_in_odd"][0], dtype=np.float32)
    perm = np.arange(ODD_IN)
    for qk in range(2):
        for h in range(4):
            base = qk * 1024 + h * 256
            perm[base:base + 128] = base + np.arange(0, 256, 2)
            perm[base + 128:base + 256] = base + np.arange(1, 256, 2)
    sh["w_in_odd"] = np.ascontiguousarray(wio[:, perm])
    sh["w_out_odd"] = f(inp["w_out_odd"][0])
    for k in ("ln1_g", "ln1_b", "ln2_g", "ln2_b", "router_w", "router_b", "moe_w1", "moe_w2", "moe_b2", "ple_w"):
        sh[k] = f(inp[k])
    sh["moe_b1"] = np.ascontiguousarray(np.asarray(inp["moe_b1"], dtype=np.float32).reshape(2, NE, 16, 128).transpose(0, 1, 3, 2))
    sh["ple_gw"] = f(inp["ple_gate_w"])
    for k, v in host_consts().items():
        if k in CONST_SHAPES:
            sh["c_" + k] = np.ascontiguousarray(v.astype(np.float32)).reshape(CONST_SHAPES[k])
    return sh


def core_inputs(inp, sh, prompt_ids, sample_id, L):
    m = dict(sh)
    xp = [np.asarray(inp["x_prompt"][i][:L], dtype=np.float32) for i in prompt_ids]
    m["xin"] = np.ascontiguousarray(np.concatenate(xp + [np.asarray(inp["x_sample"][sample_id], dtype=np.float32)], axis=0))
    pp = [np.asarray(inp["p_prompt"][:, i, :L], dtype=np.float32) for i in prompt_ids]
    m["pin"] = np.ascontiguousarray(np.concatenate(pp + [np.asarray(inp["p_sample"][:, sample_id], dtype=np.float32)], axis=1))
    m["st_s5re"] = np.ascontiguousarray(np.asarray(inp["state_s5_re"][0, sample_id], dtype=np.float32).reshape(16, 128).T)
    m["st_s5im"] = np.ascontiguousarray(np.asarray(inp["state_s5_im"][0, sample_id], dtype=np.float32).reshape(16, 128).T)
    m["st_gdn"] = np.ascontiguousarray(np.asarray(inp["state_gdn"][0, sample_id], dtype=np.float32))
    m["st_conv"] = np.ascontiguousarray(np.asarray(inp["state_gdn_conv"][0, sample_id], dtype=np.float32).reshape(3, 12, 128).transpose(2, 1, 0))
    m["st_ret"] = np.ascontiguousarray(np.asarray(inp["state_ret"][0, sample_id], dtype=np.float32))
    return m


def unperm(k, a):
    a = np.asarray(a)
    if k in ("o_s5re", "o_s5im"):
        return a.reshape(128, 16).T.reshape(32, 64)
    if k == "o_conv":
        return a.reshape(128, 12, 3).transpose(2, 1, 0).reshape(3, 1536)
    return a


_CACHE = {}


def kernel(**inputs):
    NPS, L, C = 2, 2048, 768
    key = (NPS, L, C)
    if key not in _CACHE:
        _CACHE[key] = build(NPS, L, C)
    nc = _CACHE[key]
    sh = prep_shared(inputs)
    in_maps = [core_inputs(inputs, sh, [2 * c, 2 * c + 1], c, L) for c in range(8)]
    res = run_bass_kernel_spmd(nc, in_maps, core_ids=list(range(8)))
    R = res.results
    B, DB = 16, 8
    y_p = np.zeros((B, L, D), np.float32)
    y_s = np.zeros((DB, 16, D), np.float32)
    outs = {k: (np.zeros((1, B) + shp, np.float32), np.zeros((1, DB) + shp, np.float32))
            for k, shp in (("o_s5re", (32, 64)), ("o_s5im", (32, 64)), ("o_gdn", (4, 128, 128)), ("o_conv", (3, 1536)), ("o_ret", (4, 256, 512)))}
    for c in range(8):
        r = R[c]
        y = r["y_out"]
        for j in range(NPS):
            y_p[2 * c + j] = y[j * L:(j + 1) * L]
        y_s[c] = y[NPS * L:NPS * L + 16]
        for k, (po, so) in outs.items():
            a = r[k]
            for j in range(NPS):
                po[0, 2 * c + j] = unperm(k, a[j]).reshape(po.shape[2:])
            so[0, c] = unperm(k, a[NPS]).reshape(so.shape[2:])
    return (y_p, y_s, outs["o_s5re"][0], outs["o_s5im"][0], outs["o_gdn"][0], outs["o_conv"][0], outs["o_ret"][0],
            outs["o_s5re"][1], outs["o_s5im"][1], outs["o_gdn"][1], outs["o_conv"][1], outs["o_ret"][1])
```

```python
from contextlib import ExitStack
import math
import os
CUT = int(os.environ.get('KCUT', '99'))
CUT2 = int(os.environ.get('KCUT2', '99'))
import numpy as np
import concourse.bass as bass
import concourse.mybir as mybir
from concourse.bass_utils import run_bass_kernel_spmd

F32 = mybir.dt.float32
BF16 = mybir.dt.bfloat16
I32 = mybir.dt.int32
U32 = mybir.dt.uint32
ALU = mybir.AluOpType
AF = mybir.ActivationFunctionType

ENGS = ("pe", "dve", "act", "pool", "sp")
SEM_CHUNK = 30000
SAME_ENG_DIST = int(os.environ.get('KSED', '1000000000'))

D = 1024
NE = 32
TOPK = 4
ALPHA = 4.0 ** 0.25
LN_EPS = 1e-5
NORM_EPS = 1e-6
EVEN_IN = 2568
ODD_IN = 6144


class Instr:
    __slots__ = ("eng", "fn", "waits", "is_dma", "sig", "key", "val", "idx", "clock", "sval")

    def __init__(self, eng, fn, is_dma):
        self.eng = eng
        self.fn = fn
        self.is_dma = is_dma
        self.waits = []
        self.sig = False
        self.key = None
        self.val = 0
        self.idx = 0
        self.clock = None
        self.sval = None


class Trk:
    def __init__(self, name=""):
        self.name = name
        self.ent = {}

    def _conf(self, k):
        if k == "*":
            return list(self.ent.values())
        out = []
        e = self.ent.get(k)
        if e is not None:
            out.append(e)
        e = self.ent.get("*")
        if e is not None:
            out.append(e)
        return out


class Buf:
    def __init__(self, h, name):
        self.h = h
        self.trk = Trk(name)

    def __getitem__(self, k):
        return self.h[k]


def alias(ap, parent, name="alias"):
    b = Buf(ap, name)
    b.trk = parent.trk
    return b


class Prog:
    def __init__(self, nc, sb_words):
        self.nc = nc
        self.q = {e: [] for e in ENGS}
        self.clock = {e: {} for e in ENGS}
        self.es = ExitStack()
        self.dma_sems = {}
        self.dma_rr = {e: 0 for e in ENGS}
        self.n_dma_sems = 8
        self.pending = {e: [] for e in ENGS}
        self.big = self.es.enter_context(nc.sbuf_tensor("big", [128, sb_words], F32))
        self.sb_words = sb_words
        self.top = 0
        self.psn = 0

    def sb(self, name, shape, dtype=F32):
        isz = 2 if dtype == BF16 else 4
        n = 1
        for s in shape[1:]:
            n *= s
        words = (n * isz + 3) // 4
        off = self.top
        self.top += words
        assert self.top <= self.sb_words, (name, self.top, self.sb_words)
        v = self.big[0:shape[0], off:off + words]
        if dtype != F32:
            v = v.bitcast(dtype)
        if dtype == BF16 and n % 2 == 1:
            v = v[:, 0:n]
        if len(shape) == 3:
            v = v.rearrange("p (a b) -> p a b", a=shape[1])
        elif len(shape) == 4:
            v = v.rearrange("p (a b c) -> p a b c", a=shape[1], b=shape[2])
        return Buf(v, name)

    def mark(self):
        return self.top

    def release(self, m):
        self.barrier()
        self.top = m

    def ps(self, name):
        t = self.es.enter_context(self.nc.psum_tensor(name, [128, 512], F32))
        b = Buf(t, name)
        b.trk.excl = True
        return b

    def barrier(self):
        lasts = []
        for e in ENGS:
            if self.q[e]:
                for ins in reversed(self.q[e]):
                    if not ins.is_dma:
                        lasts.append(ins)
                        break
        for q, lst in self.dma_sems.items():
            for s in lst:
                if s[2] is not None:
                    lasts.append(s[2])
        for e in ENGS:
            self.pending[e] = list(lasts)

    def _norm(self, lst):
        out = []
        for x in lst:
            if isinstance(x, tuple):
                t, k = x
            else:
                t, k = x, "*"
            trk = t if isinstance(t, Trk) else t.trk
            out.append((trk, k))
        return out

    def _add_dep(self, ins, prod):
        if prod is None or prod is ins:
            return
        eng = ins.eng
        if prod.eng == "pe" and eng == "pe" and not prod.is_dma:
            return
        if (not prod.is_dma) and (not ins.is_dma) and prod.eng == eng and eng in ("dve", "act") \
                and ins.idx - prod.idx >= SAME_ENG_DIST:
            return
        clk = self.clock[eng]
        if clk.get(prod.key, -1) >= prod.val:
            return
        prod.sig = True
        ins.waits.append(prod)
        new = dict(clk)
        new[prod.key] = prod.val
        if prod.clock:
            for k, v in prod.clock.items():
                if new.get(k, -1) < v:
                    new[k] = v
        self.clock[eng] = new

    def _record(self, ins, reads, writes):
        if self.pending[ins.eng]:
            for pr in self.pending[ins.eng]:
                self._add_dep(ins, pr)
            self.pending[ins.eng] = []
        reads = self._norm(reads)
        writes = self._norm(writes)
        excl = [x for x in reads if getattr(x[0], "excl", False)]
        if excl:
            reads = [x for x in reads if not getattr(x[0], "excl", False)]
            writes = writes + [x for x in excl if x not in writes]
        for trk, k in reads:
            for e in trk._conf(k):
                self._add_dep(ins, e[0])
        for trk, k in writes:
            for e in trk._conf(k):
                self._add_dep(ins, e[0])
                for r in e[1]:
                    self._add_dep(ins, r)
        for trk, k in reads:
            e = trk.ent.get(k)
            if e is None:
                e = trk.ent[k] = [None, []]
            e[1].append(ins)
        for trk, k in writes:
            if k == "*":
                trk.ent.clear()
            trk.ent[k] = [ins, []]
        ins.clock = self.clock[ins.eng]
        self.q[ins.eng].append(ins)

    def op(self, eng, fn, r=(), w=()):
        ins = Instr(eng, fn, False)
        ins.idx = len(self.q[eng])
        ins.key = eng
        ins.val = ins.idx
        self._record(ins, r, w)
        return ins

    def V(self, eng, meth, *args, r=(), w=(), **kw):
        return self.op(eng, lambda e: getattr(e, meth)(*args, **kw), r=r, w=w)

    def dma(self, eng, fn, r=(), w=()):
        ins = Instr(eng, fn, True)
        ins.idx = len(self.q[eng])
        sems = self.dma_sems.setdefault(eng, [])
        if len(sems) < self.n_dma_sems:
            s = [f"dq_{eng}_{len(sems)}", 0, None]
            sems.append(s)
        else:
            s = sems[self.dma_rr[eng] % self.n_dma_sems]
        self.dma_rr[eng] += 1
        if s[2] is not None:
            self._add_dep(ins, s[2])
        s[1] += 1
        s[2] = ins
        ins.key = s[0]
        ins.val = s[1]
        ins.sig = True
        self._record(ins, r, w)
        return ins

    def DM(self, eng, out, in_, r=(), w=(), **kw):
        return self.dma(eng, lambda e: e.dma_start(out=out, in_=in_, **kw), r=r, w=w)

    def finalize(self):
        nc = self.nc
        sem_names = set()
        for e in ENGS:
            cnt = 0
            for ins in self.q[e]:
                if ins.is_dma:
                    sem_names.add(ins.key)
                elif ins.sig:
                    ep, v = divmod(cnt, SEM_CHUNK)
                    ins.sval = (f"e_{e}_{ep}", v + 1)
                    sem_names.add(ins.sval[0])
                    cnt += 1
        sems = {}
        for n in sorted(sem_names):
            sems[n] = self.es.enter_context(nc.semaphore(n))

        def semval(prod):
            if prod.is_dma:
                return sems[prod.key], prod.val * 16
            return sems[prod.sval[0]], prod.sval[1]

        def run(e, eng_name):
            for ins in self.q[eng_name]:
                best = {}
                for pr in ins.waits:
                    s, v = semval(pr)
                    k = id(s)
                    if k not in best or best[k][1] < v:
                        best[k] = (s, v)
                ws = list(best.values())
                attach = None
                if ws and eng_name != "pe":
                    attach = ws.pop()
                for s, v in ws:
                    e.wait_ge(s, v)
                bi = ins.fn(e)
                if attach is not None:
                    bi._wait_ge(attach[0], attach[1])
                if ins.is_dma:
                    bi.then_inc(sems[ins.key], 16)
                elif ins.sig:
                    bi.then_inc(sems[ins.sval[0]], 1)

        block = self.es.enter_context(nc.Block())

        @block.tensor
        def _(e):
            run(e, "pe")

        @block.vector
        def _(e):
            run(e, "dve")

        @block.scalar
        def _(e):
            run(e, "act")

        @block.gpsimd
        def _(e):
            run(e, "pool")

        @block.sync
        def _(e):
            run(e, "sp")
            for q, lst in self.dma_sems.items():
                for s in lst:
                    if s[1] > 0:
                        e.wait_ge(sems[s[0]], s[1] * 16)

        self.es.close()
        return nc


def host_consts():
    c = {}
    c["identf"] = np.eye(128, dtype=np.float32)
    jj = np.arange(128)[:, None]
    ii = np.arange(128)[None, :]
    c["uincl"] = (jj <= ii).astype(np.float32)
    c["ustrict"] = (jj < ii).astype(np.float32)
    c["ones"] = np.ones((128, 128), np.float32)
    c["iota_e"] = np.tile(np.arange(32, dtype=np.float32)[None, :], (128, 1))
    c["tau"] = np.tile(np.arange(1, 129, dtype=np.float32)[None, :], (128, 1))
    c["pidx"] = np.arange(128, dtype=np.float32)[:, None].copy()
    log_g = np.log(1.0 - 2.0 ** (-5.0 - np.arange(4, dtype=np.float32))).astype(np.float32)
    M = np.zeros((4, 128, 128), np.float32)
    for h in range(4):
        m = np.exp(log_g[h] * np.abs(ii - jj).astype(np.float32))
        m = np.where((jj >= 64) & (ii < 64), 0.0, m)
        M[h] = m
    c["retmask"] = np.ascontiguousarray(M.transpose(1, 0, 2)).astype(np.float32)
    qs = np.zeros((128, 4, 128), np.float32)
    for h in range(4):
        qs[:, h, :] = np.exp(log_g[h] * (np.arange(128, dtype=np.float32) + 1.0))[None, :]
    c["retqs"] = qs
    ks = np.zeros((128, 8), np.float32)
    for h in range(4):
        ks[:, h] = np.exp(log_g[h] * (127.0 - np.arange(128, dtype=np.float32)))
        ks[:, 4 + h] = np.exp(log_g[h] * (15.0 - np.arange(128, dtype=np.float32)))
    c["retks"] = ks * np.float32(256 ** -0.5)
    c["retcdec"] = [[float(np.exp(log_g[h] * 128.0)) for h in range(4)],
                    [float(np.exp(log_g[h] * 16.0)) for h in range(4)]]
    freq = (1.0 / (10000.0 ** np.linspace(0.0, 1.0, 128, dtype=np.float32))).astype(np.float32)
    pos = np.arange(2048, dtype=np.float32)
    ang = (pos[None, :] * freq[:, None]).astype(np.float32)
    c["rcos"] = np.cos(ang).astype(np.float32)
    c["rsin"] = np.sin(ang).astype(np.float32)
    return c


LAST_DIN = {}
CONST_SHAPES = {"identf": [128, 128], "uincl": [128, 128], "ustrict": [128, 128], "ones": [128, 128],
                "iota_e": [128, 32], "tau": [128, 128], "pidx": [128, 1], "retmask": [128, 4, 128],
                "retqs": [128, 4, 128], "retks": [128, 8], "rcos": [128, 2048], "rsin": [128, 2048]}


def build(NPS, L, C, stages=("A0", "M0", "C0", "A1", "M1", "C1"), dbg=()):
    nc = bass.Bass("TRN2", target_bir_lowering=False)
    NSEQ = NPS + 1
    NTOK = NPS * L + 16
    TPS = L // 128
    tiles = []
    for s in range(NPS):
        for t in range(TPS):
            tiles.append(dict(row0=s * L + t * 128, nt=128, seq=s, first=(t == 0), last=(t == TPS - 1),
                              pos0=t * 128, ti=len(tiles)))
    tiles.append(dict(row0=NPS * L, nt=16, seq=NPS, first=True, last=True, pos0=1024, ti=len(tiles)))
    NT = len(tiles)
    NROWP = NT * 128
    CT = C // 128
    TRASH = NE * C
    cgs = []
    c0 = 0
    while c0 < C:
        cgs.append((c0, min(C, c0 + 512)))
        c0 += 512

    def din(name, shape, dt=F32):
        LAST_DIN[name] = list(shape)
        return nc.dram_tensor(name, list(shape), dt, kind="ExternalInput").ap()

    def dout(name, shape, dt=F32):
        return nc.dram_tensor(name, list(shape), dt, kind="ExternalOutput").ap()

    def dint(name, shape, dt=F32):
        return nc.dram_tensor(name, list(shape), dt, kind="Internal").ap()

    xin = din("xin", [NTOK, D])
    pin = din("pin", [2, NTOK, 256])
    st_s5re = din("st_s5re", [128, 16])
    st_s5im = din("st_s5im", [128, 16])
    st_gdn = din("st_gdn", [4, 128, 128])
    st_conv = din("st_conv", [128, 12, 3])
    st_ret = din("st_ret", [4, 256, 512])
    w_in_even = din("w_in_even", [D, EVEN_IN])
    s5_are = din("s5_are", [128, 16])
    s5_aim = din("s5_aim", [128, 16])
    s5_ldt = din("s5_ldt", [128, 16])
    s5_bst = din("s5_bst", [2, 16, 128, 128])
    s5_cst = din("s5_cst", [2, 16, 128, 128])
    s5_d = din("s5_d", [128, 4])
    s5_wglu = din("s5_wglu", [512, 512])
    s5_bglu = din("s5_bglu", [128, 4])
    gdn_convw = din("gdn_convw", [128, 12, 4])
    gdn_alog = din("gdn_alog", [1, 4])
    gdn_dtb = din("gdn_dtb", [1, 4])
    gdn_normw = din("gdn_normw", [1, 128])
    w_out_even = din("w_out_even", [D, D])
    w_in_odd = din("w_in_odd", [D, ODD_IN])
    w_out_odd = din("w_out_odd", [2048, D])
    ln1_g = din("ln1_g", [2, D])
    ln1_b = din("ln1_b", [2, D])
    ln2_g = din("ln2_g", [2, D])
    ln2_b = din("ln2_b", [2, D])
    router_w = din("router_w", [2, D, NE])
    router_b = din("router_b", [2, NE])
    moe_w1 = din("moe_w1", [2, NE, D, 2 * D])
    moe_b1 = din("moe_b1", [2, NE, 128, 16])
    moe_w2 = din("moe_w2", [2, NE, D, D])
    moe_b2 = din("moe_b2", [2, NE, D])
    ple_w = din("ple_w", [2, 256, D])
    ple_gw = din("ple_gw", [2, D, D])
    cst = {k: din("c_" + k, v) for k, v in CONST_SHAPES.items()}

    y_out = dout("y_out", [NTOK, D])
    o_s5re = dout("o_s5re", [NSEQ, 128, 16])
    o_s5im = dout("o_s5im", [NSEQ, 128, 16])
    o_gdn = dout("o_gdn", [NSEQ, 4, 128, 128])
    o_conv = dout("o_conv", [NSEQ, 128, 12, 3])
    o_ret = dout("o_ret", [NSEQ, 4, 256, 512])
    dbg_out = {k: dout("dbg_" + k, [NTOK, D]) for k in dbg if k.startswith("x")}
    taps = {}

    def tap(name, ap, ti, npart, width, dt=F32):
        if ("t_" + name) not in dbg:
            return
        if name not in taps:
            taps[name] = dout("tap_" + name, [NT, 128, width], dt)
        p.DM("sp", taps[name][ti, 0:npart, :], ap, r=[tapsrc[0]], w=[T_out])

    tapsrc = [None]

    x1s = dint("x1s", [NROWP, D])
    x3s = dint("x3s", [NROWP, D])
    yas = dint("yas", [NT, 128, 4, 128], BF16)
    T_yas = Trk("yas")
    xs = dint("xs", [NE * C + 128, D], BF16)
    ys = dint("ys", [NE * C + 128, D])

    p = Prog(nc, 52900)
    DR = Trk("dram_in")
    T_x1s, T_x3s, T_xs, T_ys, T_out = Trk("x1s"), Trk("x3s"), Trk("xs"), Trk("ys"), Trk("out")
    PS = [p.ps(f"ps{i}") for i in range(8)]

    def psbf(b, n):
        return PS[b][:, 0:n // 2].bitcast(BF16)

    identf = p.sb("identf", [128, 128])
    identb = p.sb("identb", [128, 128], BF16)
    uincl = p.sb("uincl", [128, 128])
    ustr_b = p.sb("ustr_b", [128, 128], BF16)
    ones_f = p.sb("ones_f", [128, 128])
    ones_b = p.sb("ones_b", [128, 128], BF16)
    iota_e = p.sb("iota_e", [128, 32])
    pidx = p.sb("pidx", [128, 1])
    gates_all = p.sb("gates_all", [128, NT, 4])
    slots_all = p.sb("slots_all", [128, NT, 4], I32)
    tmpc = p.sb("tmpc", [128, 128])
    for nm, b in (("identf", identf), ("uincl", uincl), ("ones", ones_f), ("iota_e", iota_e), ("pidx", pidx)):
        p.DM("sp", b[:], cst[nm], r=[DR], w=[b])
    p.DM("sp", tmpc[:], cst["ustrict"], r=[DR], w=[tmpc])
    p.V("dve", "tensor_copy", ustr_b[:], tmpc[:], r=[tmpc], w=[ustr_b])
    p.V("dve", "tensor_copy", identb[:], identf[:], r=[identf], w=[identb])
    p.V("dve", "tensor_copy", ones_b[:], ones_f[:], r=[ones_f], w=[ones_b])

    rr = {"ev": 0}

    def evac(out_ap, in_ap, r, w):
        rr["ev"] += 1
        if rr["ev"] % 2:
            p.V("act", "activation", out_ap, in_ap, AF.Copy, r=r, w=w)
        else:
            p.V("dve", "tensor_copy", out_ap, in_ap, r=r, w=w)

    def bcast_load(buf, src_row):
        p.DM("sp", buf[:], src_row.partition_broadcast(128), r=[DR], w=[buf])

    def load_w_bf16(buf, src, kc):
        n = src.shape[1]
        nch = (n + 2047) // 2048
        step = (n + nch - 1) // nch
        v = src.rearrange("(kc q) n -> q kc n", q=128)
        for c0 in range(0, n, step):
            c1 = min(n, c0 + step)
            p.DM("pool", buf[:, :, c0:c1], v[:, :, c0:c1], r=[DR], w=[(buf, c0)] if nch > 1 else [buf])

    def layernorm(eng_h, h, nt, gt, bt, out, scr6, scr2):
        p.V("dve", "bn_stats", scr6[0:nt, 0, :], h[0:nt, 0:512], r=[h], w=[(scr6, 0)])
        p.V("dve", "bn_stats", scr6[0:nt, 1, :], h[0:nt, 512:1024], r=[h], w=[(scr6, 1)])
        p.V("dve", "bn_aggr", scr2[0:nt, 0:2], scr6[0:nt, :, :].rearrange("p a b -> p (a b)"), r=[scr6], w=[scr2])
        p.V("act", "activation", scr2[0:nt, 2:3], scr2[0:nt, 1:2], AF.Sqrt, bias=epsln[0:nt, 0:1], r=[scr2, epsln], w=[(scr2, "s")])
        p.V("dve", "reciprocal", scr2[0:nt, 3:4], scr2[0:nt, 2:3], r=[(scr2, "s")], w=[(scr2, "r")])
        p.V("dve", "tensor_scalar", out[0:nt, :], h[0:nt, :], scr2[0:nt, 0:1], scr2[0:nt, 3:4], ALU.subtract, ALU.mult,
            r=[h, scr2, (scr2, "r")], w=[out])
        p.V("pool", "tensor_tensor", out[0:nt, :], out[0:nt, :], gt[0:nt, :], ALU.mult, r=[out, gt], w=[out])
        p.V("pool", "tensor_tensor", out[0:nt, :], out[0:nt, :], bt[0:nt, :], ALU.add, r=[out, bt], w=[out])

    epsln = p.sb("epsln", [128, 2])
    p.V("dve", "memset", epsln[:, 0:1], LN_EPS, w=[(epsln, 0)])
    p.V("dve", "memset", epsln[:, 1:2], NORM_EPS, w=[(epsln, 1)])

    def transpose_to(dstT, src_bf, nt, nblk, bank):
        for b0 in range(0, nblk, 8):
            nb = min(8, nblk - b0)
            for b in range(nb):
                p.V("pe", "transpose", psbf(bank, 1024)[:, b * 128:b * 128 + nt], src_bf[0:nt, (b0 + b) * 128:(b0 + b + 1) * 128],
                    identb[0:nt, 0:nt], r=[src_bf, identb], w=[PS[bank]])
            evac(dstT[:, b0:b0 + nb, 0:nt], psbf(bank, 1024).rearrange("p (a b) -> p a b", a=8)[:, 0:nb, 0:nt],
                 r=[PS[bank]], w=[dstT])

    def phaseA_tail(li, tl, xt, mixps, lnw, rw, rb, work):
        nt, ti = tl["nt"], tl["ti"]
        h, x1, xrow, x1T, lg, scr6, scr2, small, Mb, tot = work
        p.V("dve", "scalar_tensor_tensor", h[0:nt, 0:512], xt[0:nt, 0:512], ALPHA, PS[mixps[0]][0:nt, :], ALU.mult, ALU.add,
            r=[xt, PS[mixps[0]]], w=[(h, 0)])
        p.V("dve", "scalar_tensor_tensor", h[0:nt, 512:1024], xt[0:nt, 512:1024], ALPHA, PS[mixps[1]][0:nt, :], ALU.mult, ALU.add,
            r=[xt, PS[mixps[1]]], w=[(h, 1)])
        layernorm("dve", h, nt, lnw[0], lnw[1], x1, scr6, scr2)
        p.DM("sp", x1s[ti * 128:ti * 128 + nt, :], x1[0:nt, :], r=[x1], w=[(T_x1s, ti)])
        if ("x1_%d" % li) in dbg_out:
            p.DM("sp", dbg_out["x1_%d" % li][tl["row0"]:tl["row0"] + nt, :], x1[0:nt, :], r=[x1], w=[T_out])
        p.V("act", "activation", xrow[0:nt, :], x1[0:nt, :], AF.Copy, r=[x1], w=[xrow])
        for half in range(2):
            for b in range(4):
                kc = half * 4 + b
                p.V("pe", "transpose", PS[6][:, b * 128:b * 128 + nt], x1[0:nt, kc * 128:(kc + 1) * 128], identf[0:nt, 0:nt],
                    r=[x1, identf], w=[PS[6]])
            evac(x1T[:, half * 4:half * 4 + 4, 0:nt], PS[6][:, :].rearrange("p (a b) -> p a b", a=4)[:, :, 0:nt], r=[PS[6]], w=[x1T])
        for kc in range(8):
            p.V("pe", "matmul", PS[7][0:nt, 0:32], x1T[:, kc, 0:nt], rw[:, kc, :], start=(kc == 0), stop=(kc == 7),
                r=[x1T, rw], w=[PS[7]])
        p.V("dve", "tensor_tensor", lg[0:nt, :], PS[7][0:nt, 0:32], rb[0:nt, :], ALU.add, r=[PS[7], rb], w=[lg])
        top, ti8, nt0, ex, gs, ef, rk, sl, ov, tmp32, rnk = small
        p.V("dve", "max", top[0:nt, :], lg[0:nt, :], r=[lg], w=[top])
        p.V("dve", "max_index", ti8[0:nt, :], top[0:nt, :], lg[0:nt, :], r=[lg, top], w=[ti8])
        p.V("dve", "tensor_scalar", nt0[0:nt, :], top[0:nt, 0:1], -1.0, None, ALU.mult, r=[top], w=[nt0])
        p.V("act", "activation", ex[0:nt, :], top[0:nt, 0:4], AF.Exp, bias=nt0[0:nt, 0:1], r=[top, nt0], w=[ex])
        p.V("dve", "reduce_sum", gs[0:nt, 0:1], ex[0:nt, :], mybir.AxisListType.X, r=[ex], w=[gs])
        p.V("dve", "reciprocal", gs[0:nt, 1:2], gs[0:nt, 0:1], r=[gs], w=[(gs, "r")])
        p.V("dve", "tensor_scalar", gates_all[0:nt, ti, :], ex[0:nt, :], gs[0:nt, 1:2], None, ALU.mult, r=[ex, (gs, "r")], w=[(gates_all, ti)])
        p.V("pool", "memset", Mb[:], 0.0, w=[Mb])
        p.V("dve", "tensor_scalar", Mb[0:nt, :], lg[0:nt, :], top[0:nt, 3:4], None, ALU.is_ge, r=[lg, top], w=[Mb])
        p.V("pe", "matmul", PS[7][:, 64:96], ustr_b[:, :], Mb[:, :], start=True, stop=True, r=[ustr_b, Mb], w=[PS[7]])
        p.V("pe", "matmul", PS[7][:, 96:128], ones_b[:, :], Mb[:, :], start=True, stop=True, r=[ones_b, Mb], w=[PS[7]])
        p.V("dve", "tensor_tensor", rnk[:, :], PS[7][:, 64:96], tot[:, :], ALU.add, r=[PS[7], tot], w=[rnk])
        p.V("dve", "tensor_tensor", tot[:, :], PS[7][:, 96:128], tot[:, :], ALU.add, r=[PS[7], tot], w=[tot])
        p.V("dve", "tensor_copy", ef[0:nt, :], ti8[0:nt, 0:4], r=[ti8], w=[ef])
        for k in range(4):
            p.V("dve", "scalar_tensor_tensor", tmp32[0:nt, :], iota_e[0:nt, :], ef[0:nt, k:k + 1], rnk[0:nt, :], ALU.is_equal, ALU.mult,
                accum_out=rk[0:nt, k:k + 1], r=[iota_e, ef, rnk], w=[tmp32, (rk, k)])
        p.V("dve", "scalar_tensor_tensor", sl[0:nt, :], ef[0:nt, :], float(C), rk[0:nt, :], ALU.mult, ALU.add, r=[ef, rk], w=[sl])
        p.V("dve", "tensor_scalar", ov[0:nt, :], rk[0:nt, :], float(C), None, ALU.is_ge, r=[rk], w=[ov])
        p.V("dve", "tensor_scalar", tmp32[0:nt, 0:4], sl[0:nt, :], -1.0, pidx[0:nt, 0:1], ALU.mult, ALU.add, r=[sl, pidx], w=[tmp32])
        p.V("dve", "tensor_scalar", tmp32[0:nt, 0:4], tmp32[0:nt, 0:4], float(TRASH), None, ALU.add, r=[tmp32], w=[tmp32])
        p.V("dve", "tensor_tensor", tmp32[0:nt, 0:4], tmp32[0:nt, 0:4], ov[0:nt, :], ALU.mult, r=[tmp32, ov], w=[tmp32])
        p.V("dve", "tensor_tensor", sl[0:nt, :], sl[0:nt, :], tmp32[0:nt, 0:4], ALU.add, r=[sl, tmp32], w=[sl])
        if nt < 128:
            p.V("dve", "tensor_scalar", tmp32[:, 0:4], pidx[:, 0:1].to_broadcast([128, 4]), float(TRASH), None, ALU.add, r=[pidx], w=[tmp32])
            p.V("dve", "tensor_copy", slots_all[:, ti, :], tmp32[:, 0:4], r=[tmp32], w=[(slots_all, ti)])
        p.V("dve", "tensor_copy", slots_all[0:nt, ti, :], sl[0:nt, :], r=[sl], w=[(slots_all, ti)])
        for k in range(4):
            p.dma("pool", lambda e, k=k, ti=ti: e.indirect_dma_start(
                out=xs[:, :], out_offset=bass.IndirectOffsetOnAxis(ap=slots_all[:, ti, k:k + 1], axis=0),
                in_=xrow[:, :], in_offset=None), r=[xrow, (slots_all, ti), (T_xs, "*")], w=[])

    def alloc_tail_work(h=None, x1=None, x1T=None):
        if h is None:
            h = p.sb("h", [128, D])
        if x1 is None:
            x1 = p.sb("x1", [128, D])
        xrow = p.sb("xrow", [128, D], BF16)
        if x1T is None:
            x1T = p.sb("x1T", [128, 8, 128])
        lg = p.sb("lg", [128, 32])
        scr6 = p.sb("scr6", [128, 2, 6])
        scr2 = p.sb("scr2", [128, 4])
        small = (p.sb("top", [128, 8]), p.sb("ti8", [128, 8], U32), p.sb("nt0", [128, 1]), p.sb("ex", [128, 4]),
                 p.sb("gs", [128, 2]), p.sb("ef", [128, 4]), p.sb("rk", [128, 4]), p.sb("sl", [128, 4]),
                 p.sb("ov", [128, 4]), p.sb("tmp32", [128, 32]), p.sb("rnk", [128, 32]))
        Mb = p.sb("Mb", [128, 32], BF16)
        tot = p.sb("tot", [128, 32])
        p.V("dve", "memset", tot[:], 0.0, w=[tot])
        p.V("pool", "memset", xrow[:, :], 0.0, w=[xrow])
        return (h, x1, xrow, x1T, lg, scr6, scr2, small, Mb, tot)

    def load_ln_router(li):
        g1 = p.sb("ln1g", [128, D]); b1 = p.sb("ln1b", [128, D])
        bcast_load(g1, ln1_g[li:li + 1, :]); bcast_load(b1, ln1_b[li:li + 1, :])
        rw = p.sb("rw", [128, 8, NE])
        p.DM("sp", rw[:], router_w[li].rearrange("(kc q) n -> q kc n", q=128), r=[DR], w=[rw])
        rb = p.sb("rb", [128, NE])
        bcast_load(rb, router_b[li:li + 1, :])
        return (g1, b1), rw, rb

    def phaseA0():
        m0 = p.mark()
        win = p.sb("win", [128, 8, EVEN_IN], BF16)
        load_w_bf16(win, w_in_even, 8)
        wout = p.sb("wout", [128, 8, D], BF16)
        load_w_bf16(wout, w_out_even, 8)
        wglu = p.sb("wglu", [128, 4, 512], BF16)
        load_w_bf16(wglu, s5_wglu, 4)
        bst = p.sb("bst", [128, 2, 16, 128], BF16)
        cstt = p.sb("cstt", [128, 2, 16, 128], BF16)
        for ri in range(2):
            p.DM("pool", bst[:, ri, :, :], s5_bst[ri].rearrange("b k m -> k b m"), r=[DR], w=[(bst, ri)])
            p.DM("pool", cstt[:, ri, :, :], s5_cst[ri].rearrange("b k m -> k b m"), r=[DR], w=[(cstt, ri)])
        lnw, rw, rb = load_ln_router(0)
        are = p.sb("are", [128, 16]); aim = p.sb("aim", [128, 16]); ldt = p.sb("ldt", [128, 16])
        for b, s in ((are, s5_are), (aim, s5_aim), (ldt, s5_ldt)):
            p.DM("sp", b[:], s, r=[DR], w=[b])
        dsk = p.sb("dsk", [128, 4]); bgl = p.sb("bgl", [128, 4])
        p.DM("sp", dsk[:], s5_d, r=[DR], w=[dsk])
        p.DM("sp", bgl[:], s5_bglu, r=[DR], w=[bgl])
        tau = p.sb("tau", [128, 128])
        p.DM("sp", tau[:], cst["tau"], r=[DR], w=[tau])
        lam = p.sb("lam", [128, 16]); li_ = p.sb("li", [128, 16]); dtt = p.sb("dtt", [128, 16])
        p.V("act", "activation", dtt[:], ldt[:], AF.Exp, r=[ldt], w=[dtt])
        p.V("dve", "tensor_tensor", li_[:], aim[:], dtt[:], ALU.mult, r=[aim, dtt], w=[li_])
        p.V("dve", "tensor_tensor", lam[:], are[:], dtt[:], ALU.mult, r=[are, dtt], w=[lam])
        p.V("act", "activation", lam[:], lam[:], AF.Exp, r=[lam], w=[lam])
        cosT = p.sb("cosT", [128, 16, 128]); sinT = p.sb("sinT", [128, 16, 128])
        crT = p.sb("crT", [128, 16, 128]); ciT = p.sb("ciT", [128, 16, 128])
        ang = crT
        kq = ciT
        gsc = p.sb("gsc", [128, 2, 16, 128])
        ki = Buf(gsc[:, 0, :, :].bitcast(I32), "ki")
        ki.trk = gsc.trk
        TWO_PI = 2.0 * math.pi

        def sin_of(dst, shift):
            p.V("dve", "tensor_tensor", ang[:], li_[:, :].unsqueeze(2).to_broadcast([128, 16, 128]),
                tau[:, :].unsqueeze(1).to_broadcast([128, 16, 128]), ALU.mult, r=[li_, tau], w=[ang])
            if shift != 0.0:
                p.V("dve", "tensor_scalar", ang[:], ang[:], shift, None, ALU.add, r=[ang], w=[ang])
            p.V("dve", "tensor_scalar", kq[:], ang[:], 1.0 / TWO_PI, None, ALU.mult, r=[ang], w=[kq])
            p.V("dve", "tensor_copy", ki[:], kq[:], r=[kq], w=[ki])
            p.V("dve", "tensor_copy", kq[:], ki[:], r=[ki], w=[kq])
            p.V("dve", "scalar_tensor_tensor", ang[:], kq[:], -TWO_PI, ang[:], ALU.mult, ALU.add, r=[kq, ang], w=[ang])
            p.V("dve", "tensor_scalar", kq[:], ang[:], math.pi, TWO_PI, ALU.is_gt, ALU.mult, r=[ang], w=[kq])
            p.V("dve", "tensor_tensor", ang[:], ang[:], kq[:], ALU.subtract, r=[ang, kq], w=[ang])
            p.V("dve", "tensor_scalar", kq[:], ang[:], -math.pi, TWO_PI, ALU.is_lt, ALU.mult, r=[ang], w=[kq])
            p.V("dve", "tensor_tensor", ang[:], ang[:], kq[:], ALU.add, r=[ang, kq], w=[ang])
            p.V("dve", "tensor_scalar", ang[:], ang[:], math.pi, -math.pi, ALU.min, ALU.max, r=[ang], w=[ang])
            p.V("act", "activation", dst[:], ang[:], AF.Sin, r=[ang], w=[dst])

        sin_of(sinT, 0.0)
        sin_of(cosT, math.pi / 2)
        sm = p.sb("s5sm", [128, 8, 16])
        abre, abim, den, t1, t2, cfre, cfim, t3 = [sm[:, i, :] for i in range(8)]
        S = [sm]
        p.V("dve", "tensor_tensor", abre, lam[:], cosT[:, :, 0], ALU.mult, r=[lam, cosT], w=S)
        p.V("dve", "tensor_tensor", abim, lam[:], sinT[:, :, 0], ALU.mult, r=[lam, sinT], w=S)
        p.V("dve", "tensor_scalar", abre, abre, -1.0, None, ALU.add, r=S, w=S)
        p.V("dve", "tensor_tensor", t1, are[:], are[:], ALU.mult, r=[are], w=S)
        p.V("dve", "tensor_tensor", t2, aim[:], aim[:], ALU.mult, r=[aim], w=S)
        p.V("dve", "tensor_tensor", den, t1, t2, ALU.add, r=S, w=S)
        p.V("dve", "reciprocal", den, den, r=S, w=S)
        p.V("dve", "tensor_tensor", t1, abre, are[:], ALU.mult, r=S + [are], w=S)
        p.V("dve", "tensor_tensor", t2, abim, aim[:], ALU.mult, r=S + [aim], w=S)
        p.V("dve", "tensor_tensor", cfre, t1, t2, ALU.add, r=S, w=S)
        p.V("dve", "tensor_tensor", cfre, cfre, den, ALU.mult, r=S, w=S)
        p.V("dve", "tensor_tensor", t1, abim, are[:], ALU.mult, r=S + [are], w=S)
        p.V("dve", "tensor_tensor", t2, abre, aim[:], ALU.mult, r=S + [aim], w=S)
        p.V("dve", "tensor_tensor", cfim, t1, t2, ALU.subtract, r=S, w=S)
        p.V("dve", "tensor_tensor", cfim, cfim, den, ALU.mult, r=S, w=S)
        sc3 = Buf(gsc[:, 1, :, :], "sc3")
        sc3.trk = gsc.trk
        bc = lambda a: a.unsqueeze(2).to_broadcast([128, 16, 128])
        p.V("dve", "tensor_tensor", crT[:], cosT[:], bc(cfre), ALU.mult, r=[cosT] + S, w=[crT])
        p.V("dve", "tensor_tensor", sc3[:], sinT[:], bc(cfim), ALU.mult, r=[sinT] + S, w=[sc3])
        p.V("dve", "tensor_tensor", crT[:], crT[:], sc3[:], ALU.add, r=[crT, sc3], w=[crT])
        p.V("dve", "tensor_tensor", ciT[:], cosT[:], bc(cfim), ALU.mult, r=[cosT] + S, w=[ciT])
        p.V("dve", "tensor_tensor", sc3[:], sinT[:], bc(cfre), ALU.mult, r=[sinT] + S, w=[sc3])
        p.V("dve", "tensor_tensor", ciT[:], ciT[:], sc3[:], ALU.subtract, r=[ciT, sc3], w=[ciT])
        wc = p.sb("wc", [128, 12, 4])
        p.DM("sp", wc[:], gdn_convw, r=[DR], w=[wc])
        alog = p.sb("alog", [128, 4]); dtb = p.sb("dtb", [128, 4]); nrmw = p.sb("nrmw", [128, 128])
        bcast_load(alog, gdn_alog); bcast_load(dtb, gdn_dtb); bcast_load(nrmw, gdn_normw)
        p.V("act", "activation", alog[:], alog[:], AF.Exp, r=[alog], w=[alog])
        Hre = p.sb("Hre", [128, 16]); Him = p.sb("Him", [128, 16])
        Sg = p.sb("Sg", [128, 4, 128])
        ctx3 = p.sb("ctx3", [128, 12, 3])
        xt_one = p.sb("xt0", [128, D])
        xts = [xt_one, xt_one]
        xb = p.sb("xb", [128, D], BF16)
        xT = p.sb("xT", [128, 8, 128], BF16)
        uTf = p.sb("uTf", [128, 4, 128]); uTb = p.sb("uTb", [128, 4, 128], BF16)
        cb = p.sb("cb", [128, 12, 131])
        cacc = p.sb("cacc", [128, 12, 128])
        g8 = p.sb("g8", [128, 16, 128])
        ctmp = Buf(g8[:, 0:12, :], "ctmp"); ctmp.trk = g8.trk
        ztok = p.sb("ztok", [128, 8])
        rbuf = p.sb("rbuf", [128, 2, 8, 128]); rtmp = p.sb("rtmp", [128, 2, 8, 128])
        hbf = p.sb("hbf", [128, 2, 16, 128], BF16)
        hl = p.sb("hl", [128, 4, 16])
        yA = Buf(rtmp[:, 0, 0:4, :], "yA"); yA.trk = rtmp.trk
        ysq = Buf(rtmp[:, 0, 4:8, :], "ysq"); ysq.trk = rtmp.trk
        gaf = Buf(rtmp[:, 1, 0:4, :], "gaf"); gaf.trk = rtmp.trk
        gab = p.sb("gab", [128, 4, 128], BF16)
        mixT = p.sb("mixT", [128, 8, 128], BF16)
        qkn = Buf(g8[:, 8:16, :], "qkn"); qkn.trk = g8.trk
        sq8 = Buf(g8[:, 0:8, :], "sq8"); sq8.trk = g8.trk
        kvtok = alias(rbuf[:, 0, :, :], rbuf, "kvtok")
        gd = p.sb("gd", [128, 16])
        gd2 = p.sb("gd2", [128, 16])
        _gt = [alias(gsc[:, 0, i, :], gsc, "gt%d" % i) for i in range(16)]
        gbc, dec, erow, attn, attnT, rv, rk_, nwT, ub, qdT, kd, Yt = _gt[0:12]
        Mm = [_gt[12], _gt[13]]
        MT = [_gt[14], _gt[15]]
        yB = p.sb("yB", [128, 512], BF16); osb = alias(gsc[:, 1, 0, :], gsc, "osb"); ssq = p.sb("ssq", [128, 4])
        zs = p.sb("zs", [128, 512], BF16)
        x1a = alias(g8[:, 0:8, :].rearrange("p a b -> p (a b)"), g8, "x1a")
        x1Ta = alias(cacc[:, 0:8, :], cacc, "x1Ta")
        work = alloc_tail_work(h=xt_one, x1=x1a, x1T=x1Ta)
        print("A0 sbuf words", p.top)

        def load_x(tl, buf):
            if tl["nt"] < 128:
                p.V("pool", "memset", buf[:], 0.0, w=[buf])
            p.DM("sp", buf[0:tl["nt"], :], xin[tl["row0"]:tl["row0"] + tl["nt"], :], r=[DR], w=[buf])

        for tl in tiles:
            nt, ti, sq = tl["nt"], tl["ti"], tl["seq"]
            xt = xts[ti % 2]
            load_x(tl, xt)
            if tl["first"]:
                if sq < NPS:
                    p.V("pool", "memset", Hre[:], 0.0, w=[Hre]); p.V("pool", "memset", Him[:], 0.0, w=[Him])
                    p.V("pool", "memset", Sg[:], 0.0, w=[Sg]); p.V("pool", "memset", ctx3[:], 0.0, w=[ctx3])
                else:
                    p.DM("sp", Hre[:], st_s5re, r=[DR], w=[Hre])
                    p.DM("sp", Him[:], st_s5im, r=[DR], w=[Him])
                    p.DM("sp", Sg[:], st_gdn.rearrange("h k v -> k h v"), r=[DR], w=[Sg])
                    p.DM("sp", ctx3[:], st_conv, r=[DR], w=[ctx3])
            if CUT <= 1:
                continue
            p.V("act", "activation", xb[0:nt, :], xt[0:nt, :], AF.Copy, r=[xt], w=[xb])
            transpose_to(xT, xb, nt, 8, 0)
            tapsrc[0] = xt; tap("xt", xt[0:nt, :], ti, nt, D)
            tapsrc[0] = xb; tap("xb", xb[0:nt, :], ti, nt, D, BF16)
            tapsrc[0] = xT; tap("xT", xT[:, :, :].rearrange("p a b -> p (a b)"), ti, 128, 1024, BF16)
            for ob in range(4):
                for kc in range(8):
                    p.V("pe", "matmul", PS[1][:, ob * 128:ob * 128 + nt], win[:, kc, ob * 128:(ob + 1) * 128], xT[:, kc, 0:nt],
                        start=(kc == 0), stop=(kc == 7), r=[win, xT], w=[PS[1]])
            ps1v = PS[1][:, :].rearrange("p (a b) -> p a b", a=4)[:, :, 0:nt]
            p.V("act", "activation", uTf[:, :, 0:nt], ps1v, AF.Copy, r=[PS[1]], w=[uTf])
            p.V("dve", "tensor_copy", uTb[:, :, 0:nt], ps1v, r=[PS[1]], w=[uTb])
            p.V("pool", "tensor_copy", cb[:, :, 0:3], ctx3[:, :, :], r=[ctx3], w=[(cb, "c")])
            for g4 in range(3):
                bank = 2 + (g4 % 2)
                for b in range(4):
                    blk = g4 * 4 + b
                    for kc in range(8):
                        p.V("pe", "matmul", PS[bank][:, b * 128:b * 128 + nt], win[:, kc, 512 + blk * 128:512 + (blk + 1) * 128],
                            xT[:, kc, 0:nt], start=(kc == 0), stop=(kc == 7), r=[win, xT], w=[PS[bank]])
                evac(cb[:, g4 * 4:g4 * 4 + 4, 3:3 + nt], PS[bank][:, :].rearrange("p (a b) -> p a b", a=4)[:, :, 0:nt],
                     r=[PS[bank]], w=[(cb, g4)])
            tapsrc[0] = cb; tap("cb", cb[:, :, :].rearrange("p a b -> p (a b)"), ti, 128, 12 * 131)
            tapsrc[0] = uTf; tap("uTf", uTf[:, :, :].rearrange("p a b -> p (a b)"), ti, 128, 512)
            for kc in range(8):
                p.V("pe", "matmul", PS[4][0:nt, 0:512], xT[:, kc, 0:nt], win[:, kc, 2048:2560], start=(kc == 0), stop=(kc == 7),
                    r=[xT, win], w=[PS[4]])
            for kc in range(8):
                p.V("pe", "matmul", PS[5][0:nt, 0:8], xT[:, kc, 0:nt], win[:, kc, 2560:2568], start=(kc == 0), stop=(kc == 7),
                    r=[xT, win], w=[PS[5]])
            p.V("act", "activation", zs[0:nt, :], PS[4][0:nt, 0:512], AF.Silu, r=[PS[4]], w=[zs])
            p.V("dve", "tensor_copy", ztok[0:nt, 0:8], PS[5][0:nt, 0:8], r=[PS[5]], w=[ztok])
            if CUT <= 2:
                continue
            for hf in range(2):
                for ri in range(2):
                    for b8 in range(8):
                        blk = hf * 8 + b8
                        bank = 4 + ri * 2 + (b8 // 4)
                        p.V("pe", "matmul", PS[bank][:, (b8 % 4) * 128:(b8 % 4) * 128 + nt], bst[:, ri, blk, :], uTb[:, blk // 4, 0:nt],
                            start=True, stop=True, r=[bst, uTb], w=[PS[bank]])
                for q4 in range(2):
                    bre = PS[4 + q4][:, :].rearrange("p (a b) -> p a b", a=4)[:, :, 0:nt]
                    bim = PS[6 + q4][:, :].rearrange("p (a b) -> p a b", a=4)[:, :, 0:nt]
                    bs = slice(hf * 8 + q4 * 4, hf * 8 + q4 * 4 + 4)
                    o4 = slice(q4 * 4, q4 * 4 + 4)
                    p.V("dve", "tensor_tensor", rbuf[:, 0, o4, 0:nt], bre, crT[:, bs, 0:nt], ALU.mult, r=[PS[4 + q4], crT], w=[(rbuf, 0)])
                    p.V("dve", "tensor_tensor", rtmp[:, 0, o4, 0:nt], bim, ciT[:, bs, 0:nt], ALU.mult, r=[PS[6 + q4], ciT], w=[(rtmp, 0)])
                    p.V("dve", "tensor_tensor", rbuf[:, 1, o4, 0:nt], bre, ciT[:, bs, 0:nt], ALU.mult, r=[PS[4 + q4], ciT], w=[(rbuf, 1)])
                    p.V("dve", "tensor_tensor", rtmp[:, 1, o4, 0:nt], bim, crT[:, bs, 0:nt], ALU.mult, r=[PS[6 + q4], crT], w=[(rtmp, 1)])
                p.V("pool", "tensor_tensor", rbuf[:, 0, :, 0:nt], rbuf[:, 0, :, 0:nt], rtmp[:, 0, :, 0:nt], ALU.subtract, r=[(rbuf, 0), (rtmp, 0)], w=[(rbuf, 0)])
                p.V("pool", "tensor_tensor", rbuf[:, 1, :, 0:nt], rbuf[:, 1, :, 0:nt], rtmp[:, 1, :, 0:nt], ALU.add, r=[(rbuf, 1), (rtmp, 1)], w=[(rbuf, 1)])
                for b8 in range(8):
                    blk = hf * 8 + b8
                    p.V("dve", "tensor_tensor_scan", gsc[:, 0, blk, 0:nt], lam[:, blk:blk + 1].to_broadcast([128, nt]), rbuf[:, 0, b8, 0:nt],
                        Hre[:, blk:blk + 1], ALU.mult, ALU.add, r=[lam, (rbuf, 0), Hre], w=[(gsc, (0, blk))])
                    p.V("dve", "tensor_tensor_scan", gsc[:, 1, blk, 0:nt], lam[:, blk:blk + 1].to_broadcast([128, nt]), rbuf[:, 1, b8, 0:nt],
                        Him[:, blk:blk + 1], ALU.mult, ALU.add, r=[lam, (rbuf, 1), Him], w=[(gsc, (1, blk))])
            t_a, t_b = rbuf[:, :, :, :].rearrange("p a b c -> p (a b) c"), rtmp[:, :, :, :].rearrange("p a b c -> p (a b) c")
            p.V("pool", "tensor_tensor", t_a[:, :, 0:nt], gsc[:, 0, :, 0:nt], cosT[:, :, 0:nt], ALU.mult, r=[gsc, cosT], w=[rbuf])
            p.V("pool", "tensor_tensor", t_b[:, :, 0:nt], gsc[:, 1, :, 0:nt], sinT[:, :, 0:nt], ALU.mult, r=[gsc, sinT], w=[rtmp])
            p.V("dve", "tensor_tensor", hbf[:, 0, :, 0:nt], t_a[:, :, 0:nt], t_b[:, :, 0:nt], ALU.subtract, r=[rbuf, rtmp], w=[(hbf, 0)])
            lc = nt - 1
            p.V("dve", "tensor_tensor", hl[:, 0, :], gsc[:, 0, :, lc], cosT[:, :, lc], ALU.mult, r=[gsc, cosT], w=[(hl, 0)])
            p.V("dve", "tensor_tensor", hl[:, 1, :], gsc[:, 1, :, lc], sinT[:, :, lc], ALU.mult, r=[gsc, sinT], w=[(hl, 1)])
            p.V("dve", "tensor_tensor", hl[:, 2, :], gsc[:, 0, :, lc], sinT[:, :, lc], ALU.mult, r=[gsc, sinT], w=[(hl, 2)])
            p.V("dve", "tensor_tensor", hl[:, 3, :], gsc[:, 1, :, lc], cosT[:, :, lc], ALU.mult, r=[gsc, cosT], w=[(hl, 3)])
            p.V("pool", "tensor_tensor", t_a[:, :, 0:nt], gsc[:, 0, :, 0:nt], sinT[:, :, 0:nt], ALU.mult, r=[gsc, sinT, (hbf, 0)], w=[rbuf])
            p.V("pool", "tensor_tensor", t_b[:, :, 0:nt], gsc[:, 1, :, 0:nt], cosT[:, :, 0:nt], ALU.mult, r=[gsc, cosT, (hbf, 0)], w=[rtmp])
            p.V("dve", "scalar_tensor_tensor", hbf[:, 1, :, 0:nt], t_a[:, :, 0:nt], -1.0, t_b[:, :, 0:nt], ALU.mult, ALU.subtract,
                r=[rbuf, rtmp], w=[(hbf, 1)])
            p.V("dve", "tensor_tensor", Hre[:], hl[:, 0, :], hl[:, 1, :], ALU.subtract, r=[hl], w=[Hre])
            p.V("dve", "tensor_tensor", Him[:], hl[:, 2, :], hl[:, 3, :], ALU.add, r=[hl], w=[Him])
            if tl["last"]:
                p.DM("sp", o_s5re[sq], Hre[:], r=[Hre], w=[T_out])
                p.DM("sp", o_s5im[sq], Him[:], r=[Him], w=[T_out])
            for ob in range(4):
                n = 0
                for b4 in range(4):
                    blk = ob * 4 + b4
                    for ri in range(2):
                        p.V("pe", "matmul", PS[1][:, ob * 128:ob * 128 + nt], cstt[:, ri, blk, :], hbf[:, ri, blk, 0:nt],
                            start=(n == 0), stop=(n == 7), r=[cstt, hbf], w=[PS[1]])
                        n += 1
            for ob in range(4):
                p.V("dve", "scalar_tensor_tensor", yA[:, ob, 0:nt], uTf[:, ob, 0:nt], dsk[:, ob:ob + 1], PS[1][:, ob * 128:ob * 128 + nt],
                    ALU.mult, ALU.add, r=[uTf, dsk, PS[1]], w=[yA])
            cg = math.sqrt(2.0 / math.pi)
            p.V("act", "activation", ysq[:, :, 0:nt], yA[:, :, 0:nt], AF.Square, r=[yA], w=[ysq])
            p.V("dve", "tensor_scalar", ysq[:, :, 0:nt], ysq[:, :, 0:nt], 2.0 * cg * 0.044715, 2.0 * cg, ALU.mult, ALU.add, r=[ysq], w=[ysq])
            p.V("dve", "tensor_tensor", ysq[:, :, 0:nt], ysq[:, :, 0:nt], yA[:, :, 0:nt], ALU.mult, r=[ysq, yA], w=[ysq])
            p.V("act", "activation", ysq[:, :, 0:nt], ysq[:, :, 0:nt], AF.Sigmoid, r=[ysq], w=[ysq])
            p.V("dve", "tensor_tensor", gaf[:, :, 0:nt], ysq[:, :, 0:nt], yA[:, :, 0:nt], ALU.mult, r=[ysq, yA], w=[gaf])
            p.V("act", "activation", gab[:, :, 0:nt], gaf[:, :, 0:nt], AF.Copy, r=[gaf], w=[gab])
            for ob in range(4):
                for kc in range(4):
                    p.V("pe", "matmul", PS[0][:, ob * 128:ob * 128 + nt], wglu[:, kc, ob * 128:(ob + 1) * 128], gab[:, kc, 0:nt],
                        start=(kc == 0), stop=(kc == 3), r=[wglu, gab], w=[PS[0]])
            for ob in range(4):
                p.V("act", "activation", ysq[:, ob, 0:nt], PS[0][:, ob * 128:ob * 128 + nt], AF.Sigmoid, bias=bgl[:, ob:ob + 1],
                    r=[PS[0], bgl], w=[ysq])
            p.V("dve", "tensor_tensor", mixT[:, 0:4, 0:nt], ysq[:, :, 0:nt], gaf[:, :, 0:nt], ALU.mult, r=[ysq, gaf], w=[(mixT, "a")])
            if CUT <= 3:
                continue
            for j in range(4):
                wj = wc[:, :, j:j + 1].to_broadcast([128, 12, nt])
                if j == 0:
                    p.V("dve", "tensor_tensor", cacc[:, :, 0:nt], cb[:, :, 0:nt], wj, ALU.mult, r=[cb, wc], w=[cacc])
                else:
                    p.V("pool", "tensor_tensor", ctmp[:, :, 0:nt], cb[:, :, j:j + nt], wj, ALU.mult, r=[cb, wc], w=[ctmp])
                    p.V("dve", "tensor_tensor", cacc[:, :, 0:nt], cacc[:, :, 0:nt], ctmp[:, :, 0:nt], ALU.add, r=[cacc, ctmp], w=[cacc])
            p.V("pool", "tensor_copy", ctx3[:, :, :], cb[:, :, nt:nt + 3], r=[cb], w=[ctx3])
            if tl["last"]:
                p.DM("sp", o_conv[sq], ctx3[:], r=[ctx3], w=[T_out])
            p.V("act", "activation", cacc[:, :, 0:nt], cacc[:, :, 0:nt], AF.Silu, r=[cacc], w=[cacc])
            p.V("act", "activation", sq8[:, :, 0:nt], cacc[:, 0:8, 0:nt], AF.Square, r=[cacc], w=[sq8])
            for hb in range(2):
                for b in range(4):
                    p.V("pe", "matmul", PS[2 + hb][:, b * 128:b * 128 + nt], ones_f[:, :], sq8[:, hb * 4 + b, 0:nt], start=True, stop=True,
                        r=[ones_f, sq8], w=[PS[2 + hb]])
            for hb in range(2):
                v = PS[2 + hb][:, :].rearrange("p (a b) -> p a b", a=4)[:, :, 0:nt]
                p.V("act", "activation", sq8[:, hb * 4:hb * 4 + 4, 0:nt], v, AF.Sqrt, bias=epsln[:, 1:2], r=[PS[2 + hb], epsln], w=[(sq8, hb)])
            p.V("dve", "reciprocal", sq8[:, :, 0:nt], sq8[:, :, 0:nt], r=[sq8], w=[sq8])
            p.V("dve", "scalar_tensor_tensor", qkn[:, 0:4, 0:nt], cacc[:, 0:4, 0:nt], 128.0 ** -0.5, sq8[:, 0:4, 0:nt], ALU.mult, ALU.mult,
                r=[cacc, sq8], w=[(qkn, "q")])
            p.V("dve", "tensor_tensor", qkn[:, 4:8, 0:nt], cacc[:, 4:8, 0:nt], sq8[:, 4:8, 0:nt], ALU.mult, r=[cacc, sq8], w=[(qkn, "k")])
            for b in range(4):
                p.V("pe", "transpose", PS[2][0:nt, b * 128:(b + 1) * 128], qkn[:, 4 + b, 0:nt], identf[:, :], r=[qkn, identf], w=[PS[2]])
                p.V("pe", "transpose", PS[3][0:nt, b * 128:(b + 1) * 128], cacc[:, 8 + b, 0:nt], identf[:, :], r=[cacc, identf], w=[PS[3]])
            evac(kvtok[0:nt, 0:4, :], PS[2][0:nt, :].rearrange("p (a b) -> p a b", a=4), r=[PS[2]], w=[kvtok])
            evac(kvtok[0:nt, 4:8, :], PS[3][0:nt, :].rearrange("p (a b) -> p a b", a=4), r=[PS[3]], w=[kvtok])
            if CUT2 <= 1:
                continue
            p.V("act", "activation", gd[0:nt, 0:4], ztok[0:nt, 0:4], AF.Sigmoid, r=[ztok], w=[(gd, "b")])
            p.V("dve", "tensor_scalar", gd[0:nt, 4:8], gd[0:nt, 0:4], -1.0, None, ALU.mult, r=[(gd, "b")], w=[(gd, "nb")])
            p.V("dve", "tensor_tensor", gd[0:nt, 8:12], ztok[0:nt, 4:8], dtb[0:nt, :], ALU.add, r=[ztok, dtb], w=[(gd, "g")])
            p.V("act", "activation", gd[0:nt, 8:12], gd[0:nt, 8:12], AF.Exp, r=[(gd, "g")], w=[(gd, "g")])
            p.V("act", "activation", gd[0:nt, 8:12], gd[0:nt, 8:12], AF.Ln, bias=1.0, r=[(gd, "g")], w=[(gd, "g")])
            p.V("dve", "scalar_tensor_tensor", gd[0:nt, 8:12], gd[0:nt, 8:12], -1.0, alog[0:nt, :], ALU.mult, ALU.mult, r=[(gd, "g"), alog], w=[(gd, "g")])
            p.V("pe", "matmul", PS[0][0:nt, 0:4], uincl[0:nt, 0:nt], gd[0:nt, 8:12], start=True, stop=True, r=[uincl, (gd, "g")], w=[PS[0]])
            p.V("dve", "tensor_copy", gd[0:nt, 12:16], PS[0][0:nt, 0:4], r=[PS[0]], w=[(gd, "G")])
            p.V("act", "activation", gd2[0:nt, 0:4], gd[0:nt, 12:16], AF.Exp, r=[(gd, "G")], w=[(gd2, "e")])
            p.V("dve", "tensor_tensor", gd2[0:nt, 4:8], gd2[0:nt, 0:4], gd[0:nt, 0:4], ALU.mult, r=[(gd2, "e"), (gd, "b")], w=[(gd2, "be")])
            if CUT2 <= 2:
                continue
            for hd in range(4):
                kT = qkn[:, 4 + hd, 0:nt]
                qT = qkn[:, hd, 0:nt]
                p.V("dve", "tensor_scalar", gbc[0:nt, :], ones_f[0:nt, :], gd[0:nt, 8 + hd:9 + hd], None, ALU.mult, r=[ones_f, (gd, "g")], w=[gbc])
                p.V("pe", "matmul", PS[0][:, 128:128 + nt], gbc[0:nt, :], uincl[0:nt, 0:nt], start=True, stop=True, r=[gbc, uincl], w=[PS[0]])
                p.V("pe", "matmul", PS[0][0:nt, 256:256 + nt], kT, kT, start=True, stop=True, r=[(qkn, "k")], w=[PS[0]])
                p.V("pe", "matmul", PS[0][0:nt, 384:384 + nt], qT, kT, start=True, stop=True, r=[(qkn, "q"), (qkn, "k")], w=[PS[0]])
                grow = PS[0][0:nt, 128:128 + nt]
                p.V("act", "activation", dec[0:nt, 0:nt], grow, AF.Exp, bias=gd[0:nt, 12 + hd:13 + hd], scale=-1.0, r=[PS[0], (gd, "G")], w=[dec])
                p.V("act", "activation", erow[:, 0:nt], PS[0][:, 128:128 + nt], AF.Exp, r=[PS[0]], w=[erow])
                p.V("pool", "affine_select", dec[0:nt, 0:nt], dec[0:nt, 0:nt], [[-1, nt]], ALU.is_ge, 0.0, base=0, channel_multiplier=1, r=[dec], w=[dec])
                p.V("dve", "scalar_tensor_tensor", Mm[0][0:nt, 0:nt], PS[0][0:nt, 256:256 + nt], gd[0:nt, 4 + hd:5 + hd], dec[0:nt, 0:nt], ALU.mult, ALU.mult,
                    r=[PS[0], (gd, "nb"), dec], w=[Mm[0]])
                p.V("pool", "affine_select", Mm[0][0:nt, 0:nt], Mm[0][0:nt, 0:nt], [[-1, nt]], ALU.is_gt, 0.0, base=0, channel_multiplier=1, r=[Mm[0]], w=[Mm[0]])
                p.V("dve", "tensor_tensor", attn[0:nt, 0:nt], PS[0][0:nt, 384:384 + nt], dec[0:nt, 0:nt], ALU.mult, r=[PS[0], dec], w=[attn])
                p.V("pe", "transpose", PS[1][0:nt, 0:nt], Mm[0][0:nt, 0:nt], identf[0:nt, 0:nt], r=[Mm[0], identf], w=[PS[1]])
                p.V("pe", "transpose", PS[1][0:nt, 128:128 + nt], attn[0:nt, 0:nt], identf[0:nt, 0:nt], r=[attn, identf], w=[PS[1]])
                p.V("act", "activation", MT[0][0:nt, 0:nt], PS[1][0:nt, 0:nt], AF.Copy, r=[PS[1]], w=[MT[0]])
                p.V("dve", "tensor_tensor", Yt[0:nt, 0:nt], PS[1][0:nt, 0:nt], identf[0:nt, 0:nt], ALU.add, r=[PS[1], identf], w=[Yt])
                p.V("act", "activation", attnT[0:nt, 0:nt], PS[1][0:nt, 128:128 + nt], AF.Copy, r=[PS[1]], w=[attnT])
                if CUT2 <= 3:
                    continue
                nlev = 6 if nt == 128 else 3
                for lv in range(nlev):
                    a, b_ = lv % 2, (lv + 1) % 2
                    bank = 6 + (lv % 2)
                    p.V("pe", "matmul", PS[bank][0:nt, 0:nt], MT[a][0:nt, 0:nt], Mm[a][0:nt, 0:nt], start=True, stop=True, r=[MT[a], Mm[a]], w=[PS[bank]])
                    if lv < nlev - 1:
                        p.V("pe", "matmul", PS[bank][0:nt, 128:128 + nt], Mm[a][0:nt, 0:nt], MT[a][0:nt, 0:nt], start=True, stop=True, r=[MT[a], Mm[a]], w=[PS[bank]])
                    p.V("act", "activation", Mm[b_][0:nt, 0:nt], PS[bank][0:nt, 0:nt], AF.Copy, r=[PS[bank]], w=[Mm[b_]])
                    if lv < nlev - 1:
                        p.V("dve", "tensor_copy", MT[b_][0:nt, 0:nt], PS[bank][0:nt, 128:128 + nt], r=[PS[bank]], w=[MT[b_]])
                    p.V("pe", "matmul", PS[bank][0:nt, 256:256 + nt], Mm[b_][0:nt, 0:nt], Yt[0:nt, 0:nt], start=True, stop=True, r=[Mm[b_], Yt], w=[PS[bank]])
                    p.V("dve", "tensor_tensor", Yt[0:nt, 0:nt], Yt[0:nt, 0:nt], PS[bank][0:nt, 256:256 + nt], ALU.add, r=[Yt, PS[bank]], w=[Yt])
                if CUT2 <= 4:
                    continue
                p.V("dve", "tensor_scalar", rv[0:nt, :], kvtok[0:nt, 4 + hd, :], gd[0:nt, hd:hd + 1], None, ALU.mult, r=[kvtok, (gd, "b")], w=[rv])
                p.V("dve", "tensor_scalar", rk_[0:nt, :], kvtok[0:nt, hd, :], gd2[0:nt, 4 + hd:5 + hd], None, ALU.mult, r=[kvtok, (gd2, "be")], w=[rk_])
                p.V("pe", "matmul", PS[1][:, 256:256 + nt], rk_[0:nt, :], Yt[0:nt, 0:nt], start=True, stop=True, r=[rk_, Yt], w=[PS[1]])
                p.V("dve", "tensor_scalar", nwT[:, 0:nt], PS[1][:, 256:256 + nt], -1.0, None, ALU.mult, r=[PS[1]], w=[nwT])
                p.V("pe", "matmul", PS[5][0:nt, 0:128], Yt[0:nt, 0:nt], rv[0:nt, :], start=True, stop=False, r=[Yt, rv], w=[PS[5]])
                p.V("pe", "matmul", PS[5][0:nt, 0:128], nwT[:, 0:nt], Sg[:, hd, :], start=False, stop=True, r=[nwT, Sg], w=[PS[5]])
                p.V("act", "activation", ub[0:nt, :], PS[5][0:nt, 0:128], AF.Copy, r=[PS[5]], w=[ub])
                p.V("dve", "tensor_tensor", qdT[:, 0:nt], qT, erow[:, 0:nt], ALU.mult, r=[(qkn, "q"), erow], w=[qdT])
                p.V("pe", "matmul", PS[5][0:nt, 128:256], qdT[:, 0:nt], Sg[:, hd, :], start=True, stop=False, r=[qdT, Sg], w=[PS[5]])
                p.V("pe", "matmul", PS[5][0:nt, 128:256], attnT[0:nt, 0:nt], ub[0:nt, :], start=False, stop=True, r=[attnT, ub], w=[PS[5]])
                if CUT2 <= 5:
                    continue
                p.V("dve", "tensor_copy", gd2[:, 12:13], PS[0][:, 128 + nt - 1:128 + nt], r=[PS[0]], w=[(gd2, "gl")])
                p.V("act", "activation", gd2[0:nt, 8 + hd:9 + hd], gd[0:nt, 12 + hd:13 + hd], AF.Exp, bias=gd2[0:nt, 12:13], scale=-1.0,
                    r=[(gd, "G"), (gd2, "gl")], w=[(gd2, ("kd", hd))])
                p.V("dve", "tensor_scalar", kd[0:nt, :], kvtok[0:nt, hd, :], gd2[0:nt, 8 + hd:9 + hd], None, ALU.mult, r=[kvtok, (gd2, ("kd", hd))], w=[kd])
                p.V("pe", "matmul", PS[5][:, 256:384], kd[0:nt, :], ub[0:nt, :], start=True, stop=True, r=[kd, ub], w=[PS[5]])
                p.V("act", "activation", gd2[:, 13:14], gd2[:, 12:13], AF.Exp, r=[(gd2, "gl")], w=[(gd2, "egl")])
                p.V("dve", "scalar_tensor_tensor", Sg[:, hd, :], Sg[:, hd, :], gd2[:, 13:14], PS[5][:, 256:384], ALU.mult, ALU.add,
                    r=[Sg, (gd2, "egl"), PS[5]], w=[Sg])
                if CUT2 <= 6:
                    continue
                p.V("act", "activation", osb[0:nt, :], PS[5][0:nt, 128:256], AF.Square, r=[PS[5]], w=[osb])
                p.V("dve", "reduce_sum", ssq[0:nt, hd:hd + 1], osb[0:nt, :], mybir.AxisListType.X, r=[osb], w=[(ssq, hd)])
                p.V("act", "activation", ssq[0:nt, hd:hd + 1], ssq[0:nt, hd:hd + 1], AF.Sqrt, bias=epsln[0:nt, 1:2], scale=1.0 / 128.0, r=[(ssq, hd), epsln], w=[(ssq, hd)])
                p.V("dve", "reciprocal", ssq[0:nt, hd:hd + 1], ssq[0:nt, hd:hd + 1], r=[(ssq, hd)], w=[(ssq, hd)])
                if CUT2 <= 7:
                    continue
                p.V("dve", "scalar_tensor_tensor", osb[0:nt, :], PS[5][0:nt, 128:256], ssq[0:nt, hd:hd + 1], nrmw[0:nt, :], ALU.mult, ALU.mult,
                    r=[PS[5], (ssq, hd), nrmw], w=[osb])
                p.V("dve", "tensor_tensor", yB[0:nt, hd * 128:(hd + 1) * 128], osb[0:nt, :], zs[0:nt, hd * 128:(hd + 1) * 128], ALU.mult, r=[osb, zs], w=[(yB, hd)])
            if tl["last"]:
                p.DM("sp", o_gdn[sq].rearrange("h k v -> k h v"), Sg[:], r=[Sg], w=[T_out])
            if CUT <= 4:
                continue
            for b in range(4):
                p.V("pe", "transpose", psbf(2, 1024)[:, b * 128:b * 128 + nt], yB[0:nt, b * 128:(b + 1) * 128], identb[0:nt, 0:nt], r=[yB, identb], w=[PS[2]])
            evac(mixT[:, 4:8, 0:nt], psbf(2, 1024).rearrange("p (a b) -> p a b", a=8)[:, 0:4, 0:nt], r=[PS[2]], w=[(mixT, "b")])
            for half in range(2):
                for kc in range(8):
                    p.V("pe", "matmul", PS[2 + half][0:nt, :], mixT[:, kc, 0:nt], wout[:, kc, half * 512:(half + 1) * 512], start=(kc == 0), stop=(kc == 7),
                        r=[mixT, wout], w=[PS[2 + half]])
            if CUT <= 5:
                continue
            phaseA_tail(0, tl, xt, (2, 3), lnw, rw, rb, work)
        p.release(m0)

    def run_streams(gens):
        gens = list(gens)
        while gens:
            for g in list(gens):
                try:
                    next(g)
                except StopIteration:
                    gens.remove(g)

    def phaseS0():
        m0 = p.mark()
        win_u = p.sb("win_u", [128, 8, 512], BF16)
        p.DM("pool", win_u[:], w_in_even[:, 0:512].rearrange("(kc q) n -> q kc n", q=128), r=[DR], w=[win_u])
        wglu = p.sb("wglu", [128, 4, 512], BF16)
        load_w_bf16(wglu, s5_wglu, 4)
        bst = p.sb("bst", [128, 2, 16, 128], BF16)
        cstt = p.sb("cstt", [128, 2, 16, 128], BF16)
        for ri in range(2):
            p.DM("pool", bst[:, ri, :, :], s5_bst[ri].rearrange("b k m -> k b m"), r=[DR], w=[(bst, ri)])
            p.DM("pool", cstt[:, ri, :, :], s5_cst[ri].rearrange("b k m -> k b m"), r=[DR], w=[(cstt, ri)])
        are = p.sb("are", [128, 16]); aim = p.sb("aim", [128, 16]); ldt = p.sb("ldt", [128, 16])
        for b, s_ in ((are, s5_are), (aim, s5_aim), (ldt, s5_ldt)):
            p.DM("sp", b[:], s_, r=[DR], w=[b])
        dsk = p.sb("dsk", [128, 4]); bgl = p.sb("bgl", [128, 4])
        p.DM("sp", dsk[:], s5_d, r=[DR], w=[dsk])
        p.DM("sp", bgl[:], s5_bglu, r=[DR], w=[bgl])
        tau = p.sb("tau", [128, 128])
        p.DM("sp", tau[:], cst["tau"], r=[DR], w=[tau])
        lam = p.sb("lam", [128, 16]); li_ = p.sb("li", [128, 16]); dtt = p.sb("dtt", [128, 16])
        p.V("act", "activation", dtt[:], ldt[:], AF.Exp, r=[ldt], w=[dtt])
        p.V("dve", "tensor_tensor", li_[:], aim[:], dtt[:], ALU.mult, r=[aim, dtt], w=[li_])
        p.V("dve", "tensor_tensor", lam[:], are[:], dtt[:], ALU.mult, r=[are, dtt], w=[lam])
        p.V("act", "activation", lam[:], lam[:], AF.Exp, r=[lam], w=[lam])
        cosT = p.sb("cosT", [128, 16, 128]); sinT = p.sb("sinT", [128, 16, 128])
        crT = p.sb("crT", [128, 16, 128]); ciT = p.sb("ciT", [128, 16, 128])
        ang = crT
        kq = ciT
        kif = p.sb("kif", [128, 16, 128])
        ki = alias(kif[:, :, :].bitcast(I32), kif, "ki")
        sc3 = p.sb("sc3", [128, 16, 128])
        TWO_PI = 2.0 * math.pi

        def sin_of(dst, shift):
            p.V("dve", "tensor_tensor", ang[:], li_[:, :].unsqueeze(2).to_broadcast([128, 16, 128]),
                tau[:, :].unsqueeze(1).to_broadcast([128, 16, 128]), ALU.mult, r=[li_, tau], w=[ang])
            if shift != 0.0:
                p.V("dve", "tensor_scalar", ang[:], ang[:], shift, None, ALU.add, r=[ang], w=[ang])
            p.V("dve", "tensor_scalar", kq[:], ang[:], 1.0 / TWO_PI, None, ALU.mult, r=[ang], w=[kq])
            p.V("dve", "tensor_copy", ki[:], kq[:], r=[kq], w=[ki])
            p.V("dve", "tensor_copy", kq[:], ki[:], r=[ki], w=[kq])
            p.V("dve", "scalar_tensor_tensor", ang[:], kq[:], -TWO_PI, ang[:], ALU.mult, ALU.add, r=[kq, ang], w=[ang])
            p.V("dve", "tensor_scalar", kq[:], ang[:], math.pi, TWO_PI, ALU.is_gt, ALU.mult, r=[ang], w=[kq])
            p.V("dve", "tensor_tensor", ang[:], ang[:], kq[:], ALU.subtract, r=[ang, kq], w=[ang])
            p.V("dve", "tensor_scalar", kq[:], ang[:], -math.pi, TWO_PI, ALU.is_lt, ALU.mult, r=[ang], w=[kq])
            p.V("dve", "tensor_tensor", ang[:], ang[:], kq[:], ALU.add, r=[ang, kq], w=[ang])
            p.V("dve", "tensor_scalar", ang[:], ang[:], math.pi, -math.pi, ALU.min, ALU.max, r=[ang], w=[ang])
            p.V("act", "activation", dst[:], ang[:], AF.Sin, r=[ang], w=[dst])

        sin_of(sinT, 0.0)
        sin_of(cosT, math.pi / 2)
        sm = p.sb("s5sm", [128, 8, 16])
        abre, abim, den, t1, t2, cfre, cfim, t3 = [sm[:, i, :] for i in range(8)]
        S = [sm]
        p.V("dve", "tensor_tensor", abre, lam[:], cosT[:, :, 0], ALU.mult, r=[lam, cosT], w=S)
        p.V("dve", "tensor_tensor", abim, lam[:], sinT[:, :, 0], ALU.mult, r=[lam, sinT], w=S)
        p.V("dve", "tensor_scalar", abre, abre, -1.0, None, ALU.add, r=S, w=S)
        p.V("dve", "tensor_tensor", t1, are[:], are[:], ALU.mult, r=[are], w=S)
        p.V("dve", "tensor_tensor", t2, aim[:], aim[:], ALU.mult, r=[aim], w=S)
        p.V("dve", "tensor_tensor", den, t1, t2, ALU.add, r=S, w=S)
        p.V("dve", "reciprocal", den, den, r=S, w=S)
        p.V("dve", "tensor_tensor", t1, abre, are[:], ALU.mult, r=S + [are], w=S)
        p.V("dve", "tensor_tensor", t2, abim, aim[:], ALU.mult, r=S + [aim], w=S)
        p.V("dve", "tensor_tensor", cfre, t1, t2, ALU.add, r=S, w=S)
        p.V("dve", "tensor_tensor", cfre, cfre, den, ALU.mult, r=S, w=S)
        p.V("dve", "tensor_tensor", t1, abim, are[:], ALU.mult, r=S + [are], w=S)
        p.V("dve", "tensor_tensor", t2, abre, aim[:], ALU.mult, r=S + [aim], w=S)
        p.V("dve", "tensor_tensor", cfim, t1, t2, ALU.subtract, r=S, w=S)
        p.V("dve", "tensor_tensor", cfim, cfim, den, ALU.mult, r=S, w=S)
        bc = lambda a: a.unsqueeze(2).to_broadcast([128, 16, 128])
        p.V("dve", "tensor_tensor", crT[:], cosT[:], bc(cfre), ALU.mult, r=[cosT] + S, w=[crT])
        p.V("dve", "tensor_tensor", sc3[:], sinT[:], bc(cfim), ALU.mult, r=[sinT] + S, w=[sc3])
        p.V("dve", "tensor_tensor", crT[:], crT[:], sc3[:], ALU.add, r=[crT, sc3], w=[crT])
        p.V("dve", "tensor_tensor", ciT[:], cosT[:], bc(cfim), ALU.mult, r=[cosT] + S, w=[ciT])
        p.V("dve", "tensor_tensor", sc3[:], sinT[:], bc(cfre), ALU.mult, r=[sinT] + S, w=[sc3])
        p.V("dve", "tensor_tensor", ciT[:], ciT[:], sc3[:], ALU.subtract, r=[ciT, sc3], w=[ciT])
        cg = math.sqrt(2.0 / math.pi)

        def stream(sx, tlist):
            B0, B1, B2, B3 = 4 * sx, 4 * sx + 1, 4 * sx + 2, 4 * sx + 3
            n_ = lambda s_: "%s_%d" % (s_, sx)
            xt = p.sb(n_("xt"), [128, D]); xb = p.sb(n_("xb"), [128, D], BF16); xT = p.sb(n_("xT"), [128, 8, 128], BF16)
            uTf = p.sb(n_("uTf"), [128, 4, 128]); uTb = p.sb(n_("uTb"), [128, 4, 128], BF16)
            rbuf = p.sb(n_("rbuf"), [128, 2, 4, 128]); rtmp = p.sb(n_("rtmp"), [128, 2, 4, 128]); gsc = p.sb(n_("gsc"), [128, 2, 4, 128])
            hbf = p.sb(n_("hbf"), [128, 2, 16, 128], BF16); hl = p.sb(n_("hl"), [128, 4, 16])
            yA = p.sb(n_("yA"), [128, 4, 128]); ysq = p.sb(n_("ysq"), [128, 4, 128]); gaf = p.sb(n_("gaf"), [128, 4, 128])
            gab = p.sb(n_("gab"), [128, 4, 128], BF16); yag = p.sb(n_("yag"), [128, 4, 128], BF16)
            Hre = p.sb(n_("Hre"), [128, 16]); Him = p.sb(n_("Him"), [128, 16])
            Hn = p.sb(n_("Hn"), [128, 2, 16])
            for tl in tlist:
                nt, ti, sq = tl["nt"], tl["ti"], tl["seq"]
                if nt < 128:
                    p.V("pool", "memset", xt[:], 0.0, w=[xt])
                p.DM("sp", xt[0:nt, :], xin[tl["row0"]:tl["row0"] + nt, :], r=[DR], w=[xt])
                if tl["first"]:
                    if sq < NPS:
                        p.V("pool", "memset", Hre[:], 0.0, w=[Hre]); p.V("pool", "memset", Him[:], 0.0, w=[Him])
                    else:
                        p.DM("sp", Hre[:], st_s5re, r=[DR], w=[Hre])
                        p.DM("sp", Him[:], st_s5im, r=[DR], w=[Him])
                yield
                p.V("act", "activation", xb[0:nt, :], xt[0:nt, :], AF.Copy, r=[xt], w=[xb])
                yield
                for b in range(8):
                    p.V("pe", "transpose", psbf(B0, 1024)[:, b * 128:b * 128 + nt], xb[0:nt, b * 128:(b + 1) * 128], identb[0:nt, 0:nt], r=[xb, identb], w=[PS[B0]])
                p.V("dve", "tensor_copy", xT[:, :, 0:nt], psbf(B0, 1024).rearrange("p (a b) -> p a b", a=8)[:, :, 0:nt], r=[PS[B0]], w=[xT])
                yield
                for ob in range(4):
                    for kc in range(8):
                        p.V("pe", "matmul", PS[B1][:, ob * 128:ob * 128 + nt], win_u[:, kc, ob * 128:(ob + 1) * 128], xT[:, kc, 0:nt],
                            start=(kc == 0), stop=(kc == 7), r=[win_u, xT], w=[PS[B1]])
                ps1v = PS[B1][:, :].rearrange("p (a b) -> p a b", a=4)[:, :, 0:nt]
                p.V("act", "activation", uTf[:, :, 0:nt], ps1v, AF.Copy, r=[PS[B1]], w=[uTf])
                p.V("act", "activation", uTb[:, :, 0:nt], uTf[:, :, 0:nt], AF.Copy, r=[uTf], w=[uTb])
                yield
                lc = nt - 1
                for g4 in range(4):
                    bs = slice(g4 * 4, g4 * 4 + 4)
                    for ri in range(2):
                        for b in range(4):
                            blk = g4 * 4 + b
                            p.V("pe", "matmul", PS[B2 + ri][:, b * 128:b * 128 + nt], bst[:, ri, blk, :], uTb[:, blk // 4, 0:nt], start=True, stop=True,
                                r=[bst, uTb], w=[PS[B2 + ri]])
                    bre = PS[B2][:, :].rearrange("p (a b) -> p a b", a=4)[:, :, 0:nt]
                    bim = PS[B3][:, :].rearrange("p (a b) -> p a b", a=4)[:, :, 0:nt]
                    p.V("dve", "tensor_tensor", rbuf[:, 0, :, 0:nt], bre, crT[:, bs, 0:nt], ALU.mult, r=[PS[B2], crT], w=[(rbuf, 0)])
                    p.V("dve", "tensor_tensor", rbuf[:, 1, :, 0:nt], bre, ciT[:, bs, 0:nt], ALU.mult, r=[PS[B2], ciT], w=[(rbuf, 1)])
                    yield
                    p.V("dve", "tensor_tensor", rtmp[:, 0, :, 0:nt], bim, ciT[:, bs, 0:nt], ALU.mult, r=[PS[B3], ciT], w=[(rtmp, 0)])
                    p.V("dve", "tensor_tensor", rtmp[:, 1, :, 0:nt], bim, crT[:, bs, 0:nt], ALU.mult, r=[PS[B3], crT], w=[(rtmp, 1)])
                    yield
                    p.V("pool", "tensor_tensor", rbuf[:, 0, :, 0:nt], rbuf[:, 0, :, 0:nt], rtmp[:, 0, :, 0:nt], ALU.subtract, r=[(rbuf, 0), (rtmp, 0)], w=[(rbuf, 0)])
                    p.V("pool", "tensor_tensor", rbuf[:, 1, :, 0:nt], rbuf[:, 1, :, 0:nt], rtmp[:, 1, :, 0:nt], ALU.add, r=[(rbuf, 1), (rtmp, 1)], w=[(rbuf, 1)])
                    yield
                    for b in range(4):
                        blk = g4 * 4 + b
                        p.V("dve", "tensor_tensor_scan", gsc[:, 0, b, 0:nt], lam[:, blk:blk + 1].to_broadcast([128, nt]), rbuf[:, 0, b, 0:nt],
                            Hre[:, blk:blk + 1], ALU.mult, ALU.add, r=[lam, (rbuf, 0), Hre], w=[(gsc, (0, b))])
                        p.V("dve", "tensor_tensor_scan", gsc[:, 1, b, 0:nt], lam[:, blk:blk + 1].to_broadcast([128, nt]), rbuf[:, 1, b, 0:nt],
                            Him[:, blk:blk + 1], ALU.mult, ALU.add, r=[lam, (rbuf, 1), Him], w=[(gsc, (1, b))])
                        yield
                    p.V("pool", "tensor_tensor", rbuf[:, 0, :, 0:nt], gsc[:, 0, :, 0:nt], cosT[:, bs, 0:nt], ALU.mult, r=[gsc, cosT], w=[(rbuf, 0)])
                    p.V("pool", "tensor_tensor", rbuf[:, 1, :, 0:nt], gsc[:, 1, :, 0:nt], sinT[:, bs, 0:nt], ALU.mult, r=[gsc, sinT], w=[(rbuf, 1)])
                    yield
                    p.V("pool", "tensor_tensor", rtmp[:, 0, :, 0:nt], gsc[:, 0, :, 0:nt], sinT[:, bs, 0:nt], ALU.mult, r=[gsc, sinT], w=[(rtmp, 0)])
                    p.V("pool", "tensor_tensor", rtmp[:, 1, :, 0:nt], gsc[:, 1, :, 0:nt], cosT[:, bs, 0:nt], ALU.mult, r=[gsc, cosT], w=[(rtmp, 1)])
                    yield
                    p.V("dve", "tensor_tensor", hbf[:, 0, bs, 0:nt], rbuf[:, 0, :, 0:nt], rbuf[:, 1, :, 0:nt], ALU.subtract, r=[rbuf], w=[(hbf, (0, g4))])
                    p.V("dve", "scalar_tensor_tensor", hbf[:, 1, bs, 0:nt], rtmp[:, 0, :, 0:nt], -1.0, rtmp[:, 1, :, 0:nt], ALU.mult, ALU.subtract,
                        r=[rtmp], w=[(hbf, (1, g4))])
                    yield
                    p.V("dve", "tensor_tensor", Hn[:, 0, bs], rbuf[:, 0, :, lc], rbuf[:, 1, :, lc], ALU.subtract, r=[rbuf], w=[(Hn, (0, g4))])
                    p.V("dve", "tensor_tensor", Hn[:, 1, bs], rtmp[:, 0, :, lc], rtmp[:, 1, :, lc], ALU.add, r=[rtmp], w=[(Hn, (1, g4))])
                    yield
                p.V("dve", "tensor_copy", Hre[:], Hn[:, 0, :], r=[Hn], w=[Hre])
                p.V("dve", "tensor_copy", Him[:], Hn[:, 1, :], r=[Hn], w=[Him])
                if tl["last"]:
                    p.DM("sp", o_s5re[sq], Hre[:], r=[Hre], w=[T_out])
                    p.DM("sp", o_s5im[sq], Him[:], r=[Him], w=[T_out])
                yield
                for ob in range(4):
                    n = 0
                    for b4 in range(4):
                        blk = ob * 4 + b4
                        for ri in range(2):
                            p.V("pe", "matmul", PS[B0][:, ob * 128:ob * 128 + nt], cstt[:, ri, blk, :], hbf[:, ri, blk, 0:nt],
                                start=(n == 0), stop=(n == 7), r=[cstt, hbf], w=[PS[B0]])
                            n += 1
                yield
                for ob in range(4):
                    p.V("dve", "scalar_tensor_tensor", yA[:, ob, 0:nt], uTf[:, ob, 0:nt], dsk[:, ob:ob + 1], PS[B0][:, ob * 128:ob * 128 + nt],
                        ALU.mult, ALU.add, r=[uTf, dsk, PS[B0]], w=[(yA, ob)])
                yield
                p.V("act", "activation", ysq[:, :, 0:nt], yA[:, :, 0:nt], AF.Square, r=[yA], w=[ysq])
                yield
                p.V("dve", "tensor_scalar", ysq[:, :, 0:nt], ysq[:, :, 0:nt], 2.0 * cg * 0.044715, 2.0 * cg, ALU.mult, ALU.add, r=[ysq], w=[ysq])
                yield
                p.V("pool", "tensor_tensor", ysq[:, :, 0:nt], ysq[:, :, 0:nt], yA[:, :, 0:nt], ALU.mult, r=[ysq, yA], w=[ysq])
                yield
                p.V("act", "activation", ysq[:, :, 0:nt], ysq[:, :, 0:nt], AF.Sigmoid, r=[ysq], w=[ysq])
                yield
                p.V("pool", "tensor_tensor", gaf[:, :, 0:nt], ysq[:, :, 0:nt], yA[:, :, 0:nt], ALU.mult, r=[ysq, yA], w=[gaf])
                yield
                p.V("act", "activation", gab[:, :, 0:nt], gaf[:, :, 0:nt], AF.Copy, r=[gaf], w=[gab])
                yield
                for ob in range(4):
                    for kc in range(4):
                        p.V("pe", "matmul", PS[B1][:, ob * 128:ob * 128 + nt], wglu[:, kc, ob * 128:(ob + 1) * 128], gab[:, kc, 0:nt],
                            start=(kc == 0), stop=(kc == 3), r=[wglu, gab], w=[PS[B1]])
                yield
                for ob in range(4):
                    p.V("act", "activation", ysq[:, ob, 0:nt], PS[B1][:, ob * 128:ob * 128 + nt], AF.Sigmoid, bias=bgl[:, ob:ob + 1],
                        r=[PS[B1], bgl], w=[ysq])
                yield
                p.V("pool", "tensor_tensor", yag[:, :, 0:nt], ysq[:, :, 0:nt], gaf[:, :, 0:nt], ALU.mult, r=[ysq, gaf], w=[yag])
                p.DM("sp", yas[ti, :, :, 0:nt], yag[:, :, 0:nt], r=[yag], w=[(T_yas, ti)])
                yield

        lists = [[], []]
        for tl in tiles:
            lists[tl["seq"] % 2].append(tl)
        run_streams([stream(0, lists[0]), stream(1, lists[1])])
        p.release(m0)

    def phaseG0():
        m0 = p.mark()
        NG = EVEN_IN - 512
        win = p.sb("win_g", [128, 8, NG], BF16)
        vsrc = w_in_even.rearrange("(kc q) n -> q kc n", q=128)
        p.DM("pool", win[:, :, 0:1024], vsrc[:, :, 512:1536], r=[DR], w=[(win, 0)])
        p.DM("pool", win[:, :, 1024:NG], vsrc[:, :, 1536:EVEN_IN], r=[DR], w=[(win, 1)])
        wout = p.sb("wout", [128, 8, D], BF16)
        load_w_bf16(wout, w_out_even, 8)
        lnw, rw, rb = load_ln_router(0)
        wc = p.sb("wc", [128, 12, 4])
        p.DM("sp", wc[:], gdn_convw, r=[DR], w=[wc])
        alog = p.sb("alog", [128, 4]); dtb = p.sb("dtb", [128, 4]); nrmw = p.sb("nrmw", [128, 128])
        bcast_load(alog, gdn_alog); bcast_load(dtb, gdn_dtb); bcast_load(nrmw, gdn_normw)
        p.V("act", "activation", alog[:], alog[:], AF.Exp, r=[alog], w=[alog])
        Sg = p.sb("Sg", [128, 4, 128])
        ctx3 = p.sb("ctx3", [128, 12, 3])
        xts = [p.sb("xt0", [128, D]), p.sb("xt1", [128, D])]
        xb = p.sb("xb", [128, D], BF16)
        xT = p.sb("xT", [128, 8, 128], BF16)
        cb = p.sb("cb", [128, 12, 131])
        cacc = p.sb("cacc", [128, 12, 128]); ctmp = p.sb("ctmp", [128, 12, 128])
        ztok = p.sb("ztok", [128, 8])
        mixT = p.sb("mixT", [128, 8, 128], BF16)
        qkn = p.sb("qkn", [128, 8, 128]); sq8 = p.sb("sq8", [128, 8, 128])
        kvtok = p.sb("kvtok", [128, 8, 128])
        gd = p.sb("gd", [128, 16]); gd2 = p.sb("gd2", [128, 4, 8])
        HT = []
        for hd in range(4):
            HT.append([p.sb("gt%d_%d" % (hd, i), [128, 128]) for i in range(17)])
        yB = p.sb("yB", [128, 512], BF16); ssq = p.sb("ssq", [128, 4])
        zs = p.sb("zs", [128, 512], BF16)
        work = alloc_tail_work()

        def load_x(tl, buf):
            if tl["nt"] < 128:
                p.V("pool", "memset", buf[:], 0.0, w=[buf])
            p.DM("sp", buf[0:tl["nt"], :], xin[tl["row0"]:tl["row0"] + tl["nt"], :], r=[DR], w=[buf])

        def head(hd, nt):
            A, B = 2 * hd, 2 * hd + 1
            gbc, dec, erow, attn, attnT, rv, rk_, nwT, ub, qdT, kd, Yt, M0, M1, T0, T1, osb = HT[hd]
            Mm = [M0, M1]; MT = [T0, T1]
            g2 = gd2[:, hd, :]
            kT = qkn[:, 4 + hd, 0:nt]
            qT = qkn[:, hd, 0:nt]
            p.V("dve", "tensor_scalar", gbc[0:nt, :], ones_f[0:nt, :], gd[0:nt, 8 + hd:9 + hd], None, ALU.mult, r=[ones_f, (gd, "g")], w=[gbc])
            yield
            p.V("pe", "matmul", PS[A][:, 0:nt], gbc[0:nt, :], uincl[0:nt, 0:nt], start=True, stop=True, r=[gbc, uincl], w=[PS[A]])
            p.V("pe", "matmul", PS[A][0:nt, 128:128 + nt], kT, kT, start=True, stop=True, r=[(qkn, "k")], w=[PS[A]])
            p.V("pe", "matmul", PS[A][0:nt, 256:256 + nt], qT, kT, start=True, stop=True, r=[(qkn, "q"), (qkn, "k")], w=[PS[A]])
            yield
            p.V("act", "activation", dec[0:nt, 0:nt], PS[A][0:nt, 0:nt], AF.Exp, bias=gd[0:nt, 12 + hd:13 + hd], scale=-1.0, r=[PS[A], (gd, "G")], w=[dec])
            yield
            p.V("act", "activation", erow[:, 0:nt], PS[A][:, 0:nt], AF.Exp, r=[PS[A]], w=[erow])
            yield
            p.V("dve", "tensor_copy", g2[:, 4:5], PS[A][:, nt - 1:nt], r=[PS[A]], w=[(gd2, (hd, "gl"))])
            yield
            p.V("pool", "affine_select", dec[0:nt, 0:nt], dec[0:nt, 0:nt], [[-1, nt]], ALU.is_ge, 0.0, base=0, channel_multiplier=1, r=[dec], w=[dec])
            yield
            p.V("dve", "scalar_tensor_tensor", Mm[0][0:nt, 0:nt], PS[A][0:nt, 128:128 + nt], gd[0:nt, 4 + hd:5 + hd], dec[0:nt, 0:nt], ALU.mult, ALU.mult,
                r=[PS[A], (gd, "nb"), dec], w=[Mm[0]])
            yield
            p.V("pool", "affine_select", Mm[0][0:nt, 0:nt], Mm[0][0:nt, 0:nt], [[-1, nt]], ALU.is_gt, 0.0, base=0, channel_multiplier=1, r=[Mm[0]], w=[Mm[0]])
            yield
            p.V("dve", "tensor_tensor", attn[0:nt, 0:nt], PS[A][0:nt, 256:256 + nt], dec[0:nt, 0:nt], ALU.mult, r=[PS[A], dec], w=[attn])
            yield
            p.V("pe", "transpose", PS[B][0:nt, 0:nt], Mm[0][0:nt, 0:nt], identf[0:nt, 0:nt], r=[Mm[0], identf], w=[PS[B]])
            p.V("pe", "transpose", PS[B][0:nt, 128:128 + nt], attn[0:nt, 0:nt], identf[0:nt, 0:nt], r=[attn, identf], w=[PS[B]])
            yield
            p.V("act", "activation", MT[0][0:nt, 0:nt], PS[B][0:nt, 0:nt], AF.Copy, r=[PS[B]], w=[MT[0]])
            yield
            p.V("dve", "tensor_tensor", Yt[0:nt, 0:nt], PS[B][0:nt, 0:nt], identf[0:nt, 0:nt], ALU.add, r=[PS[B], identf], w=[Yt])
            yield
            p.V("act", "activation", attnT[0:nt, 0:nt], PS[B][0:nt, 128:128 + nt], AF.Copy, r=[PS[B]], w=[attnT])
            yield
            nlev = 6 if nt == 128 else 3
            for lv in range(nlev):
                a, b_ = lv % 2, (lv + 1) % 2
                bank = A if lv % 2 == 0 else B
                p.V("pe", "matmul", PS[bank][0:nt, 0:nt], MT[a][0:nt, 0:nt], Mm[a][0:nt, 0:nt], start=True, stop=True, r=[MT[a], Mm[a]], w=[PS[bank]])
                if lv < nlev - 1:
                    p.V("pe", "matmul", PS[bank][0:nt, 128:128 + nt], Mm[a][0:nt, 0:nt], MT[a][0:nt, 0:nt], start=True, stop=True, r=[MT[a], Mm[a]], w=[PS[bank]])
                yield
                p.V("act", "activation", Mm[b_][0:nt, 0:nt], PS[bank][0:nt, 0:nt], AF.Copy, r=[PS[bank]], w=[Mm[b_]])
                yield
                if lv < nlev - 1:
                    p.V("dve", "tensor_copy", MT[b_][0:nt, 0:nt], PS[bank][0:nt, 128:128 + nt], r=[PS[bank]], w=[MT[b_]])
                    yield
                p.V("pe", "matmul", PS[bank][0:nt, 256:256 + nt], Mm[b_][0:nt, 0:nt], Yt[0:nt, 0:nt], start=True, stop=True, r=[Mm[b_], Yt], w=[PS[bank]])
                yield
                p.V("dve", "tensor_tensor", Yt[0:nt, 0:nt], Yt[0:nt, 0:nt], PS[bank][0:nt, 256:256 + nt], ALU.add, r=[Yt, PS[bank]], w=[Yt])
                yield
            p.V("dve", "tensor_scalar", rv[0:nt, :], kvtok[0:nt, 4 + hd, :], gd[0:nt, hd:hd + 1], None, ALU.mult, r=[(kvtok, "v"), (gd, "b")], w=[rv])
            p.V("pool", "tensor_scalar", rk_[0:nt, :], kvtok[0:nt, hd, :], gd[0:nt, 16 + hd:17 + hd] if False else g2[0:nt, 0:1], None, ALU.mult, r=[(kvtok, "k"), (gd2, (hd, "be"))], w=[rk_])
            yield
            p.V("pe", "matmul", PS[A][:, 0:nt], rk_[0:nt, :], Yt[0:nt, 0:nt], start=True, stop=True, r=[rk_, Yt], w=[PS[A]])
            yield
            p.V("dve", "tensor_scalar", nwT[:, 0:nt], PS[A][:, 0:nt], -1.0, None, ALU.mult, r=[PS[A]], w=[nwT])
            yield
            p.V("pe", "matmul", PS[B][0:nt, 0:128], Yt[0:nt, 0:nt], rv[0:nt, :], start=True, stop=False, r=[Yt, rv], w=[PS[B]])
            p.V("pe", "matmul", PS[B][0:nt, 0:128], nwT[:, 0:nt], Sg[:, hd, :], start=False, stop=True, r=[nwT, (Sg, hd)], w=[PS[B]])
            yield
            p.V("act", "activation", ub[0:nt, :], PS[B][0:nt, 0:128], AF.Copy, r=[PS[B]], w=[ub])
            yield
            p.V("pool", "tensor_tensor", qdT[:, 0:nt], qT, erow[:, 0:nt], ALU.mult, r=[(qkn, "q"), erow], w=[qdT])
            yield
            p.V("pe", "matmul", PS[A][0:nt, 128:256], qdT[:, 0:nt], Sg[:, hd, :], start=True, stop=False, r=[qdT, (Sg, hd)], w=[PS[A]])
            p.V("pe", "matmul", PS[A][0:nt, 128:256], attnT[0:nt, 0:nt], ub[0:nt, :], start=False, stop=True, r=[attnT, ub], w=[PS[A]])
            yield
            p.V("act", "activation", g2[0:nt, 1:2], gd[0:nt, 12 + hd:13 + hd], AF.Exp, bias=g2[0:nt, 4:5], scale=-1.0,
                r=[(gd, "G"), (gd2, (hd, "gl"))], w=[(gd2, (hd, "kd"))])
            yield
            p.V("dve", "tensor_scalar", kd[0:nt, :], kvtok[0:nt, hd, :], g2[0:nt, 1:2], None, ALU.mult, r=[(kvtok, "k"), (gd2, (hd, "kd"))], w=[kd])
            yield
            p.V("pe", "matmul", PS[B][:, 128:256], kd[0:nt, :], ub[0:nt, :], start=True, stop=True, r=[kd, ub], w=[PS[B]])
            yield
            p.V("act", "activation", g2[:, 5:6], g2[:, 4:5], AF.Exp, r=[(gd2, (hd, "gl"))], w=[(gd2, (hd, "egl"))])
            yield
            p.V("dve", "scalar_tensor_tensor", Sg[:, hd, :], Sg[:, hd, :], g2[:, 5:6], PS[B][:, 128:256], ALU.mult, ALU.add,
                r=[(Sg, hd), (gd2, (hd, "egl")), PS[B]], w=[(Sg, hd)])
            yield
            p.V("act", "activation", osb[0:nt, :], PS[A][0:nt, 128:256], AF.Square, r=[PS[A]], w=[osb])
            yield
            p.V("dve", "reduce_sum", ssq[0:nt, hd:hd + 1], osb[0:nt, :], mybir.AxisListType.X, r=[osb], w=[(ssq, hd)])
            yield
            p.V("act", "activation", ssq[0:nt, hd:hd + 1], ssq[0:nt, hd:hd + 1], AF.Sqrt, bias=epsln[0:nt, 1:2], scale=1.0 / 128.0, r=[(ssq, hd), epsln], w=[(ssq, hd)])
            yield
            p.V("dve", "reciprocal", ssq[0:nt, hd:hd + 1], ssq[0:nt, hd:hd + 1], r=[(ssq, hd)], w=[(ssq, hd)])
            yield
            p.V("dve", "scalar_tensor_tensor", osb[0:nt, :], PS[A][0:nt, 128:256], ssq[0:nt, hd:hd + 1], nrmw[0:nt, :], ALU.mult, ALU.mult,
                r=[PS[A], (ssq, hd), nrmw], w=[osb])
            yield
            p.V("pool", "tensor_tensor", yB[0:nt, hd * 128:(hd + 1) * 128], osb[0:nt, :], zs[0:nt, hd * 128:(hd + 1) * 128], ALU.mult, r=[osb, zs], w=[(yB, hd)])
            yield

        load_x(tiles[0], xts[0])
        for tl in tiles:
            nt, ti, sq = tl["nt"], tl["ti"], tl["seq"]
            xt = xts[ti % 2]
            if ti + 1 < NT:
                load_x(tiles[ti + 1], xts[(ti + 1) % 2])
            if tl["first"]:
                if sq < NPS:
                    p.V("pool", "memset", Sg[:], 0.0, w=[Sg]); p.V("pool", "memset", ctx3[:], 0.0, w=[ctx3])
                else:
                    p.DM("sp", Sg[:], st_gdn.rearrange("h k v -> k h v"), r=[DR], w=[Sg])
                    p.DM("sp", ctx3[:], st_conv, r=[DR], w=[ctx3])
            p.DM("sp", mixT[:, 0:4, 0:nt], yas[ti, :, :, 0:nt], r=[(T_yas, ti)], w=[(mixT, "a")])
            p.V("act", "activation", xb[0:nt, :], xt[0:nt, :], AF.Copy, r=[xt], w=[xb])
            transpose_to(xT, xb, nt, 8, 0)
            p.V("pool", "tensor_copy", cb[:, :, 0:3], ctx3[:, :, :], r=[ctx3], w=[(cb, "c")])
            for g4 in range(3):
                bank = 1 + g4
                for b in range(4):
                    blk = g4 * 4 + b
                    for kc in range(8):
                        p.V("pe", "matmul", PS[bank][:, b * 128:b * 128 + nt], win[:, kc, blk * 128:(blk + 1) * 128],
                            xT[:, kc, 0:nt], start=(kc == 0), stop=(kc == 7), r=[win, xT], w=[PS[bank]])
                evac(cb[:, g4 * 4:g4 * 4 + 4, 3:3 + nt], PS[bank][:, :].rearrange("p (a b) -> p a b", a=4)[:, :, 0:nt],
                     r=[PS[bank]], w=[(cb, g4)])
            for kc in range(8):
                p.V("pe", "matmul", PS[4][0:nt, 0:512], xT[:, kc, 0:nt], win[:, kc, 1536:2048], start=(kc == 0), stop=(kc == 7),
                    r=[xT, win], w=[PS[4]])
            for kc in range(8):
                p.V("pe", "matmul", PS[5][0:nt, 0:8], xT[:, kc, 0:nt], win[:, kc, 2048:2056], start=(kc == 0), stop=(kc == 7),
                    r=[xT, win], w=[PS[5]])
            p.V("act", "activation", zs[0:nt, :], PS[4][0:nt, 0:512], AF.Silu, r=[PS[4]], w=[zs])
            p.V("dve", "tensor_copy", ztok[0:nt, 0:8], PS[5][0:nt, 0:8], r=[PS[5]], w=[ztok])
            for j in range(4):
                wj = wc[:, :, j:j + 1].to_broadcast([128, 12, nt])
                if j == 0:
                    p.V("dve", "tensor_tensor", cacc[:, :, 0:nt], cb[:, :, 0:nt], wj, ALU.mult, r=[cb, wc], w=[cacc])
                else:
                    p.V("pool", "tensor_tensor", ctmp[:, :, 0:nt], cb[:, :, j:j + nt], wj, ALU.mult, r=[cb, wc], w=[ctmp])
                    p.V("dve", "tensor_tensor", cacc[:, :, 0:nt], cacc[:, :, 0:nt], ctmp[:, :, 0:nt], ALU.add, r=[cacc, ctmp], w=[cacc])
            p.V("pool", "tensor_copy", ctx3[:, :, :], cb[:, :, nt:nt + 3], r=[cb], w=[ctx3])
            if tl["last"]:
                p.DM("sp", o_conv[sq], ctx3[:], r=[ctx3], w=[T_out])
            p.V("act", "activation", cacc[:, :, 0:nt], cacc[:, :, 0:nt], AF.Silu, r=[cacc], w=[cacc])
            p.V("act", "activation", sq8[:, :, 0:nt], cacc[:, 0:8, 0:nt], AF.Square, r=[cacc], w=[sq8])
            for hb in range(2):
                for b in range(4):
                    p.V("pe", "matmul", PS[2 + hb][:, b * 128:b * 128 + nt], ones_f[:, :], sq8[:, hb * 4 + b, 0:nt], start=True, stop=True,
                        r=[ones_f, sq8], w=[PS[2 + hb]])
            for hb in range(2):
                v = PS[2 + hb][:, :].rearrange("p (a b) -> p a b", a=4)[:, :, 0:nt]
                p.V("act", "activation", sq8[:, hb * 4:hb * 4 + 4, 0:nt], v, AF.Sqrt, bias=epsln[:, 1:2], r=[PS[2 + hb], epsln], w=[sq8])
            p.V("dve", "reciprocal", sq8[:, :, 0:nt], sq8[:, :, 0:nt], r=[sq8], w=[sq8])
            p.V("dve", "scalar_tensor_tensor", qkn[:, 0:4, 0:nt], cacc[:, 0:4, 0:nt], 128.0 ** -0.5, sq8[:, 0:4, 0:nt], ALU.mult, ALU.mult,
                r=[cacc, sq8], w=[(qkn, "q")])
            p.V("pool", "tensor_tensor", qkn[:, 4:8, 0:nt], cacc[:, 4:8, 0:nt], sq8[:, 4:8, 0:nt], ALU.mult, r=[cacc, sq8], w=[(qkn, "k")])
            for b in range(4):
                p.V("pe", "transpose", PS[2][0:nt, b * 128:(b + 1) * 128], qkn[:, 4 + b, 0:nt], identf[:, :], r=[(qkn, "k"), identf], w=[PS[2]])
                p.V("pe", "transpose", PS[3][0:nt, b * 128:(b + 1) * 128], cacc[:, 8 + b, 0:nt], identf[:, :], r=[cacc, identf], w=[PS[3]])
            evac(kvtok[0:nt, 0:4, :], PS[2][0:nt, :].rearrange("p (a b) -> p a b", a=4), r=[PS[2]], w=[(kvtok, "k")])
            evac(kvtok[0:nt, 4:8, :], PS[3][0:nt, :].rearrange("p (a b) -> p a b", a=4), r=[PS[3]], w=[(kvtok, "v")])
            p.V("act", "activation", gd[0:nt, 0:4], ztok[0:nt, 0:4], AF.Sigmoid, r=[ztok], w=[(gd, "b")])
            p.V("dve", "tensor_scalar", gd[0:nt, 4:8], gd[0:nt, 0:4], -1.0, None, ALU.mult, r=[(gd, "b")], w=[(gd, "nb")])
            p.V("dve", "tensor_tensor", gd[0:nt, 8:12], ztok[0:nt, 4:8], dtb[0:nt, :], ALU.add, r=[ztok, dtb], w=[(gd, "g")])
            p.V("act", "activation", gd[0:nt, 8:12], gd[0:nt, 8:12], AF.Exp, r=[(gd, "g")], w=[(gd, "g")])
            p.V("act", "activation", gd[0:nt, 8:12], gd[0:nt, 8:12], AF.Ln, bias=1.0, r=[(gd, "g")], w=[(gd, "g")])
            p.V("dve", "scalar_tensor_tensor", gd[0:nt, 8:12], gd[0:nt, 8:12], -1.0, alog[0:nt, :], ALU.mult, ALU.mult, r=[(gd, "g"), alog], w=[(gd, "g")])
            p.V("pe", "matmul", PS[0][0:nt, 0:4], uincl[0:nt, 0:nt], gd[0:nt, 8:12], start=True, stop=True, r=[uincl, (gd, "g")], w=[PS[0]])
            p.V("dve", "tensor_copy", gd[0:nt, 12:16], PS[0][0:nt, 0:4], r=[PS[0]], w=[(gd, "G")])
            p.V("act", "activation", gd2[0:nt, :, 2], gd[0:nt, 12:16], AF.Exp, r=[(gd, "G")], w=[(gd2, "e")])
            p.V("dve", "tensor_tensor", gd2[0:nt, :, 0], gd2[0:nt, :, 2], gd[0:nt, 0:4], ALU.mult, r=[(gd2, "e"), (gd, "b")],
                w=[(gd2, (0, "be")), (gd2, (1, "be")), (gd2, (2, "be")), (gd2, (3, "be"))])
            run_streams([head(hd, nt) for hd in range(4)])
            if tl["last"]:
                p.DM("sp", o_gdn[sq].rearrange("h k v -> k h v"), Sg[:], r=[Sg], w=[T_out])
            for b in range(4):
                p.V("pe", "transpose", psbf(2, 1024)[:, b * 128:b * 128 + nt], yB[0:nt, b * 128:(b + 1) * 128], identb[0:nt, 0:nt], r=[yB, identb], w=[PS[2]])
            evac(mixT[:, 4:8, 0:nt], psbf(2, 1024).rearrange("p (a b) -> p a b", a=8)[:, 0:4, 0:nt], r=[PS[2]], w=[(mixT, "b")])
            for half in range(2):
                for kc in range(8):
                    p.V("pe", "matmul", PS[2 + half][0:nt, :], mixT[:, kc, 0:nt], wout[:, kc, half * 512:(half + 1) * 512], start=(kc == 0), stop=(kc == 7),
                        r=[mixT, wout], w=[PS[2 + half]])
            phaseA_tail(0, tl, xt, (2, 3), lnw, rw, rb, work)
        p.release(m0)

    def phaseA1():
        m0 = p.mark()
        win = p.sb("win1", [128, 8, ODD_IN], BF16)
        load_w_bf16(win, w_in_odd, 8)
        wout = p.sb("wout1", [128, 16, D], BF16)
        load_w_bf16(wout, w_out_odd, 16)
        lnw, rw, rb = load_ln_router(1)
        retmask = p.sb("retmask", [128, 4, 128]); retqs = p.sb("retqs", [128, 4, 128]); retks = p.sb("retks", [128, 8])
        for b, nm in ((retmask, "retmask"), (retqs, "retqs"), (retks, "retks")):
            p.DM("sp", b[:], cst[nm], r=[DR], w=[b])
        Rf = p.sb("Rf", [128, 4, 2, 512])
        xt_one = p.sb("xt0", [128, D])
        xts = [xt_one, xt_one]
        xb = p.sb("xb", [128, D], BF16)
        xT = p.sb("xT", [128, 8, 128], BF16)
        cs = p.sb("cs", [128, 2, 128])
        qkT = p.sb("qkT", [128, 16, 128], BF16)
        qsT = p.sb("qsT", [128, 2, 128])
        rt1 = p.sb("rt1", [128, 128]); rt2 = p.sb("rt2", [128, 128])
        ktok = p.sb("ktok", [128, 8, 128], BF16)
        vtok = p.sb("vtok", [128, 2048], BF16); gsil = p.sb("gsil", [128, 2048], BF16)
        sT = p.sb("sT", [128, 128], BF16)
        og = vtok
        ogT = p.sb("ogT", [128, 16, 128], BF16)
        onrm = p.sb("onrm", [128, 512]); st6 = p.sb("st6", [128, 6]); st2 = p.sb("st2", [128, 4])
        work = alloc_tail_work(h=xt_one)

        def load_x(tl, buf):
            if tl["nt"] < 128:
                p.V("pool", "memset", buf[:], 0.0, w=[buf])
            p.DM("sp", buf[0:tl["nt"], :], x3s[tl["ti"] * 128:tl["ti"] * 128 + tl["nt"], :], r=[(T_x3s, tl["ti"])], w=[buf])

        for tl in tiles:
            nt, ti, sq = tl["nt"], tl["ti"], tl["seq"]
            Lc = 0 if nt == 128 else 1
            xt = xts[ti % 2]
            load_x(tl, xt)
            if tl["first"]:
                if sq < NPS:
                    p.V("pool", "memset", Rf[:], 0.0, w=[Rf])
                else:
                    for h in range(4):
                        for par in range(2):
                            p.DM("sp", Rf[:, h, par, :], st_ret[h].rearrange("(q two) v -> q two v", two=2)[:, par, :], r=[DR], w=[(Rf, (h, par))])
            p.DM("sp", cs[:, 0, 0:nt], cst["rcos"][:, tl["pos0"]:tl["pos0"] + nt], r=[DR], w=[(cs, 0)])
            p.DM("sp", cs[:, 1, 0:nt], cst["rsin"][:, tl["pos0"]:tl["pos0"] + nt], r=[DR], w=[(cs, 1)])
            p.V("act", "activation", xb[0:nt, :], xt[0:nt, :], AF.Copy, r=[xt], w=[xb])
            transpose_to(xT, xb, nt, 8, 0)
            for qk in range(2):
                for h in range(4):
                    bank = 1 + ((qk * 4 + h) % 2)
                    for par in range(2):
                        col0 = qk * 1024 + h * 256 + par * 128
                        for kc in range(8):
                            p.V("pe", "matmul", PS[bank][:, par * 128:par * 128 + nt], win[:, kc, col0:col0 + 128], xT[:, kc, 0:nt],
                                start=(kc == 0), stop=(kc == 7), r=[win, xT], w=[PS[bank]])
                    x0 = PS[bank][:, 0:nt]
                    x1_ = PS[bank][:, 128:128 + nt]
                    blk = qk * 8 + h * 2
                    p.V("dve", "tensor_tensor", rt1[:, 0:nt], x0, cs[:, 0, 0:nt], ALU.mult, r=[PS[bank], cs], w=[rt1])
                    p.V("dve", "tensor_tensor", rt2[:, 0:nt], x1_, cs[:, 1, 0:nt], ALU.mult, r=[PS[bank], cs], w=[rt2])
                    p.V("pool", "tensor_tensor", qkT[:, blk, 0:nt], rt1[:, 0:nt], rt2[:, 0:nt], ALU.subtract, r=[rt1, rt2], w=[(qkT, blk)])
                    p.V("dve", "tensor_tensor", rt1[:, 0:nt], x0, cs[:, 1, 0:nt], ALU.mult, r=[PS[bank], cs, (qkT, blk)], w=[rt1])
                    p.V("dve", "tensor_tensor", rt2[:, 0:nt], x1_, cs[:, 0, 0:nt], ALU.mult, r=[PS[bank], cs, (qkT, blk)], w=[rt2])
                    p.V("pool", "tensor_tensor", qkT[:, blk + 1, 0:nt], rt1[:, 0:nt], rt2[:, 0:nt], ALU.add, r=[rt1, rt2], w=[(qkT, blk + 1)])
            for cg in range(8):
                bank = 3 + (cg % 2)
                for kc in range(8):
                    p.V("pe", "matmul", PS[bank][0:nt, :], xT[:, kc, 0:nt], win[:, kc, 2048 + cg * 512:2048 + (cg + 1) * 512], start=(kc == 0), stop=(kc == 7),
                        r=[xT, win], w=[PS[bank]])
                if cg < 4:
                    p.V("dve", "tensor_copy", vtok[0:nt, cg * 512:(cg + 1) * 512], PS[bank][0:nt, :], r=[PS[bank]], w=[(vtok, cg)])
                else:
                    p.V("act", "activation", gsil[0:nt, (cg - 4) * 512:(cg - 3) * 512], PS[bank][0:nt, :], AF.Silu, r=[PS[bank]], w=[(gsil, cg - 4)])
            for b in range(8):
                p.V("pe", "transpose", psbf(5, 1024)[0:nt, b * 128:(b + 1) * 128], qkT[:, 8 + b, 0:nt], identb[:, :], r=[(qkT, 8 + b), identb], w=[PS[5]])
            for h in range(4):
                p.V("dve", "tensor_scalar", ktok[0:nt, 2 * h:2 * h + 2, :], psbf(5, 1024).rearrange("p (a b) -> p a b", a=8)[0:nt, 2 * h:2 * h + 2, :],
                    retks[0:nt, Lc * 4 + h:Lc * 4 + h + 1], None, ALU.mult, r=[PS[5], retks], w=[(ktok, h)])
            for h in range(4):
                for dc in range(2):
                    p.V("pe", "matmul", PS[6][0:nt, 0:nt], qkT[:, 8 + 2 * h + dc, 0:nt], qkT[:, 2 * h + dc, 0:nt], start=(dc == 0), stop=(dc == 1),
                        r=[(qkT, 8 + 2 * h + dc), (qkT, 2 * h + dc)], w=[PS[6]])
                p.V("dve", "scalar_tensor_tensor", sT[0:nt, 0:nt], PS[6][0:nt, 0:nt], 256.0 ** -0.5, retmask[0:nt, h, 0:nt], ALU.mult, ALU.mult,
                    r=[PS[6], retmask], w=[sT])
                for dc in range(2):
                    p.V("pool", "tensor_tensor", qsT[:, dc, 0:nt], qkT[:, 2 * h + dc, 0:nt], retqs[:, h, 0:nt], ALU.mult, r=[(qkT, 2 * h + dc), retqs], w=[(qsT, dc)])
                p.V("pe", "matmul", PS[7][0:nt, :], sT[0:nt, 0:nt], vtok[0:nt, h * 512:(h + 1) * 512], start=True, stop=False, r=[sT, (vtok, h)], w=[PS[7]])
                for dc in range(2):
                    p.V("pe", "matmul", PS[7][0:nt, :], qsT[:, dc, 0:nt], Rf[:, h, dc, :], start=False, stop=(dc == 1), r=[(qsT, dc), Rf], w=[PS[7]])
                cdec = cst_host["retcdec"][Lc][h]
                for dc in range(2):
                    bank = 1 + dc
                    p.V("pe", "matmul", PS[bank][:, :], ktok[0:nt, 2 * h + dc, :], vtok[0:nt, h * 512:(h + 1) * 512], start=True, stop=True,
                        r=[(ktok, h), (vtok, h)], w=[PS[bank]])
                    p.V("dve", "scalar_tensor_tensor", Rf[:, h, dc, :], Rf[:, h, dc, :], cdec, PS[bank][:, :], ALU.mult, ALU.add, r=[Rf, PS[bank]], w=[Rf])
                p.V("dve", "bn_stats", st6[0:nt, :], PS[7][0:nt, :], r=[PS[7]], w=[st6])
                p.V("dve", "bn_aggr", st2[0:nt, 0:2], st6[0:nt, :], r=[st6], w=[st2])
                p.V("act", "activation", st2[0:nt, 2:3], st2[0:nt, 1:2], AF.Sqrt, bias=epsln[0:nt, 0:1], r=[st2, epsln], w=[(st2, "s")])
                p.V("dve", "reciprocal", st2[0:nt, 3:4], st2[0:nt, 2:3], r=[(st2, "s")], w=[(st2, "r")])
                p.V("dve", "tensor_scalar", onrm[0:nt, :], PS[7][0:nt, :], st2[0:nt, 0:1], st2[0:nt, 3:4], ALU.subtract, ALU.mult, r=[PS[7], st2, (st2, "r")], w=[onrm])
                p.V("pool", "tensor_tensor", og[0:nt, h * 512:(h + 1) * 512], onrm[0:nt, :], gsil[0:nt, h * 512:(h + 1) * 512], ALU.mult, r=[onrm, (gsil, h)], w=[(vtok, h)])
            if tl["last"]:
                for h in range(4):
                    for par in range(2):
                        p.DM("sp", o_ret[sq, h].rearrange("(q two) v -> q two v", two=2)[:, par, :], Rf[:, h, par, :], r=[Rf], w=[T_out])
            transpose_to(ogT, og, nt, 16, 5)
            for half in range(2):
                for kc in range(16):
                    p.V("pe", "matmul", PS[2 + half][0:nt, :], ogT[:, kc, 0:nt], wout[:, kc, half * 512:(half + 1) * 512], start=(kc == 0), stop=(kc == 15),
                        r=[ogT, wout], w=[PS[2 + half]])
            phaseA_tail(1, tl, xt, (2, 3), lnw, rw, rb, work)
        p.release(m0)

    def phaseM(li):
        m0 = p.mark()
        w1b = [p.sb("w1b0", [128, 8, 2 * D], BF16), p.sb("w1b1", [128, 8, 2 * D], BF16)]
        w2b = [p.sb("w2b0", [128, 8, D], BF16), p.sb("w2b1", [128, 8, D], BF16)]
        b1t = [p.sb("b1t0", [128, 16]), p.sb("b1t1", [128, 16])]
        b2f = [p.sb("b2f0", [1, D]), p.sb("b2f1", [1, D])]
        b2b = p.sb("b2b", [1, D], BF16)
        xg = p.sb("xg", [128, CT, D], BF16)
        xgT = p.sb("xgT", [128, 8, C], BF16)
        glu = p.sb("glu", [128, C]); sig = p.sb("sig", [128, C]); lin = p.sb("lin", [128, C])
        actT = p.sb("actT", [128, 8, C], BF16)
        yo = [p.sb("yo0", [128, D]), p.sb("yo1", [128, D])]

        def load_w(e):
            s = e % 2
            load_w_bf16(w1b[s], moe_w1[li, e], 8)
            load_w_bf16(w2b[s], moe_w2[li, e], 8)
            p.DM("sp", b1t[s][:], moe_b1[li, e], r=[DR], w=[b1t[s]])
            p.DM("sp", b2f[s][:], moe_b2[li, e:e + 1, :], r=[DR], w=[b2f[s]])

        load_w(0)
        for e in range(NE):
            s = e % 2
            if e + 1 < NE:
                load_w(e + 1)
            p.DM("sp", xg[:], xs[e * C:(e + 1) * C, :].rearrange("(ct q) d -> q ct d", q=128), r=[(T_xs, "*")], w=[xg, (T_xs, e)])
            p.V("act", "activation", b2b[:], b2f[s][:], AF.Copy, r=[b2f[s]], w=[b2b])
            for ct in range(CT):
                bank = 4 + (ct % 2)
                for kc in range(8):
                    p.V("pe", "transpose", psbf(bank, 1024)[:, kc * 128:(kc + 1) * 128], xg[:, ct, kc * 128:(kc + 1) * 128], identb[:, :],
                        r=[xg, identb], w=[PS[bank]])
                evac(xgT[:, :, ct * 128:(ct + 1) * 128], psbf(bank, 1024).rearrange("p (a b) -> p a b", a=8), r=[PS[bank]], w=[(xgT, ct)])
            for i in range(8):
                for part in range(2):
                    fc = i + part * 8
                    banks = [(0, 1), (2, 3)][(i * 2 + part) % 2]
                    for gi, (ca, cb_) in enumerate(cgs):
                        for kc in range(8):
                            p.V("pe", "matmul", PS[banks[gi]][:, 0:cb_ - ca], w1b[s][:, kc, fc * 128:(fc + 1) * 128], xgT[:, kc, ca:cb_],
                                start=(kc == 0), stop=(kc == 7), r=[w1b[s], xgT], w=[PS[banks[gi]]])
                    for gi, (ca, cb_) in enumerate(cgs):
                        src = PS[banks[gi]][:, 0:cb_ - ca]
                        if part == 0:
                            p.V("dve", "tensor_scalar", glu[:, ca:cb_], src, b1t[s][:, fc:fc + 1], 7.0, ALU.add, ALU.min, r=[PS[banks[gi]], b1t[s]], w=[(glu, gi)])
                        else:
                            p.V("dve", "tensor_scalar", lin[:, ca:cb_], src, b1t[s][:, fc:fc + 1], 7.0, ALU.add, ALU.min, r=[PS[banks[gi]], b1t[s]], w=[(lin, gi)])
                    if part == 0:
                        p.V("act", "activation", sig[:, :], glu[:, :], AF.Sigmoid, scale=1.702, r=[glu], w=[sig])
                        p.V("pool", "tensor_tensor", glu[:, :], glu[:, :], sig[:, :], ALU.mult, r=[glu, sig], w=[glu])
                    else:
                        p.V("dve", "tensor_scalar", lin[:, :], lin[:, :], -7.0, 1.0, ALU.max, ALU.add, r=[lin], w=[lin])
                        p.V("pool", "tensor_tensor", actT[:, i, :], glu[:, :], lin[:, :], ALU.mult, r=[glu, lin], w=[(actT, i)])
            for ct in range(CT):
                yb = yo[ct % 2]
                for half in range(2):
                    bank = 4 + (ct % 2) * 2 + half
                    for fc in range(8):
                        p.V("pe", "matmul", PS[bank][:, :], actT[:, fc, ct * 128:(ct + 1) * 128], w2b[s][:, fc, half * 512:(half + 1) * 512],
                            start=(fc == 0), stop=False, r=[actT, w2b[s]], w=[PS[bank]])
                    p.V("pe", "matmul", PS[bank][:, :], ones_b[0:1, :], b2b[0:1, half * 512:(half + 1) * 512], start=False, stop=True,
                        r=[ones_b, b2b], w=[PS[bank]])
                    evac(yb[:, half * 512:(half + 1) * 512], PS[bank][:, :], r=[PS[bank]], w=[(yb, half)])
                p.DM("sp", ys[e * C + ct * 128:e * C + (ct + 1) * 128, :], yb[:, :], r=[yb], w=[(T_ys, (e, ct))])
        p.release(m0)

    def phaseC(li):
        m0 = p.mark()
        wg = p.sb("wg", [128, 8, D], BF16)
        load_w_bf16(wg, ple_gw[li], 8)
        wp = p.sb("wp", [128, 2, D], BF16)
        load_w_bf16(wp, ple_w[li], 2)
        g2 = p.sb("ln2g", [128, D]); b2 = p.sb("ln2b", [128, D])
        bcast_load(g2, ln2_g[li:li + 1, :]); bcast_load(b2, ln2_b[li:li + 1, :])
        rows = [[p.sb(f"row{s}{k}", [128, D]) for k in range(4)] for s in range(2)]
        x1t = [p.sb("x1t0", [128, D]), p.sb("x1t1", [128, D])]
        pt = [p.sb("pt0", [128, 256]), p.sb("pt1", [128, 256])]
        ff = p.sb("ff", [128, D]); x2 = p.sb("x2", [128, D]); x2b = p.sb("x2b", [128, D], BF16)
        x2T = p.sb("x2T", [128, 8, 128], BF16)
        pb = p.sb("pb", [128, 256], BF16); pT = p.sb("pT", [128, 2, 128], BF16)
        gt = p.sb("gt", [128, D]); x3 = p.sb("x3", [128, D])
        scr6 = p.sb("scr6", [128, 2, 6]); scr2 = p.sb("scr2", [128, 4])

        def loads(tl):
            s = tl["ti"] % 2
            ti, nt = tl["ti"], tl["nt"]
            for k in range(4):
                p.dma("pool", lambda e, k=k, ti=ti, s=s: e.indirect_dma_start(
                    out=rows[s][k][:, :], out_offset=None, in_=ys[:, :],
                    in_offset=bass.IndirectOffsetOnAxis(ap=slots_all[:, ti, k:k + 1], axis=0)),
                    r=[(T_ys, "*"), (slots_all, ti)], w=[rows[s][k]])
            p.DM("sp", x1t[s][0:nt, :], x1s[ti * 128:ti * 128 + nt, :], r=[(T_x1s, ti)], w=[x1t[s]])
            p.DM("sp", pt[s][0:nt, :], pin[li, tl["row0"]:tl["row0"] + nt, :], r=[DR], w=[pt[s]])

        loads(tiles[0])
        for tl in tiles:
            nt, ti = tl["nt"], tl["ti"]
            s = ti % 2
            if ti + 1 < NT:
                loads(tiles[ti + 1])
            p.V("dve", "tensor_scalar", ff[0:nt, :], rows[s][0][0:nt, :], gates_all[0:nt, ti, 0:1], None, ALU.mult, r=[rows[s][0], (gates_all, ti)], w=[ff])
            for k in range(1, 4):
                eng = "dve"
                p.V(eng, "scalar_tensor_tensor", ff[0:nt, :], rows[s][k][0:nt, :], gates_all[0:nt, ti, k:k + 1], ff[0:nt, :], ALU.mult, ALU.add,
                    r=[rows[s][k], (gates_all, ti), ff], w=[ff])
            p.V("dve", "scalar_tensor_tensor", ff[0:nt, :], x1t[s][0:nt, :], ALPHA, ff[0:nt, :], ALU.mult, ALU.add, r=[x1t[s], ff], w=[ff])
            layernorm("dve", ff, nt, g2, b2, x2, scr6, scr2)
            p.V("act", "activation", x2b[0:nt, :], x2[0:nt, :], AF.Copy, r=[x2], w=[x2b])
            transpose_to(x2T, x2b, nt, 8, 0)
            p.V("act", "activation", pb[0:nt, :], pt[s][0:nt, :], AF.Copy, r=[pt[s]], w=[pb])
            transpose_to(pT, pb, nt, 2, 1)
            for half in range(2):
                for kc in range(8):
                    p.V("pe", "matmul", PS[2 + half][0:nt, :], x2T[:, kc, 0:nt], wg[:, kc, half * 512:(half + 1) * 512], start=(kc == 0), stop=(kc == 7),
                        r=[x2T, wg], w=[PS[2 + half]])
                for kc in range(2):
                    p.V("pe", "matmul", PS[4 + half][0:nt, :], pT[:, kc, 0:nt], wp[:, kc, half * 512:(half + 1) * 512], start=(kc == 0), stop=(kc == 1),
                        r=[pT, wp], w=[PS[4 + half]])
                hs = slice(half * 512, (half + 1) * 512)
                p.V("act", "activation", gt[0:nt, hs], PS[2 + half][0:nt, :], AF.Sigmoid, r=[PS[2 + half]], w=[(gt, half)])
                p.V("dve", "tensor_tensor", gt[0:nt, hs], gt[0:nt, hs], PS[4 + half][0:nt, :], ALU.mult, r=[(gt, half), PS[4 + half]], w=[(gt, half)])
                p.V("pool", "tensor_tensor", x3[0:nt, hs], gt[0:nt, hs], x2[0:nt, hs], ALU.add, r=[(gt, half), x2], w=[(x3, half)])
            if li == 0:
                p.DM("sp", x3s[ti * 128:ti * 128 + nt, :], x3[0:nt, :], r=[x3], w=[(T_x3s, ti)])
                if "x3_0" in dbg_out:
                    p.DM("sp", dbg_out["x3_0"][tl["row0"]:tl["row0"] + nt, :], x3[0:nt, :], r=[x3], w=[T_out])
            else:
                p.DM("sp", y_out[tl["row0"]:tl["row0"] + nt, :], x3[0:nt, :], r=[x3], w=[T_out])
        p.release(m0)

    cst_host = host_consts()
    for st in stages:
        if st == "A0":
            if os.environ.get("KOLDA0"):
                phaseA0()
            else:
                phaseS0()
                phaseG0()
        elif st == "A1":
            phaseA1()
        elif st[0] == "M":
            phaseM(int(st[1]))
        elif st[0] == "C":
            phaseC(int(st[1]))
    p.finalize()
    return nc


def prep_shared(inp):
    f = lambda a: np.ascontiguousarray(np.asarray(a, dtype=np.float32))
    sh = {}
    sh["w_in_even"] = f(inp["w_in_even"][0])
    qb = lambda v, nb: np.ascontiguousarray(np.asarray(v, dtype=np.float32).reshape(nb, 128).T)
    sh["s5_are"] = qb(inp["s5_a_re"][0].reshape(-1), 16)
    sh["s5_aim"] = qb(inp["s5_a_im"][0].reshape(-1), 16)
    sh["s5_ldt"] = qb(np.repeat(np.asarray(inp["s5_log_dt"][0]), 64), 16)
    bst = np.zeros((2, 16, 128, 128), np.float32)
    cstt = np.zeros((2, 16, 128, 128), np.float32)
    for ri, (bsrc, csrc) in enumerate(((inp["s5_b_re"][0], inp["s5_c_re"][0]), (inp["s5_b_im"][0], inp["s5_c_im"][0]))):
        bsrc = np.asarray(bsrc)
        csrc = np.asarray(csrc)
        for g in range(32):
            blk = g // 2
            m0 = (g % 2) * 64
            k0 = (g % 8) * 16
            bst[ri, blk, k0:k0 + 16, m0:m0 + 64] = bsrc[g].T
            cstt[ri, blk, m0:m0 + 64, k0:k0 + 16] = csrc[g].T
    sh["s5_bst"] = bst
    sh["s5_cst"] = cstt
    sh["s5_d"] = qb(inp["s5_d"][0], 4)
    sh["s5_wglu"] = f(inp["s5_w_glu"][0])
    sh["s5_bglu"] = qb(inp["s5_b_glu"][0], 4)
    sh["gdn_convw"] = np.ascontiguousarray(np.asarray(inp["gdn_conv_w"][0], dtype=np.float32).reshape(4, 12, 128).transpose(2, 1, 0))
    sh["gdn_alog"] = f(inp["gdn_a_log"][0].reshape(1, 4))
    sh["gdn_dtb"] = f(inp["gdn_dt_bias"][0].reshape(1, 4))
    sh["gdn_normw"] = f(inp["gdn_norm_w"][0].reshape(1, 128))
    sh["w_out_even"] = f(inp["w_out_even"][0])
    wio = np.asarray(inp["w_in_odd"][0], dtype=np.float32)
    perm = np.arange(ODD_IN)
    for qk in range(2):
        for h in range(4):
            base = qk * 1024 + h * 256
            perm[base:base + 128] = base + np.arange(0, 256, 2)
            perm[base + 128:base + 256] = base + np.arange(1, 256, 2)
    sh["w_in_odd"] = np.ascontiguousarray(wio[:, perm])
    sh["w_out_odd"] = f(inp["w_out_odd"][0])
    for k in ("ln1_g", "ln1_b", "ln2_g", "ln2_b", "router_w", "router_b", "moe_w1", "moe_w2", "moe_b2", "ple_w"):
        sh[k] = f(inp[k])
    sh["moe_b1"] = np.ascontiguousarray(np.asarray(inp["moe_b1"], dtype=np.float32).reshape(2, NE, 16, 128).transpose(0, 1, 3, 2))
    sh["ple_gw"] = f(inp["ple_gate_w"])
    for k, v in host_consts().items():
        if k in CONST_SHAPES:
            sh["c_" + k] = np.ascontiguousarray(v.astype(np.float32)).reshape(CONST_SHAPES[k])
    return sh


def core_inputs(inp, sh, prompt_ids, sample_id, L):
    m = dict(sh)
    xp = [np.asarray(inp["x_prompt"][i][:L], dtype=np.float32) for i in prompt_ids]
    m["xin"] = np.ascontiguousarray(np.concatenate(xp + [np.asarray(inp["x_sample"][sample_id], dtype=np.float32)], axis=0))
    pp = [np.asarray(inp["p_prompt"][:, i, :L], dtype=np.float32) for i in prompt_ids]
    m["pin"] = np.ascontiguousarray(np.concatenate(pp + [np.asarray(inp["p_sample"][:, sample_id], dtype=np.float32)], axis=1))
    m["st_s5re"] = np.ascontiguousarray(np.asarray(inp["state_s5_re"][0, sample_id], dtype=np.float32).reshape(16, 128).T)
    m["st_s5im"] = np.ascontiguousarray(np.asarray(inp["state_s5_im"][0, sample_id], dtype=np.float32).reshape(16, 128).T)
    m["st_gdn"] = np.ascontiguousarray(np.asarray(inp["state_gdn"][0, sample_id], dtype=np.float32))
    m["st_conv"] = np.ascontiguousarray(np.asarray(inp["state_gdn_conv"][0, sample_id], dtype=np.float32).reshape(3, 12, 128).transpose(2, 1, 0))
    m["st_ret"] = np.ascontiguousarray(np.asarray(inp["state_ret"][0, sample_id], dtype=np.float32))
    return m


def unperm(k, a):
    a = np.asarray(a)
    if k in ("o_s5re", "o_s5im"):
        return a.reshape(128, 16).T.reshape(32, 64)
    if k == "o_conv":
        return a.reshape(128, 12, 3).transpose(2, 1, 0).reshape(3, 1536)
    return a


_CACHE = {}


def kernel(**inputs):
    NPS, L, C = 2, 2048, 768
    key = (NPS, L, C)
    if key not in _CACHE:
        _CACHE[key] = build(NPS, L, C)
    nc = _CACHE[key]
    sh = prep_shared(inputs)
    in_maps = [core_inputs(inputs, sh, [2 * c, 2 * c + 1], c, L) for c in range(8)]
    res = run_bass_kernel_spmd(nc, in_maps, core_ids=list(range(8)))
    R = res.results
    B, DB = 16, 8
    y_p = np.zeros((B, L, D), np.float32)
    y_s = np.zeros((DB, 16, D), np.float32)
    outs = {k: (np.zeros((1, B) + shp, np.float32), np.zeros((1, DB) + shp, np.float32))
            for k, shp in (("o_s5re", (32, 64)), ("o_s5im", (32, 64)), ("o_gdn", (4, 128, 128)), ("o_conv", (3, 1536)), ("o_ret", (4, 256, 512)))}
    for c in range(8):
        r = R[c]
        y = r["y_out"]
        for j in range(NPS):
            y_p[2 * c + j] = y[j * L:(j + 1) * L]
        y_s[c] = y[NPS * L:NPS * L + 16]
        for k, (po, so) in outs.items():
            a = r[k]
            for j in range(NPS):
                po[0, 2 * c + j] = unperm(k, a[j]).reshape(po.shape[2:])
            so[0, c] = unperm(k, a[NPS]).reshape(so.shape[2:])
    return (y_p, y_s, outs["o_s5re"][0], outs["o_s5im"][0], outs["o_gdn"][0], outs["o_conv"][0], outs["o_ret"][0],
            outs["o_s5re"][1], outs["o_s5im"][1], outs["o_gdn"][1], outs["o_conv"][1], outs["o_ret"][1])
```

```python
from contextlib import ExitStack
import math
import os
CUT = int(os.environ.get('KCUT', '99'))
CUT2 = int(os.environ.get('KCUT2', '99'))
import numpy as np
import concourse.bass as bass
import concourse.mybir as mybir
from concourse.bass_utils import run_bass_kernel_spmd

F32 = mybir.dt.float32
BF16 = mybir.dt.bfloat16
I32 = mybir.dt.int32
U32 = mybir.dt.uint32
ALU = mybir.AluOpType
AF = mybir.ActivationFunctionType

ENGS = ("pe", "dve", "act", "pool", "sp")
SEM_CHUNK = 30000
SAME_ENG_DIST = int(os.environ.get('KSED', '1000000000'))

D = 1024
NE = 32
TOPK = 4
ALPHA = 4.0 ** 0.25
LN_EPS = 1e-5
NORM_EPS = 1e-6
EVEN_IN = 2568
ODD_IN = 6144


class Instr:
    __slots__ = ("eng", "fn", "waits", "is_dma", "sig", "key", "val", "idx", "clock", "sval")

    def __init__(self, eng, fn, is_dma):
        self.eng = eng
        self.fn = fn
        self.is_dma = is_dma
        self.waits = []
        self.sig = False
        self.key = None
        self.val = 0
        self.idx = 0
        self.clock = None
        self.sval = None


class Trk:
    def __init__(self, name=""):
        self.name = name
        self.ent = {}

    def _conf(self, k):
        if k == "*":
            return list(self.ent.values())
        out = []
        e = self.ent.get(k)
        if e is not None:
            out.append(e)
        e = self.ent.get("*")
        if e is not None:
            out.append(e)
        return out


class Buf:
    def __init__(self, h, name):
        self.h = h
        self.trk = Trk(name)

    def __getitem__(self, k):
        return self.h[k]


def alias(ap, parent, name="alias"):
    b = Buf(ap, name)
    b.trk = parent.trk
    return b


class Prog:
    def __init__(self, nc, sb_words):
        self.nc = nc
        self.q = {e: [] for e in ENGS}
        self.clock = {e: {} for e in ENGS}
        self.es = ExitStack()
        self.dma_sems = {}
        self.dma_rr = {e: 0 for e in ENGS}
        self.n_dma_sems = 8
        self.pending = {e: [] for e in ENGS}
        self.big = self.es.enter_context(nc.sbuf_tensor("big", [128, sb_words], F32))
        self.sb_words = sb_words
        self.top = 0
        self.psn = 0

    def sb(self, name, shape, dtype=F32):
        isz = 2 if dtype == BF16 else 4
        n = 1
        for s in shape[1:]:
            n *= s
        words = (n * isz + 3) // 4
        off = self.top
        self.top += words
        assert self.top <= self.sb_words, (name, self.top, self.sb_words)
        v = self.big[0:shape[0], off:off + words]
        if dtype != F32:
            v = v.bitcast(dtype)
        if dtype == BF16 and n % 2 == 1:
            v = v[:, 0:n]
        if len(shape) == 3:
            v = v.rearrange("p (a b) -> p a b", a=shape[1])
        elif len(shape) == 4:
            v = v.rearrange("p (a b c) -> p a b c", a=shape[1], b=shape[2])
        return Buf(v, name)

    def mark(self):
        return self.top

    def release(self, m):
        self.barrier()
        self.top = m

    def ps(self, name):
        t = self.es.enter_context(self.nc.psum_tensor(name, [128, 512], F32))
        b = Buf(t, name)
        b.trk.excl = True
        return b

    def barrier(self):
        lasts = []
        for e in ENGS:
            if self.q[e]:
                for ins in reversed(self.q[e]):
                    if not ins.is_dma:
                        lasts.append(ins)
                        break
        for q, lst in self.dma_sems.items():
            for s in lst:
                if s[2] is not None:
                    lasts.append(s[2])
        for e in ENGS:
            self.pending[e] = list(lasts)

    def _norm(self, lst):
        out = []
        for x in lst:
            if isinstance(x, tuple):
                t, k = x
            else:
                t, k = x, "*"
            trk = t if isinstance(t, Trk) else t.trk
            out.append((trk, k))
        return out

    def _add_dep(self, ins, prod):
        if prod is None or prod is ins:
            return
        eng = ins.eng
        if prod.eng == "pe" and eng == "pe" and not prod.is_dma:
            return
        if (not prod.is_dma) and (not ins.is_dma) and prod.eng == eng and eng in ("dve", "act") \
                and ins.idx - prod.idx >= SAME_ENG_DIST:
            return
        clk = self.clock[eng]
        if clk.get(prod.key, -1) >= prod.val:
            return
        prod.sig = True
        ins.waits.append(prod)
        new = dict(clk)
        new[prod.key] = prod.val
        if prod.clock:
            for k, v in prod.clock.items():
                if new.get(k, -1) < v:
                    new[k] = v
        self.clock[eng] = new

    def _record(self, ins, reads, writes):
        if self.pending[ins.eng]:
            for pr in self.pending[ins.eng]:
                self._add_dep(ins, pr)
            self.pending[ins.eng] = []
        reads = self._norm(reads)
        writes = self._norm(writes)
        excl = [x for x in reads if getattr(x[0], "excl", False)]
        if excl:
            reads = [x for x in reads if not getattr(x[0], "excl", False)]
            writes = writes + [x for x in excl if x not in writes]
        for trk, k in reads:
            for e in trk._conf(k):
                self._add_dep(ins, e[0])
        for trk, k in writes:
            for e in trk._conf(k):
                self._add_dep(ins, e[0])
                for r in e[1]:
                    self._add_dep(ins, r)
        for trk, k in reads:
            e = trk.ent.get(k)
            if e is None:
                e = trk.ent[k] = [None, []]
            e[1].append(ins)
        for trk, k in writes:
            if k == "*":
                trk.ent.clear()
            trk.ent[k] = [ins, []]
        ins.clock = self.clock[ins.eng]
        self.q[ins.eng].append(ins)

    def op(self, eng, fn, r=(), w=()):
        ins = Instr(eng, fn, False)
        ins.idx = len(self.q[eng])
        ins.key = eng
        ins.val = ins.idx
        self._record(ins, r, w)
        return ins

    def V(self, eng, meth, *args, r=(), w=(), **kw):
        return self.op(eng, lambda e: getattr(e, meth)(*args, **kw), r=r, w=w)

    def dma(self, eng, fn, r=(), w=()):
        ins = Instr(eng, fn, True)
        ins.idx = len(self.q[eng])
        sems = self.dma_sems.setdefault(eng, [])
        if len(sems) < self.n_dma_sems:
            s = [f"dq_{eng}_{len(sems)}", 0, None]
            sems.append(s)
        else:
            s = sems[self.dma_rr[eng] % self.n_dma_sems]
        self.dma_rr[eng] += 1
        if s[2] is not None:
            self._add_dep(ins, s[2])
        s[1] += 1
        s[2] = ins
        ins.key = s[0]
        ins.val = s[1]
        ins.sig = True
        self._record(ins, r, w)
        return ins

    def DM(self, eng, out, in_, r=(), w=(), **kw):
        return self.dma(eng, lambda e: e.dma_start(out=out, in_=in_, **kw), r=r, w=w)

    def finalize(self):
        nc = self.nc
        sem_names = set()
        for e in ENGS:
            cnt = 0
            for ins in self.q[e]:
                if ins.is_dma:
                    sem_names.add(ins.key)
                elif ins.sig:
                    ep, v = divmod(cnt, SEM_CHUNK)
                    ins.sval = (f"e_{e}_{ep}", v + 1)
                    sem_names.add(ins.sval[0])
                    cnt += 1
        sems = {}
        for n in sorted(sem_names):
            sems[n] = self.es.enter_context(nc.semaphore(n))

        def semval(prod):
            if prod.is_dma:
                return sems[prod.key], prod.val * 16
            return sems[prod.sval[0]], prod.sval[1]

        def run(e, eng_name):
            for ins in self.q[eng_name]:
                best = {}
                for pr in ins.waits:
                    s, v = semval(pr)
                    k = id(s)
                    if k not in best or best[k][1] < v:
                        best[k] = (s, v)
                ws = list(best.values())
                attach = None
                if ws and eng_name != "pe":
                    attach = ws.pop()
                for s, v in ws:
                    e.wait_ge(s, v)
                bi = ins.fn(e)
                if attach is not None:
                    bi._wait_ge(attach[0], attach[1])
                if ins.is_dma:
                    bi.then_inc(sems[ins.key], 16)
                elif ins.sig:
                    bi.then_inc(sems[ins.sval[0]], 1)

        block = self.es.enter_context(nc.Block())

        @block.tensor
        def _(e):
            run(e, "pe")

        @block.vector
        def _(e):
            run(e, "dve")

        @block.scalar
        def _(e):
            run(e, "act")

        @block.gpsimd
        def _(e):
            run(e, "pool")

        @block.sync
        def _(e):
            run(e, "sp")
            for q, lst in self.dma_sems.items():
                for s in lst:
                    if s[1] > 0:
                        e.wait_ge(sems[s[0]], s[1] * 16)

        self.es.close()
        return nc


def host_consts():
    c = {}
    c["identf"] = np.eye(128, dtype=np.float32)
    jj = np.arange(128)[:, None]
    ii = np.arange(128)[None, :]
    c["uincl"] = (jj <= ii).astype(np.float32)
    c["ustrict"] = (jj < ii).astype(np.float32)
    c["ones"] = np.ones((128, 128), np.float32)
    c["lmi"] = (ii <= jj).astype(np.float32)
    c["lms"] = (ii < jj).astype(np.float32)
    c["iota_e"] = np.tile(np.arange(32, dtype=np.float32)[None, :], (128, 1))
    c["tau"] = np.tile(np.arange(1, 129, dtype=np.float32)[None, :], (128, 1))
    c["pidx"] = np.arange(128, dtype=np.float32)[:, None].copy()
    log_g = np.log(1.0 - 2.0 ** (-5.0 - np.arange(4, dtype=np.float32))).astype(np.float32)
    M = np.zeros((4, 128, 128), np.float32)
    for h in range(4):
        m = np.exp(log_g[h] * np.abs(ii - jj).astype(np.float32))
        m = np.where((jj >= 64) & (ii < 64), 0.0, m)
        M[h] = m
    c["retmask"] = np.ascontiguousarray(M.transpose(1, 0, 2)).astype(np.float32)
    qs = np.zeros((128, 4, 128), np.float32)
    for h in range(4):
        qs[:, h, :] = np.exp(log_g[h] * (np.arange(128, dtype=np.float32) + 1.0))[None, :]
    c["retqs"] = qs
    ks = np.zeros((128, 8), np.float32)
    for h in range(4):
        ks[:, h] = np.exp(log_g[h] * (127.0 - np.arange(128, dtype=np.float32)))
        ks[:, 4 + h] = np.exp(log_g[h] * (15.0 - np.arange(128, dtype=np.float32)))
    c["retks"] = ks * np.float32(256 ** -0.5)
    c["retcdec"] = [[float(np.exp(log_g[h] * 128.0)) for h in range(4)],
                    [float(np.exp(log_g[h] * 16.0)) for h in range(4)]]
    freq = (1.0 / (10000.0 ** np.linspace(0.0, 1.0, 128, dtype=np.float32))).astype(np.float32)
    pos = np.arange(2048, dtype=np.float32)
    ang = (pos[None, :] * freq[:, None]).astype(np.float32)
    c["rcos"] = np.cos(ang).astype(np.float32)
    c["rsin"] = np.sin(ang).astype(np.float32)
    return c


LAST_DIN = {}
CONST_SHAPES = {"lmi": [128, 128], "lms": [128, 128], "identf": [128, 128], "uincl": [128, 128], "ustrict": [128, 128], "ones": [128, 128],
                "iota_e": [128, 32], "tau": [128, 128], "pidx": [128, 1], "retmask": [128, 4, 128],
                "retqs": [128, 4, 128], "retks": [128, 8], "rcos": [128, 2048], "rsin": [128, 2048]}


def build(NPS, L, C, stages=("A0", "M0", "C0", "A1", "M1", "C1"), dbg=()):
    nc = bass.Bass("TRN2", target_bir_lowering=False)
    NSEQ = NPS + 1
    NTOK = NPS * L + 16
    TPS = L // 128
    tiles = []
    for s in range(NPS):
        for t in range(TPS):
            tiles.append(dict(row0=s * L + t * 128, nt=128, seq=s, first=(t == 0), last=(t == TPS - 1),
                              pos0=t * 128, ti=len(tiles)))
    tiles.append(dict(row0=NPS * L, nt=16, seq=NPS, first=True, last=True, pos0=1024, ti=len(tiles)))
    NT = len(tiles)
    NROWP = NT * 128
    CT = C // 128
    TRASH = NE * C
    cgs = []
    c0 = 0
    while c0 < C:
        cgs.append((c0, min(C, c0 + 512)))
        c0 += 512

    def din(name, shape, dt=F32):
        LAST_DIN[name] = list(shape)
        return nc.dram_tensor(name, list(shape), dt, kind="ExternalInput").ap()

    def dout(name, shape, dt=F32):
        return nc.dram_tensor(name, list(shape), dt, kind="ExternalOutput").ap()

    def dint(name, shape, dt=F32):
        return nc.dram_tensor(name, list(shape), dt, kind="Internal").ap()

    xin = din("xin", [NTOK, D])
    pin = din("pin", [2, NTOK, 256])
    st_s5re = din("st_s5re", [128, 16])
    st_s5im = din("st_s5im", [128, 16])
    st_gdn = din("st_gdn", [4, 128, 128])
    st_conv = din("st_conv", [128, 12, 3])
    st_ret = din("st_ret", [4, 256, 512])
    w_in_even = din("w_in_even", [D, EVEN_IN])
    s5_are = din("s5_are", [128, 16])
    s5_aim = din("s5_aim", [128, 16])
    s5_ldt = din("s5_ldt", [128, 16])
    s5_bst = din("s5_bst", [2, 16, 128, 128])
    s5_cst = din("s5_cst", [2, 16, 128, 128])
    s5_d = din("s5_d", [128, 4])
    s5_wglu = din("s5_wglu", [512, 512])
    s5_bglu = din("s5_bglu", [128, 4])
    gdn_convw = din("gdn_convw", [128, 12, 4])
    gdn_alog = din("gdn_alog", [1, 4])
    gdn_dtb = din("gdn_dtb", [1, 4])
    gdn_normw = din("gdn_normw", [1, 128])
    w_out_even = din("w_out_even", [D, D])
    w_in_odd = din("w_in_odd", [D, ODD_IN])
    w_out_odd = din("w_out_odd", [2048, D])
    ln1_g = din("ln1_g", [2, D])
    ln1_b = din("ln1_b", [2, D])
    ln2_g = din("ln2_g", [2, D])
    ln2_b = din("ln2_b", [2, D])
    router_w = din("router_w", [2, D, NE])
    router_b = din("router_b", [2, NE])
    moe_w1 = din("moe_w1", [2, NE, D, 2 * D])
    moe_b1 = din("moe_b1", [2, NE, 128, 16])
    moe_w2 = din("moe_w2", [2, NE, D, D])
    moe_b2 = din("moe_b2", [2, NE, D])
    ple_w = din("ple_w", [2, 256, D])
    ple_gw = din("ple_gw", [2, D, D])
    cst = {k: din("c_" + k, v) for k, v in CONST_SHAPES.items()}

    y_out = dout("y_out", [NTOK, D])
    o_s5re = dout("o_s5re", [NSEQ, 128, 16])
    o_s5im = dout("o_s5im", [NSEQ, 128, 16])
    o_gdn = dout("o_gdn", [NSEQ, 4, 128, 128])
    o_conv = dout("o_conv", [NSEQ, 128, 12, 3])
    o_ret = dout("o_ret", [NSEQ, 4, 256, 512])
    dbg_out = {k: dout("dbg_" + k, [NTOK, D]) for k in dbg if k.startswith("x")}
    taps = {}

    def tap(name, ap, ti, npart, width, dt=F32):
        if ("t_" + name) not in dbg:
            return
        if name not in taps:
            taps[name] = dout("tap_" + name, [NT, 128, width], dt)
        p.DM("sp", taps[name][ti, 0:npart, :], ap, r=[tapsrc[0]], w=[T_out])

    tapsrc = [None]

    x1s = dint("x1s", [NROWP, D])
    x3s = dint("x3s", [NROWP, D])
    yas = dint("yas", [NT, 128, 4, 128], BF16)
    T_yas = Trk("yas")
    xs = dint("xs", [NE * C + 128, D], BF16)
    ys = dint("ys", [NE * C + 128, D])

    p = Prog(nc, 52900)
    DR = Trk("dram_in")
    T_x1s, T_x3s, T_xs, T_ys, T_out = Trk("x1s"), Trk("x3s"), Trk("xs"), Trk("ys"), Trk("out")
    PS = [p.ps(f"ps{i}") for i in range(8)]

    def psbf(b, n):
        return PS[b][:, 0:n // 2].bitcast(BF16)

    identf = p.sb("identf", [128, 128])
    identb = p.sb("identb", [128, 128], BF16)
    uincl = p.sb("uincl", [128, 128])
    ustr_b = p.sb("ustr_b", [128, 128], BF16)
    ones_f = p.sb("ones_f", [128, 128])
    ones_b = p.sb("ones_b", [128, 128], BF16)
    iota_e = p.sb("iota_e", [128, 32])
    pidx = p.sb("pidx", [128, 1])
    gates_all = p.sb("gates_all", [128, NT, 4])
    slots_all = p.sb("slots_all", [128, NT, 4], I32)
    tmpc = p.sb("tmpc", [128, 128])
    lmi = p.sb("lmi", [128, 128]); lms = p.sb("lms", [128, 128])
    p.DM("sp", lmi[:], cst["lmi"], r=[DR], w=[lmi])
    p.DM("sp", lms[:], cst["lms"], r=[DR], w=[lms])
    for nm, b in (("identf", identf), ("uincl", uincl), ("ones", ones_f), ("iota_e", iota_e), ("pidx", pidx)):
        p.DM("sp", b[:], cst[nm], r=[DR], w=[b])
    p.DM("sp", tmpc[:], cst["ustrict"], r=[DR], w=[tmpc])
    p.V("dve", "tensor_copy", ustr_b[:], tmpc[:], r=[tmpc], w=[ustr_b])
    p.V("dve", "tensor_copy", identb[:], identf[:], r=[identf], w=[identb])
    p.V("dve", "tensor_copy", ones_b[:], ones_f[:], r=[ones_f], w=[ones_b])

    rr = {"ev": 0}

    def evac(out_ap, in_ap, r, w):
        rr["ev"] += 1
        if rr["ev"] % 2:
            p.V("act", "activation", out_ap, in_ap, AF.Copy, r=r, w=w)
        else:
            p.V("dve", "tensor_copy", out_ap, in_ap, r=r, w=w)

    def bcast_load(buf, src_row):
        p.DM("sp", buf[:], src_row.partition_broadcast(128), r=[DR], w=[buf])

    def load_w_bf16(buf, src, kc):
        n = src.shape[1]
        nch = (n + 2047) // 2048
        step = (n + nch - 1) // nch
        v = src.rearrange("(kc q) n -> q kc n", q=128)
        for c0 in range(0, n, step):
            c1 = min(n, c0 + step)
            p.DM("pool", buf[:, :, c0:c1], v[:, :, c0:c1], r=[DR], w=[(buf, c0)] if nch > 1 else [buf])

    def layernorm(eng_h, h, nt, gt, bt, out, scr6, scr2):
        p.V("dve", "bn_stats", scr6[0:nt, 0, :], h[0:nt, 0:512], r=[h], w=[(scr6, 0)])
        p.V("dve", "bn_stats", scr6[0:nt, 1, :], h[0:nt, 512:1024], r=[h], w=[(scr6, 1)])
        p.V("dve", "bn_aggr", scr2[0:nt, 0:2], scr6[0:nt, :, :].rearrange("p a b -> p (a b)"), r=[scr6], w=[scr2])
        p.V("act", "activation", scr2[0:nt, 2:3], scr2[0:nt, 1:2], AF.Sqrt, bias=epsln[0:nt, 0:1], r=[scr2, epsln], w=[(scr2, "s")])
        p.V("dve", "reciprocal", scr2[0:nt, 3:4], scr2[0:nt, 2:3], r=[(scr2, "s")], w=[(scr2, "r")])
        p.V("dve", "tensor_scalar", out[0:nt, :], h[0:nt, :], scr2[0:nt, 0:1], scr2[0:nt, 3:4], ALU.subtract, ALU.mult,
            r=[h, scr2, (scr2, "r")], w=[out])
        p.V("pool", "tensor_tensor", out[0:nt, :], out[0:nt, :], gt[0:nt, :], ALU.mult, r=[out, gt], w=[out])
        p.V("pool", "tensor_tensor", out[0:nt, :], out[0:nt, :], bt[0:nt, :], ALU.add, r=[out, bt], w=[out])

    epsln = p.sb("epsln", [128, 2])
    p.V("dve", "memset", epsln[:, 0:1], LN_EPS, w=[(epsln, 0)])
    p.V("dve", "memset", epsln[:, 1:2], NORM_EPS, w=[(epsln, 1)])

    def transpose_to(dstT, src_bf, nt, nblk, bank):
        for b0 in range(0, nblk, 8):
            nb = min(8, nblk - b0)
            for b in range(nb):
                p.V("pe", "transpose", psbf(bank, 1024)[:, b * 128:b * 128 + nt], src_bf[0:nt, (b0 + b) * 128:(b0 + b + 1) * 128],
                    identb[0:nt, 0:nt], r=[src_bf, identb], w=[PS[bank]])
            evac(dstT[:, b0:b0 + nb, 0:nt], psbf(bank, 1024).rearrange("p (a b) -> p a b", a=8)[:, 0:nb, 0:nt],
                 r=[PS[bank]], w=[dstT])

    def phaseA_tail(li, tl, xt, mixps, lnw, rw, rb, work):
        nt, ti = tl["nt"], tl["ti"]
        h, x1, xrow, x1T, lg, scr6, scr2, small, Mb, tot = work
        p.V("dve", "scalar_tensor_tensor", h[0:nt, 0:512], xt[0:nt, 0:512], ALPHA, PS[mixps[0]][0:nt, :], ALU.mult, ALU.add,
            r=[xt, PS[mixps[0]]], w=[(h, 0)])
        p.V("dve", "scalar_tensor_tensor", h[0:nt, 512:1024], xt[0:nt, 512:1024], ALPHA, PS[mixps[1]][0:nt, :], ALU.mult, ALU.add,
            r=[xt, PS[mixps[1]]], w=[(h, 1)])
        layernorm("dve", h, nt, lnw[0], lnw[1], x1, scr6, scr2)
        p.DM("sp", x1s[ti * 128:ti * 128 + nt, :], x1[0:nt, :], r=[x1], w=[(T_x1s, ti)])
        if ("x1_%d" % li) in dbg_out:
            p.DM("sp", dbg_out["x1_%d" % li][tl["row0"]:tl["row0"] + nt, :], x1[0:nt, :], r=[x1], w=[T_out])
        p.V("act", "activation", xrow[0:nt, :], x1[0:nt, :], AF.Copy, r=[x1], w=[xrow])
        for half in range(2):
            for b in range(4):
                kc = half * 4 + b
                p.V("pe", "transpose", PS[6][:, b * 128:b * 128 + nt], x1[0:nt, kc * 128:(kc + 1) * 128], identf[0:nt, 0:nt],
                    r=[x1, identf], w=[PS[6]])
            evac(x1T[:, half * 4:half * 4 + 4, 0:nt], PS[6][:, :].rearrange("p (a b) -> p a b", a=4)[:, :, 0:nt], r=[PS[6]], w=[x1T])
        for kc in range(8):
            p.V("pe", "matmul", PS[7][0:nt, 0:32], x1T[:, kc, 0:nt], rw[:, kc, :], start=(kc == 0), stop=(kc == 7),
                r=[x1T, rw], w=[PS[7]])
        p.V("dve", "tensor_tensor", lg[0:nt, :], PS[7][0:nt, 0:32], rb[0:nt, :], ALU.add, r=[PS[7], rb], w=[lg])
        top, ti8, nt0, ex, gs, ef, rk, sl, ov, tmp32, rnk = small
        p.V("dve", "max", top[0:nt, :], lg[0:nt, :], r=[lg], w=[top])
        p.V("dve", "max_index", ti8[0:nt, :], top[0:nt, :], lg[0:nt, :], r=[lg, top], w=[ti8])
        p.V("dve", "tensor_scalar", nt0[0:nt, :], top[0:nt, 0:1], -1.0, None, ALU.mult, r=[top], w=[nt0])
        p.V("act", "activation", ex[0:nt, :], top[0:nt, 0:4], AF.Exp, bias=nt0[0:nt, 0:1], r=[top, nt0], w=[ex])
        p.V("dve", "reduce_sum", gs[0:nt, 0:1], ex[0:nt, :], mybir.AxisListType.X, r=[ex], w=[gs])
        p.V("dve", "reciprocal", gs[0:nt, 1:2], gs[0:nt, 0:1], r=[gs], w=[(gs, "r")])
        p.V("dve", "tensor_scalar", gates_all[0:nt, ti, :], ex[0:nt, :], gs[0:nt, 1:2], None, ALU.mult, r=[ex, (gs, "r")], w=[(gates_all, ti)])
        p.V("pool", "memset", Mb[:], 0.0, w=[Mb])
        p.V("dve", "tensor_scalar", Mb[0:nt, :], lg[0:nt, :], top[0:nt, 3:4], None, ALU.is_ge, r=[lg, top], w=[Mb])
        p.V("pe", "matmul", PS[7][:, 64:96], ustr_b[:, :], Mb[:, :], start=True, stop=True, r=[ustr_b, Mb], w=[PS[7]])
        p.V("pe", "matmul", PS[7][:, 96:128], ones_b[:, :], Mb[:, :], start=True, stop=True, r=[ones_b, Mb], w=[PS[7]])
        p.V("dve", "tensor_tensor", rnk[:, :], PS[7][:, 64:96], tot[:, :], ALU.add, r=[PS[7], tot], w=[rnk])
        p.V("dve", "tensor_tensor", tot[:, :], PS[7][:, 96:128], tot[:, :], ALU.add, r=[PS[7], tot], w=[tot])
        p.V("dve", "tensor_copy", ef[0:nt, :], ti8[0:nt, 0:4], r=[ti8], w=[ef])
        for k in range(4):
            p.V("dve", "scalar_tensor_tensor", tmp32[0:nt, :], iota_e[0:nt, :], ef[0:nt, k:k + 1], rnk[0:nt, :], ALU.is_equal, ALU.mult,
                accum_out=rk[0:nt, k:k + 1], r=[iota_e, ef, rnk], w=[tmp32, (rk, k)])
        p.V("dve", "scalar_tensor_tensor", sl[0:nt, :], ef[0:nt, :], float(C), rk[0:nt, :], ALU.mult, ALU.add, r=[ef, rk], w=[sl])
        p.V("dve", "tensor_scalar", ov[0:nt, :], rk[0:nt, :], float(C), None, ALU.is_ge, r=[rk], w=[ov])
        p.V("dve", "tensor_scalar", tmp32[0:nt, 0:4], sl[0:nt, :], -1.0, pidx[0:nt, 0:1], ALU.mult, ALU.add, r=[sl, pidx], w=[tmp32])
        p.V("dve", "tensor_scalar", tmp32[0:nt, 0:4], tmp32[0:nt, 0:4], float(TRASH), None, ALU.add, r=[tmp32], w=[tmp32])
        p.V("dve", "tensor_tensor", tmp32[0:nt, 0:4], tmp32[0:nt, 0:4], ov[0:nt, :], ALU.mult, r=[tmp32, ov], w=[tmp32])
        p.V("dve", "tensor_tensor", sl[0:nt, :], sl[0:nt, :], tmp32[0:nt, 0:4], ALU.add, r=[sl, tmp32], w=[sl])
        if nt < 128:
            p.V("dve", "tensor_scalar", tmp32[:, 0:4], pidx[:, 0:1].to_broadcast([128, 4]), float(TRASH), None, ALU.add, r=[pidx], w=[tmp32])
            p.V("dve", "tensor_copy", slots_all[:, ti, :], tmp32[:, 0:4], r=[tmp32], w=[(slots_all, ti)])
        p.V("dve", "tensor_copy", slots_all[0:nt, ti, :], sl[0:nt, :], r=[sl], w=[(slots_all, ti)])
        for k in range(4):
            p.dma("pool", lambda e, k=k, ti=ti: e.indirect_dma_start(
                out=xs[:, :], out_offset=bass.IndirectOffsetOnAxis(ap=slots_all[:, ti, k:k + 1], axis=0),
                in_=xrow[:, :], in_offset=None), r=[xrow, (slots_all, ti), (T_xs, "*")], w=[])

    def alloc_tail_work(h=None, x1=None, x1T=None):
        if h is None:
            h = p.sb("h", [128, D])
        if x1 is None:
            x1 = p.sb("x1", [128, D])
        xrow = p.sb("xrow", [128, D], BF16)
        if x1T is None:
            x1T = p.sb("x1T", [128, 8, 128])
        lg = p.sb("lg", [128, 32])
        scr6 = p.sb("scr6", [128, 2, 6])
        scr2 = p.sb("scr2", [128, 4])
        small = (p.sb("top", [128, 8]), p.sb("ti8", [128, 8], U32), p.sb("nt0", [128, 1]), p.sb("ex", [128, 4]),
                 p.sb("gs", [128, 2]), p.sb("ef", [128, 4]), p.sb("rk", [128, 4]), p.sb("sl", [128, 4]),
                 p.sb("ov", [128, 4]), p.sb("tmp32", [128, 32]), p.sb("rnk", [128, 32]))
        Mb = p.sb("Mb", [128, 32], BF16)
        tot = p.sb("tot", [128, 32])
        p.V("dve", "memset", tot[:], 0.0, w=[tot])
        p.V("pool", "memset", xrow[:, :], 0.0, w=[xrow])
        return (h, x1, xrow, x1T, lg, scr6, scr2, small, Mb, tot)

    def load_ln_router(li):
        g1 = p.sb("ln1g", [128, D]); b1 = p.sb("ln1b", [128, D])
        bcast_load(g1, ln1_g[li:li + 1, :]); bcast_load(b1, ln1_b[li:li + 1, :])
        rw = p.sb("rw", [128, 8, NE])
        p.DM("sp", rw[:], router_w[li].rearrange("(kc q) n -> q kc n", q=128), r=[DR], w=[rw])
        rb = p.sb("rb", [128, NE])
        bcast_load(rb, router_b[li:li + 1, :])
        return (g1, b1), rw, rb

    def phaseA0():
        m0 = p.mark()
        win = p.sb("win", [128, 8, EVEN_IN], BF16)
        load_w_bf16(win, w_in_even, 8)
        wout = p.sb("wout", [128, 8, D], BF16)
        load_w_bf16(wout, w_out_even, 8)
        wglu = p.sb("wglu", [128, 4, 512], BF16)
        load_w_bf16(wglu, s5_wglu, 4)
        bst = p.sb("bst", [128, 2, 16, 128], BF16)
        cstt = p.sb("cstt", [128, 2, 16, 128], BF16)
        for ri in range(2):
            p.DM("pool", bst[:, ri, :, :], s5_bst[ri].rearrange("b k m -> k b m"), r=[DR], w=[(bst, ri)])
            p.DM("pool", cstt[:, ri, :, :], s5_cst[ri].rearrange("b k m -> k b m"), r=[DR], w=[(cstt, ri)])
        lnw, rw, rb = load_ln_router(0)
        are = p.sb("are", [128, 16]); aim = p.sb("aim", [128, 16]); ldt = p.sb("ldt", [128, 16])
        for b, s in ((are, s5_are), (aim, s5_aim), (ldt, s5_ldt)):
            p.DM("sp", b[:], s, r=[DR], w=[b])
        dsk = p.sb("dsk", [128, 4]); bgl = p.sb("bgl", [128, 4])
        p.DM("sp", dsk[:], s5_d, r=[DR], w=[dsk])
        p.DM("sp", bgl[:], s5_bglu, r=[DR], w=[bgl])
        tau = p.sb("tau", [128, 128])
        p.DM("sp", tau[:], cst["tau"], r=[DR], w=[tau])
        lam = p.sb("lam", [128, 16]); li_ = p.sb("li", [128, 16]); dtt = p.sb("dtt", [128, 16])
        p.V("act", "activation", dtt[:], ldt[:], AF.Exp, r=[ldt], w=[dtt])
        p.V("dve", "tensor_tensor", li_[:], aim[:], dtt[:], ALU.mult, r=[aim, dtt], w=[li_])
        p.V("dve", "tensor_tensor", lam[:], are[:], dtt[:], ALU.mult, r=[are, dtt], w=[lam])
        p.V("act", "activation", lam[:], lam[:], AF.Exp, r=[lam], w=[lam])
        cosT = p.sb("cosT", [128, 16, 128]); sinT = p.sb("sinT", [128, 16, 128])
        crT = p.sb("crT", [128, 16, 128]); ciT = p.sb("ciT", [128, 16, 128])
        ang = crT
        kq = ciT
        gsc = p.sb("gsc", [128, 2, 16, 128])
        ki = Buf(gsc[:, 0, :, :].bitcast(I32), "ki")
        ki.trk = gsc.trk
        TWO_PI = 2.0 * math.pi

        def sin_of(dst, shift):
            p.V("dve", "tensor_tensor", ang[:], li_[:, :].unsqueeze(2).to_broadcast([128, 16, 128]),
                tau[:, :].unsqueeze(1).to_broadcast([128, 16, 128]), ALU.mult, r=[li_, tau], w=[ang])
            if shift != 0.0:
                p.V("dve", "tensor_scalar", ang[:], ang[:], shift, None, ALU.add, r=[ang], w=[ang])
            p.V("dve", "tensor_scalar", kq[:], ang[:], 1.0 / TWO_PI, None, ALU.mult, r=[ang], w=[kq])
            p.V("dve", "tensor_copy", ki[:], kq[:], r=[kq], w=[ki])
            p.V("dve", "tensor_copy", kq[:], ki[:], r=[ki], w=[kq])
            p.V("dve", "scalar_tensor_tensor", ang[:], kq[:], -TWO_PI, ang[:], ALU.mult, ALU.add, r=[kq, ang], w=[ang])
            p.V("dve", "tensor_scalar", kq[:], ang[:], math.pi, TWO_PI, ALU.is_gt, ALU.mult, r=[ang], w=[kq])
            p.V("dve", "tensor_tensor", ang[:], ang[:], kq[:], ALU.subtract, r=[ang, kq], w=[ang])
            p.V("dve", "tensor_scalar", kq[:], ang[:], -math.pi, TWO_PI, ALU.is_lt, ALU.mult, r=[ang], w=[kq])
            p.V("dve", "tensor_tensor", ang[:], ang[:], kq[:], ALU.add, r=[ang, kq], w=[ang])
            p.V("dve", "tensor_scalar", ang[:], ang[:], math.pi, -math.pi, ALU.min, ALU.max, r=[ang], w=[ang])
            p.V("act", "activation", dst[:], ang[:], AF.Sin, r=[ang], w=[dst])

        sin_of(sinT, 0.0)
        sin_of(cosT, math.pi / 2)
        sm = p.sb("s5sm", [128, 8, 16])
        abre, abim, den, t1, t2, cfre, cfim, t3 = [sm[:, i, :] for i in range(8)]
        S = [sm]
        p.V("dve", "tensor_tensor", abre, lam[:], cosT[:, :, 0], ALU.mult, r=[lam, cosT], w=S)
        p.V("dve", "tensor_tensor", abim, lam[:], sinT[:, :, 0], ALU.mult, r=[lam, sinT], w=S)
        p.V("dve", "tensor_scalar", abre, abre, -1.0, None, ALU.add, r=S, w=S)
        p.V("dve", "tensor_tensor", t1, are[:], are[:], ALU.mult, r=[are], w=S)
        p.V("dve", "tensor_tensor", t2, aim[:], aim[:], ALU.mult, r=[aim], w=S)
        p.V("dve", "tensor_tensor", den, t1, t2, ALU.add, r=S, w=S)
        p.V("dve", "reciprocal", den, den, r=S, w=S)
        p.V("dve", "tensor_tensor", t1, abre, are[:], ALU.mult, r=S + [are], w=S)
        p.V("dve", "tensor_tensor", t2, abim, aim[:], ALU.mult, r=S + [aim], w=S)
        p.V("dve", "tensor_tensor", cfre, t1, t2, ALU.add, r=S, w=S)
        p.V("dve", "tensor_tensor", cfre, cfre, den, ALU.mult, r=S, w=S)
        p.V("dve", "tensor_tensor", t1, abim, are[:], ALU.mult, r=S + [are], w=S)
        p.V("dve", "tensor_tensor", t2, abre, aim[:], ALU.mult, r=S + [aim], w=S)
        p.V("dve", "tensor_tensor", cfim, t1, t2, ALU.subtract, r=S, w=S)
        p.V("dve", "tensor_tensor", cfim, cfim, den, ALU.mult, r=S, w=S)
        sc3 = Buf(gsc[:, 1, :, :], "sc3")
        sc3.trk = gsc.trk
        bc = lambda a: a.unsqueeze(2).to_broadcast([128, 16, 128])
        p.V("dve", "tensor_tensor", crT[:], cosT[:], bc(cfre), ALU.mult, r=[cosT] + S, w=[crT])
        p.V("dve", "tensor_tensor", sc3[:], sinT[:], bc(cfim), ALU.mult, r=[sinT] + S, w=[sc3])
        p.V("dve", "tensor_tensor", crT[:], crT[:], sc3[:], ALU.add, r=[crT, sc3], w=[crT])
        p.V("dve", "tensor_tensor", ciT[:], cosT[:], bc(cfim), ALU.mult, r=[cosT] + S, w=[ciT])
        p.V("dve", "tensor_tensor", sc3[:], sinT[:], bc(cfre), ALU.mult, r=[sinT] + S, w=[sc3])
        p.V("dve", "tensor_tensor", ciT[:], ciT[:], sc3[:], ALU.subtract, r=[ciT, sc3], w=[ciT])
        wc = p.sb("wc", [128, 12, 4])
        p.DM("sp", wc[:], gdn_convw, r=[DR], w=[wc])
        alog = p.sb("alog", [128, 4]); dtb = p.sb("dtb", [128, 4]); nrmw = p.sb("nrmw", [128, 128])
        bcast_load(alog, gdn_alog); bcast_load(dtb, gdn_dtb); bcast_load(nrmw, gdn_normw)
        p.V("act", "activation", alog[:], alog[:], AF.Exp, r=[alog], w=[alog])
        Hre = p.sb("Hre", [128, 16]); Him = p.sb("Him", [128, 16])
        Sg = p.sb("Sg", [128, 4, 128])
        ctx3 = p.sb("ctx3", [128, 12, 3])
        xt_one = p.sb("xt0", [128, D])
        xts = [xt_one, xt_one]
        xb = p.sb("xb", [128, D], BF16)
        xT = p.sb("xT", [128, 8, 128], BF16)
        uTf = p.sb("uTf", [128, 4, 128]); uTb = p.sb("uTb", [128, 4, 128], BF16)
        cb = p.sb("cb", [128, 12, 131])
        cacc = p.sb("cacc", [128, 12, 128])
        g8 = p.sb("g8", [128, 16, 128])
        ctmp = Buf(g8[:, 0:12, :], "ctmp"); ctmp.trk = g8.trk
        ztok = p.sb("ztok", [128, 8])
        rbuf = p.sb("rbuf", [128, 2, 8, 128]); rtmp = p.sb("rtmp", [128, 2, 8, 128])
        hbf = p.sb("hbf", [128, 2, 16, 128], BF16)
        hl = p.sb("hl", [128, 4, 16])
        yA = Buf(rtmp[:, 0, 0:4, :], "yA"); yA.trk = rtmp.trk
        ysq = Buf(rtmp[:, 0, 4:8, :], "ysq"); ysq.trk = rtmp.trk
        gaf = Buf(rtmp[:, 1, 0:4, :], "gaf"); gaf.trk = rtmp.trk
        gab = p.sb("gab", [128, 4, 128], BF16)
        mixT = p.sb("mixT", [128, 8, 128], BF16)
        qkn = Buf(g8[:, 8:16, :], "qkn"); qkn.trk = g8.trk
        sq8 = Buf(g8[:, 0:8, :], "sq8"); sq8.trk = g8.trk
        kvtok = alias(rbuf[:, 0, :, :], rbuf, "kvtok")
        gd = p.sb("gd", [128, 16])
        gd2 = p.sb("gd2", [128, 16])
        _gt = [alias(gsc[:, 0, i, :], gsc, "gt%d" % i) for i in range(16)]
        gbc, dec, erow, attn, attnT, rv, rk_, nwT, ub, qdT, kd, Yt = _gt[0:12]
        Mm = [_gt[12], _gt[13]]
        MT = [_gt[14], _gt[15]]
        yB = p.sb("yB", [128, 512], BF16); osb = alias(gsc[:, 1, 0, :], gsc, "osb"); ssq = p.sb("ssq", [128, 4])
        zs = p.sb("zs", [128, 512], BF16)
        x1a = alias(g8[:, 0:8, :].rearrange("p a b -> p (a b)"), g8, "x1a")
        x1Ta = alias(cacc[:, 0:8, :], cacc, "x1Ta")
        work = alloc_tail_work(h=xt_one, x1=x1a, x1T=x1Ta)
        print("A0 sbuf words", p.top)

        def load_x(tl, buf):
            if tl["nt"] < 128:
                p.V("pool", "memset", buf[:], 0.0, w=[buf])
            p.DM("sp", buf[0:tl["nt"], :], xin[tl["row0"]:tl["row0"] + tl["nt"], :], r=[DR], w=[buf])

        for tl in tiles:
            nt, ti, sq = tl["nt"], tl["ti"], tl["seq"]
            xt = xts[ti % 2]
            load_x(tl, xt)
            if tl["first"]:
                if sq < NPS:
                    p.V("pool", "memset", Hre[:], 0.0, w=[Hre]); p.V("pool", "memset", Him[:], 0.0, w=[Him])
                    p.V("pool", "memset", Sg[:], 0.0, w=[Sg]); p.V("pool", "memset", ctx3[:], 0.0, w=[ctx3])
                else:
                    p.DM("sp", Hre[:], st_s5re, r=[DR], w=[Hre])
                    p.DM("sp", Him[:], st_s5im, r=[DR], w=[Him])
                    p.DM("sp", Sg[:], st_gdn.rearrange("h k v -> k h v"), r=[DR], w=[Sg])
                    p.DM("sp", ctx3[:], st_conv, r=[DR], w=[ctx3])
            if CUT <= 1:
                continue
            p.V("act", "activation", xb[0:nt, :], xt[0:nt, :], AF.Copy, r=[xt], w=[xb])
            transpose_to(xT, xb, nt, 8, 0)
            tapsrc[0] = xt; tap("xt", xt[0:nt, :], ti, nt, D)
            tapsrc[0] = xb; tap("xb", xb[0:nt, :], ti, nt, D, BF16)
            tapsrc[0] = xT; tap("xT", xT[:, :, :].rearrange("p a b -> p (a b)"), ti, 128, 1024, BF16)
            for ob in range(4):
                for kc in range(8):
                    p.V("pe", "matmul", PS[1][:, ob * 128:ob * 128 + nt], win[:, kc, ob * 128:(ob + 1) * 128], xT[:, kc, 0:nt],
                        start=(kc == 0), stop=(kc == 7), r=[win, xT], w=[PS[1]])
            ps1v = PS[1][:, :].rearrange("p (a b) -> p a b", a=4)[:, :, 0:nt]
            p.V("act", "activation", uTf[:, :, 0:nt], ps1v, AF.Copy, r=[PS[1]], w=[uTf])
            p.V("dve", "tensor_copy", uTb[:, :, 0:nt], ps1v, r=[PS[1]], w=[uTb])
            p.V("pool", "tensor_copy", cb[:, :, 0:3], ctx3[:, :, :], r=[ctx3], w=[(cb, "c")])
            for g4 in range(3):
                bank = 2 + (g4 % 2)
                for b in range(4):
                    blk = g4 * 4 + b
                    for kc in range(8):
                        p.V("pe", "matmul", PS[bank][:, b * 128:b * 128 + nt], win[:, kc, 512 + blk * 128:512 + (blk + 1) * 128],
                            xT[:, kc, 0:nt], start=(kc == 0), stop=(kc == 7), r=[win, xT], w=[PS[bank]])
                evac(cb[:, g4 * 4:g4 * 4 + 4, 3:3 + nt], PS[bank][:, :].rearrange("p (a b) -> p a b", a=4)[:, :, 0:nt],
                     r=[PS[bank]], w=[(cb, g4)])
            tapsrc[0] = cb; tap("cb", cb[:, :, :].rearrange("p a b -> p (a b)"), ti, 128, 12 * 131)
            tapsrc[0] = uTf; tap("uTf", uTf[:, :, :].rearrange("p a b -> p (a b)"), ti, 128, 512)
            for kc in range(8):
                p.V("pe", "matmul", PS[4][0:nt, 0:512], xT[:, kc, 0:nt], win[:, kc, 2048:2560], start=(kc == 0), stop=(kc == 7),
                    r=[xT, win], w=[PS[4]])
            for kc in range(8):
                p.V("pe", "matmul", PS[5][0:nt, 0:8], xT[:, kc, 0:nt], win[:, kc, 2560:2568], start=(kc == 0), stop=(kc == 7),
                    r=[xT, win], w=[PS[5]])
            p.V("act", "activation", zs[0:nt, :], PS[4][0:nt, 0:512], AF.Silu, r=[PS[4]], w=[zs])
            p.V("dve", "tensor_copy", ztok[0:nt, 0:8], PS[5][0:nt, 0:8], r=[PS[5]], w=[ztok])
            if CUT <= 2:
                continue
            for hf in range(2):
                for ri in range(2):
                    for b8 in range(8):
                        blk = hf * 8 + b8
                        bank = 4 + ri * 2 + (b8 // 4)
                        p.V("pe", "matmul", PS[bank][:, (b8 % 4) * 128:(b8 % 4) * 128 + nt], bst[:, ri, blk, :], uTb[:, blk // 4, 0:nt],
                            start=True, stop=True, r=[bst, uTb], w=[PS[bank]])
                for q4 in range(2):
                    bre = PS[4 + q4][:, :].rearrange("p (a b) -> p a b", a=4)[:, :, 0:nt]
                    bim = PS[6 + q4][:, :].rearrange("p (a b) -> p a b", a=4)[:, :, 0:nt]
                    bs = slice(hf * 8 + q4 * 4, hf * 8 + q4 * 4 + 4)
                    o4 = slice(q4 * 4, q4 * 4 + 4)
                    p.V("dve", "tensor_tensor", rbuf[:, 0, o4, 0:nt], bre, crT[:, bs, 0:nt], ALU.mult, r=[PS[4 + q4], crT], w=[(rbuf, 0)])
                    p.V("dve", "tensor_tensor", rtmp[:, 0, o4, 0:nt], bim, ciT[:, bs, 0:nt], ALU.mult, r=[PS[6 + q4], ciT], w=[(rtmp, 0)])
                    p.V("dve", "tensor_tensor", rbuf[:, 1, o4, 0:nt], bre, ciT[:, bs, 0:nt], ALU.mult, r=[PS[4 + q4], ciT], w=[(rbuf, 1)])
                    p.V("dve", "tensor_tensor", rtmp[:, 1, o4, 0:nt], bim, crT[:, bs, 0:nt], ALU.mult, r=[PS[6 + q4], crT], w=[(rtmp, 1)])
                p.V("pool", "tensor_tensor", rbuf[:, 0, :, 0:nt], rbuf[:, 0, :, 0:nt], rtmp[:, 0, :, 0:nt], ALU.subtract, r=[(rbuf, 0), (rtmp, 0)], w=[(rbuf, 0)])
                p.V("pool", "tensor_tensor", rbuf[:, 1, :, 0:nt], rbuf[:, 1, :, 0:nt], rtmp[:, 1, :, 0:nt], ALU.add, r=[(rbuf, 1), (rtmp, 1)], w=[(rbuf, 1)])
                for b8 in range(8):
                    blk = hf * 8 + b8
                    p.V("dve", "tensor_tensor_scan", gsc[:, 0, blk, 0:nt], lam[:, blk:blk + 1].to_broadcast([128, nt]), rbuf[:, 0, b8, 0:nt],
                        Hre[:, blk:blk + 1], ALU.mult, ALU.add, r=[lam, (rbuf, 0), Hre], w=[(gsc, (0, blk))])
                    p.V("dve", "tensor_tensor_scan", gsc[:, 1, blk, 0:nt], lam[:, blk:blk + 1].to_broadcast([128, nt]), rbuf[:, 1, b8, 0:nt],
                        Him[:, blk:blk + 1], ALU.mult, ALU.add, r=[lam, (rbuf, 1), Him], w=[(gsc, (1, blk))])
            t_a, t_b = rbuf[:, :, :, :].rearrange("p a b c -> p (a b) c"), rtmp[:, :, :, :].rearrange("p a b c -> p (a b) c")
            p.V("pool", "tensor_tensor", t_a[:, :, 0:nt], gsc[:, 0, :, 0:nt], cosT[:, :, 0:nt], ALU.mult, r=[gsc, cosT], w=[rbuf])
            p.V("pool", "tensor_tensor", t_b[:, :, 0:nt], gsc[:, 1, :, 0:nt], sinT[:, :, 0:nt], ALU.mult, r=[gsc, sinT], w=[rtmp])
            p.V("dve", "tensor_tensor", hbf[:, 0, :, 0:nt], t_a[:, :, 0:nt], t_b[:, :, 0:nt], ALU.subtract, r=[rbuf, rtmp], w=[(hbf, 0)])
            lc = nt - 1
            p.V("dve", "tensor_tensor", hl[:, 0, :], gsc[:, 0, :, lc], cosT[:, :, lc], ALU.mult, r=[gsc, cosT], w=[(hl, 0)])
            p.V("dve", "tensor_tensor", hl[:, 1, :], gsc[:, 1, :, lc], sinT[:, :, lc], ALU.mult, r=[gsc, sinT], w=[(hl, 1)])
            p.V("dve", "tensor_tensor", hl[:, 2, :], gsc[:, 0, :, lc], sinT[:, :, lc], ALU.mult, r=[gsc, sinT], w=[(hl, 2)])
            p.V("dve", "tensor_tensor", hl[:, 3, :], gsc[:, 1, :, lc], cosT[:, :, lc], ALU.mult, r=[gsc, cosT], w=[(hl, 3)])
            p.V("pool", "tensor_tensor", t_a[:, :, 0:nt], gsc[:, 0, :, 0:nt], sinT[:, :, 0:nt], ALU.mult, r=[gsc, sinT, (hbf, 0)], w=[rbuf])
            p.V("pool", "tensor_tensor", t_b[:, :, 0:nt], gsc[:, 1, :, 0:nt], cosT[:, :, 0:nt], ALU.mult, r=[gsc, cosT, (hbf, 0)], w=[rtmp])
            p.V("dve", "scalar_tensor_tensor", hbf[:, 1, :, 0:nt], t_a[:, :, 0:nt], -1.0, t_b[:, :, 0:nt], ALU.mult, ALU.subtract,
                r=[rbuf, rtmp], w=[(hbf, 1)])
            p.V("dve", "tensor_tensor", Hre[:], hl[:, 0, :], hl[:, 1, :], ALU.subtract, r=[hl], w=[Hre])
            p.V("dve", "tensor_tensor", Him[:], hl[:, 2, :], hl[:, 3, :], ALU.add, r=[hl], w=[Him])
            if tl["last"]:
                p.DM("sp", o_s5re[sq], Hre[:], r=[Hre], w=[T_out])
                p.DM("sp", o_s5im[sq], Him[:], r=[Him], w=[T_out])
            for ob in range(4):
                n = 0
                for b4 in range(4):
                    blk = ob * 4 + b4
                    for ri in range(2):
                        p.V("pe", "matmul", PS[1][:, ob * 128:ob * 128 + nt], cstt[:, ri, blk, :], hbf[:, ri, blk, 0:nt],
                            start=(n == 0), stop=(n == 7), r=[cstt, hbf], w=[PS[1]])
                        n += 1
            for ob in range(4):
                p.V("dve", "scalar_tensor_tensor", yA[:, ob, 0:nt], uTf[:, ob, 0:nt], dsk[:, ob:ob + 1], PS[1][:, ob * 128:ob * 128 + nt],
                    ALU.mult, ALU.add, r=[uTf, dsk, PS[1]], w=[yA])
            cg = math.sqrt(2.0 / math.pi)
            p.V("act", "activation", ysq[:, :, 0:nt], yA[:, :, 0:nt], AF.Square, r=[yA], w=[ysq])
            p.V("dve", "tensor_scalar", ysq[:, :, 0:nt], ysq[:, :, 0:nt], 2.0 * cg * 0.044715, 2.0 * cg, ALU.mult, ALU.add, r=[ysq], w=[ysq])
            p.V("dve", "tensor_tensor", ysq[:, :, 0:nt], ysq[:, :, 0:nt], yA[:, :, 0:nt], ALU.mult, r=[ysq, yA], w=[ysq])
            p.V("act", "activation", ysq[:, :, 0:nt], ysq[:, :, 0:nt], AF.Sigmoid, r=[ysq], w=[ysq])
            p.V("dve", "tensor_tensor", gaf[:, :, 0:nt], ysq[:, :, 0:nt], yA[:, :, 0:nt], ALU.mult, r=[ysq, yA], w=[gaf])
            p.V("act", "activation", gab[:, :, 0:nt], gaf[:, :, 0:nt], AF.Copy, r=[gaf], w=[gab])
            for ob in range(4):
                for kc in range(4):
                    p.V("pe", "matmul", PS[0][:, ob * 128:ob * 128 + nt], wglu[:, kc, ob * 128:(ob + 1) * 128], gab[:, kc, 0:nt],
                        start=(kc == 0), stop=(kc == 3), r=[wglu, gab], w=[PS[0]])
            for ob in range(4):
                p.V("act", "activation", ysq[:, ob, 0:nt], PS[0][:, ob * 128:ob * 128 + nt], AF.Sigmoid, bias=bgl[:, ob:ob + 1],
                    r=[PS[0], bgl], w=[ysq])
            p.V("dve", "tensor_tensor", mixT[:, 0:4, 0:nt], ysq[:, :, 0:nt], gaf[:, :, 0:nt], ALU.mult, r=[ysq, gaf], w=[(mixT, "a")])
            if CUT <= 3:
                continue
            for j in range(4):
                wj = wc[:, :, j:j + 1].to_broadcast([128, 12, nt])
                if j == 0:
                    p.V("dve", "tensor_tensor", cacc[:, :, 0:nt], cb[:, :, 0:nt], wj, ALU.mult, r=[cb, wc], w=[cacc])
                else:
                    p.V("pool", "tensor_tensor", ctmp[:, :, 0:nt], cb[:, :, j:j + nt], wj, ALU.mult, r=[cb, wc], w=[ctmp])
                    p.V("dve", "tensor_tensor", cacc[:, :, 0:nt], cacc[:, :, 0:nt], ctmp[:, :, 0:nt], ALU.add, r=[cacc, ctmp], w=[cacc])
            p.V("pool", "tensor_copy", ctx3[:, :, :], cb[:, :, nt:nt + 3], r=[cb], w=[ctx3])
            if tl["last"]:
                p.DM("sp", o_conv[sq], ctx3[:], r=[ctx3], w=[T_out])
            p.V("act", "activation", cacc[:, :, 0:nt], cacc[:, :, 0:nt], AF.Silu, r=[cacc], w=[cacc])
            p.V("act", "activation", sq8[:, :, 0:nt], cacc[:, 0:8, 0:nt], AF.Square, r=[cacc], w=[sq8])
            for hb in range(2):
                for b in range(4):
                    p.V("pe", "matmul", PS[2 + hb][:, b * 128:b * 128 + nt], ones_f[:, :], sq8[:, hb * 4 + b, 0:nt], start=True, stop=True,
                        r=[ones_f, sq8], w=[PS[2 + hb]])
            for hb in range(2):
                v = PS[2 + hb][:, :].rearrange("p (a b) -> p a b", a=4)[:, :, 0:nt]
                p.V("act", "activation", sq8[:, hb * 4:hb * 4 + 4, 0:nt], v, AF.Sqrt, bias=epsln[:, 1:2], r=[PS[2 + hb], epsln], w=[(sq8, hb)])
            p.V("dve", "reciprocal", sq8[:, :, 0:nt], sq8[:, :, 0:nt], r=[sq8], w=[sq8])
            p.V("dve", "scalar_tensor_tensor", qkn[:, 0:4, 0:nt], cacc[:, 0:4, 0:nt], 128.0 ** -0.5, sq8[:, 0:4, 0:nt], ALU.mult, ALU.mult,
                r=[cacc, sq8], w=[(qkn, "q")])
            p.V("dve", "tensor_tensor", qkn[:, 4:8, 0:nt], cacc[:, 4:8, 0:nt], sq8[:, 4:8, 0:nt], ALU.mult, r=[cacc, sq8], w=[(qkn, "k")])
            for b in range(4):
                p.V("pe", "transpose", PS[2][0:nt, b * 128:(b + 1) * 128], qkn[:, 4 + b, 0:nt], identf[:, :], r=[qkn, identf], w=[PS[2]])
                p.V("pe", "transpose", PS[3][0:nt, b * 128:(b + 1) * 128], cacc[:, 8 + b, 0:nt], identf[:, :], r=[cacc, identf], w=[PS[3]])
            evac(kvtok[0:nt, 0:4, :], PS[2][0:nt, :].rearrange("p (a b) -> p a b", a=4), r=[PS[2]], w=[kvtok])
            evac(kvtok[0:nt, 4:8, :], PS[3][0:nt, :].rearrange("p (a b) -> p a b", a=4), r=[PS[3]], w=[kvtok])
            if CUT2 <= 1:
                continue
            p.V("act", "activation", gd[0:nt, 0:4], ztok[0:nt, 0:4], AF.Sigmoid, r=[ztok], w=[(gd, "b")])
            p.V("dve", "tensor_scalar", gd[0:nt, 4:8], gd[0:nt, 0:4], -1.0, None, ALU.mult, r=[(gd, "b")], w=[(gd, "nb")])
            p.V("dve", "tensor_tensor", gd[0:nt, 8:12], ztok[0:nt, 4:8], dtb[0:nt, :], ALU.add, r=[ztok, dtb], w=[(gd, "g")])
            p.V("act", "activation", gd[0:nt, 8:12], gd[0:nt, 8:12], AF.Exp, r=[(gd, "g")], w=[(gd, "g")])
            p.V("act", "activation", gd[0:nt, 8:12], gd[0:nt, 8:12], AF.Ln, bias=1.0, r=[(gd, "g")], w=[(gd, "g")])
            p.V("dve", "scalar_tensor_tensor", gd[0:nt, 8:12], gd[0:nt, 8:12], -1.0, alog[0:nt, :], ALU.mult, ALU.mult, r=[(gd, "g"), alog], w=[(gd, "g")])
            p.V("pe", "matmul", PS[0][0:nt, 0:4], uincl[0:nt, 0:nt], gd[0:nt, 8:12], start=True, stop=True, r=[uincl, (gd, "g")], w=[PS[0]])
            p.V("dve", "tensor_copy", gd[0:nt, 12:16], PS[0][0:nt, 0:4], r=[PS[0]], w=[(gd, "G")])
            p.V("act", "activation", gd2[0:nt, 0:4], gd[0:nt, 12:16], AF.Exp, r=[(gd, "G")], w=[(gd2, "e")])
            p.V("dve", "tensor_tensor", gd2[0:nt, 4:8], gd2[0:nt, 0:4], gd[0:nt, 0:4], ALU.mult, r=[(gd2, "e"), (gd, "b")], w=[(gd2, "be")])
            if CUT2 <= 2:
                continue
            for hd in range(4):
                kT = qkn[:, 4 + hd, 0:nt]
                qT = qkn[:, hd, 0:nt]
                p.V("dve", "tensor_scalar", gbc[0:nt, :], ones_f[0:nt, :], gd[0:nt, 8 + hd:9 + hd], None, ALU.mult, r=[ones_f, (gd, "g")], w=[gbc])
                p.V("pe", "matmul", PS[0][:, 128:128 + nt], gbc[0:nt, :], uincl[0:nt, 0:nt], start=True, stop=True, r=[gbc, uincl], w=[PS[0]])
                p.V("pe", "matmul", PS[0][0:nt, 256:256 + nt], kT, kT, start=True, stop=True, r=[(qkn, "k")], w=[PS[0]])
                p.V("pe", "matmul", PS[0][0:nt, 384:384 + nt], qT, kT, start=True, stop=True, r=[(qkn, "q"), (qkn, "k")], w=[PS[0]])
                grow = PS[0][0:nt, 128:128 + nt]
                p.V("act", "activation", dec[0:nt, 0:nt], grow, AF.Exp, bias=gd[0:nt, 12 + hd:13 + hd], scale=-1.0, r=[PS[0], (gd, "G")], w=[dec])
                p.V("act", "activation", erow[:, 0:nt], PS[0][:, 128:128 + nt], AF.Exp, r=[PS[0]], w=[erow])
                p.V("pool", "affine_select", dec[0:nt, 0:nt], dec[0:nt, 0:nt], [[-1, nt]], ALU.is_ge, 0.0, base=0, channel_multiplier=1, r=[dec], w=[dec])
                p.V("dve", "scalar_tensor_tensor", Mm[0][0:nt, 0:nt], PS[0][0:nt, 256:256 + nt], gd[0:nt, 4 + hd:5 + hd], dec[0:nt, 0:nt], ALU.mult, ALU.mult,
                    r=[PS[0], (gd, "nb"), dec], w=[Mm[0]])
                p.V("pool", "affine_select", Mm[0][0:nt, 0:nt], Mm[0][0:nt, 0:nt], [[-1, nt]], ALU.is_gt, 0.0, base=0, channel_multiplier=1, r=[Mm[0]], w=[Mm[0]])
                p.V("dve", "tensor_tensor", attn[0:nt, 0:nt], PS[0][0:nt, 384:384 + nt], dec[0:nt, 0:nt], ALU.mult, r=[PS[0], dec], w=[attn])
                p.V("pe", "transpose", PS[1][0:nt, 0:nt], Mm[0][0:nt, 0:nt], identf[0:nt, 0:nt], r=[Mm[0], identf], w=[PS[1]])
                p.V("pe", "transpose", PS[1][0:nt, 128:128 + nt], attn[0:nt, 0:nt], identf[0:nt, 0:nt], r=[attn, identf], w=[PS[1]])
                p.V("act", "activation", MT[0][0:nt, 0:nt], PS[1][0:nt, 0:nt], AF.Copy, r=[PS[1]], w=[MT[0]])
                p.V("dve", "tensor_tensor", Yt[0:nt, 0:nt], PS[1][0:nt, 0:nt], identf[0:nt, 0:nt], ALU.add, r=[PS[1], identf], w=[Yt])
                p.V("act", "activation", attnT[0:nt, 0:nt], PS[1][0:nt, 128:128 + nt], AF.Copy, r=[PS[1]], w=[attnT])
                if CUT2 <= 3:
                    continue
                nlev = 6 if nt == 128 else 3
                for lv in range(nlev):
                    a, b_ = lv % 2, (lv + 1) % 2
                    bank = 6 + (lv % 2)
                    p.V("pe", "matmul", PS[bank][0:nt, 0:nt], MT[a][0:nt, 0:nt], Mm[a][0:nt, 0:nt], start=True, stop=True, r=[MT[a], Mm[a]], w=[PS[bank]])
                    if lv < nlev - 1:
                        p.V("pe", "matmul", PS[bank][0:nt, 128:128 + nt], Mm[a][0:nt, 0:nt], MT[a][0:nt, 0:nt], start=True, stop=True, r=[MT[a], Mm[a]], w=[PS[bank]])
                    p.V("act", "activation", Mm[b_][0:nt, 0:nt], PS[bank][0:nt, 0:nt], AF.Copy, r=[PS[bank]], w=[Mm[b_]])
                    if lv < nlev - 1:
                        p.V("dve", "tensor_copy", MT[b_][0:nt, 0:nt], PS[bank][0:nt, 128:128 + nt], r=[PS[bank]], w=[MT[b_]])
                    p.V("pe", "matmul", PS[bank][0:nt, 256:256 + nt], Mm[b_][0:nt, 0:nt], Yt[0:nt, 0:nt], start=True, stop=True, r=[Mm[b_], Yt], w=[PS[bank]])
                    p.V("dve", "tensor_tensor", Yt[0:nt, 0:nt], Yt[0:nt, 0:nt], PS[bank][0:nt, 256:256 + nt], ALU.add, r=[Yt, PS[bank]], w=[Yt])
                if CUT2 <= 4:
                    continue
                p.V("dve", "tensor_scalar", rv[0:nt, :], kvtok[0:nt, 4 + hd, :], gd[0:nt, hd:hd + 1], None, ALU.mult, r=[kvtok, (gd, "b")], w=[rv])
                p.V("dve", "tensor_scalar", rk_[0:nt, :], kvtok[0:nt, hd, :], gd2[0:nt, 4 + hd:5 + hd], None, ALU.mult, r=[kvtok, (gd2, "be")], w=[rk_])
                p.V("pe", "matmul", PS[1][:, 256:256 + nt], rk_[0:nt, :], Yt[0:nt, 0:nt], start=True, stop=True, r=[rk_, Yt], w=[PS[1]])
                p.V("dve", "tensor_scalar", nwT[:, 0:nt], PS[1][:, 256:256 + nt], -1.0, None, ALU.mult, r=[PS[1]], w=[nwT])
                p.V("pe", "matmul", PS[5][0:nt, 0:128], Yt[0:nt, 0:nt], rv[0:nt, :], start=True, stop=False, r=[Yt, rv], w=[PS[5]])
                p.V("pe", "matmul", PS[5][0:nt, 0:128], nwT[:, 0:nt], Sg[:, hd, :], start=False, stop=True, r=[nwT, Sg], w=[PS[5]])
                p.V("act", "activation", ub[0:nt, :], PS[5][0:nt, 0:128], AF.Copy, r=[PS[5]], w=[ub])
                p.V("dve", "tensor_tensor", qdT[:, 0:nt], qT, erow[:, 0:nt], ALU.mult, r=[(qkn, "q"), erow], w=[qdT])
                p.V("pe", "matmul", PS[5][0:nt, 128:256], qdT[:, 0:nt], Sg[:, hd, :], start=True, stop=False, r=[qdT, Sg], w=[PS[5]])
                p.V("pe", "matmul", PS[5][0:nt, 128:256], attnT[0:nt, 0:nt], ub[0:nt, :], start=False, stop=True, r=[attnT, ub], w=[PS[5]])
                if CUT2 <= 5:
                    continue
                p.V("dve", "tensor_copy", gd2[:, 12:13], PS[0][:, 128 + nt - 1:128 + nt], r=[PS[0]], w=[(gd2, "gl")])
                p.V("act", "activation", gd2[0:nt, 8 + hd:9 + hd], gd[0:nt, 12 + hd:13 + hd], AF.Exp, bias=gd2[0:nt, 12:13], scale=-1.0,
                    r=[(gd, "G"), (gd2, "gl")], w=[(gd2, ("kd", hd))])
                p.V("dve", "tensor_scalar", kd[0:nt, :], kvtok[0:nt, hd, :], gd2[0:nt, 8 + hd:9 + hd], None, ALU.mult, r=[kvtok, (gd2, ("kd", hd))], w=[kd])
                p.V("pe", "matmul", PS[5][:, 256:384], kd[0:nt, :], ub[0:nt, :], start=True, stop=True, r=[kd, ub], w=[PS[5]])
                p.V("act", "activation", gd2[:, 13:14], gd2[:, 12:13], AF.Exp, r=[(gd2, "gl")], w=[(gd2, "egl")])
                p.V("dve", "scalar_tensor_tensor", Sg[:, hd, :], Sg[:, hd, :], gd2[:, 13:14], PS[5][:, 256:384], ALU.mult, ALU.add,
                    r=[Sg, (gd2, "egl"), PS[5]], w=[Sg])
                if CUT2 <= 6:
                    continue
                p.V("act", "activation", osb[0:nt, :], PS[5][0:nt, 128:256], AF.Square, r=[PS[5]], w=[osb])
                p.V("dve", "reduce_sum", ssq[0:nt, hd:hd + 1], osb[0:nt, :], mybir.AxisListType.X, r=[osb], w=[(ssq, hd)])
                p.V("act", "activation", ssq[0:nt, hd:hd + 1], ssq[0:nt, hd:hd + 1], AF.Sqrt, bias=epsln[0:nt, 1:2], scale=1.0 / 128.0, r=[(ssq, hd), epsln], w=[(ssq, hd)])
                p.V("dve", "reciprocal", ssq[0:nt, hd:hd + 1], ssq[0:nt, hd:hd + 1], r=[(ssq, hd)], w=[(ssq, hd)])
                if CUT2 <= 7:
                    continue
                p.V("dve", "scalar_tensor_tensor", osb[0:nt, :], PS[5][0:nt, 128:256], ssq[0:nt, hd:hd + 1], nrmw[0:nt, :], ALU.mult, ALU.mult,
                    r=[PS[5], (ssq, hd), nrmw], w=[osb])
                p.V("dve", "tensor_tensor", yB[0:nt, hd * 128:(hd + 1) * 128], osb[0:nt, :], zs[0:nt, hd * 128:(hd + 1) * 128], ALU.mult, r=[osb, zs], w=[(yB, hd)])
            if tl["last"]:
                p.DM("sp", o_gdn[sq].rearrange("h k v -> k h v"), Sg[:], r=[Sg], w=[T_out])
            if CUT <= 4:
                continue
            for b in range(4):
                p.V("pe", "transpose", psbf(2, 1024)[:, b * 128:b * 128 + nt], yB[0:nt, b * 128:(b + 1) * 128], identb[0:nt, 0:nt], r=[yB, identb], w=[PS[2]])
            evac(mixT[:, 4:8, 0:nt], psbf(2, 1024).rearrange("p (a b) -> p a b", a=8)[:, 0:4, 0:nt], r=[PS[2]], w=[(mixT, "b")])
            for half in range(2):
                for kc in range(8):
                    p.V("pe", "matmul", PS[2 + half][0:nt, :], mixT[:, kc, 0:nt], wout[:, kc, half * 512:(half + 1) * 512], start=(kc == 0), stop=(kc == 7),
                        r=[mixT, wout], w=[PS[2 + half]])
            if CUT <= 5:
                continue
            phaseA_tail(0, tl, xt, (2, 3), lnw, rw, rb, work)
        p.release(m0)

    def run_streams(gens):
        gens = list(gens)
        while gens:
            for g in list(gens):
                try:
                    next(g)
                except StopIteration:
                    gens.remove(g)

    def phaseS0():
        m0 = p.mark()
        win_u = p.sb("win_u", [128, 8, 512], BF16)
        p.DM("pool", win_u[:], w_in_even[:, 0:512].rearrange("(kc q) n -> q kc n", q=128), r=[DR], w=[win_u])
        wglu = p.sb("wglu", [128, 4, 512], BF16)
        load_w_bf16(wglu, s5_wglu, 4)
        bst = p.sb("bst", [128, 2, 16, 128], BF16)
        cstt = p.sb("cstt", [128, 2, 16, 128], BF16)
        for ri in range(2):
            p.DM("pool", bst[:, ri, :, :], s5_bst[ri].rearrange("b k m -> k b m"), r=[DR], w=[(bst, ri)])
            p.DM("pool", cstt[:, ri, :, :], s5_cst[ri].rearrange("b k m -> k b m"), r=[DR], w=[(cstt, ri)])
        are = p.sb("are", [128, 16]); aim = p.sb("aim", [128, 16]); ldt = p.sb("ldt", [128, 16])
        for b, s_ in ((are, s5_are), (aim, s5_aim), (ldt, s5_ldt)):
            p.DM("sp", b[:], s_, r=[DR], w=[b])
        dsk = p.sb("dsk", [128, 4]); bgl = p.sb("bgl", [128, 4])
        p.DM("sp", dsk[:], s5_d, r=[DR], w=[dsk])
        p.DM("sp", bgl[:], s5_bglu, r=[DR], w=[bgl])
        tau = p.sb("tau", [128, 128])
        p.DM("sp", tau[:], cst["tau"], r=[DR], w=[tau])
        lam = p.sb("lam", [128, 16]); li_ = p.sb("li", [128, 16]); dtt = p.sb("dtt", [128, 16])
        p.V("act", "activation", dtt[:], ldt[:], AF.Exp, r=[ldt], w=[dtt])
        p.V("dve", "tensor_tensor", li_[:], aim[:], dtt[:], ALU.mult, r=[aim, dtt], w=[li_])
        p.V("dve", "tensor_tensor", lam[:], are[:], dtt[:], ALU.mult, r=[are, dtt], w=[lam])
        p.V("act", "activation", lam[:], lam[:], AF.Exp, r=[lam], w=[lam])
        cosT = p.sb("cosT", [128, 16, 128]); sinT = p.sb("sinT", [128, 16, 128])
        crT = p.sb("crT", [128, 16, 128]); ciT = p.sb("ciT", [128, 16, 128])
        ang = crT
        kq = ciT
        kif = p.sb("kif", [128, 16, 128])
        ki = alias(kif[:, :, :].bitcast(I32), kif, "ki")
        sc3 = p.sb("sc3", [128, 16, 128])
        TWO_PI = 2.0 * math.pi

        def sin_of(dst, shift):
            p.V("dve", "tensor_tensor", ang[:], li_[:, :].unsqueeze(2).to_broadcast([128, 16, 128]),
                tau[:, :].unsqueeze(1).to_broadcast([128, 16, 128]), ALU.mult, r=[li_, tau], w=[ang])
            if shift != 0.0:
                p.V("dve", "tensor_scalar", ang[:], ang[:], shift, None, ALU.add, r=[ang], w=[ang])
            p.V("dve", "tensor_scalar", kq[:], ang[:], 1.0 / TWO_PI, None, ALU.mult, r=[ang], w=[kq])
            p.V("dve", "tensor_copy", ki[:], kq[:], r=[kq], w=[ki])
            p.V("dve", "tensor_copy", kq[:], ki[:], r=[ki], w=[kq])
            p.V("dve", "scalar_tensor_tensor", ang[:], kq[:], -TWO_PI, ang[:], ALU.mult, ALU.add, r=[kq, ang], w=[ang])
            p.V("dve", "tensor_scalar", kq[:], ang[:], math.pi, TWO_PI, ALU.is_gt, ALU.mult, r=[ang], w=[kq])
            p.V("dve", "tensor_tensor", ang[:], ang[:], kq[:], ALU.subtract, r=[ang, kq], w=[ang])
            p.V("dve", "tensor_scalar", kq[:], ang[:], -math.pi, TWO_PI, ALU.is_lt, ALU.mult, r=[ang], w=[kq])
            p.V("dve", "tensor_tensor", ang[:], ang[:], kq[:], ALU.add, r=[ang, kq], w=[ang])
            p.V("dve", "tensor_scalar", ang[:], ang[:], math.pi, -math.pi, ALU.min, ALU.max, r=[ang], w=[ang])
            p.V("act", "activation", dst[:], ang[:], AF.Sin, r=[ang], w=[dst])

        sin_of(sinT, 0.0)
        sin_of(cosT, math.pi / 2)
        sm = p.sb("s5sm", [128, 8, 16])
        abre, abim, den, t1, t2, cfre, cfim, t3 = [sm[:, i, :] for i in range(8)]
        S = [sm]
        p.V("dve", "tensor_tensor", abre, lam[:], cosT[:, :, 0], ALU.mult, r=[lam, cosT], w=S)
        p.V("dve", "tensor_tensor", abim, lam[:], sinT[:, :, 0], ALU.mult, r=[lam, sinT], w=S)
        p.V("dve", "tensor_scalar", abre, abre, -1.0, None, ALU.add, r=S, w=S)
        p.V("dve", "tensor_tensor", t1, are[:], are[:], ALU.mult, r=[are], w=S)
        p.V("dve", "tensor_tensor", t2, aim[:], aim[:], ALU.mult, r=[aim], w=S)
        p.V("dve", "tensor_tensor", den, t1, t2, ALU.add, r=S, w=S)
        p.V("dve", "reciprocal", den, den, r=S, w=S)
        p.V("dve", "tensor_tensor", t1, abre, are[:], ALU.mult, r=S + [are], w=S)
        p.V("dve", "tensor_tensor", t2, abim, aim[:], ALU.mult, r=S + [aim], w=S)
        p.V("dve", "tensor_tensor", cfre, t1, t2, ALU.add, r=S, w=S)
        p.V("dve", "tensor_tensor", cfre, cfre, den, ALU.mult, r=S, w=S)
        p.V("dve", "tensor_tensor", t1, abim, are[:], ALU.mult, r=S + [are], w=S)
        p.V("dve", "tensor_tensor", t2, abre, aim[:], ALU.mult, r=S + [aim], w=S)
        p.V("dve", "tensor_tensor", cfim, t1, t2, ALU.subtract, r=S, w=S)
        p.V("dve", "tensor_tensor", cfim, cfim, den, ALU.mult, r=S, w=S)
        bc = lambda a: a.unsqueeze(2).to_broadcast([128, 16, 128])
        p.V("dve", "tensor_tensor", crT[:], cosT[:], bc(cfre), ALU.mult, r=[cosT] + S, w=[crT])
        p.V("dve", "tensor_tensor", sc3[:], sinT[:], bc(cfim), ALU.mult, r=[sinT] + S, w=[sc3])
        p.V("dve", "tensor_tensor", crT[:], crT[:], sc3[:], ALU.add, r=[crT, sc3], w=[crT])
        p.V("dve", "tensor_tensor", ciT[:], cosT[:], bc(cfim), ALU.mult, r=[cosT] + S, w=[ciT])
        p.V("dve", "tensor_tensor", sc3[:], sinT[:], bc(cfre), ALU.mult, r=[sinT] + S, w=[sc3])
        p.V("dve", "tensor_tensor", ciT[:], ciT[:], sc3[:], ALU.subtract, r=[ciT, sc3], w=[ciT])
        cg = math.sqrt(2.0 / math.pi)

        def stream(sx, tlist):
            B0, B1, B2, B3 = 4 * sx, 4 * sx + 1, 4 * sx + 2, 4 * sx + 3
            n_ = lambda s_: "%s_%d" % (s_, sx)
            xt = p.sb(n_("xt"), [128, D]); xb = p.sb(n_("xb"), [128, D], BF16); xT = p.sb(n_("xT"), [128, 8, 128], BF16)
            uTf = p.sb(n_("uTf"), [128, 4, 128]); uTb = p.sb(n_("uTb"), [128, 4, 128], BF16)
            rbuf = p.sb(n_("rbuf"), [128, 2, 4, 128]); rtmp = p.sb(n_("rtmp"), [128, 2, 4, 128]); gsc = p.sb(n_("gsc"), [128, 2, 4, 128])
            hbf = p.sb(n_("hbf"), [128, 2, 16, 128], BF16); hl = p.sb(n_("hl"), [128, 4, 16])
            yA = p.sb(n_("yA"), [128, 4, 128]); ysq = p.sb(n_("ysq"), [128, 4, 128]); gaf = p.sb(n_("gaf"), [128, 4, 128])
            gab = p.sb(n_("gab"), [128, 4, 128], BF16); yag = p.sb(n_("yag"), [128, 4, 128], BF16)
            Hre = p.sb(n_("Hre"), [128, 16]); Him = p.sb(n_("Him"), [128, 16])
            Hn = p.sb(n_("Hn"), [128, 2, 16])
            for tl in tlist:
                nt, ti, sq = tl["nt"], tl["ti"], tl["seq"]
                if nt < 128:
                    p.V("pool", "memset", xt[:], 0.0, w=[xt])
                p.DM("sp", xt[0:nt, :], xin[tl["row0"]:tl["row0"] + nt, :], r=[DR], w=[xt])
                if tl["first"]:
                    if sq < NPS:
                        p.V("pool", "memset", Hre[:], 0.0, w=[Hre]); p.V("pool", "memset", Him[:], 0.0, w=[Him])
                    else:
                        p.DM("sp", Hre[:], st_s5re, r=[DR], w=[Hre])
                        p.DM("sp", Him[:], st_s5im, r=[DR], w=[Him])
                yield
                p.V("act", "activation", xb[0:nt, :], xt[0:nt, :], AF.Copy, r=[xt], w=[xb])
                yield
                for b in range(8):
                    p.V("pe", "transpose", psbf(B0, 1024)[:, b * 128:b * 128 + nt], xb[0:nt, b * 128:(b + 1) * 128], identb[0:nt, 0:nt], r=[xb, identb], w=[PS[B0]])
                p.V("dve", "tensor_copy", xT[:, :, 0:nt], psbf(B0, 1024).rearrange("p (a b) -> p a b", a=8)[:, :, 0:nt], r=[PS[B0]], w=[xT])
                yield
                for ob in range(4):
                    for kc in range(8):
                        p.V("pe", "matmul", PS[B1][:, ob * 128:ob * 128 + nt], win_u[:, kc, ob * 128:(ob + 1) * 128], xT[:, kc, 0:nt],
                            start=(kc == 0), stop=(kc == 7), r=[win_u, xT], w=[PS[B1]])
                ps1v = PS[B1][:, :].rearrange("p (a b) -> p a b", a=4)[:, :, 0:nt]
                p.V("act", "activation", uTf[:, :, 0:nt], ps1v, AF.Copy, r=[PS[B1]], w=[uTf])
                p.V("act", "activation", uTb[:, :, 0:nt], uTf[:, :, 0:nt], AF.Copy, r=[uTf], w=[uTb])
                yield
                lc = nt - 1
                for g4 in range(4):
                    bs = slice(g4 * 4, g4 * 4 + 4)
                    for ri in range(2):
                        for b in range(4):
                            blk = g4 * 4 + b
                            p.V("pe", "matmul", PS[B2 + ri][:, b * 128:b * 128 + nt], bst[:, ri, blk, :], uTb[:, blk // 4, 0:nt], start=True, stop=True,
                                r=[bst, uTb], w=[PS[B2 + ri]])
                    bre = PS[B2][:, :].rearrange("p (a b) -> p a b", a=4)[:, :, 0:nt]
                    bim = PS[B3][:, :].rearrange("p (a b) -> p a b", a=4)[:, :, 0:nt]
                    p.V("dve", "tensor_tensor", rbuf[:, 0, :, 0:nt], bre, crT[:, bs, 0:nt], ALU.mult, r=[PS[B2], crT], w=[(rbuf, 0)])
                    p.V("dve", "tensor_tensor", rbuf[:, 1, :, 0:nt], bre, ciT[:, bs, 0:nt], ALU.mult, r=[PS[B2], ciT], w=[(rbuf, 1)])
                    yield
                    p.V("dve", "tensor_tensor", rtmp[:, 0, :, 0:nt], bim, ciT[:, bs, 0:nt], ALU.mult, r=[PS[B3], ciT], w=[(rtmp, 0)])
                    p.V("dve", "tensor_tensor", rtmp[:, 1, :, 0:nt], bim, crT[:, bs, 0:nt], ALU.mult, r=[PS[B3], crT], w=[(rtmp, 1)])
                    yield
                    p.V("pool", "tensor_tensor", rbuf[:, 0, :, 0:nt], rbuf[:, 0, :, 0:nt], rtmp[:, 0, :, 0:nt], ALU.subtract, r=[(rbuf, 0), (rtmp, 0)], w=[(rbuf, 0)])
                    p.V("pool", "tensor_tensor", rbuf[:, 1, :, 0:nt], rbuf[:, 1, :, 0:nt], rtmp[:, 1, :, 0:nt], ALU.add, r=[(rbuf, 1), (rtmp, 1)], w=[(rbuf, 1)])
                    yield
                    for b in range(4):
                        blk = g4 * 4 + b
                        p.V("dve", "tensor_tensor_scan", gsc[:, 0, b, 0:nt], lam[:, blk:blk + 1].to_broadcast([128, nt]), rbuf[:, 0, b, 0:nt],
                            Hre[:, blk:blk + 1], ALU.mult, ALU.add, r=[lam, (rbuf, 0), Hre], w=[(gsc, (0, b))])
                        p.V("dve", "tensor_tensor_scan", gsc[:, 1, b, 0:nt], lam[:, blk:blk + 1].to_broadcast([128, nt]), rbuf[:, 1, b, 0:nt],
                            Him[:, blk:blk + 1], ALU.mult, ALU.add, r=[lam, (rbuf, 1), Him], w=[(gsc, (1, b))])
                        yield
                    p.V("pool", "tensor_tensor", rbuf[:, 0, :, 0:nt], gsc[:, 0, :, 0:nt], cosT[:, bs, 0:nt], ALU.mult, r=[gsc, cosT], w=[(rbuf, 0)])
                    p.V("pool", "tensor_tensor", rbuf[:, 1, :, 0:nt], gsc[:, 1, :, 0:nt], sinT[:, bs, 0:nt], ALU.mult, r=[gsc, sinT], w=[(rbuf, 1)])
                    yield
                    p.V("pool", "tensor_tensor", rtmp[:, 0, :, 0:nt], gsc[:, 0, :, 0:nt], sinT[:, bs, 0:nt], ALU.mult, r=[gsc, sinT], w=[(rtmp, 0)])
                    p.V("pool", "tensor_tensor", rtmp[:, 1, :, 0:nt], gsc[:, 1, :, 0:nt], cosT[:, bs, 0:nt], ALU.mult, r=[gsc, cosT], w=[(rtmp, 1)])
                    yield
                    p.V("dve", "tensor_tensor", hbf[:, 0, bs, 0:nt], rbuf[:, 0, :, 0:nt], rbuf[:, 1, :, 0:nt], ALU.subtract, r=[rbuf], w=[(hbf, (0, g4))])
                    p.V("dve", "scalar_tensor_tensor", hbf[:, 1, bs, 0:nt], rtmp[:, 0, :, 0:nt], -1.0, rtmp[:, 1, :, 0:nt], ALU.mult, ALU.subtract,
                        r=[rtmp], w=[(hbf, (1, g4))])
                    yield
                    p.V("dve", "tensor_tensor", Hn[:, 0, bs], rbuf[:, 0, :, lc], rbuf[:, 1, :, lc], ALU.subtract, r=[rbuf], w=[(Hn, (0, g4))])
                    p.V("dve", "tensor_tensor", Hn[:, 1, bs], rtmp[:, 0, :, lc], rtmp[:, 1, :, lc], ALU.add, r=[rtmp], w=[(Hn, (1, g4))])
                    yield
                p.V("dve", "tensor_copy", Hre[:], Hn[:, 0, :], r=[Hn], w=[Hre])
                p.V("dve", "tensor_copy", Him[:], Hn[:, 1, :], r=[Hn], w=[Him])
                if tl["last"]:
                    p.DM("sp", o_s5re[sq], Hre[:], r=[Hre], w=[T_out])
                    p.DM("sp", o_s5im[sq], Him[:], r=[Him], w=[T_out])
                yield
                for ob in range(4):
                    n = 0
                    for b4 in range(4):
                        blk = ob * 4 + b4
                        for ri in range(2):
                            p.V("pe", "matmul", PS[B0][:, ob * 128:ob * 128 + nt], cstt[:, ri, blk, :], hbf[:, ri, blk, 0:nt],
                                start=(n == 0), stop=(n == 7), r=[cstt, hbf], w=[PS[B0]])
                            n += 1
                yield
                for ob in range(4):
                    p.V("dve", "scalar_tensor_tensor", yA[:, ob, 0:nt], uTf[:, ob, 0:nt], dsk[:, ob:ob + 1], PS[B0][:, ob * 128:ob * 128 + nt],
                        ALU.mult, ALU.add, r=[uTf, dsk, PS[B0]], w=[(yA, ob)])
                yield
                p.V("act", "activation", ysq[:, :, 0:nt], yA[:, :, 0:nt], AF.Square, r=[yA], w=[ysq])
                yield
                p.V("dve", "tensor_scalar", ysq[:, :, 0:nt], ysq[:, :, 0:nt], 2.0 * cg * 0.044715, 2.0 * cg, ALU.mult, ALU.add, r=[ysq], w=[ysq])
                yield
                p.V("pool", "tensor_tensor", ysq[:, :, 0:nt], ysq[:, :, 0:nt], yA[:, :, 0:nt], ALU.mult, r=[ysq, yA], w=[ysq])
                yield
                p.V("act", "activation", ysq[:, :, 0:nt], ysq[:, :, 0:nt], AF.Sigmoid, r=[ysq], w=[ysq])
                yield
                p.V("pool", "tensor_tensor", gaf[:, :, 0:nt], ysq[:, :, 0:nt], yA[:, :, 0:nt], ALU.mult, r=[ysq, yA], w=[gaf])
                yield
                p.V("act", "activation", gab[:, :, 0:nt], gaf[:, :, 0:nt], AF.Copy, r=[gaf], w=[gab])
                yield
                for ob in range(4):
                    for kc in range(4):
                        p.V("pe", "matmul", PS[B1][:, ob * 128:ob * 128 + nt], wglu[:, kc, ob * 128:(ob + 1) * 128], gab[:, kc, 0:nt],
                            start=(kc == 0), stop=(kc == 3), r=[wglu, gab], w=[PS[B1]])
                yield
                for ob in range(4):
                    p.V("act", "activation", ysq[:, ob, 0:nt], PS[B1][:, ob * 128:ob * 128 + nt], AF.Sigmoid, bias=bgl[:, ob:ob + 1],
                        r=[PS[B1], bgl], w=[ysq])
                yield
                p.V("pool", "tensor_tensor", yag[:, :, 0:nt], ysq[:, :, 0:nt], gaf[:, :, 0:nt], ALU.mult, r=[ysq, gaf], w=[yag])
                p.DM("sp", yas[ti, :, :, 0:nt], yag[:, :, 0:nt], r=[yag], w=[(T_yas, ti)])
                yield

        lists = [[], []]
        for tl in tiles:
            lists[tl["seq"] % 2].append(tl)
        run_streams([stream(0, lists[0]), stream(1, lists[1])])
        p.release(m0)

    def phaseG0():
        m0 = p.mark()
        NG = EVEN_IN - 512
        win = p.sb("win_g", [128, 8, NG], BF16)
        vsrc = w_in_even.rearrange("(kc q) n -> q kc n", q=128)
        p.DM("pool", win[:, :, 0:1024], vsrc[:, :, 512:1536], r=[DR], w=[(win, 0)])
        p.DM("pool", win[:, :, 1024:NG], vsrc[:, :, 1536:EVEN_IN], r=[DR], w=[(win, 1)])
        wout = p.sb("wout", [128, 8, D], BF16)
        load_w_bf16(wout, w_out_even, 8)
        lnw, rw, rb = load_ln_router(0)
        wc = p.sb("wc", [128, 12, 4])
        p.DM("sp", wc[:], gdn_convw, r=[DR], w=[wc])
        alog = p.sb("alog", [128, 4]); dtb = p.sb("dtb", [128, 4]); nrmw = p.sb("nrmw", [128, 128])
        bcast_load(alog, gdn_alog); bcast_load(dtb, gdn_dtb); bcast_load(nrmw, gdn_normw)
        p.V("act", "activation", alog[:], alog[:], AF.Exp, r=[alog], w=[alog])
        Sg = p.sb("Sg", [128, 4, 128])
        ctx3 = p.sb("ctx3", [128, 12, 3])
        xts = [p.sb("xt0", [128, D]), p.sb("xt1", [128, D])]
        xb = p.sb("xb", [128, D], BF16)
        xT = p.sb("xT", [128, 8, 128], BF16)
        cb = p.sb("cb", [128, 12, 131])
        cacc = p.sb("cacc", [128, 12, 128]); ctmp = p.sb("ctmp", [128, 12, 128])
        ztok = p.sb("ztok", [128, 8])
        mixT = p.sb("mixT", [128, 8, 128], BF16)
        qkn = p.sb("qkn", [128, 8, 128]); sq8 = p.sb("sq8", [128, 8, 128])
        kvtok = p.sb("kvtok", [128, 8, 128])
        gd = p.sb("gd", [128, 16]); gd2 = p.sb("gd2", [128, 4, 8])
        HT = []
        for hd in range(4):
            HT.append([p.sb("gt%d_%d" % (hd, i), [128, 128]) for i in range(17)])
        yB = p.sb("yB", [128, 512], BF16); ssq = p.sb("ssq", [128, 4])
        zs = p.sb("zs", [128, 512], BF16)
        work = alloc_tail_work()

        def load_x(tl, buf):
            if tl["nt"] < 128:
                p.V("pool", "memset", buf[:], 0.0, w=[buf])
            p.DM("sp", buf[0:tl["nt"], :], xin[tl["row0"]:tl["row0"] + tl["nt"], :], r=[DR], w=[buf])

        def head(hd, nt):
            A, B = 2 * hd, 2 * hd + 1
            gbc, dec, erow, attn, attnT, rv, rk_, nwT, ub, qdT, kd, Yt, M0, M1, T0, T1, osb = HT[hd]
            Mm = [M0, M1]; MT = [T0, T1]
            g2 = gd2[:, hd, :]
            kT = qkn[:, 4 + hd, 0:nt]
            qT = qkn[:, hd, 0:nt]
            p.V("dve", "tensor_scalar", gbc[0:nt, :], ones_f[0:nt, :], gd[0:nt, 8 + hd:9 + hd], None, ALU.mult, r=[ones_f, (gd, "g")], w=[gbc])
            yield
            p.V("pe", "matmul", PS[A][:, 0:nt], gbc[0:nt, :], uincl[0:nt, 0:nt], start=True, stop=True, r=[gbc, uincl], w=[PS[A]])
            p.V("pe", "matmul", PS[A][0:nt, 128:128 + nt], kT, kT, start=True, stop=True, r=[(qkn, "k")], w=[PS[A]])
            p.V("pe", "matmul", PS[A][0:nt, 256:256 + nt], qT, kT, start=True, stop=True, r=[(qkn, "q"), (qkn, "k")], w=[PS[A]])
            yield
            p.V("act", "activation", dec[0:nt, 0:nt], PS[A][0:nt, 0:nt], AF.Exp, bias=gd[0:nt, 12 + hd:13 + hd], scale=-1.0, r=[PS[A], (gd, "G")], w=[dec])
            yield
            p.V("act", "activation", erow[:, 0:nt], PS[A][:, 0:nt], AF.Exp, r=[PS[A]], w=[erow])
            yield
            p.V("dve", "tensor_copy", g2[:, 4:5], PS[A][:, nt - 1:nt], r=[PS[A]], w=[(gd2, (hd, "gl"))])
            yield
            p.V("dve", "scalar_tensor_tensor", dec[0:nt, 0:nt], dec[0:nt, 0:nt], 1.0, lmi[0:nt, 0:nt], ALU.min, ALU.mult, r=[dec, lmi], w=[dec])
            yield
            p.V("dve", "scalar_tensor_tensor", Mm[0][0:nt, 0:nt], PS[A][0:nt, 128:128 + nt], gd[0:nt, 4 + hd:5 + hd], dec[0:nt, 0:nt], ALU.mult, ALU.mult,
                r=[PS[A], (gd, "nb"), dec], w=[Mm[0]])
            yield
            p.V("dve", "tensor_tensor", Mm[0][0:nt, 0:nt], Mm[0][0:nt, 0:nt], lms[0:nt, 0:nt], ALU.mult, r=[Mm[0], lms], w=[Mm[0]])
            yield
            p.V("dve", "tensor_tensor", attn[0:nt, 0:nt], PS[A][0:nt, 256:256 + nt], dec[0:nt, 0:nt], ALU.mult, r=[PS[A], dec], w=[attn])
            yield
            p.V("pe", "transpose", PS[B][0:nt, 0:nt], Mm[0][0:nt, 0:nt], identf[0:nt, 0:nt], r=[Mm[0], identf], w=[PS[B]])
            p.V("pe", "transpose", PS[B][0:nt, 128:128 + nt], attn[0:nt, 0:nt], identf[0:nt, 0:nt], r=[attn, identf], w=[PS[B]])
            yield
            p.V("act", "activation", MT[0][0:nt, 0:nt], PS[B][0:nt, 0:nt], AF.Copy, r=[PS[B]], w=[MT[0]])
            yield
            p.V("dve", "tensor_tensor", Yt[0:nt, 0:nt], PS[B][0:nt, 0:nt], identf[0:nt, 0:nt], ALU.add, r=[PS[B], identf], w=[Yt])
            yield
            p.V("act", "activation", attnT[0:nt, 0:nt], PS[B][0:nt, 128:128 + nt], AF.Copy, r=[PS[B]], w=[attnT])
            yield
            nlev = 6 if nt == 128 else 3
            for lv in range(nlev):
                a, b_ = lv % 2, (lv + 1) % 2
                bank = A if lv % 2 == 0 else B
                p.V("pe", "matmul", PS[bank][0:nt, 0:nt], MT[a][0:nt, 0:nt], Mm[a][0:nt, 0:nt], start=True, stop=True, r=[MT[a], Mm[a]], w=[PS[bank]])
                if lv < nlev - 1:
                    p.V("pe", "matmul", PS[bank][0:nt, 128:128 + nt], Mm[a][0:nt, 0:nt], MT[a][0:nt, 0:nt], start=True, stop=True, r=[MT[a], Mm[a]], w=[PS[bank]])
                yield
                p.V("act", "activation", Mm[b_][0:nt, 0:nt], PS[bank][0:nt, 0:nt], AF.Copy, r=[PS[bank]], w=[Mm[b_]])
                yield
                if lv < nlev - 1:
                    p.V("dve", "tensor_copy", MT[b_][0:nt, 0:nt], PS[bank][0:nt, 128:128 + nt], r=[PS[bank]], w=[MT[b_]])
                    yield
                p.V("pe", "matmul", PS[bank][0:nt, 256:256 + nt], Mm[b_][0:nt, 0:nt], Yt[0:nt, 0:nt], start=True, stop=True, r=[Mm[b_], Yt], w=[PS[bank]])
                yield
                p.V("dve", "tensor_tensor", Yt[0:nt, 0:nt], Yt[0:nt, 0:nt], PS[bank][0:nt, 256:256 + nt], ALU.add, r=[Yt, PS[bank]], w=[Yt])
                yield
            p.V("dve", "tensor_scalar", rv[0:nt, :], kvtok[0:nt, 4 + hd, :], gd[0:nt, hd:hd + 1], None, ALU.mult, r=[(kvtok, "v"), (gd, "b")], w=[rv])
            p.V("pool", "tensor_scalar", rk_[0:nt, :], kvtok[0:nt, hd, :], gd[0:nt, 16 + hd:17 + hd] if False else g2[0:nt, 0:1], None, ALU.mult, r=[(kvtok, "k"), (gd2, (hd, "be"))], w=[rk_])
            yield
            p.V("pe", "matmul", PS[A][:, 0:nt], rk_[0:nt, :], Yt[0:nt, 0:nt], start=True, stop=True, r=[rk_, Yt], w=[PS[A]])
            yield
            p.V("dve", "tensor_scalar", nwT[:, 0:nt], PS[A][:, 0:nt], -1.0, None, ALU.mult, r=[PS[A]], w=[nwT])
            yield
            p.V("pe", "matmul", PS[B][0:nt, 0:128], Yt[0:nt, 0:nt], rv[0:nt, :], start=True, stop=False, r=[Yt, rv], w=[PS[B]])
            p.V("pe", "matmul", PS[B][0:nt, 0:128], nwT[:, 0:nt], Sg[:, hd, :], start=False, stop=True, r=[nwT, (Sg, hd)], w=[PS[B]])
            yield
            p.V("act", "activation", ub[0:nt, :], PS[B][0:nt, 0:128], AF.Copy, r=[PS[B]], w=[ub])
            yield
            p.V("pool", "tensor_tensor", qdT[:, 0:nt], qT, erow[:, 0:nt], ALU.mult, r=[(qkn, "q"), erow], w=[qdT])
            yield
            p.V("pe", "matmul", PS[A][0:nt, 128:256], qdT[:, 0:nt], Sg[:, hd, :], start=True, stop=False, r=[qdT, (Sg, hd)], w=[PS[A]])
            p.V("pe", "matmul", PS[A][0:nt, 128:256], attnT[0:nt, 0:nt], ub[0:nt, :], start=False, stop=True, r=[attnT, ub], w=[PS[A]])
            yield
            p.V("act", "activation", g2[0:nt, 1:2], gd[0:nt, 12 + hd:13 + hd], AF.Exp, bias=g2[0:nt, 4:5], scale=-1.0,
                r=[(gd, "G"), (gd2, (hd, "gl"))], w=[(gd2, (hd, "kd"))])
            yield
            p.V("dve", "tensor_scalar", kd[0:nt, :], kvtok[0:nt, hd, :], g2[0:nt, 1:2], None, ALU.mult, r=[(kvtok, "k"), (gd2, (hd, "kd"))], w=[kd])
            yield
            p.V("pe", "matmul", PS[B][:, 128:256], kd[0:nt, :], ub[0:nt, :], start=True, stop=True, r=[kd, ub], w=[PS[B]])
            yield
            p.V("act", "activation", g2[:, 5:6], g2[:, 4:5], AF.Exp, r=[(gd2, (hd, "gl"))], w=[(gd2, (hd, "egl"))])
            yield
            p.V("dve", "scalar_tensor_tensor", Sg[:, hd, :], Sg[:, hd, :], g2[:, 5:6], PS[B][:, 128:256], ALU.mult, ALU.add,
                r=[(Sg, hd), (gd2, (hd, "egl")), PS[B]], w=[(Sg, hd)])
            yield
            p.V("act", "activation", osb[0:nt, :], PS[A][0:nt, 128:256], AF.Square, r=[PS[A]], w=[osb])
            yield
            p.V("dve", "reduce_sum", ssq[0:nt, hd:hd + 1], osb[0:nt, :], mybir.AxisListType.X, r=[osb], w=[(ssq, hd)])
            yield
            p.V("act", "activation", ssq[0:nt, hd:hd + 1], ssq[0:nt, hd:hd + 1], AF.Sqrt, bias=epsln[0:nt, 1:2], scale=1.0 / 128.0, r=[(ssq, hd), epsln], w=[(ssq, hd)])
            yield
            p.V("dve", "reciprocal", ssq[0:nt, hd:hd + 1], ssq[0:nt, hd:hd + 1], r=[(ssq, hd)], w=[(ssq, hd)])
            yield
            p.V("dve", "scalar_tensor_tensor", osb[0:nt, :], PS[A][0:nt, 128:256], ssq[0:nt, hd:hd + 1], nrmw[0:nt, :], ALU.mult, ALU.mult,
                r=[PS[A], (ssq, hd), nrmw], w=[osb])
            yield
            p.V("pool", "tensor_tensor", yB[0:nt, hd * 128:(hd + 1) * 128], osb[0:nt, :], zs[0:nt, hd * 128:(hd + 1) * 128], ALU.mult, r=[osb, zs], w=[(yB, hd)])
            yield

        load_x(tiles[0], xts[0])
        for tl in tiles:
            nt, ti, sq = tl["nt"], tl["ti"], tl["seq"]
            xt = xts[ti % 2]
            if ti + 1 < NT:
                load_x(tiles[ti + 1], xts[(ti + 1) % 2])
            if tl["first"]:
                if sq < NPS:
                    p.V("pool", "memset", Sg[:], 0.0, w=[Sg]); p.V("pool", "memset", ctx3[:], 0.0, w=[ctx3])
                else:
                    p.DM("sp", Sg[:], st_gdn.rearrange("h k v -> k h v"), r=[DR], w=[Sg])
                    p.DM("sp", ctx3[:], st_conv, r=[DR], w=[ctx3])
            p.DM("sp", mixT[:, 0:4, 0:nt], yas[ti, :, :, 0:nt], r=[(T_yas, ti)], w=[(mixT, "a")])
            p.V("act", "activation", xb[0:nt, :], xt[0:nt, :], AF.Copy, r=[xt], w=[xb])
            transpose_to(xT, xb, nt, 8, 0)
            p.V("pool", "tensor_copy", cb[:, :, 0:3], ctx3[:, :, :], r=[ctx3], w=[(cb, "c")])
            for g4 in range(3):
                bank = 1 + g4
                for b in range(4):
                    blk = g4 * 4 + b
                    for kc in range(8):
                        p.V("pe", "matmul", PS[bank][:, b * 128:b * 128 + nt], win[:, kc, blk * 128:(blk + 1) * 128],
                            xT[:, kc, 0:nt], start=(kc == 0), stop=(kc == 7), r=[win, xT], w=[PS[bank]])
                evac(cb[:, g4 * 4:g4 * 4 + 4, 3:3 + nt], PS[bank][:, :].rearrange("p (a b) -> p a b", a=4)[:, :, 0:nt],
                     r=[PS[bank]], w=[(cb, g4)])
            for kc in range(8):
                p.V("pe", "matmul", PS[4][0:nt, 0:512], xT[:, kc, 0:nt], win[:, kc, 1536:2048], start=(kc == 0), stop=(kc == 7),
                    r=[xT, win], w=[PS[4]])
            for kc in range(8):
                p.V("pe", "matmul", PS[5][0:nt, 0:8], xT[:, kc, 0:nt], win[:, kc, 2048:2056], start=(kc == 0), stop=(kc == 7),
                    r=[xT, win], w=[PS[5]])
            p.V("act", "activation", zs[0:nt, :], PS[4][0:nt, 0:512], AF.Silu, r=[PS[4]], w=[zs])
            p.V("dve", "tensor_copy", ztok[0:nt, 0:8], PS[5][0:nt, 0:8], r=[PS[5]], w=[ztok])
            for j in range(4):
                wj = wc[:, :, j:j + 1].to_broadcast([128, 12, nt])
                if j == 0:
                    p.V("dve", "tensor_tensor", cacc[:, :, 0:nt], cb[:, :, 0:nt], wj, ALU.mult, r=[cb, wc], w=[cacc])
                else:
                    p.V("pool", "tensor_tensor", ctmp[:, :, 0:nt], cb[:, :, j:j + nt], wj, ALU.mult, r=[cb, wc], w=[ctmp])
                    p.V("dve", "tensor_tensor", cacc[:, :, 0:nt], cacc[:, :, 0:nt], ctmp[:, :, 0:nt], ALU.add, r=[cacc, ctmp], w=[cacc])
            p.V("pool", "tensor_copy", ctx3[:, :, :], cb[:, :, nt:nt + 3], r=[cb], w=[ctx3])
            if tl["last"]:
                p.DM("sp", o_conv[sq], ctx3[:], r=[ctx3], w=[T_out])
            p.V("act", "activation", cacc[:, :, 0:nt], cacc[:, :, 0:nt], AF.Silu, r=[cacc], w=[cacc])
            p.V("act", "activation", sq8[:, :, 0:nt], cacc[:, 0:8, 0:nt], AF.Square, r=[cacc], w=[sq8])
            for hb in range(2):
                for b in range(4):
                    p.V("pe", "matmul", PS[2 + hb][:, b * 128:b * 128 + nt], ones_f[:, :], sq8[:, hb * 4 + b, 0:nt], start=True, stop=True,
                        r=[ones_f, sq8], w=[PS[2 + hb]])
            for hb in range(2):
                v = PS[2 + hb][:, :].rearrange("p (a b) -> p a b", a=4)[:, :, 0:nt]
                p.V("act", "activation", sq8[:, hb * 4:hb * 4 + 4, 0:nt], v, AF.Sqrt, bias=epsln[:, 1:2], r=[PS[2 + hb], epsln], w=[sq8])
            p.V("dve", "reciprocal", sq8[:, :, 0:nt], sq8[:, :, 0:nt], r=[sq8], w=[sq8])
            p.V("dve", "scalar_tensor_tensor", qkn[:, 0:4, 0:nt], cacc[:, 0:4, 0:nt], 128.0 ** -0.5, sq8[:, 0:4, 0:nt], ALU.mult, ALU.mult,
                r=[cacc, sq8], w=[(qkn, "q")])
            p.V("pool", "tensor_tensor", qkn[:, 4:8, 0:nt], cacc[:, 4:8, 0:nt], sq8[:, 4:8, 0:nt], ALU.mult, r=[cacc, sq8], w=[(qkn, "k")])
            for b in range(4):
                p.V("pe", "transpose", PS[2][0:nt, b * 128:(b + 1) * 128], qkn[:, 4 + b, 0:nt], identf[:, :], r=[(qkn, "k"), identf], w=[PS[2]])
                p.V("pe", "transpose", PS[3][0:nt, b * 128:(b + 1) * 128], cacc[:, 8 + b, 0:nt], identf[:, :], r=[cacc, identf], w=[PS[3]])
            evac(kvtok[0:nt, 0:4, :], PS[2][0:nt, :].rearrange("p (a b) -> p a b", a=4), r=[PS[2]], w=[(kvtok, "k")])
            evac(kvtok[0:nt, 4:8, :], PS[3][0:nt, :].rearrange("p (a b) -> p a b", a=4), r=[PS[3]], w=[(kvtok, "v")])
            p.V("act", "activation", gd[0:nt, 0:4], ztok[0:nt, 0:4], AF.Sigmoid, r=[ztok], w=[(gd, "b")])
            p.V("dve", "tensor_scalar", gd[0:nt, 4:8], gd[0:nt, 0:4], -1.0, None, ALU.mult, r=[(gd, "b")], w=[(gd, "nb")])
            p.V("dve", "tensor_tensor", gd[0:nt, 8:12], ztok[0:nt, 4:8], dtb[0:nt, :], ALU.add, r=[ztok, dtb], w=[(gd, "g")])
            p.V("act", "activation", gd[0:nt, 8:12], gd[0:nt, 8:12], AF.Exp, r=[(gd, "g")], w=[(gd, "g")])
            p.V("act", "activation", gd[0:nt, 8:12], gd[0:nt, 8:12], AF.Ln, bias=1.0, r=[(gd, "g")], w=[(gd, "g")])
            p.V("dve", "scalar_tensor_tensor", gd[0:nt, 8:12], gd[0:nt, 8:12], -1.0, alog[0:nt, :], ALU.mult, ALU.mult, r=[(gd, "g"), alog], w=[(gd, "g")])
            p.V("pe", "matmul", PS[0][0:nt, 0:4], uincl[0:nt, 0:nt], gd[0:nt, 8:12], start=True, stop=True, r=[uincl, (gd, "g")], w=[PS[0]])
            p.V("dve", "tensor_copy", gd[0:nt, 12:16], PS[0][0:nt, 0:4], r=[PS[0]], w=[(gd, "G")])
            p.V("act", "activation", gd2[0:nt, :, 2], gd[0:nt, 12:16], AF.Exp, r=[(gd, "G")], w=[(gd2, "e")])
            p.V("dve", "tensor_tensor", gd2[0:nt, :, 0], gd2[0:nt, :, 2], gd[0:nt, 0:4], ALU.mult, r=[(gd2, "e"), (gd, "b")],
                w=[(gd2, (0, "be")), (gd2, (1, "be")), (gd2, (2, "be")), (gd2, (3, "be"))])
            run_streams([head(hd, nt) for hd in range(4)])
            if tl["last"]:
                p.DM("sp", o_gdn[sq].rearrange("h k v -> k h v"), Sg[:], r=[Sg], w=[T_out])
            for b in range(4):
                p.V("pe", "transpose", psbf(2, 1024)[:, b * 128:b * 128 + nt], yB[0:nt, b * 128:(b + 1) * 128], identb[0:nt, 0:nt], r=[yB, identb], w=[PS[2]])
            evac(mixT[:, 4:8, 0:nt], psbf(2, 1024).rearrange("p (a b) -> p a b", a=8)[:, 0:4, 0:nt], r=[PS[2]], w=[(mixT, "b")])
            for half in range(2):
                for kc in range(8):
                    p.V("pe", "matmul", PS[2 + half][0:nt, :], mixT[:, kc, 0:nt], wout[:, kc, half * 512:(half + 1) * 512], start=(kc == 0), stop=(kc == 7),
                        r=[mixT, wout], w=[PS[2 + half]])
            phaseA_tail(0, tl, xt, (2, 3), lnw, rw, rb, work)
        p.release(m0)

    def phaseA1():
        m0 = p.mark()
        win = p.sb("win1", [128, 8, ODD_IN], BF16)
        load_w_bf16(win, w_in_odd, 8)
        wout = p.sb("wout1", [128, 16, D], BF16)
        load_w_bf16(wout, w_out_odd, 16)
        lnw, rw, rb = load_ln_router(1)
        retmask = p.sb("retmask", [128, 4, 128]); retqs = p.sb("retqs", [128, 4, 128]); retks = p.sb("retks", [128, 8])
        for b, nm in ((retmask, "retmask"), (retqs, "retqs"), (retks, "retks")):
            p.DM("sp", b[:], cst[nm], r=[DR], w=[b])
        Rf = p.sb("Rf", [128, 4, 2, 512])
        xt_one = p.sb("xt0", [128, D])
        xts = [xt_one, xt_one]
        xb = p.sb("xb", [128, D], BF16)
        xT = p.sb("xT", [128, 8, 128], BF16)
        cs = p.sb("cs", [128, 2, 128])
        qkT = p.sb("qkT", [128, 16, 128], BF16)
        qsT = p.sb("qsT", [128, 2, 128])
        rt1 = p.sb("rt1", [128, 128]); rt2 = p.sb("rt2", [128, 128])
        ktok = p.sb("ktok", [128, 8, 128], BF16)
        vtok = p.sb("vtok", [128, 2048], BF16); gsil = p.sb("gsil", [128, 2048], BF16)
        sT = p.sb("sT", [128, 128], BF16)
        og = vtok
        ogT = p.sb("ogT", [128, 16, 128], BF16)
        onrm = p.sb("onrm", [128, 512]); st6 = p.sb("st6", [128, 6]); st2 = p.sb("st2", [128, 4])
        work = alloc_tail_work(h=xt_one)

        def load_x(tl, buf):
            if tl["nt"] < 128:
                p.V("pool", "memset", buf[:], 0.0, w=[buf])
            p.DM("sp", buf[0:tl["nt"], :], x3s[tl["ti"] * 128:tl["ti"] * 128 + tl["nt"], :], r=[(T_x3s, tl["ti"])], w=[buf])

        for tl in tiles:
            nt, ti, sq = tl["nt"], tl["ti"], tl["seq"]
            Lc = 0 if nt == 128 else 1
            xt = xts[ti % 2]
            load_x(tl, xt)
            if tl["first"]:
                if sq < NPS:
                    p.V("pool", "memset", Rf[:], 0.0, w=[Rf])
                else:
                    for h in range(4):
                        for par in range(2):
                            p.DM("sp", Rf[:, h, par, :], st_ret[h].rearrange("(q two) v -> q two v", two=2)[:, par, :], r=[DR], w=[(Rf, (h, par))])
            p.DM("sp", cs[:, 0, 0:nt], cst["rcos"][:, tl["pos0"]:tl["pos0"] + nt], r=[DR], w=[(cs, 0)])
            p.DM("sp", cs[:, 1, 0:nt], cst["rsin"][:, tl["pos0"]:tl["pos0"] + nt], r=[DR], w=[(cs, 1)])
            p.V("act", "activation", xb[0:nt, :], xt[0:nt, :], AF.Copy, r=[xt], w=[xb])
            transpose_to(xT, xb, nt, 8, 0)
            for qk in range(2):
                for h in range(4):
                    bank = 1 + ((qk * 4 + h) % 2)
                    for par in range(2):
                        col0 = qk * 1024 + h * 256 + par * 128
                        for kc in range(8):
                            p.V("pe", "matmul", PS[bank][:, par * 128:par * 128 + nt], win[:, kc, col0:col0 + 128], xT[:, kc, 0:nt],
                                start=(kc == 0), stop=(kc == 7), r=[win, xT], w=[PS[bank]])
                    x0 = PS[bank][:, 0:nt]
                    x1_ = PS[bank][:, 128:128 + nt]
                    blk = qk * 8 + h * 2
                    p.V("dve", "tensor_tensor", rt1[:, 0:nt], x0, cs[:, 0, 0:nt], ALU.mult, r=[PS[bank], cs], w=[rt1])
                    p.V("dve", "tensor_tensor", rt2[:, 0:nt], x1_, cs[:, 1, 0:nt], ALU.mult, r=[PS[bank], cs], w=[rt2])
                    p.V("pool", "tensor_tensor", qkT[:, blk, 0:nt], rt1[:, 0:nt], rt2[:, 0:nt], ALU.subtract, r=[rt1, rt2], w=[(qkT, blk)])
                    p.V("dve", "tensor_tensor", rt1[:, 0:nt], x0, cs[:, 1, 0:nt], ALU.mult, r=[PS[bank], cs, (qkT, blk)], w=[rt1])
                    p.V("dve", "tensor_tensor", rt2[:, 0:nt], x1_, cs[:, 0, 0:nt], ALU.mult, r=[PS[bank], cs, (qkT, blk)], w=[rt2])
                    p.V("pool", "tensor_tensor", qkT[:, blk + 1, 0:nt], rt1[:, 0:nt], rt2[:, 0:nt], ALU.add, r=[rt1, rt2], w=[(qkT, blk + 1)])
            for cg in range(8):
                bank = 3 + (cg % 2)
                for kc in range(8):
                    p.V("pe", "matmul", PS[bank][0:nt, :], xT[:, kc, 0:nt], win[:, kc, 2048 + cg * 512:2048 + (cg + 1) * 512], start=(kc == 0), stop=(kc == 7),
                        r=[xT, win], w=[PS[bank]])
                if cg < 4:
                    p.V("dve", "tensor_copy", vtok[0:nt, cg * 512:(cg + 1) * 512], PS[bank][0:nt, :], r=[PS[bank]], w=[(vtok, cg)])
                else:
                    p.V("act", "activation", gsil[0:nt, (cg - 4) * 512:(cg - 3) * 512], PS[bank][0:nt, :], AF.Silu, r=[PS[bank]], w=[(gsil, cg - 4)])
            for b in range(8):
                p.V("pe", "transpose", psbf(5, 1024)[0:nt, b * 128:(b + 1) * 128], qkT[:, 8 + b, 0:nt], identb[:, :], r=[(qkT, 8 + b), identb], w=[PS[5]])
            for h in range(4):
                p.V("dve", "tensor_scalar", ktok[0:nt, 2 * h:2 * h + 2, :], psbf(5, 1024).rearrange("p (a b) -> p a b", a=8)[0:nt, 2 * h:2 * h + 2, :],
                    retks[0:nt, Lc * 4 + h:Lc * 4 + h + 1], None, ALU.mult, r=[PS[5], retks], w=[(ktok, h)])
            for h in range(4):
                for dc in range(2):
                    p.V("pe", "matmul", PS[6][0:nt, 0:nt], qkT[:, 8 + 2 * h + dc, 0:nt], qkT[:, 2 * h + dc, 0:nt], start=(dc == 0), stop=(dc == 1),
                        r=[(qkT, 8 + 2 * h + dc), (qkT, 2 * h + dc)], w=[PS[6]])
                p.V("dve", "scalar_tensor_tensor", sT[0:nt, 0:nt], PS[6][0:nt, 0:nt], 256.0 ** -0.5, retmask[0:nt, h, 0:nt], ALU.mult, ALU.mult,
                    r=[PS[6], retmask], w=[sT])
                for dc in range(2):
                    p.V("pool", "tensor_tensor", qsT[:, dc, 0:nt], qkT[:, 2 * h + dc, 0:nt], retqs[:, h, 0:nt], ALU.mult, r=[(qkT, 2 * h + dc), retqs], w=[(qsT, dc)])
                p.V("pe", "matmul", PS[7][0:nt, :], sT[0:nt, 0:nt], vtok[0:nt, h * 512:(h + 1) * 512], start=True, stop=False, r=[sT, (vtok, h)], w=[PS[7]])
                for dc in range(2):
                    p.V("pe", "matmul", PS[7][0:nt, :], qsT[:, dc, 0:nt], Rf[:, h, dc, :], start=False, stop=(dc == 1), r=[(qsT, dc), Rf], w=[PS[7]])
                cdec = cst_host["retcdec"][Lc][h]
                for dc in range(2):
                    bank = 1 + dc
                    p.V("pe", "matmul", PS[bank][:, :], ktok[0:nt, 2 * h + dc, :], vtok[0:nt, h * 512:(h + 1) * 512], start=True, stop=True,
                        r=[(ktok, h), (vtok, h)], w=[PS[bank]])
                    p.V("dve", "scalar_tensor_tensor", Rf[:, h, dc, :], Rf[:, h, dc, :], cdec, PS[bank][:, :], ALU.mult, ALU.add, r=[Rf, PS[bank]], w=[Rf])
                p.V("dve", "bn_stats", st6[0:nt, :], PS[7][0:nt, :], r=[PS[7]], w=[st6])
                p.V("dve", "bn_aggr", st2[0:nt, 0:2], st6[0:nt, :], r=[st6], w=[st2])
                p.V("act", "activation", st2[0:nt, 2:3], st2[0:nt, 1:2], AF.Sqrt, bias=epsln[0:nt, 0:1], r=[st2, epsln], w=[(st2, "s")])
                p.V("dve", "reciprocal", st2[0:nt, 3:4], st2[0:nt, 2:3], r=[(st2, "s")], w=[(st2, "r")])
                p.V("dve", "tensor_scalar", onrm[0:nt, :], PS[7][0:nt, :], st2[0:nt, 0:1], st2[0:nt, 3:4], ALU.subtract, ALU.mult, r=[PS[7], st2, (st2, "r")], w=[onrm])
                p.V("pool", "tensor_tensor", og[0:nt, h * 512:(h + 1) * 512], onrm[0:nt, :], gsil[0:nt, h * 512:(h + 1) * 512], ALU.mult, r=[onrm, (gsil, h)], w=[(vtok, h)])
            if tl["last"]:
                for h in range(4):
                    for par in range(2):
                        p.DM("sp", o_ret[sq, h].rearrange("(q two) v -> q two v", two=2)[:, par, :], Rf[:, h, par, :], r=[Rf], w=[T_out])
            transpose_to(ogT, og, nt, 16, 5)
            for half in range(2):
                for kc in range(16):
                    p.V("pe", "matmul", PS[2 + half][0:nt, :], ogT[:, kc, 0:nt], wout[:, kc, half * 512:(half + 1) * 512], start=(kc == 0), stop=(kc == 15),
                        r=[ogT, wout], w=[PS[2 + half]])
            phaseA_tail(1, tl, xt, (2, 3), lnw, rw, rb, work)
        p.release(m0)

    def phaseM(li):
        m0 = p.mark()
        w1b = [p.sb("w1b0", [128, 8, 2 * D], BF16), p.sb("w1b1", [128, 8, 2 * D], BF16)]
        w2b = [p.sb("w2b0", [128, 8, D], BF16), p.sb("w2b1", [128, 8, D], BF16)]
        b1t = [p.sb("b1t0", [128, 16]), p.sb("b1t1", [128, 16])]
        b2f = [p.sb("b2f0", [1, D]), p.sb("b2f1", [1, D])]
        b2b = [p.sb("b2b0", [1, D], BF16), p.sb("b2b1", [1, D], BF16)]
        xg = [p.sb("xg0", [128, CT, D], BF16), p.sb("xg1", [128, CT, D], BF16)]
        xgT = [p.sb("xgT0", [128, 8, C], BF16), p.sb("xgT1", [128, 8, C], BF16)]
        glu = p.sb("glu", [128, C]); sig = p.sb("sig", [128, C]); lin = p.sb("lin", [128, C])
        actT = p.sb("actT", [128, 8, C], BF16)
        yo = [p.sb("yo0", [128, D]), p.sb("yo1", [128, D])]

        def load_w(e):
            s = e % 2
            load_w_bf16(w1b[s], moe_w1[li, e], 8)
            load_w_bf16(w2b[s], moe_w2[li, e], 8)
            p.DM("sp", b1t[s][:], moe_b1[li, e], r=[DR], w=[b1t[s]])
            p.DM("sp", b2f[s][:], moe_b2[li, e:e + 1, :], r=[DR], w=[b2f[s]])

        def load_xg(e):
            s = e % 2
            p.DM("sp", xg[s][:], xs[e * C:(e + 1) * C, :].rearrange("(ct q) d -> q ct d", q=128), r=[(T_xs, "*")], w=[xg[s], (T_xs, e)])

        def transposes(e):
            s = e % 2
            p.V("act", "activation", b2b[s][:], b2f[s][:], AF.Copy, r=[b2f[s]], w=[b2b[s]])
            for ct in range(CT):
                bank = 4 + (ct % 2)
                for kc in range(8):
                    p.V("pe", "transpose", psbf(bank, 1024)[:, kc * 128:(kc + 1) * 128], xg[s][:, ct, kc * 128:(kc + 1) * 128], identb[:, :],
                        r=[xg[s], identb], w=[PS[bank]])
                evac(xgT[s][:, :, ct * 128:(ct + 1) * 128], psbf(bank, 1024).rearrange("p (a b) -> p a b", a=8), r=[PS[bank]], w=[(xgT[s], ct)])

        load_w(0)
        load_xg(0)
        transposes(0)
        for e in range(NE):
            s = e % 2
            if e + 1 < NE:
                load_w(e + 1)
                load_xg(e + 1)
            for i in range(8):
                for part in range(2):
                    fc = i + part * 8
                    banks = [(0, 1), (2, 3)][(i * 2 + part) % 2]
                    for gi, (ca, cb_) in enumerate(cgs):
                        for kc in range(8):
                            p.V("pe", "matmul", PS[banks[gi]][:, 0:cb_ - ca], w1b[s][:, kc, fc * 128:(fc + 1) * 128], xgT[s][:, kc, ca:cb_],
                                start=(kc == 0), stop=(kc == 7), r=[w1b[s], xgT[s]], w=[PS[banks[gi]]])
                    for gi, (ca, cb_) in enumerate(cgs):
                        src = PS[banks[gi]][:, 0:cb_ - ca]
                        if part == 0:
                            p.V("dve", "tensor_scalar", glu[:, ca:cb_], src, b1t[s][:, fc:fc + 1], 7.0, ALU.add, ALU.min, r=[PS[banks[gi]], b1t[s]], w=[(glu, gi)])
                        else:
                            p.V("dve", "tensor_scalar", lin[:, ca:cb_], src, b1t[s][:, fc:fc + 1], 7.0, ALU.add, ALU.min, r=[PS[banks[gi]], b1t[s]], w=[(lin, gi)])
                    if part == 0:
                        p.V("act", "activation", sig[:, :], glu[:, :], AF.Sigmoid, scale=1.702, r=[glu], w=[sig])
                        p.V("pool", "tensor_tensor", glu[:, :], glu[:, :], sig[:, :], ALU.mult, r=[glu, sig], w=[glu])
                    else:
                        p.V("dve", "tensor_scalar", lin[:, :], lin[:, :], -7.0, 1.0, ALU.max, ALU.add, r=[lin], w=[lin])
                        p.V("pool", "tensor_tensor", actT[:, i, :], glu[:, :], lin[:, :], ALU.mult, r=[glu, lin], w=[(actT, i)])
            if e + 1 < NE:
                transposes(e + 1)
            for ct in range(CT):
                yb = yo[ct % 2]
                for half in range(2):
                    bank = 4 + (ct % 2) * 2 + half
                    for fc in range(8):
                        p.V("pe", "matmul", PS[bank][:, :], actT[:, fc, ct * 128:(ct + 1) * 128], w2b[s][:, fc, half * 512:(half + 1) * 512],
                            start=(fc == 0), stop=False, r=[actT, w2b[s]], w=[PS[bank]])
                    p.V("pe", "matmul", PS[bank][:, :], ones_b[0:1, :], b2b[s][0:1, half * 512:(half + 1) * 512], start=False, stop=True,
                        r=[ones_b, b2b[s]], w=[PS[bank]])
                    evac(yb[:, half * 512:(half + 1) * 512], PS[bank][:, :], r=[PS[bank]], w=[(yb, half)])
                p.DM("act", ys[e * C + ct * 128:e * C + (ct + 1) * 128, :], yb[:, :], r=[yb], w=[(T_ys, (e, ct))])
        p.release(m0)

    def phaseC(li):
        m0 = p.mark()
        wg = p.sb("wg", [128, 8, D], BF16)
        load_w_bf16(wg, ple_gw[li], 8)
        wp = p.sb("wp", [128, 2, D], BF16)
        load_w_bf16(wp, ple_w[li], 2)
        g2 = p.sb("ln2g", [128, D]); b2 = p.sb("ln2b", [128, D])
        bcast_load(g2, ln2_g[li:li + 1, :]); bcast_load(b2, ln2_b[li:li + 1, :])
        rows = [[p.sb(f"row{s}{k}", [128, D]) for k in range(4)] for s in range(2)]
        x1t = [p.sb("x1t0", [128, D]), p.sb("x1t1", [128, D])]
        pt = [p.sb("pt0", [128, 256]), p.sb("pt1", [128, 256])]
        ff = p.sb("ff", [128, D]); x2 = p.sb("x2", [128, D]); x2b = p.sb("x2b", [128, D], BF16)
        x2T = p.sb("x2T", [128, 8, 128], BF16)
        pb = p.sb("pb", [128, 256], BF16); pT = p.sb("pT", [128, 2, 128], BF16)
        gt = p.sb("gt", [128, D]); x3 = p.sb("x3", [128, D])
        scr6 = p.sb("scr6", [128, 2, 6]); scr2 = p.sb("scr2", [128, 4])

        def loads(tl):
            s = tl["ti"] % 2
            ti, nt = tl["ti"], tl["nt"]
            for k in range(4):
                p.dma("pool", lambda e, k=k, ti=ti, s=s: e.indirect_dma_start(
                    out=rows[s][k][:, :], out_offset=None, in_=ys[:, :],
                    in_offset=bass.IndirectOffsetOnAxis(ap=slots_all[:, ti, k:k + 1], axis=0)),
                    r=[(T_ys, "*"), (slots_all, ti)], w=[rows[s][k]])
            p.DM("sp", x1t[s][0:nt, :], x1s[ti * 128:ti * 128 + nt, :], r=[(T_x1s, ti)], w=[x1t[s]])
            p.DM("sp", pt[s][0:nt, :], pin[li, tl["row0"]:tl["row0"] + nt, :], r=[DR], w=[pt[s]])

        loads(tiles[0])
        for tl in tiles:
            nt, ti = tl["nt"], tl["ti"]
            s = ti % 2
            if ti + 1 < NT:
                loads(tiles[ti + 1])
            p.V("dve", "tensor_scalar", ff[0:nt, :], rows[s][0][0:nt, :], gates_all[0:nt, ti, 0:1], None, ALU.mult, r=[rows[s][0], (gates_all, ti)], w=[ff])
            for k in range(1, 4):
                eng = "dve"
                p.V(eng, "scalar_tensor_tensor", ff[0:nt, :], rows[s][k][0:nt, :], gates_all[0:nt, ti, k:k + 1], ff[0:nt, :], ALU.mult, ALU.add,
                    r=[rows[s][k], (gates_all, ti), ff], w=[ff])
            p.V("dve", "scalar_tensor_tensor", ff[0:nt, :], x1t[s][0:nt, :], ALPHA, ff[0:nt, :], ALU.mult, ALU.add, r=[x1t[s], ff], w=[ff])
            layernorm("dve", ff, nt, g2, b2, x2, scr6, scr2)
            p.V("act", "activation", x2b[0:nt, :], x2[0:nt, :], AF.Copy, r=[x2], w=[x2b])
            transpose_to(x2T, x2b, nt, 8, 0)
            p.V("act", "activation", pb[0:nt, :], pt[s][0:nt, :], AF.Copy, r=[pt[s]], w=[pb])
            transpose_to(pT, pb, nt, 2, 1)
            for half in range(2):
                for kc in range(8):
                    p.V("pe", "matmul", PS[2 + half][0:nt, :], x2T[:, kc, 0:nt], wg[:, kc, half * 512:(half + 1) * 512], start=(kc == 0), stop=(kc == 7),
                        r=[x2T, wg], w=[PS[2 + half]])
                for kc in range(2):
                    p.V("pe", "matmul", PS[4 + half][0:nt, :], pT[:, kc, 0:nt], wp[:, kc, half * 512:(half + 1) * 512], start=(kc == 0), stop=(kc == 1),
                        r=[pT, wp], w=[PS[4 + half]])
                hs = slice(half * 512, (half + 1) * 512)
                p.V("act", "activation", gt[0:nt, hs], PS[2 + half][0:nt, :], AF.Sigmoid, r=[PS[2 + half]], w=[(gt, half)])
                p.V("dve", "tensor_tensor", gt[0:nt, hs], gt[0:nt, hs], PS[4 + half][0:nt, :], ALU.mult, r=[(gt, half), PS[4 + half]], w=[(gt, half)])
                p.V("pool", "tensor_tensor", x3[0:nt, hs], gt[0:nt, hs], x2[0:nt, hs], ALU.add, r=[(gt, half), x2], w=[(x3, half)])
            if li == 0:
                p.DM("sp", x3s[ti * 128:ti * 128 + nt, :], x3[0:nt, :], r=[x3], w=[(T_x3s, ti)])
                if "x3_0" in dbg_out:
                    p.DM("sp", dbg_out["x3_0"][tl["row0"]:tl["row0"] + nt, :], x3[0:nt, :], r=[x3], w=[T_out])
            else:
                p.DM("sp", y_out[tl["row0"]:tl["row0"] + nt, :], x3[0:nt, :], r=[x3], w=[T_out])
        p.release(m0)

    cst_host = host_consts()
    for st in stages:
        if st == "A0":
            if os.environ.get("KOLDA0"):
                phaseA0()
            else:
                phaseS0()
                phaseG0()
        elif st == "A1":
            phaseA1()
        elif st[0] == "M":
            phaseM(int(st[1]))
        elif st[0] == "C":
            phaseC(int(st[1]))
    p.finalize()
    return nc


def prep_shared(inp):
    f = lambda a: np.ascontiguousarray(np.asarray(a, dtype=np.float32))
    sh = {}
    sh["w_in_even"] = f(inp["w_in_even"][0])
    qb = lambda v, nb: np.ascontiguousarray(np.asarray(v, dtype=np.float32).reshape(nb, 128).T)
    sh["s5_are"] = qb(inp["s5_a_re"][0].reshape(-1), 16)
    sh["s5_aim"] = qb(inp["s5_a_im"][0].reshape(-1), 16)
    sh["s5_ldt"] = qb(np.repeat(np.asarray(inp["s5_log_dt"][0]), 64), 16)
    bst = np.zeros((2, 16, 128, 128), np.float32)
    cstt = np.zeros((2, 16, 128, 128), np.float32)
    for ri, (bsrc, csrc) in enumerate(((inp["s5_b_re"][0], inp["s5_c_re"][0]), (inp["s5_b_im"][0], inp["s5_c_im"][0]))):
        bsrc = np.asarray(bsrc)
        csrc = np.asarray(csrc)
        for g in range(32):
            blk = g // 2
            m0 = (g % 2) * 64
            k0 = (g % 8) * 16
            bst[ri, blk, k0:k0 + 16, m0:m0 + 64] = bsrc[g].T
            cstt[ri, blk, m0:m0 + 64, k0:k0 + 16] = csrc[g].T
    sh["s5_bst"] = bst
    sh["s5_cst"] = cstt
    sh["s5_d"] = qb(inp["s5_d"][0], 4)
    sh["s5_wglu"] = f(inp["s5_w_glu"][0])
    sh["s5_bglu"] = qb(inp["s5_b_glu"][0], 4)
    sh["gdn_convw"] = np.ascontiguousarray(np.asarray(inp["gdn_conv_w"][0], dtype=np.float32).reshape(4, 12, 128).transpose(2, 1, 0))
    sh["gdn_alog"] = f(inp["gdn_a_log"][0].reshape(1, 4))
    sh["gdn_dtb"] = f(inp["gdn_dt_bias"][0].reshape(1, 4))
    sh["gdn_normw"] = f(inp["gdn_norm_w"][0].reshape(1, 128))
    sh["w_out_even"] = f(inp["w_out_even"][0])
    wio = np.asarray(inp["w_in_odd"][0], dtype=np.float32)
    perm = np.arange(ODD_IN)
    for qk in range(2):
        for h in range(4):
            base = qk * 1024 + h * 256
            perm[base:base + 128] = base + np.arange(0, 256, 2)
            perm[base + 128:base + 256] = base + np.arange(1, 256, 2)
    sh["w_in_odd"] = np.ascontiguousarray(wio[:, perm])
    sh["w_out_odd"] = f(inp["w_out_odd"][0])
    for k in ("ln1_g", "ln1_b", "ln2_g", "ln2_b", "router_w", "router_b", "moe_w1", "moe_w2", "moe_b2", "ple_w"):
        sh[k] = f(inp[k])
    sh["moe_b1"] = np.ascontiguousarray(np.asarray(inp["moe_b1"], dtype=np.float32).reshape(2, NE, 16, 128).transpose(0, 1, 3, 2))
    sh["ple_gw"] = f(inp["ple_gate_w"])
    for k, v in host_consts().items():
        if k in CONST_SHAPES:
            sh["c_" + k] = np.ascontiguousarray(v.astype(np.float32)).reshape(CONST_SHAPES[k])
    return sh


def core_inputs(inp, sh, prompt_ids, sample_id, L):
    m = dict(sh)
    xp = [np.asarray(inp["x_prompt"][i][:L], dtype=np.float32) for i in prompt_ids]
    m["xin"] = np.ascontiguousarray(np.concatenate(xp + [np.asarray(inp["x_sample"][sample_id], dtype=np.float32)], axis=0))
    pp = [np.asarray(inp["p_prompt"][:, i, :L], dtype=np.float32) for i in prompt_ids]
    m["pin"] = np.ascontiguousarray(np.concatenate(pp + [np.asarray(inp["p_sample"][:, sample_id], dtype=np.float32)], axis=1))
    m["st_s5re"] = np.ascontiguousarray(np.asarray(inp["state_s5_re"][0, sample_id], dtype=np.float32).reshape(16, 128).T)
    m["st_s5im"] = np.ascontiguousarray(np.asarray(inp["state_s5_im"][0, sample_id], dtype=np.float32).reshape(16, 128).T)
    m["st_gdn"] = np.ascontiguousarray(np.asarray(inp["state_gdn"][0, sample_id], dtype=np.float32))
    m["st_conv"] = np.ascontiguousarray(np.asarray(inp["state_gdn_conv"][0, sample_id], dtype=np.float32).reshape(3, 12, 128).transpose(2, 1, 0))
    m["st_ret"] = np.ascontiguousarray(np.asarray(inp["state_ret"][0, sample_id], dtype=np.float32))
    return m


def unperm(k, a):
    a = np.asarray(a)
    if k in ("o_s5re", "o_s5im"):
        return a.reshape(128, 16).T.reshape(32, 64)
    if k == "o_conv":
        return a.reshape(128, 12, 3).transpose(2, 1, 0).reshape(3, 1536)
    return a


_CACHE = {}


def kernel(**inputs):
    NPS, L, C = 2, 2048, 768
    key = (NPS, L, C)
    if key not in _CACHE:
        _CACHE[key] = build(NPS, L, C)
    nc = _CACHE[key]
    sh = prep_shared(inputs)
    in_maps = [core_inputs(inputs, sh, [2 * c, 2 * c + 1], c, L) for c in range(8)]
    res = run_bass_kernel_spmd(nc, in_maps, core_ids=list(range(8)))
    R = res.results
    B, DB = 16, 8
    y_p = np.zeros((B, L, D), np.float32)
    y_s = np.zeros((DB, 16, D), np.float32)
    outs = {k: (np.zeros((1, B) + shp, np.float32), np.zeros((1, DB) + shp, np.float32))
            for k, shp in (("o_s5re", (32, 64)), ("o_s5im", (32, 64)), ("o_gdn", (4, 128, 128)), ("o_conv", (3, 1536)), ("o_ret", (4, 256, 512)))}
    for c in range(8):
        r = R[c]
        y = r["y_out"]
        for j in range(NPS):
            y_p[2 * c + j] = y[j * L:(j + 1) * L]
        y_s[c] = y[NPS * L:NPS * L + 16]
        for k, (po, so) in outs.items():
            a = r[k]
            for j in range(NPS):
                po[0, 2 * c + j] = unperm(k, a[j]).reshape(po.shape[2:])
            so[0, c] = unperm(k, a[NPS]).reshape(so.shape[2:])
    return (y_p, y_s, outs["o_s5re"][0], outs["o_s5im"][0], outs["o_gdn"][0], outs["o_conv"][0], outs["o_ret"][0],
            outs["o_s5re"][1], outs["o_s5im"][1], outs["o_gdn"][1], outs["o_conv"][1], outs["o_ret"][1])
```

```python
from contextlib import ExitStack
import math
import os
CUT = int(os.environ.get('KCUT', '99'))
CUT2 = int(os.environ.get('KCUT2', '99'))
import numpy as np
import concourse.bass as bass
import concourse.mybir as mybir
from concourse.bass_utils import run_bass_kernel_spmd

F32 = mybir.dt.float32
BF16 = mybir.dt.bfloat16
I32 = mybir.dt.int32
U32 = mybir.dt.uint32
ALU = mybir.AluOpType
AF = mybir.ActivationFunctionType

ENGS = ("pe", "dve", "act", "pool", "sp")
SEM_CHUNK = 30000
SAME_ENG_DIST = int(os.environ.get('KSED', '1000000000'))

D = 1024
NE = 32
TOPK = 4
ALPHA = 4.0 ** 0.25
LN_EPS = 1e-5
NORM_EPS = 1e-6
EVEN_IN = 2568
ODD_IN = 6144


class Instr:
    __slots__ = ("eng", "fn", "waits", "is_dma", "sig", "key", "val", "idx", "clock", "sval")

    def __init__(self, eng, fn, is_dma):
        self.eng = eng
        self.fn = fn
        self.is_dma = is_dma
        self.waits = []
        self.sig = False
        self.key = None
        self.val = 0
        self.idx = 0
        self.clock = None
        self.sval = None


class Trk:
    def __init__(self, name=""):
        self.name = name
        self.ent = {}

    def _conf(self, k):
        if k == "*":
            return list(self.ent.values())
        out = []
        e = self.ent.get(k)
        if e is not None:
            out.append(e)
        e = self.ent.get("*")
        if e is not None:
            out.append(e)
        return out


class Buf:
    def __init__(self, h, name):
        self.h = h
        self.trk = Trk(name)

    def __getitem__(self, k):
        return self.h[k]


def alias(ap, parent, name="alias"):
    b = Buf(ap, name)
    b.trk = parent.trk
    return b


class Prog:
    def __init__(self, nc, sb_words):
        self.nc = nc
        self.q = {e: [] for e in ENGS}
        self.clock = {e: {} for e in ENGS}
        self.es = ExitStack()
        self.dma_sems = {}
        self.dma_rr = {e: 0 for e in ENGS}
        self.n_dma_sems = 8
        self.pending = {e: [] for e in ENGS}
        self.big = self.es.enter_context(nc.sbuf_tensor("big", [128, sb_words], F32))
        self.sb_words = sb_words
        self.top = 0
        self.psn = 0

    def sb(self, name, shape, dtype=F32):
        isz = 2 if dtype == BF16 else 4
        n = 1
        for s in shape[1:]:
            n *= s
        words = (n * isz + 3) // 4
        off = self.top
        self.top += words
        assert self.top <= self.sb_words, (name, self.top, self.sb_words)
        v = self.big[0:shape[0], off:off + words]
        if dtype != F32:
            v = v.bitcast(dtype)
        if dtype == BF16 and n % 2 == 1:
            v = v[:, 0:n]
        if len(shape) == 3:
            v = v.rearrange("p (a b) -> p a b", a=shape[1])
        elif len(shape) == 4:
            v = v.rearrange("p (a b c) -> p a b c", a=shape[1], b=shape[2])
        return Buf(v, name)

    def mark(self):
        return self.top

    def release(self, m):
        self.barrier()
        self.top = m

    def ps(self, name):
        t = self.es.enter_context(self.nc.psum_tensor(name, [128, 512], F32))
        b = Buf(t, name)
        b.trk.excl = True
        return b

    def barrier(self):
        lasts = []
        for e in ENGS:
            if self.q[e]:
                for ins in reversed(self.q[e]):
                    if not ins.is_dma:
                        lasts.append(ins)
                        break
        for q, lst in self.dma_sems.items():
            for s in lst:
                if s[2] is not None:
                    lasts.append(s[2])
        for e in ENGS:
            self.pending[e] = list(lasts)

    def _norm(self, lst):
        out = []
        for x in lst:
            if isinstance(x, tuple):
                t, k = x
            else:
                t, k = x, "*"
            trk = t if isinstance(t, Trk) else t.trk
            out.append((trk, k))
        return out

    def _add_dep(self, ins, prod):
        if prod is None or prod is ins:
            return
        eng = ins.eng
        if prod.eng == "pe" and eng == "pe" and not prod.is_dma:
            return
        if (not prod.is_dma) and (not ins.is_dma) and prod.eng == eng and eng in ("dve", "act") \
                and ins.idx - prod.idx >= SAME_ENG_DIST:
            return
        clk = self.clock[eng]
        if clk.get(prod.key, -1) >= prod.val:
            return
        prod.sig = True
        ins.waits.append(prod)
        new = dict(clk)
        new[prod.key] = prod.val
        if prod.clock:
            for k, v in prod.clock.items():
                if new.get(k, -1) < v:
                    new[k] = v
        self.clock[eng] = new

    def _record(self, ins, reads, writes):
        if self.pending[ins.eng]:
            for pr in self.pending[ins.eng]:
                self._add_dep(ins, pr)
            self.pending[ins.eng] = []
        reads = self._norm(reads)
        writes = self._norm(writes)
        excl = [x for x in reads if getattr(x[0], "excl", False)]
        if excl:
            reads = [x for x in reads if not getattr(x[0], "excl", False)]
            writes = writes + [x for x in excl if x not in writes]
        for trk, k in reads:
            for e in trk._conf(k):
                self._add_dep(ins, e[0])
        for trk, k in writes:
            for e in trk._conf(k):
                self._add_dep(ins, e[0])
                for r in e[1]:
                    self._add_dep(ins, r)
        for trk, k in reads:
            e = trk.ent.get(k)
            if e is None:
                e = trk.ent[k] = [None, []]
            e[1].append(ins)
        for trk, k in writes:
            if k == "*":
                trk.ent.clear()
            trk.ent[k] = [ins, []]
        ins.clock = self.clock[ins.eng]
        self.q[ins.eng].append(ins)

    def op(self, eng, fn, r=(), w=()):
        ins = Instr(eng, fn, False)
        ins.idx = len(self.q[eng])
        ins.key = eng
        ins.val = ins.idx
        self._record(ins, r, w)
        return ins

    def V(self, eng, meth, *args, r=(), w=(), **kw):
        return self.op(eng, lambda e: getattr(e, meth)(*args, **kw), r=r, w=w)

    def dma(self, eng, fn, r=(), w=()):
        ins = Instr(eng, fn, True)
        ins.idx = len(self.q[eng])
        sems = self.dma_sems.setdefault(eng, [])
        if len(sems) < self.n_dma_sems:
            s = [f"dq_{eng}_{len(sems)}", 0, None]
            sems.append(s)
        else:
            s = sems[self.dma_rr[eng] % self.n_dma_sems]
        self.dma_rr[eng] += 1
        if s[2] is not None:
            self._add_dep(ins, s[2])
        s[1] += 1
        s[2] = ins
        ins.key = s[0]
        ins.val = s[1]
        ins.sig = True
        self._record(ins, r, w)
        return ins

    def DM(self, eng, out, in_, r=(), w=(), **kw):
        return self.dma(eng, lambda e: e.dma_start(out=out, in_=in_, **kw), r=r, w=w)

    def finalize(self):
        nc = self.nc
        sem_names = set()
        for e in ENGS:
            cnt = 0
            for ins in self.q[e]:
                if ins.is_dma:
                    sem_names.add(ins.key)
                elif ins.sig:
                    ep, v = divmod(cnt, SEM_CHUNK)
                    ins.sval = (f"e_{e}_{ep}", v + 1)
                    sem_names.add(ins.sval[0])
                    cnt += 1
        sems = {}
        for n in sorted(sem_names):
            sems[n] = self.es.enter_context(nc.semaphore(n))

        def semval(prod):
            if prod.is_dma:
                return sems[prod.key], prod.val * 16
            return sems[prod.sval[0]], prod.sval[1]

        def run(e, eng_name):
            for ins in self.q[eng_name]:
                best = {}
                for pr in ins.waits:
                    s, v = semval(pr)
                    k = id(s)
                    if k not in best or best[k][1] < v:
                        best[k] = (s, v)
                ws = list(best.values())
                attach = None
                if ws and eng_name != "pe":
                    attach = ws.pop()
                for s, v in ws:
                    e.wait_ge(s, v)
                bi = ins.fn(e)
                if attach is not None:
                    bi._wait_ge(attach[0], attach[1])
                if ins.is_dma:
                    bi.then_inc(sems[ins.key], 16)
                elif ins.sig:
                    bi.then_inc(sems[ins.sval[0]], 1)

        block = self.es.enter_context(nc.Block())

        @block.tensor
        def _(e):
            run(e, "pe")

        @block.vector
        def _(e):
            run(e, "dve")

        @block.scalar
        def _(e):
            run(e, "act")

        @block.gpsimd
        def _(e):
            run(e, "pool")

        @block.sync
        def _(e):
            run(e, "sp")
            for q, lst in self.dma_sems.items():
                for s in lst:
                    if s[1] > 0:
                        e.wait_ge(sems[s[0]], s[1] * 16)

        self.es.close()
        return nc


def host_consts():
    c = {}
    c["identf"] = np.eye(128, dtype=np.float32)
    jj = np.arange(128)[:, None]
    ii = np.arange(128)[None, :]
    c["uincl"] = (jj <= ii).astype(np.float32)
    c["ustrict"] = (jj < ii).astype(np.float32)
    c["ones"] = np.ones((128, 128), np.float32)
    c["lmi"] = (ii <= jj).astype(np.float32)
    c["lms"] = (ii < jj).astype(np.float32)
    c["iota_e"] = np.tile(np.arange(32, dtype=np.float32)[None, :], (128, 1))
    c["tau"] = np.tile(np.arange(1, 129, dtype=np.float32)[None, :], (128, 1))
    c["pidx"] = np.arange(128, dtype=np.float32)[:, None].copy()
    log_g = np.log(1.0 - 2.0 ** (-5.0 - np.arange(4, dtype=np.float32))).astype(np.float32)
    M = np.zeros((4, 128, 128), np.float32)
    for h in range(4):
        m = np.exp(log_g[h] * np.abs(ii - jj).astype(np.float32))
        m = np.where((jj >= 64) & (ii < 64), 0.0, m)
        M[h] = m
    c["retmask"] = np.ascontiguousarray(M.transpose(1, 0, 2)).astype(np.float32)
    qs = np.zeros((128, 4, 128), np.float32)
    for h in range(4):
        qs[:, h, :] = np.exp(log_g[h] * (np.arange(128, dtype=np.float32) + 1.0))[None, :]
    c["retqs"] = qs
    ks = np.zeros((128, 8), np.float32)
    for h in range(4):
        ks[:, h] = np.exp(log_g[h] * (127.0 - np.arange(128, dtype=np.float32)))
        ks[:, 4 + h] = np.exp(log_g[h] * (15.0 - np.arange(128, dtype=np.float32)))
    c["retks"] = ks * np.float32(256 ** -0.5)
    c["retcdec"] = [[float(np.exp(log_g[h] * 128.0)) for h in range(4)],
                    [float(np.exp(log_g[h] * 16.0)) for h in range(4)]]
    freq = (1.0 / (10000.0 ** np.linspace(0.0, 1.0, 128, dtype=np.float32))).astype(np.float32)
    pos = np.arange(2048, dtype=np.float32)
    ang = (pos[None, :] * freq[:, None]).astype(np.float32)
    c["rcos"] = np.cos(ang).astype(np.float32)
    c["rsin"] = np.sin(ang).astype(np.float32)
    return c


LAST_DIN = {}
CONST_SHAPES = {"lmi": [128, 128], "lms": [128, 128], "identf": [128, 128], "uincl": [128, 128], "ustrict": [128, 128], "ones": [128, 128],
                "iota_e": [128, 32], "tau": [128, 128], "pidx": [128, 1], "retmask": [128, 4, 128],
                "retqs": [128, 4, 128], "retks": [128, 8], "rcos": [128, 2048], "rsin": [128, 2048]}


def build(NPS, L, C, stages=("A0", "M0", "C0", "A1", "M1", "C1"), dbg=()):
    nc = bass.Bass("TRN2", target_bir_lowering=False)
    NSEQ = NPS + 1
    NTOK = NPS * L + 16
    TPS = L // 128
    tiles = []
    for s in range(NPS):
        for t in range(TPS):
            tiles.append(dict(row0=s * L + t * 128, nt=128, seq=s, first=(t == 0), last=(t == TPS - 1),
                              pos0=t * 128, ti=len(tiles)))
    tiles.append(dict(row0=NPS * L, nt=16, seq=NPS, first=True, last=True, pos0=1024, ti=len(tiles)))
    NT = len(tiles)
    NROWP = NT * 128
    CT = C // 128
    TRASH = NE * C
    cgs = []
    c0 = 0
    while c0 < C:
        cgs.append((c0, min(C, c0 + 512)))
        c0 += 512

    def din(name, shape, dt=F32):
        LAST_DIN[name] = list(shape)
        return nc.dram_tensor(name, list(shape), dt, kind="ExternalInput").ap()

    def dout(name, shape, dt=F32):
        return nc.dram_tensor(name, list(shape), dt, kind="ExternalOutput").ap()

    def dint(name, shape, dt=F32):
        return nc.dram_tensor(name, list(shape), dt, kind="Internal").ap()

    xin = din("xin", [NTOK, D])
    pin = din("pin", [2, NTOK, 256])
    st_s5re = din("st_s5re", [128, 16])
    st_s5im = din("st_s5im", [128, 16])
    st_gdn = din("st_gdn", [4, 128, 128])
    st_conv = din("st_conv", [128, 12, 3])
    st_ret = din("st_ret", [4, 256, 512])
    w_in_even = din("w_in_even", [D, EVEN_IN])
    s5_are = din("s5_are", [128, 16])
    s5_aim = din("s5_aim", [128, 16])
    s5_ldt = din("s5_ldt", [128, 16])
    s5_bst = din("s5_bst", [2, 16, 128, 128])
    s5_cst = din("s5_cst", [2, 16, 128, 128])
    s5_d = din("s5_d", [128, 4])
    s5_wglu = din("s5_wglu", [512, 512])
    s5_bglu = din("s5_bglu", [128, 4])
    gdn_convw = din("gdn_convw", [128, 12, 4])
    gdn_alog = din("gdn_alog", [1, 4])
    gdn_dtb = din("gdn_dtb", [1, 4])
    gdn_normw = din("gdn_normw", [1, 128])
    w_out_even = din("w_out_even", [D, D])
    w_in_odd = din("w_in_odd", [D, ODD_IN])
    w_out_odd = din("w_out_odd", [2048, D])
    ln1_g = din("ln1_g", [2, D])
    ln1_b = din("ln1_b", [2, D])
    ln2_g = din("ln2_g", [2, D])
    ln2_b = din("ln2_b", [2, D])
    router_w = din("router_w", [2, D, NE])
    router_b = din("router_b", [2, NE])
    moe_w1 = din("moe_w1", [2, NE, D, 2 * D])
    moe_b1 = din("moe_b1", [2, NE, 128, 16])
    moe_w2 = din("moe_w2", [2, NE, D, D])
    moe_b2 = din("moe_b2", [2, NE, D])
    ple_w = din("ple_w", [2, 256, D])
    ple_gw = din("ple_gw", [2, D, D])
    cst = {k: din("c_" + k, v) for k, v in CONST_SHAPES.items()}

    y_out = dout("y_out", [NTOK, D])
    o_s5re = dout("o_s5re", [NSEQ, 128, 16])
    o_s5im = dout("o_s5im", [NSEQ, 128, 16])
    o_gdn = dout("o_gdn", [NSEQ, 4, 128, 128])
    o_conv = dout("o_conv", [NSEQ, 128, 12, 3])
    o_ret = dout("o_ret", [NSEQ, 4, 256, 512])
    dbg_out = {k: dout("dbg_" + k, [NTOK, D]) for k in dbg if k.startswith("x")}
    taps = {}

    def tap(name, ap, ti, npart, width, dt=F32):
        if ("t_" + name) not in dbg:
            return
        if name not in taps:
            taps[name] = dout("tap_" + name, [NT, 128, width], dt)
        p.DM("sp", taps[name][ti, 0:npart, :], ap, r=[tapsrc[0]], w=[T_out])

    tapsrc = [None]

    x1s = dint("x1s", [NROWP, D])
    x3s = dint("x3s", [NROWP, D])
    yas = dint("yas", [NT, 128, 4, 128], BF16)
    T_yas = Trk("yas")
    xs = dint("xs", [NE * C + 128, D], BF16)
    ys = dint("ys", [NE * C + 128, D])

    p = Prog(nc, 52900)
    DR = Trk("dram_in")
    T_x1s, T_x3s, T_xs, T_ys, T_out = Trk("x1s"), Trk("x3s"), Trk("xs"), Trk("ys"), Trk("out")
    PS = [p.ps(f"ps{i}") for i in range(8)]

    def psbf(b, n):
        return PS[b][:, 0:n // 2].bitcast(BF16)

    identf = p.sb("identf", [128, 128])
    identb = p.sb("identb", [128, 128], BF16)
    uincl = p.sb("uincl", [128, 128])
    ustr_b = p.sb("ustr_b", [128, 128], BF16)
    ones_f = p.sb("ones_f", [128, 128])
    ones_b = p.sb("ones_b", [128, 128], BF16)
    iota_e = p.sb("iota_e", [128, 32])
    pidx = p.sb("pidx", [128, 1])
    gates_all = p.sb("gates_all", [128, NT, 4])
    slots_all = p.sb("slots_all", [128, NT, 4], I32)
    tmpc = p.sb("tmpc", [128, 128])
    for nm, b in (("identf", identf), ("uincl", uincl), ("ones", ones_f), ("iota_e", iota_e), ("pidx", pidx)):
        p.DM("sp", b[:], cst[nm], r=[DR], w=[b])
    p.DM("sp", tmpc[:], cst["ustrict"], r=[DR], w=[tmpc])
    p.V("dve", "tensor_copy", ustr_b[:], tmpc[:], r=[tmpc], w=[ustr_b])
    p.V("dve", "tensor_copy", identb[:], identf[:], r=[identf], w=[identb])
    p.V("dve", "tensor_copy", ones_b[:], ones_f[:], r=[ones_f], w=[ones_b])

    rr = {"ev": 0}

    def evac(out_ap, in_ap, r, w):
        rr["ev"] += 1
        if rr["ev"] % 2:
            p.V("act", "activation", out_ap, in_ap, AF.Copy, r=r, w=w)
        else:
            p.V("dve", "tensor_copy", out_ap, in_ap, r=r, w=w)

    def bcast_load(buf, src_row):
        p.DM("sp", buf[:], src_row.partition_broadcast(128), r=[DR], w=[buf])

    def load_w_bf16(buf, src, kc):
        n = src.shape[1]
        nch = (n + 2047) // 2048
        step = (n + nch - 1) // nch
        v = src.rearrange("(kc q) n -> q kc n", q=128)
        for c0 in range(0, n, step):
            c1 = min(n, c0 + step)
            p.DM("pool", buf[:, :, c0:c1], v[:, :, c0:c1], r=[DR], w=[(buf, c0)] if nch > 1 else [buf])

    def layernorm(eng_h, h, nt, gt, bt, out, scr6, scr2):
        p.V("dve", "bn_stats", scr6[0:nt, 0, :], h[0:nt, 0:512], r=[h], w=[(scr6, 0)])
        p.V("dve", "bn_stats", scr6[0:nt, 1, :], h[0:nt, 512:1024], r=[h], w=[(scr6, 1)])
        p.V("dve", "bn_aggr", scr2[0:nt, 0:2], scr6[0:nt, :, :].rearrange("p a b -> p (a b)"), r=[scr6], w=[scr2])
        p.V("act", "activation", scr2[0:nt, 2:3], scr2[0:nt, 1:2], AF.Sqrt, bias=epsln[0:nt, 0:1], r=[scr2, epsln], w=[(scr2, "s")])
        p.V("dve", "reciprocal", scr2[0:nt, 3:4], scr2[0:nt, 2:3], r=[(scr2, "s")], w=[(scr2, "r")])
        p.V("dve", "tensor_scalar", out[0:nt, :], h[0:nt, :], scr2[0:nt, 0:1], scr2[0:nt, 3:4], ALU.subtract, ALU.mult,
            r=[h, scr2, (scr2, "r")], w=[out])
        p.V("pool", "tensor_tensor", out[0:nt, :], out[0:nt, :], gt[0:nt, :], ALU.mult, r=[out, gt], w=[out])
        p.V("pool", "tensor_tensor", out[0:nt, :], out[0:nt, :], bt[0:nt, :], ALU.add, r=[out, bt], w=[out])

    epsln = p.sb("epsln", [128, 2])
    p.V("dve", "memset", epsln[:, 0:1], LN_EPS, w=[(epsln, 0)])
    p.V("dve", "memset", epsln[:, 1:2], NORM_EPS, w=[(epsln, 1)])

    def transpose_to(dstT, src_bf, nt, nblk, bank):
        for b0 in range(0, nblk, 8):
            nb = min(8, nblk - b0)
            for b in range(nb):
                p.V("pe", "transpose", psbf(bank, 1024)[:, b * 128:b * 128 + nt], src_bf[0:nt, (b0 + b) * 128:(b0 + b + 1) * 128],
                    identb[0:nt, 0:nt], r=[src_bf, identb], w=[PS[bank]])
            evac(dstT[:, b0:b0 + nb, 0:nt], psbf(bank, 1024).rearrange("p (a b) -> p a b", a=8)[:, 0:nb, 0:nt],
                 r=[PS[bank]], w=[dstT])

    def phaseA_tail(li, tl, xt, mixps, lnw, rw, rb, work):
        nt, ti = tl["nt"], tl["ti"]
        h, x1, xrow, x1T, lg, scr6, scr2, small, Mb, tot = work
        p.V("dve", "scalar_tensor_tensor", h[0:nt, 0:512], xt[0:nt, 0:512], ALPHA, PS[mixps[0]][0:nt, :], ALU.mult, ALU.add,
            r=[xt, PS[mixps[0]]], w=[(h, 0)])
        p.V("dve", "scalar_tensor_tensor", h[0:nt, 512:1024], xt[0:nt, 512:1024], ALPHA, PS[mixps[1]][0:nt, :], ALU.mult, ALU.add,
            r=[xt, PS[mixps[1]]], w=[(h, 1)])
        layernorm("dve", h, nt, lnw[0], lnw[1], x1, scr6, scr2)
        p.DM("sp", x1s[ti * 128:ti * 128 + nt, :], x1[0:nt, :], r=[x1], w=[(T_x1s, ti)])
        if ("x1_%d" % li) in dbg_out:
            p.DM("sp", dbg_out["x1_%d" % li][tl["row0"]:tl["row0"] + nt, :], x1[0:nt, :], r=[x1], w=[T_out])
        p.V("act", "activation", xrow[0:nt, :], x1[0:nt, :], AF.Copy, r=[x1], w=[xrow])
        for half in range(2):
            for b in range(4):
                kc = half * 4 + b
                p.V("pe", "transpose", PS[6][:, b * 128:b * 128 + nt], x1[0:nt, kc * 128:(kc + 1) * 128], identf[0:nt, 0:nt],
                    r=[x1, identf], w=[PS[6]])
            evac(x1T[:, half * 4:half * 4 + 4, 0:nt], PS[6][:, :].rearrange("p (a b) -> p a b", a=4)[:, :, 0:nt], r=[PS[6]], w=[x1T])
        for kc in range(8):
            p.V("pe", "matmul", PS[7][0:nt, 0:32], x1T[:, kc, 0:nt], rw[:, kc, :], start=(kc == 0), stop=(kc == 7),
                r=[x1T, rw], w=[PS[7]])
        p.V("dve", "tensor_tensor", lg[0:nt, :], PS[7][0:nt, 0:32], rb[0:nt, :], ALU.add, r=[PS[7], rb], w=[lg])
        top, ti8, nt0, ex, gs, ef, rk, sl, ov, tmp32, rnk = small
        p.V("dve", "max", top[0:nt, :], lg[0:nt, :], r=[lg], w=[top])
        p.V("dve", "max_index", ti8[0:nt, :], top[0:nt, :], lg[0:nt, :], r=[lg, top], w=[ti8])
        p.V("dve", "tensor_scalar", nt0[0:nt, :], top[0:nt, 0:1], -1.0, None, ALU.mult, r=[top], w=[nt0])
        p.V("act", "activation", ex[0:nt, :], top[0:nt, 0:4], AF.Exp, bias=nt0[0:nt, 0:1], r=[top, nt0], w=[ex])
        p.V("dve", "reduce_sum", gs[0:nt, 0:1], ex[0:nt, :], mybir.AxisListType.X, r=[ex], w=[gs])
        p.V("dve", "reciprocal", gs[0:nt, 1:2], gs[0:nt, 0:1], r=[gs], w=[(gs, "r")])
        p.V("dve", "tensor_scalar", gates_all[0:nt, ti, :], ex[0:nt, :], gs[0:nt, 1:2], None, ALU.mult, r=[ex, (gs, "r")], w=[(gates_all, ti)])
        p.V("pool", "memset", Mb[:], 0.0, w=[Mb])
        p.V("dve", "tensor_scalar", Mb[0:nt, :], lg[0:nt, :], top[0:nt, 3:4], None, ALU.is_ge, r=[lg, top], w=[Mb])
        p.V("pe", "matmul", PS[7][:, 64:96], ustr_b[:, :], Mb[:, :], start=True, stop=True, r=[ustr_b, Mb], w=[PS[7]])
        p.V("pe", "matmul", PS[7][:, 96:128], ones_b[:, :], Mb[:, :], start=True, stop=True, r=[ones_b, Mb], w=[PS[7]])
        p.V("dve", "tensor_tensor", rnk[:, :], PS[7][:, 64:96], tot[:, :], ALU.add, r=[PS[7], tot], w=[rnk])
        p.V("dve", "tensor_tensor", tot[:, :], PS[7][:, 96:128], tot[:, :], ALU.add, r=[PS[7], tot], w=[tot])
        p.V("dve", "tensor_copy", ef[0:nt, :], ti8[0:nt, 0:4], r=[ti8], w=[ef])
        for k in range(4):
            p.V("dve", "scalar_tensor_tensor", tmp32[0:nt, :], iota_e[0:nt, :], ef[0:nt, k:k + 1], rnk[0:nt, :], ALU.is_equal, ALU.mult,
                accum_out=rk[0:nt, k:k + 1], r=[iota_e, ef, rnk], w=[tmp32, (rk, k)])
        p.V("dve", "scalar_tensor_tensor", sl[0:nt, :], ef[0:nt, :], float(C), rk[0:nt, :], ALU.mult, ALU.add, r=[ef, rk], w=[sl])
        p.V("dve", "tensor_scalar", ov[0:nt, :], rk[0:nt, :], float(C), None, ALU.is_ge, r=[rk], w=[ov])
        p.V("dve", "tensor_scalar", tmp32[0:nt, 0:4], sl[0:nt, :], -1.0, pidx[0:nt, 0:1], ALU.mult, ALU.add, r=[sl, pidx], w=[tmp32])
        p.V("dve", "tensor_scalar", tmp32[0:nt, 0:4], tmp32[0:nt, 0:4], float(TRASH), None, ALU.add, r=[tmp32], w=[tmp32])
        p.V("dve", "tensor_tensor", tmp32[0:nt, 0:4], tmp32[0:nt, 0:4], ov[0:nt, :], ALU.mult, r=[tmp32, ov], w=[tmp32])
        p.V("dve", "tensor_tensor", sl[0:nt, :], sl[0:nt, :], tmp32[0:nt, 0:4], ALU.add, r=[sl, tmp32], w=[sl])
        if nt < 128:
            p.V("dve", "tensor_scalar", tmp32[:, 0:4], pidx[:, 0:1].to_broadcast([128, 4]), float(TRASH), None, ALU.add, r=[pidx], w=[tmp32])
            p.V("dve", "tensor_copy", slots_all[:, ti, :], tmp32[:, 0:4], r=[tmp32], w=[(slots_all, ti)])
        p.V("dve", "tensor_copy", slots_all[0:nt, ti, :], sl[0:nt, :], r=[sl], w=[(slots_all, ti)])
        for k in range(4):
            p.dma("pool", lambda e, k=k, ti=ti: e.indirect_dma_start(
                out=xs[:, :], out_offset=bass.IndirectOffsetOnAxis(ap=slots_all[:, ti, k:k + 1], axis=0),
                in_=xrow[:, :], in_offset=None), r=[xrow, (slots_all, ti), (T_xs, "*")], w=[])

    def alloc_tail_work(h=None, x1=None, x1T=None):
        if h is None:
            h = p.sb("h", [128, D])
        if x1 is None:
            x1 = p.sb("x1", [128, D])
        xrow = p.sb("xrow", [128, D], BF16)
        if x1T is None:
            x1T = p.sb("x1T", [128, 8, 128])
        lg = p.sb("lg", [128, 32])
        scr6 = p.sb("scr6", [128, 2, 6])
        scr2 = p.sb("scr2", [128, 4])
        small = (p.sb("top", [128, 8]), p.sb("ti8", [128, 8], U32), p.sb("nt0", [128, 1]), p.sb("ex", [128, 4]),
                 p.sb("gs", [128, 2]), p.sb("ef", [128, 4]), p.sb("rk", [128, 4]), p.sb("sl", [128, 4]),
                 p.sb("ov", [128, 4]), p.sb("tmp32", [128, 32]), p.sb("rnk", [128, 32]))
        Mb = p.sb("Mb", [128, 32], BF16)
        tot = p.sb("tot", [128, 32])
        p.V("dve", "memset", tot[:], 0.0, w=[tot])
        p.V("pool", "memset", xrow[:, :], 0.0, w=[xrow])
        return (h, x1, xrow, x1T, lg, scr6, scr2, small, Mb, tot)

    def load_ln_router(li):
        g1 = p.sb("ln1g", [128, D]); b1 = p.sb("ln1b", [128, D])
        bcast_load(g1, ln1_g[li:li + 1, :]); bcast_load(b1, ln1_b[li:li + 1, :])
        rw = p.sb("rw", [128, 8, NE])
        p.DM("sp", rw[:], router_w[li].rearrange("(kc q) n -> q kc n", q=128), r=[DR], w=[rw])
        rb = p.sb("rb", [128, NE])
        bcast_load(rb, router_b[li:li + 1, :])
        return (g1, b1), rw, rb

    def phaseA0():
        m0 = p.mark()
        win = p.sb("win", [128, 8, EVEN_IN], BF16)
        load_w_bf16(win, w_in_even, 8)
        wout = p.sb("wout", [128, 8, D], BF16)
        load_w_bf16(wout, w_out_even, 8)
        wglu = p.sb("wglu", [128, 4, 512], BF16)
        load_w_bf16(wglu, s5_wglu, 4)
        bst = p.sb("bst", [128, 2, 16, 128], BF16)
        cstt = p.sb("cstt", [128, 2, 16, 128], BF16)
        for ri in range(2):
            p.DM("pool", bst[:, ri, :, :], s5_bst[ri].rearrange("b k m -> k b m"), r=[DR], w=[(bst, ri)])
            p.DM("pool", cstt[:, ri, :, :], s5_cst[ri].rearrange("b k m -> k b m"), r=[DR], w=[(cstt, ri)])
        lnw, rw, rb = load_ln_router(0)
        are = p.sb("are", [128, 16]); aim = p.sb("aim", [128, 16]); ldt = p.sb("ldt", [128, 16])
        for b, s in ((are, s5_are), (aim, s5_aim), (ldt, s5_ldt)):
            p.DM("sp", b[:], s, r=[DR], w=[b])
        dsk = p.sb("dsk", [128, 4]); bgl = p.sb("bgl", [128, 4])
        p.DM("sp", dsk[:], s5_d, r=[DR], w=[dsk])
        p.DM("sp", bgl[:], s5_bglu, r=[DR], w=[bgl])
        tau = p.sb("tau", [128, 128])
        p.DM("sp", tau[:], cst["tau"], r=[DR], w=[tau])
        lam = p.sb("lam", [128, 16]); li_ = p.sb("li", [128, 16]); dtt = p.sb("dtt", [128, 16])
        p.V("act", "activation", dtt[:], ldt[:], AF.Exp, r=[ldt], w=[dtt])
        p.V("dve", "tensor_tensor", li_[:], aim[:], dtt[:], ALU.mult, r=[aim, dtt], w=[li_])
        p.V("dve", "tensor_tensor", lam[:], are[:], dtt[:], ALU.mult, r=[are, dtt], w=[lam])
        p.V("act", "activation", lam[:], lam[:], AF.Exp, r=[lam], w=[lam])
        cosT = p.sb("cosT", [128, 16, 128]); sinT = p.sb("sinT", [128, 16, 128])
        crT = p.sb("crT", [128, 16, 128]); ciT = p.sb("ciT", [128, 16, 128])
        ang = crT
        kq = ciT
        gsc = p.sb("gsc", [128, 2, 16, 128])
        ki = Buf(gsc[:, 0, :, :].bitcast(I32), "ki")
        ki.trk = gsc.trk
        TWO_PI = 2.0 * math.pi

        def sin_of(dst, shift):
            p.V("dve", "tensor_tensor", ang[:], li_[:, :].unsqueeze(2).to_broadcast([128, 16, 128]),
                tau[:, :].unsqueeze(1).to_broadcast([128, 16, 128]), ALU.mult, r=[li_, tau], w=[ang])
            if shift != 0.0:
                p.V("dve", "tensor_scalar", ang[:], ang[:], shift, None, ALU.add, r=[ang], w=[ang])
            p.V("dve", "tensor_scalar", kq[:], ang[:], 1.0 / TWO_PI, None, ALU.mult, r=[ang], w=[kq])
            p.V("dve", "tensor_copy", ki[:], kq[:], r=[kq], w=[ki])
            p.V("dve", "tensor_copy", kq[:], ki[:], r=[ki], w=[kq])
            p.V("dve", "scalar_tensor_tensor", ang[:], kq[:], -TWO_PI, ang[:], ALU.mult, ALU.add, r=[kq, ang], w=[ang])
            p.V("dve", "tensor_scalar", kq[:], ang[:], math.pi, TWO_PI, ALU.is_gt, ALU.mult, r=[ang], w=[kq])
            p.V("dve", "tensor_tensor", ang[:], ang[:], kq[:], ALU.subtract, r=[ang, kq], w=[ang])
            p.V("dve", "tensor_scalar", kq[:], ang[:], -math.pi, TWO_PI, ALU.is_lt, ALU.mult, r=[ang], w=[kq])
            p.V("dve", "tensor_tensor", ang[:], ang[:], kq[:], ALU.add, r=[ang, kq], w=[ang])
            p.V("dve", "tensor_scalar", ang[:], ang[:], math.pi, -math.pi, ALU.min, ALU.max, r=[ang], w=[ang])
            p.V("act", "activation", dst[:], ang[:], AF.Sin, r=[ang], w=[dst])

        sin_of(sinT, 0.0)
        sin_of(cosT, math.pi / 2)
        sm = p.sb("s5sm", [128, 8, 16])
        abre, abim, den, t1, t2, cfre, cfim, t3 = [sm[:, i, :] for i in range(8)]
        S = [sm]
        p.V("dve", "tensor_tensor", abre, lam[:], cosT[:, :, 0], ALU.mult, r=[lam, cosT], w=S)
        p.V("dve", "tensor_tensor", abim, lam[:], sinT[:, :, 0], ALU.mult, r=[lam, sinT], w=S)
        p.V("dve", "tensor_scalar", abre, abre, -1.0, None, ALU.add, r=S, w=S)
        p.V("dve", "tensor_tensor", t1, are[:], are[:], ALU.mult, r=[are], w=S)
        p.V("dve", "tensor_tensor", t2, aim[:], aim[:], ALU.mult, r=[aim], w=S)
        p.V("dve", "tensor_tensor", den, t1, t2, ALU.add, r=S, w=S)
        p.V("dve", "reciprocal", den, den, r=S, w=S)
        p.V("dve", "tensor_tensor", t1, abre, are[:], ALU.mult, r=S + [are], w=S)
        p.V("dve", "tensor_tensor", t2, abim, aim[:], ALU.mult, r=S + [aim], w=S)
        p.V("dve", "tensor_tensor", cfre, t1, t2, ALU.add, r=S, w=S)
        p.V("dve", "tensor_tensor", cfre, cfre, den, ALU.mult, r=S, w=S)
        p.V("dve", "tensor_tensor", t1, abim, are[:], ALU.mult, r=S + [are], w=S)
        p.V("dve", "tensor_tensor", t2, abre, aim[:], ALU.mult, r=S + [aim], w=S)
        p.V("dve", "tensor_tensor", cfim, t1, t2, ALU.subtract, r=S, w=S)
        p.V("dve", "tensor_tensor", cfim, cfim, den, ALU.mult, r=S, w=S)
        sc3 = Buf(gsc[:, 1, :, :], "sc3")
        sc3.trk = gsc.trk
        bc = lambda a: a.unsqueeze(2).to_broadcast([128, 16, 128])
        p.V("dve", "tensor_tensor", crT[:], cosT[:], bc(cfre), ALU.mult, r=[cosT] + S, w=[crT])
        p.V("dve", "tensor_tensor", sc3[:], sinT[:], bc(cfim), ALU.mult, r=[sinT] + S, w=[sc3])
        p.V("dve", "tensor_tensor", crT[:], crT[:], sc3[:], ALU.add, r=[crT, sc3], w=[crT])
        p.V("dve", "tensor_tensor", ciT[:], cosT[:], bc(cfim), ALU.mult, r=[cosT] + S, w=[ciT])
        p.V("dve", "tensor_tensor", sc3[:], sinT[:], bc(cfre), ALU.mult, r=[sinT] + S, w=[sc3])
        p.V("dve", "tensor_tensor", ciT[:], ciT[:], sc3[:], ALU.subtract, r=[ciT, sc3], w=[ciT])
        wc = p.sb("wc", [128, 12, 4])
        p.DM("sp", wc[:], gdn_convw, r=[DR], w=[wc])
        alog = p.sb("alog", [128, 4]); dtb = p.sb("dtb", [128, 4]); nrmw = p.sb("nrmw", [128, 128])
        bcast_load(alog, gdn_alog); bcast_load(dtb, gdn_dtb); bcast_load(nrmw, gdn_normw)
        p.V("act", "activation", alog[:], alog[:], AF.Exp, r=[alog], w=[alog])
        Hre = p.sb("Hre", [128, 16]); Him = p.sb("Him", [128, 16])
        Sg = p.sb("Sg", [128, 4, 128])
        ctx3 = p.sb("ctx3", [128, 12, 3])
        xt_one = p.sb("xt0", [128, D])
        xts = [xt_one, xt_one]
        xb = p.sb("xb", [128, D], BF16)
        xT = p.sb("xT", [128, 8, 128], BF16)
        uTf = p.sb("uTf", [128, 4, 128]); uTb = p.sb("uTb", [128, 4, 128], BF16)
        cb = p.sb("cb", [128, 12, 131])
        cacc = p.sb("cacc", [128, 12, 128])
        g8 = p.sb("g8", [128, 16, 128])
        ctmp = Buf(g8[:, 0:12, :], "ctmp"); ctmp.trk = g8.trk
        ztok = p.sb("ztok", [128, 8])
        rbuf = p.sb("rbuf", [128, 2, 8, 128]); rtmp = p.sb("rtmp", [128, 2, 8, 128])
        hbf = p.sb("hbf", [128, 2, 16, 128], BF16)
        hl = p.sb("hl", [128, 4, 16])
        yA = Buf(rtmp[:, 0, 0:4, :], "yA"); yA.trk = rtmp.trk
        ysq = Buf(rtmp[:, 0, 4:8, :], "ysq"); ysq.trk = rtmp.trk
        gaf = Buf(rtmp[:, 1, 0:4, :], "gaf"); gaf.trk = rtmp.trk
        gab = p.sb("gab", [128, 4, 128], BF16)
        mixT = p.sb("mixT", [128, 8, 128], BF16)
        qkn = Buf(g8[:, 8:16, :], "qkn"); qkn.trk = g8.trk
        sq8 = Buf(g8[:, 0:8, :], "sq8"); sq8.trk = g8.trk
        kvtok = alias(rbuf[:, 0, :, :], rbuf, "kvtok")
        gd = p.sb("gd", [128, 16])
        gd2 = p.sb("gd2", [128, 16])
        _gt = [alias(gsc[:, 0, i, :], gsc, "gt%d" % i) for i in range(16)]
        gbc, dec, erow, attn, attnT, rv, rk_, nwT, ub, qdT, kd, Yt = _gt[0:12]
        Mm = [_gt[12], _gt[13]]
        MT = [_gt[14], _gt[15]]
        yB = p.sb("yB", [128, 512], BF16); osb = alias(gsc[:, 1, 0, :], gsc, "osb"); ssq = p.sb("ssq", [128, 4])
        zs = p.sb("zs", [128, 512], BF16)
        x1a = alias(g8[:, 0:8, :].rearrange("p a b -> p (a b)"), g8, "x1a")
        x1Ta = alias(cacc[:, 0:8, :], cacc, "x1Ta")
        work = alloc_tail_work(h=xt_one, x1=x1a, x1T=x1Ta)
        print("A0 sbuf words", p.top)

        def load_x(tl, buf):
            if tl["nt"] < 128:
                p.V("pool", "memset", buf[:], 0.0, w=[buf])
            p.DM("sp", buf[0:tl["nt"], :], xin[tl["row0"]:tl["row0"] + tl["nt"], :], r=[DR], w=[buf])

        for tl in tiles:
            nt, ti, sq = tl["nt"], tl["ti"], tl["seq"]
            xt = xts[ti % 2]
            load_x(tl, xt)
            if tl["first"]:
                if sq < NPS:
                    p.V("pool", "memset", Hre[:], 0.0, w=[Hre]); p.V("pool", "memset", Him[:], 0.0, w=[Him])
                    p.V("pool", "memset", Sg[:], 0.0, w=[Sg]); p.V("pool", "memset", ctx3[:], 0.0, w=[ctx3])
                else:
                    p.DM("sp", Hre[:], st_s5re, r=[DR], w=[Hre])
                    p.DM("sp", Him[:], st_s5im, r=[DR], w=[Him])
                    p.DM("sp", Sg[:], st_gdn.rearrange("h k v -> k h v"), r=[DR], w=[Sg])
                    p.DM("sp", ctx3[:], st_conv, r=[DR], w=[ctx3])
            if CUT <= 1:
                continue
            p.V("act", "activation", xb[0:nt, :], xt[0:nt, :], AF.Copy, r=[xt], w=[xb])
            transpose_to(xT, xb, nt, 8, 0)
            tapsrc[0] = xt; tap("xt", xt[0:nt, :], ti, nt, D)
            tapsrc[0] = xb; tap("xb", xb[0:nt, :], ti, nt, D, BF16)
            tapsrc[0] = xT; tap("xT", xT[:, :, :].rearrange("p a b -> p (a b)"), ti, 128, 1024, BF16)
            for ob in range(4):
                for kc in range(8):
                    p.V("pe", "matmul", PS[1][:, ob * 128:ob * 128 + nt], win[:, kc, ob * 128:(ob + 1) * 128], xT[:, kc, 0:nt],
                        start=(kc == 0), stop=(kc == 7), r=[win, xT], w=[PS[1]])
            ps1v = PS[1][:, :].rearrange("p (a b) -> p a b", a=4)[:, :, 0:nt]
            p.V("act", "activation", uTf[:, :, 0:nt], ps1v, AF.Copy, r=[PS[1]], w=[uTf])
            p.V("dve", "tensor_copy", uTb[:, :, 0:nt], ps1v, r=[PS[1]], w=[uTb])
            p.V("pool", "tensor_copy", cb[:, :, 0:3], ctx3[:, :, :], r=[ctx3], w=[(cb, "c")])
            for g4 in range(3):
                bank = 2 + (g4 % 2)
                for b in range(4):
                    blk = g4 * 4 + b
                    for kc in range(8):
                        p.V("pe", "matmul", PS[bank][:, b * 128:b * 128 + nt], win[:, kc, 512 + blk * 128:512 + (blk + 1) * 128],
                            xT[:, kc, 0:nt], start=(kc == 0), stop=(kc == 7), r=[win, xT], w=[PS[bank]])
                evac(cb[:, g4 * 4:g4 * 4 + 4, 3:3 + nt], PS[bank][:, :].rearrange("p (a b) -> p a b", a=4)[:, :, 0:nt],
                     r=[PS[bank]], w=[(cb, g4)])
            tapsrc[0] = cb; tap("cb", cb[:, :, :].rearrange("p a b -> p (a b)"), ti, 128, 12 * 131)
            tapsrc[0] = uTf; tap("uTf", uTf[:, :, :].rearrange("p a b -> p (a b)"), ti, 128, 512)
            for kc in range(8):
                p.V("pe", "matmul", PS[4][0:nt, 0:512], xT[:, kc, 0:nt], win[:, kc, 2048:2560], start=(kc == 0), stop=(kc == 7),
                    r=[xT, win], w=[PS[4]])
            for kc in range(8):
                p.V("pe", "matmul", PS[5][0:nt, 0:8], xT[:, kc, 0:nt], win[:, kc, 2560:2568], start=(kc == 0), stop=(kc == 7),
                    r=[xT, win], w=[PS[5]])
            p.V("act", "activation", zs[0:nt, :], PS[4][0:nt, 0:512], AF.Silu, r=[PS[4]], w=[zs])
            p.V("dve", "tensor_copy", ztok[0:nt, 0:8], PS[5][0:nt, 0:8], r=[PS[5]], w=[ztok])
            if CUT <= 2:
                continue
            for hf in range(2):
                for ri in range(2):
                    for b8 in range(8):
                        blk = hf * 8 + b8
                        bank = 4 + ri * 2 + (b8 // 4)
                        p.V("pe", "matmul", PS[bank][:, (b8 % 4) * 128:(b8 % 4) * 128 + nt], bst[:, ri, blk, :], uTb[:, blk // 4, 0:nt],
                            start=True, stop=True, r=[bst, uTb], w=[PS[bank]])
                for q4 in range(2):
                    bre = PS[4 + q4][:, :].rearrange("p (a b) -> p a b", a=4)[:, :, 0:nt]
                    bim = PS[6 + q4][:, :].rearrange("p (a b) -> p a b", a=4)[:, :, 0:nt]
                    bs = slice(hf * 8 + q4 * 4, hf * 8 + q4 * 4 + 4)
                    o4 = slice(q4 * 4, q4 * 4 + 4)
                    p.V("dve", "tensor_tensor", rbuf[:, 0, o4, 0:nt], bre, crT[:, bs, 0:nt], ALU.mult, r=[PS[4 + q4], crT], w=[(rbuf, 0)])
                    p.V("dve", "tensor_tensor", rtmp[:, 0, o4, 0:nt], bim, ciT[:, bs, 0:nt], ALU.mult, r=[PS[6 + q4], ciT], w=[(rtmp, 0)])
                    p.V("dve", "tensor_tensor", rbuf[:, 1, o4, 0:nt], bre, ciT[:, bs, 0:nt], ALU.mult, r=[PS[4 + q4], ciT], w=[(rbuf, 1)])
                    p.V("dve", "tensor_tensor", rtmp[:, 1, o4, 0:nt], bim, crT[:, bs, 0:nt], ALU.mult, r=[PS[6 + q4], crT], w=[(rtmp, 1)])
                p.V("pool", "tensor_tensor", rbuf[:, 0, :, 0:nt], rbuf[:, 0, :, 0:nt], rtmp[:, 0, :, 0:nt], ALU.subtract, r=[(rbuf, 0), (rtmp, 0)], w=[(rbuf, 0)])
                p.V("pool", "tensor_tensor", rbuf[:, 1, :, 0:nt], rbuf[:, 1, :, 0:nt], rtmp[:, 1, :, 0:nt], ALU.add, r=[(rbuf, 1), (rtmp, 1)], w=[(rbuf, 1)])
                for b8 in range(8):
                    blk = hf * 8 + b8
                    p.V("dve", "tensor_tensor_scan", gsc[:, 0, blk, 0:nt], lam[:, blk:blk + 1].to_broadcast([128, nt]), rbuf[:, 0, b8, 0:nt],
                        Hre[:, blk:blk + 1], ALU.mult, ALU.add, r=[lam, (rbuf, 0), Hre], w=[(gsc, (0, blk))])
                    p.V("dve", "tensor_tensor_scan", gsc[:, 1, blk, 0:nt], lam[:, blk:blk + 1].to_broadcast([128, nt]), rbuf[:, 1, b8, 0:nt],
                        Him[:, blk:blk + 1], ALU.mult, ALU.add, r=[lam, (rbuf, 1), Him], w=[(gsc, (1, blk))])
            t_a, t_b = rbuf[:, :, :, :].rearrange("p a b c -> p (a b) c"), rtmp[:, :, :, :].rearrange("p a b c -> p (a b) c")
            p.V("pool", "tensor_tensor", t_a[:, :, 0:nt], gsc[:, 0, :, 0:nt], cosT[:, :, 0:nt], ALU.mult, r=[gsc, cosT], w=[rbuf])
            p.V("pool", "tensor_tensor", t_b[:, :, 0:nt], gsc[:, 1, :, 0:nt], sinT[:, :, 0:nt], ALU.mult, r=[gsc, sinT], w=[rtmp])
            p.V("dve", "tensor_tensor", hbf[:, 0, :, 0:nt], t_a[:, :, 0:nt], t_b[:, :, 0:nt], ALU.subtract, r=[rbuf, rtmp], w=[(hbf, 0)])
            lc = nt - 1
            p.V("dve", "tensor_tensor", hl[:, 0, :], gsc[:, 0, :, lc], cosT[:, :, lc], ALU.mult, r=[gsc, cosT], w=[(hl, 0)])
            p.V("dve", "tensor_tensor", hl[:, 1, :], gsc[:, 1, :, lc], sinT[:, :, lc], ALU.mult, r=[gsc, sinT], w=[(hl, 1)])
            p.V("dve", "tensor_tensor", hl[:, 2, :], gsc[:, 0, :, lc], sinT[:, :, lc], ALU.mult, r=[gsc, sinT], w=[(hl, 2)])
            p.V("dve", "tensor_tensor", hl[:, 3, :], gsc[:, 1, :, lc], cosT[:, :, lc], ALU.mult, r=[gsc, cosT], w=[(hl, 3)])
            p.V("pool", "tensor_tensor", t_a[:, :, 0:nt], gsc[:, 0, :, 0:nt], sinT[:, :, 0:nt], ALU.mult, r=[gsc, sinT, (hbf, 0)], w=[rbuf])
            p.V("pool", "tensor_tensor", t_b[:, :, 0:nt], gsc[:, 1, :, 0:nt], cosT[:, :, 0:nt], ALU.mult, r=[gsc, cosT, (hbf, 0)], w=[rtmp])
            p.V("dve", "scalar_tensor_tensor", hbf[:, 1, :, 0:nt], t_a[:, :, 0:nt], -1.0, t_b[:, :, 0:nt], ALU.mult, ALU.subtract,
                r=[rbuf, rtmp], w=[(hbf, 1)])
            p.V("dve", "tensor_tensor", Hre[:], hl[:, 0, :], hl[:, 1, :], ALU.subtract, r=[hl], w=[Hre])
            p.V("dve", "tensor_tensor", Him[:], hl[:, 2, :], hl[:, 3, :], ALU.add, r=[hl], w=[Him])
            if tl["last"]:
                p.DM("sp", o_s5re[sq], Hre[:], r=[Hre], w=[T_out])
                p.DM("sp", o_s5im[sq], Him[:], r=[Him], w=[T_out])
            for ob in range(4):
                n = 0
                for b4 in range(4):
                    blk = ob * 4 + b4
                    for ri in range(2):
                        p.V("pe", "matmul", PS[1][:, ob * 128:ob * 128 + nt], cstt[:, ri, blk, :], hbf[:, ri, blk, 0:nt],
                            start=(n == 0), stop=(n == 7), r=[cstt, hbf], w=[PS[1]])
                        n += 1
            for ob in range(4):
                p.V("dve", "scalar_tensor_tensor", yA[:, ob, 0:nt], uTf[:, ob, 0:nt], dsk[:, ob:ob + 1], PS[1][:, ob * 128:ob * 128 + nt],
                    ALU.mult, ALU.add, r=[uTf, dsk, PS[1]], w=[yA])
            cg = math.sqrt(2.0 / math.pi)
            p.V("act", "activation", ysq[:, :, 0:nt], yA[:, :, 0:nt], AF.Square, r=[yA], w=[ysq])
            p.V("dve", "tensor_scalar", ysq[:, :, 0:nt], ysq[:, :, 0:nt], 2.0 * cg * 0.044715, 2.0 * cg, ALU.mult, ALU.add, r=[ysq], w=[ysq])
            p.V("dve", "tensor_tensor", ysq[:, :, 0:nt], ysq[:, :, 0:nt], yA[:, :, 0:nt], ALU.mult, r=[ysq, yA], w=[ysq])
            p.V("act", "activation", ysq[:, :, 0:nt], ysq[:, :, 0:nt], AF.Sigmoid, r=[ysq], w=[ysq])
            p.V("dve", "tensor_tensor", gaf[:, :, 0:nt], ysq[:, :, 0:nt], yA[:, :, 0:nt], ALU.mult, r=[ysq, yA], w=[gaf])
            p.V("act", "activation", gab[:, :, 0:nt], gaf[:, :, 0:nt], AF.Copy, r=[gaf], w=[gab])
            for ob in range(4):
                for kc in range(4):
                    p.V("pe", "matmul", PS[0][:, ob * 128:ob * 128 + nt], wglu[:, kc, ob * 128:(ob + 1) * 128], gab[:, kc, 0:nt],
                        start=(kc == 0), stop=(kc == 3), r=[wglu, gab], w=[PS[0]])
            for ob in range(4):
                p.V("act", "activation", ysq[:, ob, 0:nt], PS[0][:, ob * 128:ob * 128 + nt], AF.Sigmoid, bias=bgl[:, ob:ob + 1],
                    r=[PS[0], bgl], w=[ysq])
            p.V("dve", "tensor_tensor", mixT[:, 0:4, 0:nt], ysq[:, :, 0:nt], gaf[:, :, 0:nt], ALU.mult, r=[ysq, gaf], w=[(mixT, "a")])
            if CUT <= 3:
                continue
            for j in range(4):
                wj = wc[:, :, j:j + 1].to_broadcast([128, 12, nt])
                if j == 0:
                    p.V("dve", "tensor_tensor", cacc[:, :, 0:nt], cb[:, :, 0:nt], wj, ALU.mult, r=[cb, wc], w=[cacc])
                else:
                    p.V("pool", "tensor_tensor", ctmp[:, :, 0:nt], cb[:, :, j:j + nt], wj, ALU.mult, r=[cb, wc], w=[ctmp])
                    p.V("dve", "tensor_tensor", cacc[:, :, 0:nt], cacc[:, :, 0:nt], ctmp[:, :, 0:nt], ALU.add, r=[cacc, ctmp], w=[cacc])
            p.V("pool", "tensor_copy", ctx3[:, :, :], cb[:, :, nt:nt + 3], r=[cb], w=[ctx3])
            if tl["last"]:
                p.DM("sp", o_conv[sq], ctx3[:], r=[ctx3], w=[T_out])
            p.V("act", "activation", cacc[:, :, 0:nt], cacc[:, :, 0:nt], AF.Silu, r=[cacc], w=[cacc])
            p.V("act", "activation", sq8[:, :, 0:nt], cacc[:, 0:8, 0:nt], AF.Square, r=[cacc], w=[sq8])
            for hb in range(2):
                for b in range(4):
                    p.V("pe", "matmul", PS[2 + hb][:, b * 128:b * 128 + nt], ones_f[:, :], sq8[:, hb * 4 + b, 0:nt], start=True, stop=True,
                        r=[ones_f, sq8], w=[PS[2 + hb]])
            for hb in range(2):
                v = PS[2 + hb][:, :].rearrange("p (a b) -> p a b", a=4)[:, :, 0:nt]
                p.V("act", "activation", sq8[:, hb * 4:hb * 4 + 4, 0:nt], v, AF.Sqrt, bias=epsln[:, 1:2], r=[PS[2 + hb], epsln], w=[(sq8, hb)])
            p.V("dve", "reciprocal", sq8[:, :, 0:nt], sq8[:, :, 0:nt], r=[sq8], w=[sq8])
            p.V("dve", "scalar_tensor_tensor", qkn[:, 0:4, 0:nt], cacc[:, 0:4, 0:nt], 128.0 ** -0.5, sq8[:, 0:4, 0:nt], ALU.mult, ALU.mult,
                r=[cacc, sq8], w=[(qkn, "q")])
            p.V("dve", "tensor_tensor", qkn[:, 4:8, 0:nt], cacc[:, 4:8, 0:nt], sq8[:, 4:8, 0:nt], ALU.mult, r=[cacc, sq8], w=[(qkn, "k")])
            for b in range(4):
                p.V("pe", "transpose", PS[2][0:nt, b * 128:(b + 1) * 128], qkn[:, 4 + b, 0:nt], identf[:, :], r=[qkn, identf], w=[PS[2]])
                p.V("pe", "transpose", PS[3][0:nt, b * 128:(b + 1) * 128], cacc[:, 8 + b, 0:nt], identf[:, :], r=[cacc, identf], w=[PS[3]])
            evac(kvtok[0:nt, 0:4, :], PS[2][0:nt, :].rearrange("p (a b) -> p a b", a=4), r=[PS[2]], w=[kvtok])
            evac(kvtok[0:nt, 4:8, :], PS[3][0:nt, :].rearrange("p (a b) -> p a b", a=4), r=[PS[3]], w=[kvtok])
            if CUT2 <= 1:
                continue
            p.V("act", "activation", gd[0:nt, 0:4], ztok[0:nt, 0:4], AF.Sigmoid, r=[ztok], w=[(gd, "b")])
            p.V("dve", "tensor_scalar", gd[0:nt, 4:8], gd[0:nt, 0:4], -1.0, None, ALU.mult, r=[(gd, "b")], w=[(gd, "nb")])
            p.V("dve", "tensor_tensor", gd[0:nt, 8:12], ztok[0:nt, 4:8], dtb[0:nt, :], ALU.add, r=[ztok, dtb], w=[(gd, "g")])
            p.V("act", "activation", gd[0:nt, 8:12], gd[0:nt, 8:12], AF.Exp, r=[(gd, "g")], w=[(gd, "g")])
            p.V("act", "activation", gd[0:nt, 8:12], gd[0:nt, 8:12], AF.Ln, bias=1.0, r=[(gd, "g")], w=[(gd, "g")])
            p.V("dve", "scalar_tensor_tensor", gd[0:nt, 8:12], gd[0:nt, 8:12], -1.0, alog[0:nt, :], ALU.mult, ALU.mult, r=[(gd, "g"), alog], w=[(gd, "g")])
            p.V("pe", "matmul", PS[0][0:nt, 0:4], uincl[0:nt, 0:nt], gd[0:nt, 8:12], start=True, stop=True, r=[uincl, (gd, "g")], w=[PS[0]])
            p.V("dve", "tensor_copy", gd[0:nt, 12:16], PS[0][0:nt, 0:4], r=[PS[0]], w=[(gd, "G")])
            p.V("act", "activation", gd2[0:nt, 0:4], gd[0:nt, 12:16], AF.Exp, r=[(gd, "G")], w=[(gd2, "e")])
            p.V("dve", "tensor_tensor", gd2[0:nt, 4:8], gd2[0:nt, 0:4], gd[0:nt, 0:4], ALU.mult, r=[(gd2, "e"), (gd, "b")], w=[(gd2, "be")])
            if CUT2 <= 2:
                continue
            for hd in range(4):
                kT = qkn[:, 4 + hd, 0:nt]
                qT = qkn[:, hd, 0:nt]
                p.V("dve", "tensor_scalar", gbc[0:nt, :], ones_f[0:nt, :], gd[0:nt, 8 + hd:9 + hd], None, ALU.mult, r=[ones_f, (gd, "g")], w=[gbc])
                p.V("pe", "matmul", PS[0][:, 128:128 + nt], gbc[0:nt, :], uincl[0:nt, 0:nt], start=True, stop=True, r=[gbc, uincl], w=[PS[0]])
                p.V("pe", "matmul", PS[0][0:nt, 256:256 + nt], kT, kT, start=True, stop=True, r=[(qkn, "k")], w=[PS[0]])
                p.V("pe", "matmul", PS[0][0:nt, 384:384 + nt], qT, kT, start=True, stop=True, r=[(qkn, "q"), (qkn, "k")], w=[PS[0]])
                grow = PS[0][0:nt, 128:128 + nt]
                p.V("act", "activation", dec[0:nt, 0:nt], grow, AF.Exp, bias=gd[0:nt, 12 + hd:13 + hd], scale=-1.0, r=[PS[0], (gd, "G")], w=[dec])
                p.V("act", "activation", erow[:, 0:nt], PS[0][:, 128:128 + nt], AF.Exp, r=[PS[0]], w=[erow])
                p.V("pool", "affine_select", dec[0:nt, 0:nt], dec[0:nt, 0:nt], [[-1, nt]], ALU.is_ge, 0.0, base=0, channel_multiplier=1, r=[dec], w=[dec])
                p.V("dve", "scalar_tensor_tensor", Mm[0][0:nt, 0:nt], PS[0][0:nt, 256:256 + nt], gd[0:nt, 4 + hd:5 + hd], dec[0:nt, 0:nt], ALU.mult, ALU.mult,
                    r=[PS[0], (gd, "nb"), dec], w=[Mm[0]])
                p.V("pool", "affine_select", Mm[0][0:nt, 0:nt], Mm[0][0:nt, 0:nt], [[-1, nt]], ALU.is_gt, 0.0, base=0, channel_multiplier=1, r=[Mm[0]], w=[Mm[0]])
                p.V("dve", "tensor_tensor", attn[0:nt, 0:nt], PS[0][0:nt, 384:384 + nt], dec[0:nt, 0:nt], ALU.mult, r=[PS[0], dec], w=[attn])
                p.V("pe", "transpose", PS[1][0:nt, 0:nt], Mm[0][0:nt, 0:nt], identf[0:nt, 0:nt], r=[Mm[0], identf], w=[PS[1]])
                p.V("pe", "transpose", PS[1][0:nt, 128:128 + nt], attn[0:nt, 0:nt], identf[0:nt, 0:nt], r=[attn, identf], w=[PS[1]])
                p.V("act", "activation", MT[0][0:nt, 0:nt], PS[1][0:nt, 0:nt], AF.Copy, r=[PS[1]], w=[MT[0]])
                p.V("dve", "tensor_tensor", Yt[0:nt, 0:nt], PS[1][0:nt, 0:nt], identf[0:nt, 0:nt], ALU.add, r=[PS[1], identf], w=[Yt])
                p.V("act", "activation", attnT[0:nt, 0:nt], PS[1][0:nt, 128:128 + nt], AF.Copy, r=[PS[1]], w=[attnT])
                if CUT2 <= 3:
                    continue
                nlev = 6 if nt == 128 else 3
                for lv in range(nlev):
                    a, b_ = lv % 2, (lv + 1) % 2
                    bank = 6 + (lv % 2)
                    p.V("pe", "matmul", PS[bank][0:nt, 0:nt], MT[a][0:nt, 0:nt], Mm[a][0:nt, 0:nt], start=True, stop=True, r=[MT[a], Mm[a]], w=[PS[bank]])
                    if lv < nlev - 1:
                        p.V("pe", "matmul", PS[bank][0:nt, 128:128 + nt], Mm[a][0:nt, 0:nt], MT[a][0:nt, 0:nt], start=True, stop=True, r=[MT[a], Mm[a]], w=[PS[bank]])
                    p.V("act", "activation", Mm[b_][0:nt, 0:nt], PS[bank][0:nt, 0:nt], AF.Copy, r=[PS[bank]], w=[Mm[b_]])
                    if lv < nlev - 1:
                        p.V("dve", "tensor_copy", MT[b_][0:nt, 0:nt], PS[bank][0:nt, 128:128 + nt], r=[PS[bank]], w=[MT[b_]])
                    p.V("pe", "matmul", PS[bank][0:nt, 256:256 + nt], Mm[b_][0:nt, 0:nt], Yt[0:nt, 0:nt], start=True, stop=True, r=[Mm[b_], Yt], w=[PS[bank]])
                    p.V("dve", "tensor_tensor", Yt[0:nt, 0:nt], Yt[0:nt, 0:nt], PS[bank][0:nt, 256:256 + nt], ALU.add, r=[Yt, PS[bank]], w=[Yt])
                if CUT2 <= 4:
                    continue
                p.V("dve", "tensor_scalar", rv[0:nt, :], kvtok[0:nt, 4 + hd, :], gd[0:nt, hd:hd + 1], None, ALU.mult, r=[kvtok, (gd, "b")], w=[rv])
                p.V("dve", "tensor_scalar", rk_[0:nt, :], kvtok[0:nt, hd, :], gd2[0:nt, 4 + hd:5 + hd], None, ALU.mult, r=[kvtok, (gd2, "be")], w=[rk_])
                p.V("pe", "matmul", PS[1][:, 256:256 + nt], rk_[0:nt, :], Yt[0:nt, 0:nt], start=True, stop=True, r=[rk_, Yt], w=[PS[1]])
                p.V("dve", "tensor_scalar", nwT[:, 0:nt], PS[1][:, 256:256 + nt], -1.0, None, ALU.mult, r=[PS[1]], w=[nwT])
                p.V("pe", "matmul", PS[5][0:nt, 0:128], Yt[0:nt, 0:nt], rv[0:nt, :], start=True, stop=False, r=[Yt, rv], w=[PS[5]])
                p.V("pe", "matmul", PS[5][0:nt, 0:128], nwT[:, 0:nt], Sg[:, hd, :], start=False, stop=True, r=[nwT, Sg], w=[PS[5]])
                p.V("act", "activation", ub[0:nt, :], PS[5][0:nt, 0:128], AF.Copy, r=[PS[5]], w=[ub])
                p.V("dve", "tensor_tensor", qdT[:, 0:nt], qT, erow[:, 0:nt], ALU.mult, r=[(qkn, "q"), erow], w=[qdT])
                p.V("pe", "matmul", PS[5][0:nt, 128:256], qdT[:, 0:nt], Sg[:, hd, :], start=True, stop=False, r=[qdT, Sg], w=[PS[5]])
                p.V("pe", "matmul", PS[5][0:nt, 128:256], attnT[0:nt, 0:nt], ub[0:nt, :], start=False, stop=True, r=[attnT, ub], w=[PS[5]])
                if CUT2 <= 5:
                    continue
                p.V("dve", "tensor_copy", gd2[:, 12:13], PS[0][:, 128 + nt - 1:128 + nt], r=[PS[0]], w=[(gd2, "gl")])
                p.V("act", "activation", gd2[0:nt, 8 + hd:9 + hd], gd[0:nt, 12 + hd:13 + hd], AF.Exp, bias=gd2[0:nt, 12:13], scale=-1.0,
                    r=[(gd, "G"), (gd2, "gl")], w=[(gd2, ("kd", hd))])
                p.V("dve", "tensor_scalar", kd[0:nt, :], kvtok[0:nt, hd, :], gd2[0:nt, 8 + hd:9 + hd], None, ALU.mult, r=[kvtok, (gd2, ("kd", hd))], w=[kd])
                p.V("pe", "matmul", PS[5][:, 256:384], kd[0:nt, :], ub[0:nt, :], start=True, stop=True, r=[kd, ub], w=[PS[5]])
                p.V("act", "activation", gd2[:, 13:14], gd2[:, 12:13], AF.Exp, r=[(gd2, "gl")], w=[(gd2, "egl")])
                p.V("dve", "scalar_tensor_tensor", Sg[:, hd, :], Sg[:, hd, :], gd2[:, 13:14], PS[5][:, 256:384], ALU.mult, ALU.add,
                    r=[Sg, (gd2, "egl"), PS[5]], w=[Sg])
                if CUT2 <= 6:
                    continue
                p.V("act", "activation", osb[0:nt, :], PS[5][0:nt, 128:256], AF.Square, r=[PS[5]], w=[osb])
                p.V("dve", "reduce_sum", ssq[0:nt, hd:hd + 1], osb[0:nt, :], mybir.AxisListType.X, r=[osb], w=[(ssq, hd)])
                p.V("act", "activation", ssq[0:nt, hd:hd + 1], ssq[0:nt, hd:hd + 1], AF.Sqrt, bias=epsln[0:nt, 1:2], scale=1.0 / 128.0, r=[(ssq, hd), epsln], w=[(ssq, hd)])
                p.V("dve", "reciprocal", ssq[0:nt, hd:hd + 1], ssq[0:nt, hd:hd + 1], r=[(ssq, hd)], w=[(ssq, hd)])
                if CUT2 <= 7:
                    continue
                p.V("dve", "scalar_tensor_tensor", osb[0:nt, :], PS[5][0:nt, 128:256], ssq[0:nt, hd:hd + 1], nrmw[0:nt, :], ALU.mult, ALU.mult,
                    r=[PS[5], (ssq, hd), nrmw], w=[osb])
                p.V("dve", "tensor_tensor", yB[0:nt, hd * 128:(hd + 1) * 128], osb[0:nt, :], zs[0:nt, hd * 128:(hd + 1) * 128], ALU.mult, r=[osb, zs], w=[(yB, hd)])
            if tl["last"]:
                p.DM("sp", o_gdn[sq].rearrange("h k v -> k h v"), Sg[:], r=[Sg], w=[T_out])
            if CUT <= 4:
                continue
            for b in range(4):
                p.V("pe", "transpose", psbf(2, 1024)[:, b * 128:b * 128 + nt], yB[0:nt, b * 128:(b + 1) * 128], identb[0:nt, 0:nt], r=[yB, identb], w=[PS[2]])
            evac(mixT[:, 4:8, 0:nt], psbf(2, 1024).rearrange("p (a b) -> p a b", a=8)[:, 0:4, 0:nt], r=[PS[2]], w=[(mixT, "b")])
            for half in range(2):
                for kc in range(8):
                    p.V("pe", "matmul", PS[2 + half][0:nt, :], mixT[:, kc, 0:nt], wout[:, kc, half * 512:(half + 1) * 512], start=(kc == 0), stop=(kc == 7),
                        r=[mixT, wout], w=[PS[2 + half]])
            if CUT <= 5:
                continue
            phaseA_tail(0, tl, xt, (2, 3), lnw, rw, rb, work)
        p.release(m0)

    def run_streams(gens):
        gens = list(gens)
        while gens:
            for g in list(gens):
                try:
                    next(g)
                except StopIteration:
                    gens.remove(g)

    def phaseS0():
        m0 = p.mark()
        win_u = p.sb("win_u", [128, 8, 512], BF16)
        p.DM("pool", win_u[:], w_in_even[:, 0:512].rearrange("(kc q) n -> q kc n", q=128), r=[DR], w=[win_u])
        wglu = p.sb("wglu", [128, 4, 512], BF16)
        load_w_bf16(wglu, s5_wglu, 4)
        bst = p.sb("bst", [128, 2, 16, 128], BF16)
        cstt = p.sb("cstt", [128, 2, 16, 128], BF16)
        for ri in range(2):
            p.DM("pool", bst[:, ri, :, :], s5_bst[ri].rearrange("b k m -> k b m"), r=[DR], w=[(bst, ri)])
            p.DM("pool", cstt[:, ri, :, :], s5_cst[ri].rearrange("b k m -> k b m"), r=[DR], w=[(cstt, ri)])
        are = p.sb("are", [128, 16]); aim = p.sb("aim", [128, 16]); ldt = p.sb("ldt", [128, 16])
        for b, s_ in ((are, s5_are), (aim, s5_aim), (ldt, s5_ldt)):
            p.DM("sp", b[:], s_, r=[DR], w=[b])
        dsk = p.sb("dsk", [128, 4]); bgl = p.sb("bgl", [128, 4])
        p.DM("sp", dsk[:], s5_d, r=[DR], w=[dsk])
        p.DM("sp", bgl[:], s5_bglu, r=[DR], w=[bgl])
        tau = p.sb("tau", [128, 128])
        p.DM("sp", tau[:], cst["tau"], r=[DR], w=[tau])
        lam = p.sb("lam", [128, 16]); li_ = p.sb("li", [128, 16]); dtt = p.sb("dtt", [128, 16])
        p.V("act", "activation", dtt[:], ldt[:], AF.Exp, r=[ldt], w=[dtt])
        p.V("dve", "tensor_tensor", li_[:], aim[:], dtt[:], ALU.mult, r=[aim, dtt], w=[li_])
        p.V("dve", "tensor_tensor", lam[:], are[:], dtt[:], ALU.mult, r=[are, dtt], w=[lam])
        p.V("act", "activation", lam[:], lam[:], AF.Exp, r=[lam], w=[lam])
        cosT = p.sb("cosT", [128, 16, 128]); sinT = p.sb("sinT", [128, 16, 128])
        crT = p.sb("crT", [128, 16, 128]); ciT = p.sb("ciT", [128, 16, 128])
        ang = crT
        kq = ciT
        kif = p.sb("kif", [128, 16, 128])
        ki = alias(kif[:, :, :].bitcast(I32), kif, "ki")
        sc3 = p.sb("sc3", [128, 16, 128])
        TWO_PI = 2.0 * math.pi

        def sin_of(dst, shift):
            p.V("dve", "tensor_tensor", ang[:], li_[:, :].unsqueeze(2).to_broadcast([128, 16, 128]),
                tau[:, :].unsqueeze(1).to_broadcast([128, 16, 128]), ALU.mult, r=[li_, tau], w=[ang])
            if shift != 0.0:
                p.V("dve", "tensor_scalar", ang[:], ang[:], shift, None, ALU.add, r=[ang], w=[ang])
            p.V("dve", "tensor_scalar", kq[:], ang[:], 1.0 / TWO_PI, None, ALU.mult, r=[ang], w=[kq])
            p.V("dve", "tensor_copy", ki[:], kq[:], r=[kq], w=[ki])
            p.V("dve", "tensor_copy", kq[:], ki[:], r=[ki], w=[kq])
            p.V("dve", "scalar_tensor_tensor", ang[:], kq[:], -TWO_PI, ang[:], ALU.mult, ALU.add, r=[kq, ang], w=[ang])
            p.V("dve", "tensor_scalar", kq[:], ang[:], math.pi, TWO_PI, ALU.is_gt, ALU.mult, r=[ang], w=[kq])
            p.V("dve", "tensor_tensor", ang[:], ang[:], kq[:], ALU.subtract, r=[ang, kq], w=[ang])
            p.V("dve", "tensor_scalar", kq[:], ang[:], -math.pi, TWO_PI, ALU.is_lt, ALU.mult, r=[ang], w=[kq])
            p.V("dve", "tensor_tensor", ang[:], ang[:], kq[:], ALU.add, r=[ang, kq], w=[ang])
            p.V("dve", "tensor_scalar", ang[:], ang[:], math.pi, -math.pi, ALU.min, ALU.max, r=[ang], w=[ang])
            p.V("act", "activation", dst[:], ang[:], AF.Sin, r=[ang], w=[dst])

        sin_of(sinT, 0.0)
        sin_of(cosT, math.pi / 2)
        sm = p.sb("s5sm", [128, 8, 16])
        abre, abim, den, t1, t2, cfre, cfim, t3 = [sm[:, i, :] for i in range(8)]
        S = [sm]
        p.V("dve", "tensor_tensor", abre, lam[:], cosT[:, :, 0], ALU.mult, r=[lam, cosT], w=S)
        p.V("dve", "tensor_tensor", abim, lam[:], sinT[:, :, 0], ALU.mult, r=[lam, sinT], w=S)
        p.V("dve", "tensor_scalar", abre, abre, -1.0, None, ALU.add, r=S, w=S)
        p.V("dve", "tensor_tensor", t1, are[:], are[:], ALU.mult, r=[are], w=S)
        p.V("dve", "tensor_tensor", t2, aim[:], aim[:], ALU.mult, r=[aim], w=S)
        p.V("dve", "tensor_tensor", den, t1, t2, ALU.add, r=S, w=S)
        p.V("dve", "reciprocal", den, den, r=S, w=S)
        p.V("dve", "tensor_tensor", t1, abre, are[:], ALU.mult, r=S + [are], w=S)
        p.V("dve", "tensor_tensor", t2, abim, aim[:], ALU.mult, r=S + [aim], w=S)
        p.V("dve", "tensor_tensor", cfre, t1, t2, ALU.add, r=S, w=S)
        p.V("dve", "tensor_tensor", cfre, cfre, den, ALU.mult, r=S, w=S)
        p.V("dve", "tensor_tensor", t1, abim, are[:], ALU.mult, r=S + [are], w=S)
        p.V("dve", "tensor_tensor", t2, abre, aim[:], ALU.mult, r=S + [aim], w=S)
        p.V("dve", "tensor_tensor", cfim, t1, t2, ALU.subtract, r=S, w=S)
        p.V("dve", "tensor_tensor", cfim, cfim, den, ALU.mult, r=S, w=S)
        bc = lambda a: a.unsqueeze(2).to_broadcast([128, 16, 128])
        p.V("dve", "tensor_tensor", crT[:], cosT[:], bc(cfre), ALU.mult, r=[cosT] + S, w=[crT])
        p.V("dve", "tensor_tensor", sc3[:], sinT[:], bc(cfim), ALU.mult, r=[sinT] + S, w=[sc3])
        p.V("dve", "tensor_tensor", crT[:], crT[:], sc3[:], ALU.add, r=[crT, sc3], w=[crT])
        p.V("dve", "tensor_tensor", ciT[:], cosT[:], bc(cfim), ALU.mult, r=[cosT] + S, w=[ciT])
        p.V("dve", "tensor_tensor", sc3[:], sinT[:], bc(cfre), ALU.mult, r=[sinT] + S, w=[sc3])
        p.V("dve", "tensor_tensor", ciT[:], ciT[:], sc3[:], ALU.subtract, r=[ciT, sc3], w=[ciT])
        cg = math.sqrt(2.0 / math.pi)

        def stream(sx, tlist):
            B0, B1, B2, B3 = 4 * sx, 4 * sx + 1, 4 * sx + 2, 4 * sx + 3
            n_ = lambda s_: "%s_%d" % (s_, sx)
            xt = p.sb(n_("xt"), [128, D]); xb = p.sb(n_("xb"), [128, D], BF16); xT = p.sb(n_("xT"), [128, 8, 128], BF16)
            uTf = p.sb(n_("uTf"), [128, 4, 128]); uTb = p.sb(n_("uTb"), [128, 4, 128], BF16)
            rbuf = p.sb(n_("rbuf"), [128, 2, 4, 128]); rtmp = p.sb(n_("rtmp"), [128, 2, 4, 128]); gsc = p.sb(n_("gsc"), [128, 2, 4, 128])
            hbf = p.sb(n_("hbf"), [128, 2, 16, 128], BF16); hl = p.sb(n_("hl"), [128, 4, 16])
            yA = p.sb(n_("yA"), [128, 4, 128]); ysq = p.sb(n_("ysq"), [128, 4, 128]); gaf = p.sb(n_("gaf"), [128, 4, 128])
            gab = p.sb(n_("gab"), [128, 4, 128], BF16); yag = p.sb(n_("yag"), [128, 4, 128], BF16)
            Hre = p.sb(n_("Hre"), [128, 16]); Him = p.sb(n_("Him"), [128, 16])
            Hn = p.sb(n_("Hn"), [128, 2, 16])
            for tl in tlist:
                nt, ti, sq = tl["nt"], tl["ti"], tl["seq"]
                if nt < 128:
                    p.V("pool", "memset", xt[:], 0.0, w=[xt])
                p.DM("sp", xt[0:nt, :], xin[tl["row0"]:tl["row0"] + nt, :], r=[DR], w=[xt])
                if tl["first"]:
                    if sq < NPS:
                        p.V("pool", "memset", Hre[:], 0.0, w=[Hre]); p.V("pool", "memset", Him[:], 0.0, w=[Him])
                    else:
                        p.DM("sp", Hre[:], st_s5re, r=[DR], w=[Hre])
                        p.DM("sp", Him[:], st_s5im, r=[DR], w=[Him])
                yield
                p.V("act", "activation", xb[0:nt, :], xt[0:nt, :], AF.Copy, r=[xt], w=[xb])
                yield
                for b in range(8):
                    p.V("pe", "transpose", psbf(B0, 1024)[:, b * 128:b * 128 + nt], xb[0:nt, b * 128:(b + 1) * 128], identb[0:nt, 0:nt], r=[xb, identb], w=[PS[B0]])
                p.V("dve", "tensor_copy", xT[:, :, 0:nt], psbf(B0, 1024).rearrange("p (a b) -> p a b", a=8)[:, :, 0:nt], r=[PS[B0]], w=[xT])
                yield
                for ob in range(4):
                    for kc in range(8):
                        p.V("pe", "matmul", PS[B1][:, ob * 128:ob * 128 + nt], win_u[:, kc, ob * 128:(ob + 1) * 128], xT[:, kc, 0:nt],
                            start=(kc == 0), stop=(kc == 7), r=[win_u, xT], w=[PS[B1]])
                ps1v = PS[B1][:, :].rearrange("p (a b) -> p a b", a=4)[:, :, 0:nt]
                p.V("act", "activation", uTf[:, :, 0:nt], ps1v, AF.Copy, r=[PS[B1]], w=[uTf])
                p.V("act", "activation", uTb[:, :, 0:nt], uTf[:, :, 0:nt], AF.Copy, r=[uTf], w=[uTb])
                yield
                lc = nt - 1
                for g4 in range(4):
                    bs = slice(g4 * 4, g4 * 4 + 4)
                    for ri in range(2):
                        for b in range(4):
                            blk = g4 * 4 + b
                            p.V("pe", "matmul", PS[B2 + ri][:, b * 128:b * 128 + nt], bst[:, ri, blk, :], uTb[:, blk // 4, 0:nt], start=True, stop=True,
                                r=[bst, uTb], w=[PS[B2 + ri]])
                    bre = PS[B2][:, :].rearrange("p (a b) -> p a b", a=4)[:, :, 0:nt]
                    bim = PS[B3][:, :].rearrange("p (a b) -> p a b", a=4)[:, :, 0:nt]
                    p.V("dve", "tensor_tensor", rbuf[:, 0, :, 0:nt], bre, crT[:, bs, 0:nt], ALU.mult, r=[PS[B2], crT], w=[(rbuf, 0)])
                    p.V("dve", "tensor_tensor", rbuf[:, 1, :, 0:nt], bre, ciT[:, bs, 0:nt], ALU.mult, r=[PS[B2], ciT], w=[(rbuf, 1)])
                    yield
                    p.V("dve", "tensor_tensor", rtmp[:, 0, :, 0:nt], bim, ciT[:, bs, 0:nt], ALU.mult, r=[PS[B3], ciT], w=[(rtmp, 0)])
                    p.V("dve", "tensor_tensor", rtmp[:, 1, :, 0:nt], bim, crT[:, bs, 0:nt], ALU.mult, r=[PS[B3], crT], w=[(rtmp, 1)])
                    yield
                    p.V("pool", "tensor_tensor", rbuf[:, 0, :, 0:nt], rbuf[:, 0, :, 0:nt], rtmp[:, 0, :, 0:nt], ALU.subtract, r=[(rbuf, 0), (rtmp, 0)], w=[(rbuf, 0)])
                    p.V("pool", "tensor_tensor", rbuf[:, 1, :, 0:nt], rbuf[:, 1, :, 0:nt], rtmp[:, 1, :, 0:nt], ALU.add, r=[(rbuf, 1), (rtmp, 1)], w=[(rbuf, 1)])
                    yield
                    for b in range(4):
                        blk = g4 * 4 + b
                        p.V("dve", "tensor_tensor_scan", gsc[:, 0, b, 0:nt], lam[:, blk:blk + 1].to_broadcast([128, nt]), rbuf[:, 0, b, 0:nt],
                            Hre[:, blk:blk + 1], ALU.mult, ALU.add, r=[lam, (rbuf, 0), Hre], w=[(gsc, (0, b))])
                        p.V("dve", "tensor_tensor_scan", gsc[:, 1, b, 0:nt], lam[:, blk:blk + 1].to_broadcast([128, nt]), rbuf[:, 1, b, 0:nt],
                            Him[:, blk:blk + 1], ALU.mult, ALU.add, r=[lam, (rbuf, 1), Him], w=[(gsc, (1, b))])
                        yield
                    p.V("pool", "tensor_tensor", rbuf[:, 0, :, 0:nt], gsc[:, 0, :, 0:nt], cosT[:, bs, 0:nt], ALU.mult, r=[gsc, cosT], w=[(rbuf, 0)])
                    p.V("pool", "tensor_tensor", rbuf[:, 1, :, 0:nt], gsc[:, 1, :, 0:nt], sinT[:, bs, 0:nt], ALU.mult, r=[gsc, sinT], w=[(rbuf, 1)])
                    yield
                    p.V("pool", "tensor_tensor", rtmp[:, 0, :, 0:nt], gsc[:, 0, :, 0:nt], sinT[:, bs, 0:nt], ALU.mult, r=[gsc, sinT], w=[(rtmp, 0)])
                    p.V("pool", "tensor_tensor", rtmp[:, 1, :, 0:nt], gsc[:, 1, :, 0:nt], cosT[:, bs, 0:nt], ALU.mult, r=[gsc, cosT], w=[(rtmp, 1)])
                    yield
                    p.V("dve", "tensor_tensor", hbf[:, 0, bs, 0:nt], rbuf[:, 0, :, 0:nt], rbuf[:, 1, :, 0:nt], ALU.subtract, r=[rbuf], w=[(hbf, (0, g4))])
                    p.V("dve", "scalar_tensor_tensor", hbf[:, 1, bs, 0:nt], rtmp[:, 0, :, 0:nt], -1.0, rtmp[:, 1, :, 0:nt], ALU.mult, ALU.subtract,
                        r=[rtmp], w=[(hbf, (1, g4))])
                    yield
                    p.V("dve", "tensor_tensor", Hn[:, 0, bs], rbuf[:, 0, :, lc], rbuf[:, 1, :, lc], ALU.subtract, r=[rbuf], w=[(Hn, (0, g4))])
                    p.V("dve", "tensor_tensor", Hn[:, 1, bs], rtmp[:, 0, :, lc], rtmp[:, 1, :, lc], ALU.add, r=[rtmp], w=[(Hn, (1, g4))])
                    yield
                p.V("dve", "tensor_copy", Hre[:], Hn[:, 0, :], r=[Hn], w=[Hre])
                p.V("dve", "tensor_copy", Him[:], Hn[:, 1, :], r=[Hn], w=[Him])
                if tl["last"]:
                    p.DM("sp", o_s5re[sq], Hre[:], r=[Hre], w=[T_out])
                    p.DM("sp", o_s5im[sq], Him[:], r=[Him], w=[T_out])
                yield
                for ob in range(4):
                    n = 0
                    for b4 in range(4):
                        blk = ob * 4 + b4
                        for ri in range(2):
                            p.V("pe", "matmul", PS[B0][:, ob * 128:ob * 128 + nt], cstt[:, ri, blk, :], hbf[:, ri, blk, 0:nt],
                                start=(n == 0), stop=(n == 7), r=[cstt, hbf], w=[PS[B0]])
                            n += 1
                yield
                for ob in range(4):
                    p.V("dve", "scalar_tensor_tensor", yA[:, ob, 0:nt], uTf[:, ob, 0:nt], dsk[:, ob:ob + 1], PS[B0][:, ob * 128:ob * 128 + nt],
                        ALU.mult, ALU.add, r=[uTf, dsk, PS[B0]], w=[(yA, ob)])
                yield
                p.V("act", "activation", ysq[:, :, 0:nt], yA[:, :, 0:nt], AF.Square, r=[yA], w=[ysq])
                yield
                p.V("dve", "tensor_scalar", ysq[:, :, 0:nt], ysq[:, :, 0:nt], 2.0 * cg * 0.044715, 2.0 * cg, ALU.mult, ALU.add, r=[ysq], w=[ysq])
                yield
                p.V("pool", "tensor_tensor", ysq[:, :, 0:nt], ysq[:, :, 0:nt], yA[:, :, 0:nt], ALU.mult, r=[ysq, yA], w=[ysq])
                yield
                p.V("act", "activation", ysq[:, :, 0:nt], ysq[:, :, 0:nt], AF.Sigmoid, r=[ysq], w=[ysq])
                yield
                p.V("pool", "tensor_tensor", gaf[:, :, 0:nt], ysq[:, :, 0:nt], yA[:, :, 0:nt], ALU.mult, r=[ysq, yA], w=[gaf])
                yield
                p.V("act", "activation", gab[:, :, 0:nt], gaf[:, :, 0:nt], AF.Copy, r=[gaf], w=[gab])
                yield
                for ob in range(4):
                    for kc in range(4):
                        p.V("pe", "matmul", PS[B1][:, ob * 128:ob * 128 + nt], wglu[:, kc, ob * 128:(ob + 1) * 128], gab[:, kc, 0:nt],
                            start=(kc == 0), stop=(kc == 3), r=[wglu, gab], w=[PS[B1]])
                yield
                for ob in range(4):
                    p.V("act", "activation", ysq[:, ob, 0:nt], PS[B1][:, ob * 128:ob * 128 + nt], AF.Sigmoid, bias=bgl[:, ob:ob + 1],
                        r=[PS[B1], bgl], w=[ysq])
                yield
                p.V("pool", "tensor_tensor", yag[:, :, 0:nt], ysq[:, :, 0:nt], gaf[:, :, 0:nt], ALU.mult, r=[ysq, gaf], w=[yag])
                p.DM("sp", yas[ti, :, :, 0:nt], yag[:, :, 0:nt], r=[yag], w=[(T_yas, ti)])
                yield

        lists = [[], []]
        for tl in tiles:
            lists[tl["seq"] % 2].append(tl)
        run_streams([stream(0, lists[0]), stream(1, lists[1])])
        p.release(m0)

    def phaseG0():
        m0 = p.mark()
        NG = EVEN_IN - 512
        lmi = p.sb("lmi", [128, 128]); lms = p.sb("lms", [128, 128])
        p.DM("sp", lmi[:], cst["lmi"], r=[DR], w=[lmi])
        p.DM("sp", lms[:], cst["lms"], r=[DR], w=[lms])
        win = p.sb("win_g", [128, 8, NG], BF16)
        vsrc = w_in_even.rearrange("(kc q) n -> q kc n", q=128)
        p.DM("pool", win[:, :, 0:1024], vsrc[:, :, 512:1536], r=[DR], w=[(win, 0)])
        p.DM("pool", win[:, :, 1024:NG], vsrc[:, :, 1536:EVEN_IN], r=[DR], w=[(win, 1)])
        wout = p.sb("wout", [128, 8, D], BF16)
        load_w_bf16(wout, w_out_even, 8)
        lnw, rw, rb = load_ln_router(0)
        wc = p.sb("wc", [128, 12, 4])
        p.DM("sp", wc[:], gdn_convw, r=[DR], w=[wc])
        alog = p.sb("alog", [128, 4]); dtb = p.sb("dtb", [128, 4]); nrmw = p.sb("nrmw", [128, 128])
        bcast_load(alog, gdn_alog); bcast_load(dtb, gdn_dtb); bcast_load(nrmw, gdn_normw)
        p.V("act", "activation", alog[:], alog[:], AF.Exp, r=[alog], w=[alog])
        Sg = p.sb("Sg", [128, 4, 128])
        ctx3 = p.sb("ctx3", [128, 12, 3])
        xts = [p.sb("xt0", [128, D]), p.sb("xt1", [128, D])]
        xb = p.sb("xb", [128, D], BF16)
        xT = p.sb("xT", [128, 8, 128], BF16)
        cb = p.sb("cb", [128, 12, 131])
        cacc = p.sb("cacc", [128, 12, 128]); ctmp = p.sb("ctmp", [128, 12, 128])
        ztok = p.sb("ztok", [128, 8])
        mixT = p.sb("mixT", [128, 8, 128], BF16)
        qkn = p.sb("qkn", [128, 8, 128]); sq8 = p.sb("sq8", [128, 8, 128])
        kvtok = p.sb("kvtok", [128, 8, 128])
        gd = p.sb("gd", [128, 16]); gd2 = p.sb("gd2", [128, 4, 8])
        HT = []
        for hd in range(4):
            HT.append([p.sb("gt%d_%d" % (hd, i), [128, 128]) for i in range(17)])
        yB = p.sb("yB", [128, 512], BF16); ssq = p.sb("ssq", [128, 4])
        zs = p.sb("zs", [128, 512], BF16)
        work = alloc_tail_work()

        def load_x(tl, buf):
            if tl["nt"] < 128:
                p.V("pool", "memset", buf[:], 0.0, w=[buf])
            p.DM("sp", buf[0:tl["nt"], :], xin[tl["row0"]:tl["row0"] + tl["nt"], :], r=[DR], w=[buf])

        def head(hd, nt):
            A, B = 2 * hd, 2 * hd + 1
            gbc, dec, erow, attn, attnT, rv, rk_, nwT, ub, qdT, kd, Yt, M0, M1, T0, T1, osb = HT[hd]
            Mm = [M0, M1]; MT = [T0, T1]
            g2 = gd2[:, hd, :]
            kT = qkn[:, 4 + hd, 0:nt]
            qT = qkn[:, hd, 0:nt]
            p.V("dve", "tensor_scalar", gbc[0:nt, :], ones_f[0:nt, :], gd[0:nt, 8 + hd:9 + hd], None, ALU.mult, r=[ones_f, (gd, "g")], w=[gbc])
            yield
            p.V("pe", "matmul", PS[A][:, 0:nt], gbc[0:nt, :], uincl[0:nt, 0:nt], start=True, stop=True, r=[gbc, uincl], w=[PS[A]])
            p.V("pe", "matmul", PS[A][0:nt, 128:128 + nt], kT, kT, start=True, stop=True, r=[(qkn, "k")], w=[PS[A]])
            p.V("pe", "matmul", PS[A][0:nt, 256:256 + nt], qT, kT, start=True, stop=True, r=[(qkn, "q"), (qkn, "k")], w=[PS[A]])
            yield
            p.V("act", "activation", dec[0:nt, 0:nt], PS[A][0:nt, 0:nt], AF.Exp, bias=gd[0:nt, 12 + hd:13 + hd], scale=-1.0, r=[PS[A], (gd, "G")], w=[dec])
            yield
            p.V("act", "activation", erow[:, 0:nt], PS[A][:, 0:nt], AF.Exp, r=[PS[A]], w=[erow])
            yield
            p.V("dve", "tensor_copy", g2[:, 4:5], PS[A][:, nt - 1:nt], r=[PS[A]], w=[(gd2, (hd, "gl"))])
            yield
            p.V("dve", "scalar_tensor_tensor", dec[0:nt, 0:nt], dec[0:nt, 0:nt], 1.0, lmi[0:nt, 0:nt], ALU.min, ALU.mult, r=[dec, lmi], w=[dec])
            yield
            p.V("dve", "scalar_tensor_tensor", Mm[0][0:nt, 0:nt], PS[A][0:nt, 128:128 + nt], gd[0:nt, 4 + hd:5 + hd], dec[0:nt, 0:nt], ALU.mult, ALU.mult,
                r=[PS[A], (gd, "nb"), dec], w=[Mm[0]])
            yield
            p.V("dve", "tensor_tensor", Mm[0][0:nt, 0:nt], Mm[0][0:nt, 0:nt], lms[0:nt, 0:nt], ALU.mult, r=[Mm[0], lms], w=[Mm[0]])
            yield
            p.V("dve", "tensor_tensor", attn[0:nt, 0:nt], PS[A][0:nt, 256:256 + nt], dec[0:nt, 0:nt], ALU.mult, r=[PS[A], dec], w=[attn])
            yield
            p.V("pe", "transpose", PS[B][0:nt, 0:nt], Mm[0][0:nt, 0:nt], identf[0:nt, 0:nt], r=[Mm[0], identf], w=[PS[B]])
            p.V("pe", "transpose", PS[B][0:nt, 128:128 + nt], attn[0:nt, 0:nt], identf[0:nt, 0:nt], r=[attn, identf], w=[PS[B]])
            yield
            p.V("act", "activation", MT[0][0:nt, 0:nt], PS[B][0:nt, 0:nt], AF.Copy, r=[PS[B]], w=[MT[0]])
            yield
            p.V("dve", "tensor_tensor", Yt[0:nt, 0:nt], PS[B][0:nt, 0:nt], identf[0:nt, 0:nt], ALU.add, r=[PS[B], identf], w=[Yt])
            yield
            p.V("act", "activation", attnT[0:nt, 0:nt], PS[B][0:nt, 128:128 + nt], AF.Copy, r=[PS[B]], w=[attnT])
            yield
            nlev = 6 if nt == 128 else 3
            for lv in range(nlev):
                a, b_ = lv % 2, (lv + 1) % 2
                bank = A if lv % 2 == 0 else B
                p.V("pe", "matmul", PS[bank][0:nt, 0:nt], MT[a][0:nt, 0:nt], Mm[a][0:nt, 0:nt], start=True, stop=True, r=[MT[a], Mm[a]], w=[PS[bank]])
                if lv < nlev - 1:
                    p.V("pe", "matmul", PS[bank][0:nt, 128:128 + nt], Mm[a][0:nt, 0:nt], MT[a][0:nt, 0:nt], start=True, stop=True, r=[MT[a], Mm[a]], w=[PS[bank]])
                yield
                p.V("act", "activation", Mm[b_][0:nt, 0:nt], PS[bank][0:nt, 0:nt], AF.Copy, r=[PS[bank]], w=[Mm[b_]])
                yield
                if lv < nlev - 1:
                    p.V("dve", "tensor_copy", MT[b_][0:nt, 0:nt], PS[bank][0:nt, 128:128 + nt], r=[PS[bank]], w=[MT[b_]])
                    yield
                p.V("pe", "matmul", PS[bank][0:nt, 256:256 + nt], Mm[b_][0:nt, 0:nt], Yt[0:nt, 0:nt], start=True, stop=True, r=[Mm[b_], Yt], w=[PS[bank]])
                yield
                p.V("dve", "tensor_tensor", Yt[0:nt, 0:nt], Yt[0:nt, 0:nt], PS[bank][0:nt, 256:256 + nt], ALU.add, r=[Yt, PS[bank]], w=[Yt])
                yield
            p.V("dve", "tensor_scalar", rv[0:nt, :], kvtok[0:nt, 4 + hd, :], gd[0:nt, hd:hd + 1], None, ALU.mult, r=[(kvtok, "v"), (gd, "b")], w=[rv])
            p.V("pool", "tensor_scalar", rk_[0:nt, :], kvtok[0:nt, hd, :], gd[0:nt, 16 + hd:17 + hd] if False else g2[0:nt, 0:1], None, ALU.mult, r=[(kvtok, "k"), (gd2, (hd, "be"))], w=[rk_])
            yield
            p.V("pe", "matmul", PS[A][:, 0:nt], rk_[0:nt, :], Yt[0:nt, 0:nt], start=True, stop=True, r=[rk_, Yt], w=[PS[A]])
            yield
            p.V("dve", "tensor_scalar", nwT[:, 0:nt], PS[A][:, 0:nt], -1.0, None, ALU.mult, r=[PS[A]], w=[nwT])
            yield
            p.V("pe", "matmul", PS[B][0:nt, 0:128], Yt[0:nt, 0:nt], rv[0:nt, :], start=True, stop=False, r=[Yt, rv], w=[PS[B]])
            p.V("pe", "matmul", PS[B][0:nt, 0:128], nwT[:, 0:nt], Sg[:, hd, :], start=False, stop=True, r=[nwT, (Sg, hd)], w=[PS[B]])
            yield
            p.V("act", "activation", ub[0:nt, :], PS[B][0:nt, 0:128], AF.Copy, r=[PS[B]], w=[ub])
            yield
            p.V("pool", "tensor_tensor", qdT[:, 0:nt], qT, erow[:, 0:nt], ALU.mult, r=[(qkn, "q"), erow], w=[qdT])
            yield
            p.V("pe", "matmul", PS[A][0:nt, 128:256], qdT[:, 0:nt], Sg[:, hd, :], start=True, stop=False, r=[qdT, (Sg, hd)], w=[PS[A]])
            p.V("pe", "matmul", PS[A][0:nt, 128:256], attnT[0:nt, 0:nt], ub[0:nt, :], start=False, stop=True, r=[attnT, ub], w=[PS[A]])
            yield
            p.V("act", "activation", g2[0:nt, 1:2], gd[0:nt, 12 + hd:13 + hd], AF.Exp, bias=g2[0:nt, 4:5], scale=-1.0,
                r=[(gd, "G"), (gd2, (hd, "gl"))], w=[(gd2, (hd, "kd"))])
            yield
            p.V("dve", "tensor_scalar", kd[0:nt, :], kvtok[0:nt, hd, :], g2[0:nt, 1:2], None, ALU.mult, r=[(kvtok, "k"), (gd2, (hd, "kd"))], w=[kd])
            yield
            p.V("pe", "matmul", PS[B][:, 128:256], kd[0:nt, :], ub[0:nt, :], start=True, stop=True, r=[kd, ub], w=[PS[B]])
            yield
            p.V("act", "activation", g2[:, 5:6], g2[:, 4:5], AF.Exp, r=[(gd2, (hd, "gl"))], w=[(gd2, (hd, "egl"))])
            yield
            p.V("dve", "scalar_tensor_tensor", Sg[:, hd, :], Sg[:, hd, :], g2[:, 5:6], PS[B][:, 128:256], ALU.mult, ALU.add,
                r=[(Sg, hd), (gd2, (hd, "egl")), PS[B]], w=[(Sg, hd)])
            yield
            p.V("act", "activation", osb[0:nt, :], PS[A][0:nt, 128:256], AF.Square, r=[PS[A]], w=[osb])
            yield
            p.V("dve", "reduce_sum", ssq[0:nt, hd:hd + 1], osb[0:nt, :], mybir.AxisListType.X, r=[osb], w=[(ssq, hd)])
            yield
            p.V("act", "activation", ssq[0:nt, hd:hd + 1], ssq[0:nt, hd:hd + 1], AF.Sqrt, bias=epsln[0:nt, 1:2], scale=1.0 / 128.0, r=[(ssq, hd), epsln], w=[(ssq, hd)])
            yield
            p.V("dve", "reciprocal", ssq[0:nt, hd:hd + 1], ssq[0:nt, hd:hd + 1], r=[(ssq, hd)], w=[(ssq, hd)])
            yield
            p.V("dve", "scalar_tensor_tensor", osb[0:nt, :], PS[A][0:nt, 128:256], ssq[0:nt, hd:hd + 1], nrmw[0:nt, :], ALU.mult, ALU.mult,
                r=[PS[A], (ssq, hd), nrmw], w=[osb])
            yield
            p.V("pool", "tensor_tensor", yB[0:nt, hd * 128:(hd + 1) * 128], osb[0:nt, :], zs[0:nt, hd * 128:(hd + 1) * 128], ALU.mult, r=[osb, zs], w=[(yB, hd)])
            yield

        load_x(tiles[0], xts[0])
        for tl in tiles:
            nt, ti, sq = tl["nt"], tl["ti"], tl["seq"]
            xt = xts[ti % 2]
            if ti + 1 < NT:
                load_x(tiles[ti + 1], xts[(ti + 1) % 2])
            if tl["first"]:
                if sq < NPS:
                    p.V("pool", "memset", Sg[:], 0.0, w=[Sg]); p.V("pool", "memset", ctx3[:], 0.0, w=[ctx3])
                else:
                    p.DM("sp", Sg[:], st_gdn.rearrange("h k v -> k h v"), r=[DR], w=[Sg])
                    p.DM("sp", ctx3[:], st_conv, r=[DR], w=[ctx3])
            p.DM("sp", mixT[:, 0:4, 0:nt], yas[ti, :, :, 0:nt], r=[(T_yas, ti)], w=[(mixT, "a")])
            p.V("act", "activation", xb[0:nt, :], xt[0:nt, :], AF.Copy, r=[xt], w=[xb])
            transpose_to(xT, xb, nt, 8, 0)
            p.V("pool", "tensor_copy", cb[:, :, 0:3], ctx3[:, :, :], r=[ctx3], w=[(cb, "c")])
            for g4 in range(3):
                bank = 1 + g4
                for b in range(4):
                    blk = g4 * 4 + b
                    for kc in range(8):
                        p.V("pe", "matmul", PS[bank][:, b * 128:b * 128 + nt], win[:, kc, blk * 128:(blk + 1) * 128],
                            xT[:, kc, 0:nt], start=(kc == 0), stop=(kc == 7), r=[win, xT], w=[PS[bank]])
                evac(cb[:, g4 * 4:g4 * 4 + 4, 3:3 + nt], PS[bank][:, :].rearrange("p (a b) -> p a b", a=4)[:, :, 0:nt],
                     r=[PS[bank]], w=[(cb, g4)])
            for kc in range(8):
                p.V("pe", "matmul", PS[4][0:nt, 0:512], xT[:, kc, 0:nt], win[:, kc, 1536:2048], start=(kc == 0), stop=(kc == 7),
                    r=[xT, win], w=[PS[4]])
            for kc in range(8):
                p.V("pe", "matmul", PS[5][0:nt, 0:8], xT[:, kc, 0:nt], win[:, kc, 2048:2056], start=(kc == 0), stop=(kc == 7),
                    r=[xT, win], w=[PS[5]])
            p.V("act", "activation", zs[0:nt, :], PS[4][0:nt, 0:512], AF.Silu, r=[PS[4]], w=[zs])
            p.V("dve", "tensor_copy", ztok[0:nt, 0:8], PS[5][0:nt, 0:8], r=[PS[5]], w=[ztok])
            for j in range(4):
                wj = wc[:, :, j:j + 1].to_broadcast([128, 12, nt])
                if j == 0:
                    p.V("dve", "tensor_tensor", cacc[:, :, 0:nt], cb[:, :, 0:nt], wj, ALU.mult, r=[cb, wc], w=[cacc])
                else:
                    p.V("pool", "tensor_tensor", ctmp[:, :, 0:nt], cb[:, :, j:j + nt], wj, ALU.mult, r=[cb, wc], w=[ctmp])
                    p.V("dve", "tensor_tensor", cacc[:, :, 0:nt], cacc[:, :, 0:nt], ctmp[:, :, 0:nt], ALU.add, r=[cacc, ctmp], w=[cacc])
            p.V("pool", "tensor_copy", ctx3[:, :, :], cb[:, :, nt:nt + 3], r=[cb], w=[ctx3])
            if tl["last"]:
                p.DM("sp", o_conv[sq], ctx3[:], r=[ctx3], w=[T_out])
            p.V("act", "activation", cacc[:, :, 0:nt], cacc[:, :, 0:nt], AF.Silu, r=[cacc], w=[cacc])
            p.V("act", "activation", sq8[:, :, 0:nt], cacc[:, 0:8, 0:nt], AF.Square, r=[cacc], w=[sq8])
            for hb in range(2):
                for b in range(4):
                    p.V("pe", "matmul", PS[2 + hb][:, b * 128:b * 128 + nt], ones_f[:, :], sq8[:, hb * 4 + b, 0:nt], start=True, stop=True,
                        r=[ones_f, sq8], w=[PS[2 + hb]])
            for hb in range(2):
                v = PS[2 + hb][:, :].rearrange("p (a b) -> p a b", a=4)[:, :, 0:nt]
                p.V("act", "activation", sq8[:, hb * 4:hb * 4 + 4, 0:nt], v, AF.Sqrt, bias=epsln[:, 1:2], r=[PS[2 + hb], epsln], w=[sq8])
            p.V("dve", "reciprocal", sq8[:, :, 0:nt], sq8[:, :, 0:nt], r=[sq8], w=[sq8])
            p.V("dve", "scalar_tensor_tensor", qkn[:, 0:4, 0:nt], cacc[:, 0:4, 0:nt], 128.0 ** -0.5, sq8[:, 0:4, 0:nt], ALU.mult, ALU.mult,
                r=[cacc, sq8], w=[(qkn, "q")])
            p.V("pool", "tensor_tensor", qkn[:, 4:8, 0:nt], cacc[:, 4:8, 0:nt], sq8[:, 4:8, 0:nt], ALU.mult, r=[cacc, sq8], w=[(qkn, "k")])
            for b in range(4):
                p.V("pe", "transpose", PS[2][0:nt, b * 128:(b + 1) * 128], qkn[:, 4 + b, 0:nt], identf[:, :], r=[(qkn, "k"), identf], w=[PS[2]])
                p.V("pe", "transpose", PS[3][0:nt, b * 128:(b + 1) * 128], cacc[:, 8 + b, 0:nt], identf[:, :], r=[cacc, identf], w=[PS[3]])
            evac(kvtok[0:nt, 0:4, :], PS[2][0:nt, :].rearrange("p (a b) -> p a b", a=4), r=[PS[2]], w=[(kvtok, "k")])
            evac(kvtok[0:nt, 4:8, :], PS[3][0:nt, :].rearrange("p (a b) -> p a b", a=4), r=[PS[3]], w=[(kvtok, "v")])
            p.V("act", "activation", gd[0:nt, 0:4], ztok[0:nt, 0:4], AF.Sigmoid, r=[ztok], w=[(gd, "b")])
            p.V("dve", "tensor_scalar", gd[0:nt, 4:8], gd[0:nt, 0:4], -1.0, None, ALU.mult, r=[(gd, "b")], w=[(gd, "nb")])
            p.V("dve", "tensor_tensor", gd[0:nt, 8:12], ztok[0:nt, 4:8], dtb[0:nt, :], ALU.add, r=[ztok, dtb], w=[(gd, "g")])
            p.V("act", "activation", gd[0:nt, 8:12], gd[0:nt, 8:12], AF.Exp, r=[(gd, "g")], w=[(gd, "g")])
            p.V("act", "activation", gd[0:nt, 8:12], gd[0:nt, 8:12], AF.Ln, bias=1.0, r=[(gd, "g")], w=[(gd, "g")])
            p.V("dve", "scalar_tensor_tensor", gd[0:nt, 8:12], gd[0:nt, 8:12], -1.0, alog[0:nt, :], ALU.mult, ALU.mult, r=[(gd, "g"), alog], w=[(gd, "g")])
            p.V("pe", "matmul", PS[0][0:nt, 0:4], uincl[0:nt, 0:nt], gd[0:nt, 8:12], start=True, stop=True, r=[uincl, (gd, "g")], w=[PS[0]])
            p.V("dve", "tensor_copy", gd[0:nt, 12:16], PS[0][0:nt, 0:4], r=[PS[0]], w=[(gd, "G")])
            p.V("act", "activation", gd2[0:nt, :, 2], gd[0:nt, 12:16], AF.Exp, r=[(gd, "G")], w=[(gd2, "e")])
            p.V("dve", "tensor_tensor", gd2[0:nt, :, 0], gd2[0:nt, :, 2], gd[0:nt, 0:4], ALU.mult, r=[(gd2, "e"), (gd, "b")],
                w=[(gd2, (0, "be")), (gd2, (1, "be")), (gd2, (2, "be")), (gd2, (3, "be"))])
            run_streams([head(hd, nt) for hd in range(4)])
            if tl["last"]:
                p.DM("sp", o_gdn[sq].rearrange("h k v -> k h v"), Sg[:], r=[Sg], w=[T_out])
            for b in range(4):
                p.V("pe", "transpose", psbf(2, 1024)[:, b * 128:b * 128 + nt], yB[0:nt, b * 128:(b + 1) * 128], identb[0:nt, 0:nt], r=[yB, identb], w=[PS[2]])
            evac(mixT[:, 4:8, 0:nt], psbf(2, 1024).rearrange("p (a b) -> p a b", a=8)[:, 0:4, 0:nt], r=[PS[2]], w=[(mixT, "b")])
            for half in range(2):
                for kc in range(8):
                    p.V("pe", "matmul", PS[2 + half][0:nt, :], mixT[:, kc, 0:nt], wout[:, kc, half * 512:(half + 1) * 512], start=(kc == 0), stop=(kc == 7),
                        r=[mixT, wout], w=[PS[2 + half]])
            phaseA_tail(0, tl, xt, (2, 3), lnw, rw, rb, work)
        p.release(m0)

    def phaseA1():
        m0 = p.mark()
        win = p.sb("win1", [128, 8, ODD_IN], BF16)
        load_w_bf16(win, w_in_odd, 8)
        wout = p.sb("wout1", [128, 16, D], BF16)
        load_w_bf16(wout, w_out_odd, 16)
        lnw, rw, rb = load_ln_router(1)
        retmask = p.sb("retmask", [128, 4, 128]); retqs = p.sb("retqs", [128, 4, 128]); retks = p.sb("retks", [128, 8])
        for b, nm in ((retmask, "retmask"), (retqs, "retqs"), (retks, "retks")):
            p.DM("sp", b[:], cst[nm], r=[DR], w=[b])
        Rf = p.sb("Rf", [128, 4, 2, 512])
        xt_one = p.sb("xt0", [128, D])
        xts = [xt_one, xt_one]
        xb = p.sb("xb", [128, D], BF16)
        xT = p.sb("xT", [128, 8, 128], BF16)
        cs = p.sb("cs", [128, 2, 128])
        qkT = p.sb("qkT", [128, 16, 128], BF16)
        qsT = p.sb("qsT", [128, 2, 128])
        rt1 = p.sb("rt1", [128, 128]); rt2 = p.sb("rt2", [128, 128])
        ktok = p.sb("ktok", [128, 8, 128], BF16)
        vtok = p.sb("vtok", [128, 2048], BF16); gsil = p.sb("gsil", [128, 2048], BF16)
        sT = p.sb("sT", [128, 128], BF16)
        og = vtok
        ogT = p.sb("ogT", [128, 16, 128], BF16)
        onrm = p.sb("onrm", [128, 512]); st6 = p.sb("st6", [128, 6]); st2 = p.sb("st2", [128, 4])
        work = alloc_tail_work(h=xt_one)

        sT_h = [sT, p.sb("sT1", [128, 128], BF16)] * 2
        qsT_h = [qsT, p.sb("qsT1", [128, 2, 128])] * 2
        onrm_h = [onrm, p.sb("onrm1", [128, 512])] * 2
        st6_h = [st6, p.sb("st61", [128, 6])] * 2
        st2_h = [st2, p.sb("st21", [128, 4])] * 2
        print("A1 sbuf words", p.top)

        def rhead(h, nt, Lc):
            A, B = 2 * h, 2 * h + 1
            sT, qsT, onrm, st6, st2 = sT_h[h], qsT_h[h], onrm_h[h], st6_h[h], st2_h[h]
            for dc in range(2):
                p.V("pe", "matmul", PS[A][0:nt, 0:nt], qkT[:, 8 + 2 * h + dc, 0:nt], qkT[:, 2 * h + dc, 0:nt], start=(dc == 0), stop=(dc == 1),
                    r=[(qkT, 8 + 2 * h + dc), (qkT, 2 * h + dc)], w=[PS[A]])
            yield
            p.V("dve", "scalar_tensor_tensor", sT[0:nt, 0:nt], PS[A][0:nt, 0:nt], 256.0 ** -0.5, retmask[0:nt, h, 0:nt], ALU.mult, ALU.mult,
                r=[PS[A], retmask], w=[sT])
            yield
            for dc in range(2):
                p.V("pool", "tensor_tensor", qsT[:, dc, 0:nt], qkT[:, 2 * h + dc, 0:nt], retqs[:, h, 0:nt], ALU.mult, r=[(qkT, 2 * h + dc), retqs], w=[(qsT, dc)])
                yield
            p.V("pe", "matmul", PS[B][0:nt, :], sT[0:nt, 0:nt], vtok[0:nt, h * 512:(h + 1) * 512], start=True, stop=False, r=[sT, (vtok, h)], w=[PS[B]])
            for dc in range(2):
                p.V("pe", "matmul", PS[B][0:nt, :], qsT[:, dc, 0:nt], Rf[:, h, dc, :], start=False, stop=(dc == 1), r=[(qsT, dc), (Rf, (h, dc))], w=[PS[B]])
            yield
            cdec = cst_host["retcdec"][Lc][h]
            for dc in range(2):
                p.V("pe", "matmul", PS[A][:, :], ktok[0:nt, 2 * h + dc, :], vtok[0:nt, h * 512:(h + 1) * 512], start=True, stop=True,
                    r=[(ktok, h), (vtok, h)], w=[PS[A]])
                yield
                p.V("dve", "scalar_tensor_tensor", Rf[:, h, dc, :], Rf[:, h, dc, :], cdec, PS[A][:, :], ALU.mult, ALU.add, r=[(Rf, (h, dc)), PS[A]], w=[(Rf, (h, dc))])
                yield
            p.V("dve", "bn_stats", st6[0:nt, :], PS[B][0:nt, :], r=[PS[B]], w=[st6])
            yield
            p.V("dve", "bn_aggr", st2[0:nt, 0:2], st6[0:nt, :], r=[st6], w=[st2])
            yield
            p.V("act", "activation", st2[0:nt, 2:3], st2[0:nt, 1:2], AF.Sqrt, bias=epsln[0:nt, 0:1], r=[st2, epsln], w=[(st2, "s")])
            yield
            p.V("dve", "reciprocal", st2[0:nt, 3:4], st2[0:nt, 2:3], r=[(st2, "s")], w=[(st2, "r")])
            yield
            p.V("dve", "tensor_scalar", onrm[0:nt, :], PS[B][0:nt, :], st2[0:nt, 0:1], st2[0:nt, 3:4], ALU.subtract, ALU.mult, r=[PS[B], st2, (st2, "r")], w=[onrm])
            yield
            p.V("pool", "tensor_tensor", og[0:nt, h * 512:(h + 1) * 512], onrm[0:nt, :], gsil[0:nt, h * 512:(h + 1) * 512], ALU.mult, r=[onrm, (gsil, h)], w=[(vtok, h)])
            yield

        def load_x(tl, buf):
            if tl["nt"] < 128:
                p.V("pool", "memset", buf[:], 0.0, w=[buf])
            p.DM("sp", buf[0:tl["nt"], :], x3s[tl["ti"] * 128:tl["ti"] * 128 + tl["nt"], :], r=[(T_x3s, tl["ti"])], w=[buf])

        for tl in tiles:
            nt, ti, sq = tl["nt"], tl["ti"], tl["seq"]
            Lc = 0 if nt == 128 else 1
            xt = xts[ti % 2]
            load_x(tl, xt)
            if tl["first"]:
                if sq < NPS:
                    p.V("pool", "memset", Rf[:], 0.0, w=[Rf])
                else:
                    for h in range(4):
                        for par in range(2):
                            p.DM("sp", Rf[:, h, par, :], st_ret[h].rearrange("(q two) v -> q two v", two=2)[:, par, :], r=[DR], w=[(Rf, (h, par))])
            p.DM("sp", cs[:, 0, 0:nt], cst["rcos"][:, tl["pos0"]:tl["pos0"] + nt], r=[DR], w=[(cs, 0)])
            p.DM("sp", cs[:, 1, 0:nt], cst["rsin"][:, tl["pos0"]:tl["pos0"] + nt], r=[DR], w=[(cs, 1)])
            p.V("act", "activation", xb[0:nt, :], xt[0:nt, :], AF.Copy, r=[xt], w=[xb])
            transpose_to(xT, xb, nt, 8, 0)
            for qk in range(2):
                for h in range(4):
                    bank = 1 + ((qk * 4 + h) % 2)
                    for par in range(2):
                        col0 = qk * 1024 + h * 256 + par * 128
                        for kc in range(8):
                            p.V("pe", "matmul", PS[bank][:, par * 128:par * 128 + nt], win[:, kc, col0:col0 + 128], xT[:, kc, 0:nt],
                                start=(kc == 0), stop=(kc == 7), r=[win, xT], w=[PS[bank]])
                    x0 = PS[bank][:, 0:nt]
                    x1_ = PS[bank][:, 128:128 + nt]
                    blk = qk * 8 + h * 2
                    p.V("dve", "tensor_tensor", rt1[:, 0:nt], x0, cs[:, 0, 0:nt], ALU.mult, r=[PS[bank], cs], w=[rt1])
                    p.V("dve", "tensor_tensor", rt2[:, 0:nt], x1_, cs[:, 1, 0:nt], ALU.mult, r=[PS[bank], cs], w=[rt2])
                    p.V("pool", "tensor_tensor", qkT[:, blk, 0:nt], rt1[:, 0:nt], rt2[:, 0:nt], ALU.subtract, r=[rt1, rt2], w=[(qkT, blk)])
                    p.V("dve", "tensor_tensor", rt1[:, 0:nt], x0, cs[:, 1, 0:nt], ALU.mult, r=[PS[bank], cs, (qkT, blk)], w=[rt1])
                    p.V("dve", "tensor_tensor", rt2[:, 0:nt], x1_, cs[:, 0, 0:nt], ALU.mult, r=[PS[bank], cs, (qkT, blk)], w=[rt2])
                    p.V("pool", "tensor_tensor", qkT[:, blk + 1, 0:nt], rt1[:, 0:nt], rt2[:, 0:nt], ALU.add, r=[rt1, rt2], w=[(qkT, blk + 1)])
            for cg in range(8):
                bank = 3 + (cg % 2)
                for kc in range(8):
                    p.V("pe", "matmul", PS[bank][0:nt, :], xT[:, kc, 0:nt], win[:, kc, 2048 + cg * 512:2048 + (cg + 1) * 512], start=(kc == 0), stop=(kc == 7),
                        r=[xT, win], w=[PS[bank]])
                if cg < 4:
                    p.V("dve", "tensor_copy", vtok[0:nt, cg * 512:(cg + 1) * 512], PS[bank][0:nt, :], r=[PS[bank]], w=[(vtok, cg)])
                else:
                    p.V("act", "activation", gsil[0:nt, (cg - 4) * 512:(cg - 3) * 512], PS[bank][0:nt, :], AF.Silu, r=[PS[bank]], w=[(gsil, cg - 4)])
            for b in range(8):
                p.V("pe", "transpose", psbf(5, 1024)[0:nt, b * 128:(b + 1) * 128], qkT[:, 8 + b, 0:nt], identb[:, :], r=[(qkT, 8 + b), identb], w=[PS[5]])
            for h in range(4):
                p.V("dve", "tensor_scalar", ktok[0:nt, 2 * h:2 * h + 2, :], psbf(5, 1024).rearrange("p (a b) -> p a b", a=8)[0:nt, 2 * h:2 * h + 2, :],
                    retks[0:nt, Lc * 4 + h:Lc * 4 + h + 1], None, ALU.mult, r=[PS[5], retks], w=[(ktok, h)])
            def pair(x):
                yield from rhead(x, nt, Lc)
                yield from rhead(x + 2, nt, Lc)

            run_streams([pair(0), pair(1)])
            if tl["last"]:
                for h in range(4):
                    for par in range(2):
                        p.DM("sp", o_ret[sq, h].rearrange("(q two) v -> q two v", two=2)[:, par, :], Rf[:, h, par, :], r=[Rf], w=[T_out])
            transpose_to(ogT, og, nt, 16, 5)
            for half in range(2):
                for kc in range(16):
                    p.V("pe", "matmul", PS[2 + half][0:nt, :], ogT[:, kc, 0:nt], wout[:, kc, half * 512:(half + 1) * 512], start=(kc == 0), stop=(kc == 15),
                        r=[ogT, wout], w=[PS[2 + half]])
            phaseA_tail(1, tl, xt, (2, 3), lnw, rw, rb, work)
        p.release(m0)

    def phaseM(li):
        m0 = p.mark()
        w1b = [p.sb("w1b0", [128, 8, 2 * D], BF16), p.sb("w1b1", [128, 8, 2 * D], BF16)]
        w2b = [p.sb("w2b0", [128, 8, D], BF16), p.sb("w2b1", [128, 8, D], BF16)]
        b1t = [p.sb("b1t0", [128, 16]), p.sb("b1t1", [128, 16])]
        b2f = [p.sb("b2f0", [1, D]), p.sb("b2f1", [1, D])]
        b2b = [p.sb("b2b0", [1, D], BF16), p.sb("b2b1", [1, D], BF16)]
        xg = [p.sb("xg0", [128, CT, D], BF16), p.sb("xg1", [128, CT, D], BF16)]
        xgT = [p.sb("xgT0", [128, 8, C], BF16), p.sb("xgT1", [128, 8, C], BF16)]
        glu = p.sb("glu", [128, C]); sig = p.sb("sig", [128, C]); lin = p.sb("lin", [128, C])
        actT = p.sb("actT", [128, 8, C], BF16)
        yo = [p.sb("yo0", [128, D]), p.sb("yo1", [128, D])]

        def load_w(e):
            s = e % 2
            load_w_bf16(w1b[s], moe_w1[li, e], 8)
            load_w_bf16(w2b[s], moe_w2[li, e], 8)
            p.DM("sp", b1t[s][:], moe_b1[li, e], r=[DR], w=[b1t[s]])
            p.DM("sp", b2f[s][:], moe_b2[li, e:e + 1, :], r=[DR], w=[b2f[s]])

        def load_xg(e):
            s = e % 2
            p.DM("sp", xg[s][:], xs[e * C:(e + 1) * C, :].rearrange("(ct q) d -> q ct d", q=128), r=[(T_xs, "*")], w=[xg[s], (T_xs, e)])

        def transposes(e):
            s = e % 2
            p.V("act", "activation", b2b[s][:], b2f[s][:], AF.Copy, r=[b2f[s]], w=[b2b[s]])
            for ct in range(CT):
                bank = 4 + (ct % 2)
                for kc in range(8):
                    p.V("pe", "transpose", psbf(bank, 1024)[:, kc * 128:(kc + 1) * 128], xg[s][:, ct, kc * 128:(kc + 1) * 128], identb[:, :],
                        r=[xg[s], identb], w=[PS[bank]])
                evac(xgT[s][:, :, ct * 128:(ct + 1) * 128], psbf(bank, 1024).rearrange("p (a b) -> p a b", a=8), r=[PS[bank]], w=[(xgT[s], ct)])

        load_w(0)
        load_xg(0)
        transposes(0)
        for e in range(NE):
            s = e % 2
            if e + 1 < NE:
                load_w(e + 1)
                load_xg(e + 1)
            for i in range(8):
                for part in range(2):
                    fc = i + part * 8
                    banks = [(0, 1), (2, 3)][(i * 2 + part) % 2]
                    for gi, (ca, cb_) in enumerate(cgs):
                        for kc in range(8):
                            p.V("pe", "matmul", PS[banks[gi]][:, 0:cb_ - ca], w1b[s][:, kc, fc * 128:(fc + 1) * 128], xgT[s][:, kc, ca:cb_],
                                start=(kc == 0), stop=(kc == 7), r=[w1b[s], xgT[s]], w=[PS[banks[gi]]])
                    for gi, (ca, cb_) in enumerate(cgs):
                        src = PS[banks[gi]][:, 0:cb_ - ca]
                        if part == 0:
                            p.V("dve", "tensor_scalar", glu[:, ca:cb_], src, b1t[s][:, fc:fc + 1], 7.0, ALU.add, ALU.min, r=[PS[banks[gi]], b1t[s]], w=[(glu, gi)])
                        else:
                            p.V("dve", "tensor_scalar", lin[:, ca:cb_], src, b1t[s][:, fc:fc + 1], 7.0, ALU.add, ALU.min, r=[PS[banks[gi]], b1t[s]], w=[(lin, gi)])
                    if part == 0:
                        p.V("act", "activation", sig[:, :], glu[:, :], AF.Sigmoid, scale=1.702, r=[glu], w=[sig])
                        p.V("pool", "tensor_tensor", glu[:, :], glu[:, :], sig[:, :], ALU.mult, r=[glu, sig], w=[glu])
                    else:
                        p.V("dve", "tensor_scalar", lin[:, :], lin[:, :], -7.0, 1.0, ALU.max, ALU.add, r=[lin], w=[lin])
                        p.V("pool", "tensor_tensor", actT[:, i, :], glu[:, :], lin[:, :], ALU.mult, r=[glu, lin], w=[(actT, i)])
            if e + 1 < NE:
                transposes(e + 1)
            for ct in range(CT):
                yb = yo[ct % 2]
                for half in range(2):
                    bank = 4 + (ct % 2) * 2 + half
                    for fc in range(8):
                        p.V("pe", "matmul", PS[bank][:, :], actT[:, fc, ct * 128:(ct + 1) * 128], w2b[s][:, fc, half * 512:(half + 1) * 512],
                            start=(fc == 0), stop=False, r=[actT, w2b[s]], w=[PS[bank]])
                    p.V("pe", "matmul", PS[bank][:, :], ones_b[0:1, :], b2b[s][0:1, half * 512:(half + 1) * 512], start=False, stop=True,
                        r=[ones_b, b2b[s]], w=[PS[bank]])
                    evac(yb[:, half * 512:(half + 1) * 512], PS[bank][:, :], r=[PS[bank]], w=[(yb, half)])
                p.DM("act", ys[e * C + ct * 128:e * C + (ct + 1) * 128, :], yb[:, :], r=[yb], w=[(T_ys, (e, ct))])
        p.release(m0)

    def phaseC(li):
        m0 = p.mark()
        wg = p.sb("wg", [128, 8, D], BF16)
        load_w_bf16(wg, ple_gw[li], 8)
        wp = p.sb("wp", [128, 2, D], BF16)
        load_w_bf16(wp, ple_w[li], 2)
        g2 = p.sb("ln2g", [128, D]); b2 = p.sb("ln2b", [128, D])
        bcast_load(g2, ln2_g[li:li + 1, :]); bcast_load(b2, ln2_b[li:li + 1, :])

        def stream(sx, tlist):
            n_ = lambda s_: "%s_%d" % (s_, sx)
            rows = [[p.sb(n_("row%d%d" % (b, k)), [128, D]) for k in range(4)] for b in range(2)]
            x1t = [p.sb(n_("x1t0"), [128, D]), p.sb(n_("x1t1"), [128, D])]
            pt = [p.sb(n_("pt0"), [128, 256]), p.sb(n_("pt1"), [128, 256])]
            ff = p.sb(n_("ff"), [128, D]); x2 = p.sb(n_("x2"), [128, D]); x2b = p.sb(n_("x2b"), [128, D], BF16)
            x2T = p.sb(n_("x2T"), [128, 8, 128], BF16)
            pb = p.sb(n_("pb"), [128, 256], BF16); pT = p.sb(n_("pT"), [128, 2, 128], BF16)
            gt = p.sb(n_("gt"), [128, D]); x3 = p.sb(n_("x3"), [128, D])
            scr6 = p.sb(n_("scr6"), [128, 2, 6]); scr2 = p.sb(n_("scr2"), [128, 4])
            B0 = 4 * sx

            def loads(j):
                tl = tlist[j]
                b = j % 2
                ti, nt = tl["ti"], tl["nt"]
                for k in range(4):
                    p.dma("pool", lambda e, k=k, ti=ti, b=b: e.indirect_dma_start(
                        out=rows[b][k][:, :], out_offset=None, in_=ys[:, :],
                        in_offset=bass.IndirectOffsetOnAxis(ap=slots_all[:, ti, k:k + 1], axis=0)),
                        r=[(T_ys, "*"), (slots_all, ti)], w=[rows[b][k]])
                p.DM("sp", x1t[b][0:nt, :], x1s[ti * 128:ti * 128 + nt, :], r=[(T_x1s, ti)], w=[x1t[b]])
                p.DM("sp", pt[b][0:nt, :], pin[li, tl["row0"]:tl["row0"] + nt, :], r=[DR], w=[pt[b]])

            if tlist:
                loads(0)
            for j, tl in enumerate(tlist):
                nt, ti = tl["nt"], tl["ti"]
                b = j % 2
                if j + 1 < len(tlist):
                    loads(j + 1)
                yield
                p.V("dve", "tensor_scalar", ff[0:nt, :], rows[b][0][0:nt, :], gates_all[0:nt, ti, 0:1], None, ALU.mult, r=[rows[b][0], (gates_all, ti)], w=[ff])
                yield
                for k in range(1, 4):
                    p.V("dve", "scalar_tensor_tensor", ff[0:nt, :], rows[b][k][0:nt, :], gates_all[0:nt, ti, k:k + 1], ff[0:nt, :], ALU.mult, ALU.add,
                        r=[rows[b][k], (gates_all, ti), ff], w=[ff])
                    yield
                p.V("dve", "scalar_tensor_tensor", ff[0:nt, :], x1t[b][0:nt, :], ALPHA, ff[0:nt, :], ALU.mult, ALU.add, r=[x1t[b], ff], w=[ff])
                yield
                layernorm("dve", ff, nt, g2, b2, x2, scr6, scr2)
                yield
                p.V("act", "activation", x2b[0:nt, :], x2[0:nt, :], AF.Copy, r=[x2], w=[x2b])
                yield
                transpose_to(x2T, x2b, nt, 8, B0)
                yield
                p.V("act", "activation", pb[0:nt, :], pt[b][0:nt, :], AF.Copy, r=[pt[b]], w=[pb])
                transpose_to(pT, pb, nt, 2, B0 + 1)
                yield
                for half in range(2):
                    for kc in range(2):
                        p.V("pe", "matmul", PS[B0 + 2 + half][0:nt, :], pT[:, kc, 0:nt], wp[:, kc, half * 512:(half + 1) * 512], start=(kc == 0), stop=(kc == 1),
                            r=[pT, wp], w=[PS[B0 + 2 + half]])
                yield
                for half in range(2):
                    for kc in range(8):
                        p.V("pe", "matmul", PS[B0 + half][0:nt, :], x2T[:, kc, 0:nt], wg[:, kc, half * 512:(half + 1) * 512], start=(kc == 0), stop=(kc == 7),
                            r=[x2T, wg], w=[PS[B0 + half]])
                    yield
                    hs = slice(half * 512, (half + 1) * 512)
                    p.V("act", "activation", gt[0:nt, hs], PS[B0 + half][0:nt, :], AF.Sigmoid, r=[PS[B0 + half]], w=[(gt, half)])
                    yield
                    p.V("dve", "tensor_tensor", gt[0:nt, hs], gt[0:nt, hs], PS[B0 + 2 + half][0:nt, :], ALU.mult, r=[(gt, half), PS[B0 + 2 + half]], w=[(gt, half)])
                    yield
                    p.V("pool", "tensor_tensor", x3[0:nt, hs], gt[0:nt, hs], x2[0:nt, hs], ALU.add, r=[(gt, half), x2], w=[(x3, half)])
                    yield
                if li == 0:
                    p.DM("sp", x3s[ti * 128:ti * 128 + nt, :], x3[0:nt, :], r=[x3], w=[(T_x3s, ti)])
                    if "x3_0" in dbg_out:
                        p.DM("sp", dbg_out["x3_0"][tl["row0"]:tl["row0"] + nt, :], x3[0:nt, :], r=[x3], w=[T_out])
                else:
                    p.DM("sp", y_out[tl["row0"]:tl["row0"] + nt, :], x3[0:nt, :], r=[x3], w=[T_out])
                yield

        run_streams([stream(0, tiles[0::2]), stream(1, tiles[1::2])])
        p.release(m0)

    cst_host = host_consts()
    for st in stages:
        if st == "A0":
            if os.environ.get("KOLDA0"):
                phaseA0()
            else:
                phaseS0()
                phaseG0()
        elif st == "A1":
            phaseA1()
        elif st[0] == "M":
            phaseM(int(st[1]))
        elif st[0] == "C":
            phaseC(int(st[1]))
    p.finalize()
    return nc


def prep_shared(inp):
    f = lambda a: np.ascontiguousarray(np.asarray(a, dtype=np.float32))
    sh = {}
    sh["w_in_even"] = f(inp["w_in_even"][0])
    qb = lambda v, nb: np.ascontiguousarray(np.asarray(v, dtype=np.float32).reshape(nb, 128).T)
    sh["s5_are"] = qb(inp["s5_a_re"][0].reshape(-1), 16)
    sh["s5_aim"] = qb(inp["s5_a_im"][0].reshape(-1), 16)
    sh["s5_ldt"] = qb(np.repeat(np.asarray(inp["s5_log_dt"][0]), 64), 16)
    bst = np.zeros((2, 16, 128, 128), np.float32)
    cstt = np.zeros((2, 16, 128, 128), np.float32)
    for ri, (bsrc, csrc) in enumerate(((inp["s5_b_re"][0], inp["s5_c_re"][0]), (inp["s5_b_im"][0], inp["s5_c_im"][0]))):
        bsrc = np.asarray(bsrc)
        csrc = np.asarray(csrc)
        for g in range(32):
            blk = g // 2
            m0 = (g % 2) * 64
            k0 = (g % 8) * 16
            bst[ri, blk, k0:k0 + 16, m0:m0 + 64] = bsrc[g].T
            cstt[ri, blk, m0:m0 + 64, k0:k0 + 16] = csrc[g].T
    sh["s5_bst"] = bst
    sh["s5_cst"] = cstt
    sh["s5_d"] = qb(inp["s5_d"][0], 4)
    sh["s5_wglu"] = f(inp["s5_w_glu"][0])
    sh["s5_bglu"] = qb(inp["s5_b_glu"][0], 4)
    sh["gdn_convw"] = np.ascontiguousarray(np.asarray(inp["gdn_conv_w"][0], dtype=np.float32).reshape(4, 12, 128).transpose(2, 1, 0))
    sh["gdn_alog"] = f(inp["gdn_a_log"][0].reshape(1, 4))
    sh["gdn_dtb"] = f(inp["gdn_dt_bias"][0].reshape(1, 4))
    sh["gdn_normw"] = f(inp["gdn_norm_w"][0].reshape(1, 128))
    sh["w_out_even"] = f(inp["w_out_even"][0])
    wio = np.asarray(inp["w_in_odd"][0], dtype=np.float32)
    perm = np.arange(ODD_IN)
    for qk in range(2):
        for h in range(4):
            base = qk * 1024 + h * 256
            perm[base:base + 128] = base + np.arange(0, 256, 2)
            perm[base + 128:base + 256] = base + np.arange(1, 256, 2)
    sh["w_in_odd"] = np.ascontiguousarray(wio[:, perm])
    sh["w_out_odd"] = f(inp["w_out_odd"][0])
    for k in ("ln1_g", "ln1_b", "ln2_g", "ln2_b", "router_w", "router_b", "moe_w1", "moe_w2", "moe_b2", "ple_w"):
        sh[k] = f(inp[k])
    sh["moe_b1"] = np.ascontiguousarray(np.asarray(inp["moe_b1"], dtype=np.float32).reshape(2, NE, 16, 128).transpose(0, 1, 3, 2))
    sh["ple_gw"] = f(inp["ple_gate_w"])
    for k, v in host_consts().items():
        if k in CONST_SHAPES:
            sh["c_" + k] = np.ascontiguousarray(v.astype(np.float32)).reshape(CONST_SHAPES[k])
    return sh


def core_inputs(inp, sh, prompt_ids, sample_id, L):
    m = dict(sh)
    xp = [np.asarray(inp["x_prompt"][i][:L], dtype=np.float32) for i in prompt_ids]
    m["xin"] = np.ascontiguousarray(np.concatenate(xp + [np.asarray(inp["x_sample"][sample_id], dtype=np.float32)], axis=0))
    pp = [np.asarray(inp["p_prompt"][:, i, :L], dtype=np.float32) for i in prompt_ids]
    m["pin"] = np.ascontiguousarray(np.concatenate(pp + [np.asarray(inp["p_sample"][:, sample_id], dtype=np.float32)], axis=1))
    m["st_s5re"] = np.ascontiguousarray(np.asarray(inp["state_s5_re"][0, sample_id], dtype=np.float32).reshape(16, 128).T)
    m["st_s5im"] = np.ascontiguousarray(np.asarray(inp["state_s5_im"][0, sample_id], dtype=np.float32).reshape(16, 128).T)
    m["st_gdn"] = np.ascontiguousarray(np.asarray(inp["state_gdn"][0, sample_id], dtype=np.float32))
    m["st_conv"] = np.ascontiguousarray(np.asarray(inp["state_gdn_conv"][0, sample_id], dtype=np.float32).reshape(3, 12, 128).transpose(2, 1, 0))
    m["st_ret"] = np.ascontiguousarray(np.asarray(inp["state_ret"][0, sample_id], dtype=np.float32))
    return m


def unperm(k, a):
    a = np.asarray(a)
    if k in ("o_s5re", "o_s5im"):
        return a.reshape(128, 16).T.reshape(32, 64)
    if k == "o_conv":
        return a.reshape(128, 12, 3).transpose(2, 1, 0).reshape(3, 1536)
    return a


_CACHE = {}


def kernel(**inputs):
    NPS, L, C = 2, 2048, 768
    key = (NPS, L, C)
    if key not in _CACHE:
        _CACHE[key] = build(NPS, L, C)
    nc = _CACHE[key]
    sh = prep_shared(inputs)
    in_maps = [core_inputs(inputs, sh, [2 * c, 2 * c + 1], c, L) for c in range(8)]
    res = run_bass_kernel_spmd(nc, in_maps, core_ids=list(range(8)))
    R = res.results
    B, DB = 16, 8
    y_p = np.zeros((B, L, D), np.float32)
    y_s = np.zeros((DB, 16, D), np.float32)
    outs = {k: (np.zeros((1, B) + shp, np.float32), np.zeros((1, DB) + shp, np.float32))
            for k, shp in (("o_s5re", (32, 64)), ("o_s5im", (32, 64)), ("o_gdn", (4, 128, 128)), ("o_conv", (3, 1536)), ("o_ret", (4, 256, 512)))}
    for c in range(8):
        r = R[c]
        y = r["y_out"]
        for j in range(NPS):
            y_p[2 * c + j] = y[j * L:(j + 1) * L]
        y_s[c] = y[NPS * L:NPS * L + 16]
        for k, (po, so) in outs.items():
            a = r[k]
            for j in range(NPS):
                po[0, 2 * c + j] = unperm(k, a[j]).reshape(po.shape[2:])
            so[0, c] = unperm(k, a[NPS]).reshape(so.shape[2:])
    return (y_p, y_s, outs["o_s5re"][0], outs["o_s5im"][0], outs["o_gdn"][0], outs["o_conv"][0], outs["o_ret"][0],
            outs["o_s5re"][1], outs["o_s5im"][1], outs["o_gdn"][1], outs["o_conv"][1], outs["o_ret"][1])
```

```python
from contextlib import ExitStack
import math
import os
CUT = int(os.environ.get('KCUT', '99'))
CUT2 = int(os.environ.get('KCUT2', '99'))
import numpy as np
import concourse.bass as bass
import concourse.mybir as mybir
from concourse.bass_utils import run_bass_kernel_spmd

F32 = mybir.dt.float32
BF16 = mybir.dt.bfloat16
I32 = mybir.dt.int32
U32 = mybir.dt.uint32
ALU = mybir.AluOpType
AF = mybir.ActivationFunctionType

ENGS = ("pe", "dve", "act", "pool", "sp")
SEM_CHUNK = 30000
SAME_ENG_DIST = int(os.environ.get('KSED', '1000000000'))

D = 1024
NE = 32
TOPK = 4
ALPHA = 4.0 ** 0.25
LN_EPS = 1e-5
NORM_EPS = 1e-6
EVEN_IN = 2568
ODD_IN = 6144


class Instr:
    __slots__ = ("eng", "fn", "waits", "is_dma", "sig", "key", "val", "idx", "clock", "sval")

    def __init__(self, eng, fn, is_dma):
        self.eng = eng
        self.fn = fn
        self.is_dma = is_dma
        self.waits = []
        self.sig = False
        self.key = None
        self.val = 0
        self.idx = 0
        self.clock = None
        self.sval = None


class Trk:
    def __init__(self, name=""):
        self.name = name
        self.ent = {}

    def _conf(self, k):
        if k == "*":
            return list(self.ent.values())
        out = []
        e = self.ent.get(k)
        if e is not None:
            out.append(e)
        e = self.ent.get("*")
        if e is not None:
            out.append(e)
        return out


class Buf:
    def __init__(self, h, name):
        self.h = h
        self.trk = Trk(name)

    def __getitem__(self, k):
        return self.h[k]


def alias(ap, parent, name="alias"):
    b = Buf(ap, name)
    b.trk = parent.trk
    return b


class Prog:
    def __init__(self, nc, sb_words):
        self.nc = nc
        self.q = {e: [] for e in ENGS}
        self.clock = {e: {} for e in ENGS}
        self.es = ExitStack()
        self.dma_sems = {}
        self.dma_rr = {e: 0 for e in ENGS}
        self.n_dma_sems = 8
        self.pending = {e: [] for e in ENGS}
        self.big = self.es.enter_context(nc.sbuf_tensor("big", [128, sb_words], F32))
        self.sb_words = sb_words
        self.top = 0
        self.psn = 0

    def sb(self, name, shape, dtype=F32):
        isz = 2 if dtype == BF16 else 4
        n = 1
        for s in shape[1:]:
            n *= s
        words = (n * isz + 3) // 4
        off = self.top
        self.top += words
        assert self.top <= self.sb_words, (name, self.top, self.sb_words)
        v = self.big[0:shape[0], off:off + words]
        if dtype != F32:
            v = v.bitcast(dtype)
        if dtype == BF16 and n % 2 == 1:
            v = v[:, 0:n]
        if len(shape) == 3:
            v = v.rearrange("p (a b) -> p a b", a=shape[1])
        elif len(shape) == 4:
            v = v.rearrange("p (a b c) -> p a b c", a=shape[1], b=shape[2])
        return Buf(v, name)

    def mark(self):
        return self.top

    def release(self, m):
        self.barrier()
        self.top = m

    def ps(self, name):
        t = self.es.enter_context(self.nc.psum_tensor(name, [128, 512], F32))
        b = Buf(t, name)
        b.trk.excl = True
        return b

    def barrier(self):
        lasts = []
        for e in ENGS:
            if self.q[e]:
                for ins in reversed(self.q[e]):
                    if not ins.is_dma:
                        lasts.append(ins)
                        break
        for q, lst in self.dma_sems.items():
            for s in lst:
                if s[2] is not None:
                    lasts.append(s[2])
        for e in ENGS:
            self.pending[e] = list(lasts)

    def _norm(self, lst):
        out = []
        for x in lst:
            if isinstance(x, tuple):
                t, k = x
            else:
                t, k = x, "*"
            trk = t if isinstance(t, Trk) else t.trk
            out.append((trk, k))
        return out

    def _add_dep(self, ins, prod):
        if prod is None or prod is ins:
            return
        eng = ins.eng
        if prod.eng == "pe" and eng == "pe" and not prod.is_dma:
            return
        if (not prod.is_dma) and (not ins.is_dma) and prod.eng == eng and eng in ("dve", "act") \
                and ins.idx - prod.idx >= SAME_ENG_DIST:
            return
        clk = self.clock[eng]
        if clk.get(prod.key, -1) >= prod.val:
            return
        prod.sig = True
        ins.waits.append(prod)
        new = dict(clk)
        new[prod.key] = prod.val
        if prod.clock:
            for k, v in prod.clock.items():
                if new.get(k, -1) < v:
                    new[k] = v
        self.clock[eng] = new

    def _record(self, ins, reads, writes):
        if self.pending[ins.eng]:
            for pr in self.pending[ins.eng]:
                self._add_dep(ins, pr)
            self.pending[ins.eng] = []
        reads = self._norm(reads)
        writes = self._norm(writes)
        excl = [x for x in reads if getattr(x[0], "excl", False)]
        if excl:
            reads = [x for x in reads if not getattr(x[0], "excl", False)]
            writes = writes + [x for x in excl if x not in writes]
        for trk, k in reads:
            for e in trk._conf(k):
                self._add_dep(ins, e[0])
        for trk, k in writes:
            for e in trk._conf(k):
                self._add_dep(ins, e[0])
                for r in e[1]:
                    self._add_dep(ins, r)
        for trk, k in reads:
            e = trk.ent.get(k)
            if e is None:
                e = trk.ent[k] = [None, []]
            e[1].append(ins)
        for trk, k in writes:
            if k == "*":
                trk.ent.clear()
            trk.ent[k] = [ins, []]
        ins.clock = self.clock[ins.eng]
        self.q[ins.eng].append(ins)

    def op(self, eng, fn, r=(), w=()):
        ins = Instr(eng, fn, False)
        ins.idx = len(self.q[eng])
        ins.key = eng
        ins.val = ins.idx
        self._record(ins, r, w)
        return ins

    def V(self, eng, meth, *args, r=(), w=(), **kw):
        return self.op(eng, lambda e: getattr(e, meth)(*args, **kw), r=r, w=w)

    def dma(self, eng, fn, r=(), w=()):
        ins = Instr(eng, fn, True)
        ins.idx = len(self.q[eng])
        sems = self.dma_sems.setdefault(eng, [])
        if len(sems) < self.n_dma_sems:
            s = [f"dq_{eng}_{len(sems)}", 0, None]
            sems.append(s)
        else:
            s = sems[self.dma_rr[eng] % self.n_dma_sems]
        self.dma_rr[eng] += 1
        if s[2] is not None:
            self._add_dep(ins, s[2])
        s[1] += 1
        s[2] = ins
        ins.key = s[0]
        ins.val = s[1]
        ins.sig = True
        self._record(ins, r, w)
        return ins

    def DM(self, eng, out, in_, r=(), w=(), **kw):
        return self.dma(eng, lambda e: e.dma_start(out=out, in_=in_, **kw), r=r, w=w)

    def finalize(self):
        nc = self.nc
        sem_names = set()
        for e in ENGS:
            cnt = 0
            for ins in self.q[e]:
                if ins.is_dma:
                    sem_names.add(ins.key)
                elif ins.sig:
                    ep, v = divmod(cnt, SEM_CHUNK)
                    ins.sval = (f"e_{e}_{ep}", v + 1)
                    sem_names.add(ins.sval[0])
                    cnt += 1
        sems = {}
        for n in sorted(sem_names):
            sems[n] = self.es.enter_context(nc.semaphore(n))

        def semval(prod):
            if prod.is_dma:
                return sems[prod.key], prod.val * 16
            return sems[prod.sval[0]], prod.sval[1]

        def run(e, eng_name):
            for ins in self.q[eng_name]:
                best = {}
                for pr in ins.waits:
                    s, v = semval(pr)
                    k = id(s)
                    if k not in best or best[k][1] < v:
                        best[k] = (s, v)
                ws = list(best.values())
                attach = None
                if ws and eng_name != "pe":
                    attach = ws.pop()
                for s, v in ws:
                    e.wait_ge(s, v)
                bi = ins.fn(e)
                if attach is not None:
                    bi._wait_ge(attach[0], attach[1])
                if ins.is_dma:
                    bi.then_inc(sems[ins.key], 16)
                elif ins.sig:
                    bi.then_inc(sems[ins.sval[0]], 1)

        block = self.es.enter_context(nc.Block())

        @block.tensor
        def _(e):
            run(e, "pe")

        @block.vector
        def _(e):
            run(e, "dve")

        @block.scalar
        def _(e):
            run(e, "act")

        @block.gpsimd
        def _(e):
            run(e, "pool")

        @block.sync
        def _(e):
            run(e, "sp")
            for q, lst in self.dma_sems.items():
                for s in lst:
                    if s[1] > 0:
                        e.wait_ge(sems[s[0]], s[1] * 16)

        self.es.close()
        return nc


def host_consts():
    c = {}
    c["identf"] = np.eye(128, dtype=np.float32)
    jj = np.arange(128)[:, None]
    ii = np.arange(128)[None, :]
    c["uincl"] = (jj <= ii).astype(np.float32)
    c["ustrict"] = (jj < ii).astype(np.float32)
    c["ones"] = np.ones((128, 128), np.float32)
    c["lmi"] = (ii <= jj).astype(np.float32)
    c["lms"] = (ii < jj).astype(np.float32)
    c["iota_e"] = np.tile(np.arange(32, dtype=np.float32)[None, :], (128, 1))
    c["tau"] = np.tile(np.arange(1, 129, dtype=np.float32)[None, :], (128, 1))
    c["pidx"] = np.arange(128, dtype=np.float32)[:, None].copy()
    log_g = np.log(1.0 - 2.0 ** (-5.0 - np.arange(4, dtype=np.float32))).astype(np.float32)
    M = np.zeros((4, 128, 128), np.float32)
    for h in range(4):
        m = np.exp(log_g[h] * np.abs(ii - jj).astype(np.float32))
        m = np.where((jj >= 64) & (ii < 64), 0.0, m)
        M[h] = m
    c["retmask"] = np.ascontiguousarray(M.transpose(1, 0, 2)).astype(np.float32)
    qs = np.zeros((128, 4, 128), np.float32)
    for h in range(4):
        qs[:, h, :] = np.exp(log_g[h] * (np.arange(128, dtype=np.float32) + 1.0))[None, :]
    c["retqs"] = qs
    ks = np.zeros((128, 8), np.float32)
    for h in range(4):
        ks[:, h] = np.exp(log_g[h] * (127.0 - np.arange(128, dtype=np.float32)))
        ks[:, 4 + h] = np.exp(log_g[h] * (15.0 - np.arange(128, dtype=np.float32)))
    c["retks"] = ks * np.float32(256 ** -0.5)
    c["retcdec"] = [[float(np.exp(log_g[h] * 128.0)) for h in range(4)],
                    [float(np.exp(log_g[h] * 16.0)) for h in range(4)]]
    freq = (1.0 / (10000.0 ** np.linspace(0.0, 1.0, 128, dtype=np.float32))).astype(np.float32)
    pos = np.arange(2048, dtype=np.float32)
    ang = (pos[None, :] * freq[:, None]).astype(np.float32)
    c["rcos"] = np.cos(ang).astype(np.float32)
    c["rsin"] = np.sin(ang).astype(np.float32)
    return c


LAST_DIN = {}
CONST_SHAPES = {"lmi": [128, 128], "lms": [128, 128], "identf": [128, 128], "uincl": [128, 128], "ustrict": [128, 128], "ones": [128, 128],
                "iota_e": [128, 32], "tau": [128, 128], "pidx": [128, 1], "retmask": [128, 4, 128],
                "retqs": [128, 4, 128], "retks": [128, 8], "rcos": [128, 2048], "rsin": [128, 2048]}


def build(NPS, L, C, stages=("A0", "M0", "C0", "A1", "M1", "C1"), dbg=()):
    nc = bass.Bass("TRN2", target_bir_lowering=False)
    NSEQ = NPS + 1
    NTOK = NPS * L + 16
    TPS = L // 128
    tiles = []
    for s in range(NPS):
        for t in range(TPS):
            tiles.append(dict(row0=s * L + t * 128, nt=128, seq=s, first=(t == 0), last=(t == TPS - 1),
                              pos0=t * 128, ti=len(tiles)))
    tiles.append(dict(row0=NPS * L, nt=16, seq=NPS, first=True, last=True, pos0=1024, ti=len(tiles)))
    NT = len(tiles)
    NROWP = NT * 128
    CT = C // 128
    TRASH = NE * C
    cgs = []
    c0 = 0
    while c0 < C:
        cgs.append((c0, min(C, c0 + 512)))
        c0 += 512

    def din(name, shape, dt=F32):
        LAST_DIN[name] = list(shape)
        return nc.dram_tensor(name, list(shape), dt, kind="ExternalInput").ap()

    def dout(name, shape, dt=F32):
        return nc.dram_tensor(name, list(shape), dt, kind="ExternalOutput").ap()

    def dint(name, shape, dt=F32):
        return nc.dram_tensor(name, list(shape), dt, kind="Internal").ap()

    xin = din("xin", [NTOK, D])
    pin = din("pin", [2, NTOK, 256])
    st_s5re = din("st_s5re", [128, 16])
    st_s5im = din("st_s5im", [128, 16])
    st_gdn = din("st_gdn", [4, 128, 128])
    st_conv = din("st_conv", [128, 12, 3])
    st_ret = din("st_ret", [4, 256, 512])
    w_in_even = din("w_in_even", [D, EVEN_IN])
    s5_are = din("s5_are", [128, 16])
    s5_aim = din("s5_aim", [128, 16])
    s5_ldt = din("s5_ldt", [128, 16])
    s5_bst = din("s5_bst", [2, 16, 128, 128])
    s5_cst = din("s5_cst", [2, 16, 128, 128])
    s5_d = din("s5_d", [128, 4])
    s5_wglu = din("s5_wglu", [512, 512])
    s5_bglu = din("s5_bglu", [128, 4])
    gdn_convw = din("gdn_convw", [128, 12, 4])
    gdn_alog = din("gdn_alog", [1, 4])
    gdn_dtb = din("gdn_dtb", [1, 4])
    gdn_normw = din("gdn_normw", [1, 128])
    w_out_even = din("w_out_even", [D, D])
    w_in_odd = din("w_in_odd", [D, ODD_IN])
    w_out_odd = din("w_out_odd", [2048, D])
    ln1_g = din("ln1_g", [2, D])
    ln1_b = din("ln1_b", [2, D])
    ln2_g = din("ln2_g", [2, D])
    ln2_b = din("ln2_b", [2, D])
    router_w = din("router_w", [2, D, NE])
    router_b = din("router_b", [2, NE])
    moe_w1 = din("moe_w1", [2, NE, D, 2 * D])
    moe_b1 = din("moe_b1", [2, NE, 128, 16])
    moe_w2 = din("moe_w2", [2, NE, D, D])
    moe_b2 = din("moe_b2", [2, NE, D])
    ple_w = din("ple_w", [2, 256, D])
    ple_gw = din("ple_gw", [2, D, D])
    cst = {k: din("c_" + k, v) for k, v in CONST_SHAPES.items()}

    y_out = dout("y_out", [NTOK, D])
    o_s5re = dout("o_s5re", [NSEQ, 128, 16])
    o_s5im = dout("o_s5im", [NSEQ, 128, 16])
    o_gdn = dout("o_gdn", [NSEQ, 4, 128, 128])
    o_conv = dout("o_conv", [NSEQ, 128, 12, 3])
    o_ret = dout("o_ret", [NSEQ, 4, 256, 512])
    dbg_out = {k: dout("dbg_" + k, [NTOK, D]) for k in dbg if k.startswith("x")}
    taps = {}

    def tap(name, ap, ti, npart, width, dt=F32):
        if ("t_" + name) not in dbg:
            return
        if name not in taps:
            taps[name] = dout("tap_" + name, [NT, 128, width], dt)
        p.DM("sp", taps[name][ti, 0:npart, :], ap, r=[tapsrc[0]], w=[T_out])

    tapsrc = [None]

    x1s = dint("x1s", [NROWP, D])
    x3s = dint("x3s", [NROWP, D])
    yas = dint("yas", [NT, 128, 4, 128], BF16)
    T_yas = Trk("yas")
    xs = dint("xs", [NE * C + 128, D], BF16)
    ys = dint("ys", [NE * C + 128, D])

    p = Prog(nc, 52900)
    DR = Trk("dram_in")
    T_x1s, T_x3s, T_xs, T_ys, T_out = Trk("x1s"), Trk("x3s"), Trk("xs"), Trk("ys"), Trk("out")
    PS = [p.ps(f"ps{i}") for i in range(8)]

    def psbf(b, n):
        return PS[b][:, 0:n // 2].bitcast(BF16)

    identf = p.sb("identf", [128, 128])
    identb = p.sb("identb", [128, 128], BF16)
    uincl = p.sb("uincl", [128, 128])
    ustr_b = p.sb("ustr_b", [128, 128], BF16)
    ones_f = p.sb("ones_f", [128, 128])
    ones_b = p.sb("ones_b", [128, 128], BF16)
    iota_e = p.sb("iota_e", [128, 32])
    pidx = p.sb("pidx", [128, 1])
    gates_all = p.sb("gates_all", [128, NT, 4])
    slots_all = p.sb("slots_all", [128, NT, 4], I32)
    tmpc = p.sb("tmpc", [128, 128])
    for nm, b in (("identf", identf), ("uincl", uincl), ("ones", ones_f), ("iota_e", iota_e), ("pidx", pidx)):
        p.DM("sp", b[:], cst[nm], r=[DR], w=[b])
    p.DM("sp", tmpc[:], cst["ustrict"], r=[DR], w=[tmpc])
    p.V("dve", "tensor_copy", ustr_b[:], tmpc[:], r=[tmpc], w=[ustr_b])
    p.V("dve", "tensor_copy", identb[:], identf[:], r=[identf], w=[identb])
    p.V("dve", "tensor_copy", ones_b[:], ones_f[:], r=[ones_f], w=[ones_b])

    rr = {"ev": 0}

    def evac(out_ap, in_ap, r, w):
        rr["ev"] += 1
        if rr["ev"] % 2:
            p.V("act", "activation", out_ap, in_ap, AF.Copy, r=r, w=w)
        else:
            p.V("dve", "tensor_copy", out_ap, in_ap, r=r, w=w)

    def bcast_load(buf, src_row):
        p.DM("sp", buf[:], src_row.partition_broadcast(128), r=[DR], w=[buf])

    def load_w_bf16(buf, src, kc):
        n = src.shape[1]
        nch = (n + 2047) // 2048
        step = (n + nch - 1) // nch
        v = src.rearrange("(kc q) n -> q kc n", q=128)
        for c0 in range(0, n, step):
            c1 = min(n, c0 + step)
            p.DM("pool", buf[:, :, c0:c1], v[:, :, c0:c1], r=[DR], w=[(buf, c0)] if nch > 1 else [buf])

    def layernorm(eng_h, h, nt, gt, bt, out, scr6, scr2):
        p.V("dve", "bn_stats", scr6[0:nt, 0, :], h[0:nt, 0:512], r=[h], w=[(scr6, 0)])
        p.V("dve", "bn_stats", scr6[0:nt, 1, :], h[0:nt, 512:1024], r=[h], w=[(scr6, 1)])
        p.V("dve", "bn_aggr", scr2[0:nt, 0:2], scr6[0:nt, :, :].rearrange("p a b -> p (a b)"), r=[scr6], w=[scr2])
        p.V("act", "activation", scr2[0:nt, 2:3], scr2[0:nt, 1:2], AF.Sqrt, bias=epsln[0:nt, 0:1], r=[scr2, epsln], w=[(scr2, "s")])
        p.V("dve", "reciprocal", scr2[0:nt, 3:4], scr2[0:nt, 2:3], r=[(scr2, "s")], w=[(scr2, "r")])
        p.V("dve", "tensor_scalar", out[0:nt, :], h[0:nt, :], scr2[0:nt, 0:1], scr2[0:nt, 3:4], ALU.subtract, ALU.mult,
            r=[h, scr2, (scr2, "r")], w=[out])
        p.V("pool", "tensor_tensor", out[0:nt, :], out[0:nt, :], gt[0:nt, :], ALU.mult, r=[out, gt], w=[out])
        p.V("pool", "tensor_tensor", out[0:nt, :], out[0:nt, :], bt[0:nt, :], ALU.add, r=[out, bt], w=[out])

    epsln = p.sb("epsln", [128, 2])
    p.V("dve", "memset", epsln[:, 0:1], LN_EPS, w=[(epsln, 0)])
    p.V("dve", "memset", epsln[:, 1:2], NORM_EPS, w=[(epsln, 1)])

    def transpose_to(dstT, src_bf, nt, nblk, bank):
        for b0 in range(0, nblk, 8):
            nb = min(8, nblk - b0)
            for b in range(nb):
                p.V("pe", "transpose", psbf(bank, 1024)[:, b * 128:b * 128 + nt], src_bf[0:nt, (b0 + b) * 128:(b0 + b + 1) * 128],
                    identb[0:nt, 0:nt], r=[src_bf, identb], w=[PS[bank]])
            evac(dstT[:, b0:b0 + nb, 0:nt], psbf(bank, 1024).rearrange("p (a b) -> p a b", a=8)[:, 0:nb, 0:nt],
                 r=[PS[bank]], w=[dstT])

    def phaseA_tail(li, tl, xt, mixps, lnw, rw, rb, work):
        nt, ti = tl["nt"], tl["ti"]
        h, x1, xrow, x1T, lg, scr6, scr2, small, Mb, tot = work
        p.V("dve", "scalar_tensor_tensor", h[0:nt, 0:512], xt[0:nt, 0:512], ALPHA, PS[mixps[0]][0:nt, :], ALU.mult, ALU.add,
            r=[xt, PS[mixps[0]]], w=[(h, 0)])
        p.V("dve", "scalar_tensor_tensor", h[0:nt, 512:1024], xt[0:nt, 512:1024], ALPHA, PS[mixps[1]][0:nt, :], ALU.mult, ALU.add,
            r=[xt, PS[mixps[1]]], w=[(h, 1)])
        layernorm("dve", h, nt, lnw[0], lnw[1], x1, scr6, scr2)
        p.DM("sp", x1s[ti * 128:ti * 128 + nt, :], x1[0:nt, :], r=[x1], w=[(T_x1s, ti)])
        if ("x1_%d" % li) in dbg_out:
            p.DM("sp", dbg_out["x1_%d" % li][tl["row0"]:tl["row0"] + nt, :], x1[0:nt, :], r=[x1], w=[T_out])
        p.V("act", "activation", xrow[0:nt, :], x1[0:nt, :], AF.Copy, r=[x1], w=[xrow])
        for half in range(2):
            for b in range(4):
                kc = half * 4 + b
                p.V("pe", "transpose", PS[6][:, b * 128:b * 128 + nt], x1[0:nt, kc * 128:(kc + 1) * 128], identf[0:nt, 0:nt],
                    r=[x1, identf], w=[PS[6]])
            evac(x1T[:, half * 4:half * 4 + 4, 0:nt], PS[6][:, :].rearrange("p (a b) -> p a b", a=4)[:, :, 0:nt], r=[PS[6]], w=[x1T])
        for kc in range(8):
            p.V("pe", "matmul", PS[7][0:nt, 0:32], x1T[:, kc, 0:nt], rw[:, kc, :], start=(kc == 0), stop=(kc == 7),
                r=[x1T, rw], w=[PS[7]])
        p.V("dve", "tensor_tensor", lg[0:nt, :], PS[7][0:nt, 0:32], rb[0:nt, :], ALU.add, r=[PS[7], rb], w=[lg])
        top, ti8, nt0, ex, gs, ef, rk, sl, ov, tmp32, rnk = small
        p.V("dve", "max", top[0:nt, :], lg[0:nt, :], r=[lg], w=[top])
        p.V("dve", "max_index", ti8[0:nt, :], top[0:nt, :], lg[0:nt, :], r=[lg, top], w=[ti8])
        p.V("dve", "tensor_scalar", nt0[0:nt, :], top[0:nt, 0:1], -1.0, None, ALU.mult, r=[top], w=[nt0])
        p.V("act", "activation", ex[0:nt, :], top[0:nt, 0:4], AF.Exp, bias=nt0[0:nt, 0:1], r=[top, nt0], w=[ex])
        p.V("dve", "reduce_sum", gs[0:nt, 0:1], ex[0:nt, :], mybir.AxisListType.X, r=[ex], w=[gs])
        p.V("dve", "reciprocal", gs[0:nt, 1:2], gs[0:nt, 0:1], r=[gs], w=[(gs, "r")])
        p.V("dve", "tensor_scalar", gates_all[0:nt, ti, :], ex[0:nt, :], gs[0:nt, 1:2], None, ALU.mult, r=[ex, (gs, "r")], w=[(gates_all, ti)])
        p.V("pool", "memset", Mb[:], 0.0, w=[Mb])
        p.V("dve", "tensor_scalar", Mb[0:nt, :], lg[0:nt, :], top[0:nt, 3:4], None, ALU.is_ge, r=[lg, top], w=[Mb])
        p.V("pe", "matmul", PS[7][:, 64:96], ustr_b[:, :], Mb[:, :], start=True, stop=True, r=[ustr_b, Mb], w=[PS[7]])
        p.V("pe", "matmul", PS[7][:, 96:128], ones_b[:, :], Mb[:, :], start=True, stop=True, r=[ones_b, Mb], w=[PS[7]])
        p.V("dve", "tensor_tensor", rnk[:, :], PS[7][:, 64:96], tot[:, :], ALU.add, r=[PS[7], tot], w=[rnk])
        p.V("dve", "tensor_tensor", tot[:, :], PS[7][:, 96:128], tot[:, :], ALU.add, r=[PS[7], tot], w=[tot])
        p.V("dve", "tensor_copy", ef[0:nt, :], ti8[0:nt, 0:4], r=[ti8], w=[ef])
        for k in range(4):
            p.V("dve", "scalar_tensor_tensor", tmp32[0:nt, :], iota_e[0:nt, :], ef[0:nt, k:k + 1], rnk[0:nt, :], ALU.is_equal, ALU.mult,
                accum_out=rk[0:nt, k:k + 1], r=[iota_e, ef, rnk], w=[tmp32, (rk, k)])
        p.V("dve", "scalar_tensor_tensor", sl[0:nt, :], ef[0:nt, :], float(C), rk[0:nt, :], ALU.mult, ALU.add, r=[ef, rk], w=[sl])
        p.V("dve", "tensor_scalar", ov[0:nt, :], rk[0:nt, :], float(C), None, ALU.is_ge, r=[rk], w=[ov])
        p.V("dve", "tensor_scalar", tmp32[0:nt, 0:4], sl[0:nt, :], -1.0, pidx[0:nt, 0:1], ALU.mult, ALU.add, r=[sl, pidx], w=[tmp32])
        p.V("dve", "tensor_scalar", tmp32[0:nt, 0:4], tmp32[0:nt, 0:4], float(TRASH), None, ALU.add, r=[tmp32], w=[tmp32])
        p.V("dve", "tensor_tensor", tmp32[0:nt, 0:4], tmp32[0:nt, 0:4], ov[0:nt, :], ALU.mult, r=[tmp32, ov], w=[tmp32])
        p.V("dve", "tensor_tensor", sl[0:nt, :], sl[0:nt, :], tmp32[0:nt, 0:4], ALU.add, r=[sl, tmp32], w=[sl])
        if nt < 128:
            p.V("dve", "tensor_scalar", tmp32[:, 0:4], pidx[:, 0:1].to_broadcast([128, 4]), float(TRASH), None, ALU.add, r=[pidx], w=[tmp32])
            p.V("dve", "tensor_copy", slots_all[:, ti, :], tmp32[:, 0:4], r=[tmp32], w=[(slots_all, ti)])
        p.V("dve", "tensor_copy", slots_all[0:nt, ti, :], sl[0:nt, :], r=[sl], w=[(slots_all, ti)])
        for k in range(4):
            p.dma("pool", lambda e, k=k, ti=ti: e.indirect_dma_start(
                out=xs[:, :], out_offset=bass.IndirectOffsetOnAxis(ap=slots_all[:, ti, k:k + 1], axis=0),
                in_=xrow[:, :], in_offset=None), r=[xrow, (slots_all, ti), (T_xs, "*")], w=[])

    def alloc_tail_work(h=None, x1=None, x1T=None):
        if h is None:
            h = p.sb("h", [128, D])
        if x1 is None:
            x1 = p.sb("x1", [128, D])
        xrow = p.sb("xrow", [128, D], BF16)
        if x1T is None:
            x1T = p.sb("x1T", [128, 8, 128])
        lg = p.sb("lg", [128, 32])
        scr6 = p.sb("scr6", [128, 2, 6])
        scr2 = p.sb("scr2", [128, 4])
        small = (p.sb("top", [128, 8]), p.sb("ti8", [128, 8], U32), p.sb("nt0", [128, 1]), p.sb("ex", [128, 4]),
                 p.sb("gs", [128, 2]), p.sb("ef", [128, 4]), p.sb("rk", [128, 4]), p.sb("sl", [128, 4]),
                 p.sb("ov", [128, 4]), p.sb("tmp32", [128, 32]), p.sb("rnk", [128, 32]))
        Mb = p.sb("Mb", [128, 32], BF16)
        tot = p.sb("tot", [128, 32])
        p.V("dve", "memset", tot[:], 0.0, w=[tot])
        p.V("pool", "memset", xrow[:, :], 0.0, w=[xrow])
        return (h, x1, xrow, x1T, lg, scr6, scr2, small, Mb, tot)

    def load_ln_router(li):
        g1 = p.sb("ln1g", [128, D]); b1 = p.sb("ln1b", [128, D])
        bcast_load(g1, ln1_g[li:li + 1, :]); bcast_load(b1, ln1_b[li:li + 1, :])
        rw = p.sb("rw", [128, 8, NE])
        p.DM("sp", rw[:], router_w[li].rearrange("(kc q) n -> q kc n", q=128), r=[DR], w=[rw])
        rb = p.sb("rb", [128, NE])
        bcast_load(rb, router_b[li:li + 1, :])
        return (g1, b1), rw, rb

    def phaseA0():
        m0 = p.mark()
        win = p.sb("win", [128, 8, EVEN_IN], BF16)
        load_w_bf16(win, w_in_even, 8)
        wout = p.sb("wout", [128, 8, D], BF16)
        load_w_bf16(wout, w_out_even, 8)
        wglu = p.sb("wglu", [128, 4, 512], BF16)
        load_w_bf16(wglu, s5_wglu, 4)
        bst = p.sb("bst", [128, 2, 16, 128], BF16)
        cstt = p.sb("cstt", [128, 2, 16, 128], BF16)
        for ri in range(2):
            p.DM("pool", bst[:, ri, :, :], s5_bst[ri].rearrange("b k m -> k b m"), r=[DR], w=[(bst, ri)])
            p.DM("pool", cstt[:, ri, :, :], s5_cst[ri].rearrange("b k m -> k b m"), r=[DR], w=[(cstt, ri)])
        lnw, rw, rb = load_ln_router(0)
        are = p.sb("are", [128, 16]); aim = p.sb("aim", [128, 16]); ldt = p.sb("ldt", [128, 16])
        for b, s in ((are, s5_are), (aim, s5_aim), (ldt, s5_ldt)):
            p.DM("sp", b[:], s, r=[DR], w=[b])
        dsk = p.sb("dsk", [128, 4]); bgl = p.sb("bgl", [128, 4])
        p.DM("sp", dsk[:], s5_d, r=[DR], w=[dsk])
        p.DM("sp", bgl[:], s5_bglu, r=[DR], w=[bgl])
        tau = p.sb("tau", [128, 128])
        p.DM("sp", tau[:], cst["tau"], r=[DR], w=[tau])
        lam = p.sb("lam", [128, 16]); li_ = p.sb("li", [128, 16]); dtt = p.sb("dtt", [128, 16])
        p.V("act", "activation", dtt[:], ldt[:], AF.Exp, r=[ldt], w=[dtt])
        p.V("dve", "tensor_tensor", li_[:], aim[:], dtt[:], ALU.mult, r=[aim, dtt], w=[li_])
        p.V("dve", "tensor_tensor", lam[:], are[:], dtt[:], ALU.mult, r=[are, dtt], w=[lam])
        p.V("act", "activation", lam[:], lam[:], AF.Exp, r=[lam], w=[lam])
        cosT = p.sb("cosT", [128, 16, 128]); sinT = p.sb("sinT", [128, 16, 128])
        crT = p.sb("crT", [128, 16, 128]); ciT = p.sb("ciT", [128, 16, 128])
        ang = crT
        kq = ciT
        gsc = p.sb("gsc", [128, 2, 16, 128])
        ki = Buf(gsc[:, 0, :, :].bitcast(I32), "ki")
        ki.trk = gsc.trk
        TWO_PI = 2.0 * math.pi

        def sin_of(dst, shift):
            p.V("dve", "tensor_tensor", ang[:], li_[:, :].unsqueeze(2).to_broadcast([128, 16, 128]),
                tau[:, :].unsqueeze(1).to_broadcast([128, 16, 128]), ALU.mult, r=[li_, tau], w=[ang])
            if shift != 0.0:
                p.V("dve", "tensor_scalar", ang[:], ang[:], shift, None, ALU.add, r=[ang], w=[ang])
            p.V("dve", "tensor_scalar", kq[:], ang[:], 1.0 / TWO_PI, None, ALU.mult, r=[ang], w=[kq])
            p.V("dve", "tensor_copy", ki[:], kq[:], r=[kq], w=[ki])
            p.V("dve", "tensor_copy", kq[:], ki[:], r=[ki], w=[kq])
            p.V("dve", "scalar_tensor_tensor", ang[:], kq[:], -TWO_PI, ang[:], ALU.mult, ALU.add, r=[kq, ang], w=[ang])
            p.V("dve", "tensor_scalar", kq[:], ang[:], math.pi, TWO_PI, ALU.is_gt, ALU.mult, r=[ang], w=[kq])
            p.V("dve", "tensor_tensor", ang[:], ang[:], kq[:], ALU.subtract, r=[ang, kq], w=[ang])
            p.V("dve", "tensor_scalar", kq[:], ang[:], -math.pi, TWO_PI, ALU.is_lt, ALU.mult, r=[ang], w=[kq])
            p.V("dve", "tensor_tensor", ang[:], ang[:], kq[:], ALU.add, r=[ang, kq], w=[ang])
            p.V("dve", "tensor_scalar", ang[:], ang[:], math.pi, -math.pi, ALU.min, ALU.max, r=[ang], w=[ang])
            p.V("act", "activation", dst[:], ang[:], AF.Sin, r=[ang], w=[dst])

        sin_of(sinT, 0.0)
        sin_of(cosT, math.pi / 2)
        sm = p.sb("s5sm", [128, 8, 16])
        abre, abim, den, t1, t2, cfre, cfim, t3 = [sm[:, i, :] for i in range(8)]
        S = [sm]
        p.V("dve", "tensor_tensor", abre, lam[:], cosT[:, :, 0], ALU.mult, r=[lam, cosT], w=S)
        p.V("dve", "tensor_tensor", abim, lam[:], sinT[:, :, 0], ALU.mult, r=[lam, sinT], w=S)
        p.V("dve", "tensor_scalar", abre, abre, -1.0, None, ALU.add, r=S, w=S)
        p.V("dve", "tensor_tensor", t1, are[:], are[:], ALU.mult, r=[are], w=S)
        p.V("dve", "tensor_tensor", t2, aim[:], aim[:], ALU.mult, r=[aim], w=S)
        p.V("dve", "tensor_tensor", den, t1, t2, ALU.add, r=S, w=S)
        p.V("dve", "reciprocal", den, den, r=S, w=S)
        p.V("dve", "tensor_tensor", t1, abre, are[:], ALU.mult, r=S + [are], w=S)
        p.V("dve", "tensor_tensor", t2, abim, aim[:], ALU.mult, r=S + [aim], w=S)
        p.V("dve", "tensor_tensor", cfre, t1, t2, ALU.add, r=S, w=S)
        p.V("dve", "tensor_tensor", cfre, cfre, den, ALU.mult, r=S, w=S)
        p.V("dve", "tensor_tensor", t1, abim, are[:], ALU.mult, r=S + [are], w=S)
        p.V("dve", "tensor_tensor", t2, abre, aim[:], ALU.mult, r=S + [aim], w=S)
        p.V("dve", "tensor_tensor", cfim, t1, t2, ALU.subtract, r=S, w=S)
        p.V("dve", "tensor_tensor", cfim, cfim, den, ALU.mult, r=S, w=S)
        sc3 = Buf(gsc[:, 1, :, :], "sc3")
        sc3.trk = gsc.trk
        bc = lambda a: a.unsqueeze(2).to_broadcast([128, 16, 128])
        p.V("dve", "tensor_tensor", crT[:], cosT[:], bc(cfre), ALU.mult, r=[cosT] + S, w=[crT])
        p.V("dve", "tensor_tensor", sc3[:], sinT[:], bc(cfim), ALU.mult, r=[sinT] + S, w=[sc3])
        p.V("dve", "tensor_tensor", crT[:], crT[:], sc3[:], ALU.add, r=[crT, sc3], w=[crT])
        p.V("dve", "tensor_tensor", ciT[:], cosT[:], bc(cfim), ALU.mult, r=[cosT] + S, w=[ciT])
        p.V("dve", "tensor_tensor", sc3[:], sinT[:], bc(cfre), ALU.mult, r=[sinT] + S, w=[sc3])
        p.V("dve", "tensor_tensor", ciT[:], ciT[:], sc3[:], ALU.subtract, r=[ciT, sc3], w=[ciT])
        wc = p.sb("wc", [128, 12, 4])
        p.DM("sp", wc[:], gdn_convw, r=[DR], w=[wc])
        alog = p.sb("alog", [128, 4]); dtb = p.sb("dtb", [128, 4]); nrmw = p.sb("nrmw", [128, 128])
        bcast_load(alog, gdn_alog); bcast_load(dtb, gdn_dtb); bcast_load(nrmw, gdn_normw)
        p.V("act", "activation", alog[:], alog[:], AF.Exp, r=[alog], w=[alog])
        Hre = p.sb("Hre", [128, 16]); Him = p.sb("Him", [128, 16])
        Sg = p.sb("Sg", [128, 4, 128])
        ctx3 = p.sb("ctx3", [128, 12, 3])
        xt_one = p.sb("xt0", [128, D])
        xts = [xt_one, xt_one]
        xb = p.sb("xb", [128, D], BF16)
        xT = p.sb("xT", [128, 8, 128], BF16)
        uTf = p.sb("uTf", [128, 4, 128]); uTb = p.sb("uTb", [128, 4, 128], BF16)
        cb = p.sb("cb", [128, 12, 131])
        cacc = p.sb("cacc", [128, 12, 128])
        g8 = p.sb("g8", [128, 16, 128])
        ctmp = Buf(g8[:, 0:12, :], "ctmp"); ctmp.trk = g8.trk
        ztok = p.sb("ztok", [128, 8])
        rbuf = p.sb("rbuf", [128, 2, 8, 128]); rtmp = p.sb("rtmp", [128, 2, 8, 128])
        hbf = p.sb("hbf", [128, 2, 16, 128], BF16)
        hl = p.sb("hl", [128, 4, 16])
        yA = Buf(rtmp[:, 0, 0:4, :], "yA"); yA.trk = rtmp.trk
        ysq = Buf(rtmp[:, 0, 4:8, :], "ysq"); ysq.trk = rtmp.trk
        gaf = Buf(rtmp[:, 1, 0:4, :], "gaf"); gaf.trk = rtmp.trk
        gab = p.sb("gab", [128, 4, 128], BF16)
        mixT = p.sb("mixT", [128, 8, 128], BF16)
        qkn = Buf(g8[:, 8:16, :], "qkn"); qkn.trk = g8.trk
        sq8 = Buf(g8[:, 0:8, :], "sq8"); sq8.trk = g8.trk
        kvtok = alias(rbuf[:, 0, :, :], rbuf, "kvtok")
        gd = p.sb("gd", [128, 16])
        gd2 = p.sb("gd2", [128, 16])
        _gt = [alias(gsc[:, 0, i, :], gsc, "gt%d" % i) for i in range(16)]
        gbc, dec, erow, attn, attnT, rv, rk_, nwT, ub, qdT, kd, Yt = _gt[0:12]
        Mm = [_gt[12], _gt[13]]
        MT = [_gt[14], _gt[15]]
        yB = p.sb("yB", [128, 512], BF16); osb = alias(gsc[:, 1, 0, :], gsc, "osb"); ssq = p.sb("ssq", [128, 4])
        zs = p.sb("zs", [128, 512], BF16)
        x1a = alias(g8[:, 0:8, :].rearrange("p a b -> p (a b)"), g8, "x1a")
        x1Ta = alias(cacc[:, 0:8, :], cacc, "x1Ta")
        work = alloc_tail_work(h=xt_one, x1=x1a, x1T=x1Ta)
        print("A0 sbuf words", p.top)

        def load_x(tl, buf):
            if tl["nt"] < 128:
                p.V("pool", "memset", buf[:], 0.0, w=[buf])
            p.DM("sp", buf[0:tl["nt"], :], xin[tl["row0"]:tl["row0"] + tl["nt"], :], r=[DR], w=[buf])

        for tl in tiles:
            nt, ti, sq = tl["nt"], tl["ti"], tl["seq"]
            xt = xts[ti % 2]
            load_x(tl, xt)
            if tl["first"]:
                if sq < NPS:
                    p.V("pool", "memset", Hre[:], 0.0, w=[Hre]); p.V("pool", "memset", Him[:], 0.0, w=[Him])
                    p.V("pool", "memset", Sg[:], 0.0, w=[Sg]); p.V("pool", "memset", ctx3[:], 0.0, w=[ctx3])
                else:
                    p.DM("sp", Hre[:], st_s5re, r=[DR], w=[Hre])
                    p.DM("sp", Him[:], st_s5im, r=[DR], w=[Him])
                    p.DM("sp", Sg[:], st_gdn.rearrange("h k v -> k h v"), r=[DR], w=[Sg])
                    p.DM("sp", ctx3[:], st_conv, r=[DR], w=[ctx3])
            if CUT <= 1:
                continue
            p.V("act", "activation", xb[0:nt, :], xt[0:nt, :], AF.Copy, r=[xt], w=[xb])
            transpose_to(xT, xb, nt, 8, 0)
            tapsrc[0] = xt; tap("xt", xt[0:nt, :], ti, nt, D)
            tapsrc[0] = xb; tap("xb", xb[0:nt, :], ti, nt, D, BF16)
            tapsrc[0] = xT; tap("xT", xT[:, :, :].rearrange("p a b -> p (a b)"), ti, 128, 1024, BF16)
            for ob in range(4):
                for kc in range(8):
                    p.V("pe", "matmul", PS[1][:, ob * 128:ob * 128 + nt], win[:, kc, ob * 128:(ob + 1) * 128], xT[:, kc, 0:nt],
                        start=(kc == 0), stop=(kc == 7), r=[win, xT], w=[PS[1]])
            ps1v = PS[1][:, :].rearrange("p (a b) -> p a b", a=4)[:, :, 0:nt]
            p.V("act", "activation", uTf[:, :, 0:nt], ps1v, AF.Copy, r=[PS[1]], w=[uTf])
            p.V("dve", "tensor_copy", uTb[:, :, 0:nt], ps1v, r=[PS[1]], w=[uTb])
            p.V("pool", "tensor_copy", cb[:, :, 0:3], ctx3[:, :, :], r=[ctx3], w=[(cb, "c")])
            for g4 in range(3):
                bank = 2 + (g4 % 2)
                for b in range(4):
                    blk = g4 * 4 + b
                    for kc in range(8):
                        p.V("pe", "matmul", PS[bank][:, b * 128:b * 128 + nt], win[:, kc, 512 + blk * 128:512 + (blk + 1) * 128],
                            xT[:, kc, 0:nt], start=(kc == 0), stop=(kc == 7), r=[win, xT], w=[PS[bank]])
                evac(cb[:, g4 * 4:g4 * 4 + 4, 3:3 + nt], PS[bank][:, :].rearrange("p (a b) -> p a b", a=4)[:, :, 0:nt],
                     r=[PS[bank]], w=[(cb, g4)])
            tapsrc[0] = cb; tap("cb", cb[:, :, :].rearrange("p a b -> p (a b)"), ti, 128, 12 * 131)
            tapsrc[0] = uTf; tap("uTf", uTf[:, :, :].rearrange("p a b -> p (a b)"), ti, 128, 512)
            for kc in range(8):
                p.V("pe", "matmul", PS[4][0:nt, 0:512], xT[:, kc, 0:nt], win[:, kc, 2048:2560], start=(kc == 0), stop=(kc == 7),
                    r=[xT, win], w=[PS[4]])
            for kc in range(8):
                p.V("pe", "matmul", PS[5][0:nt, 0:8], xT[:, kc, 0:nt], win[:, kc, 2560:2568], start=(kc == 0), stop=(kc == 7),
                    r=[xT, win], w=[PS[5]])
            p.V("act", "activation", zs[0:nt, :], PS[4][0:nt, 0:512], AF.Silu, r=[PS[4]], w=[zs])
            p.V("dve", "tensor_copy", ztok[0:nt, 0:8], PS[5][0:nt, 0:8], r=[PS[5]], w=[ztok])
            if CUT <= 2:
                continue
            for hf in range(2):
                for ri in range(2):
                    for b8 in range(8):
                        blk = hf * 8 + b8
                        bank = 4 + ri * 2 + (b8 // 4)
                        p.V("pe", "matmul", PS[bank][:, (b8 % 4) * 128:(b8 % 4) * 128 + nt], bst[:, ri, blk, :], uTb[:, blk // 4, 0:nt],
                            start=True, stop=True, r=[bst, uTb], w=[PS[bank]])
                for q4 in range(2):
                    bre = PS[4 + q4][:, :].rearrange("p (a b) -> p a b", a=4)[:, :, 0:nt]
                    bim = PS[6 + q4][:, :].rearrange("p (a b) -> p a b", a=4)[:, :, 0:nt]
                    bs = slice(hf * 8 + q4 * 4, hf * 8 + q4 * 4 + 4)
                    o4 = slice(q4 * 4, q4 * 4 + 4)
                    p.V("dve", "tensor_tensor", rbuf[:, 0, o4, 0:nt], bre, crT[:, bs, 0:nt], ALU.mult, r=[PS[4 + q4], crT], w=[(rbuf, 0)])
                    p.V("dve", "tensor_tensor", rtmp[:, 0, o4, 0:nt], bim, ciT[:, bs, 0:nt], ALU.mult, r=[PS[6 + q4], ciT], w=[(rtmp, 0)])
                    p.V("dve", "tensor_tensor", rbuf[:, 1, o4, 0:nt], bre, ciT[:, bs, 0:nt], ALU.mult, r=[PS[4 + q4], ciT], w=[(rbuf, 1)])
                    p.V("dve", "tensor_tensor", rtmp[:, 1, o4, 0:nt], bim, crT[:, bs, 0:nt], ALU.mult, r=[PS[6 + q4], crT], w=[(rtmp, 1)])
                p.V("pool", "tensor_tensor", rbuf[:, 0, :, 0:nt], rbuf[:, 0, :, 0:nt], rtmp[:, 0, :, 0:nt], ALU.subtract, r=[(rbuf, 0), (rtmp, 0)], w=[(rbuf, 0)])
                p.V("pool", "tensor_tensor", rbuf[:, 1, :, 0:nt], rbuf[:, 1, :, 0:nt], rtmp[:, 1, :, 0:nt], ALU.add, r=[(rbuf, 1), (rtmp, 1)], w=[(rbuf, 1)])
                for b8 in range(8):
                    blk = hf * 8 + b8
                    p.V("dve", "tensor_tensor_scan", gsc[:, 0, blk, 0:nt], lam[:, blk:blk + 1].to_broadcast([128, nt]), rbuf[:, 0, b8, 0:nt],
                        Hre[:, blk:blk + 1], ALU.mult, ALU.add, r=[lam, (rbuf, 0), Hre], w=[(gsc, (0, blk))])
                    p.V("dve", "tensor_tensor_scan", gsc[:, 1, blk, 0:nt], lam[:, blk:blk + 1].to_broadcast([128, nt]), rbuf[:, 1, b8, 0:nt],
                        Him[:, blk:blk + 1], ALU.mult, ALU.add, r=[lam, (rbuf, 1), Him], w=[(gsc, (1, blk))])
            t_a, t_b = rbuf[:, :, :, :].rearrange("p a b c -> p (a b) c"), rtmp[:, :, :, :].rearrange("p a b c -> p (a b) c")
            p.V("pool", "tensor_tensor", t_a[:, :, 0:nt], gsc[:, 0, :, 0:nt], cosT[:, :, 0:nt], ALU.mult, r=[gsc, cosT], w=[rbuf])
            p.V("pool", "tensor_tensor", t_b[:, :, 0:nt], gsc[:, 1, :, 0:nt], sinT[:, :, 0:nt], ALU.mult, r=[gsc, sinT], w=[rtmp])
            p.V("dve", "tensor_tensor", hbf[:, 0, :, 0:nt], t_a[:, :, 0:nt], t_b[:, :, 0:nt], ALU.subtract, r=[rbuf, rtmp], w=[(hbf, 0)])
            lc = nt - 1
            p.V("dve", "tensor_tensor", hl[:, 0, :], gsc[:, 0, :, lc], cosT[:, :, lc], ALU.mult, r=[gsc, cosT], w=[(hl, 0)])
            p.V("dve", "tensor_tensor", hl[:, 1, :], gsc[:, 1, :, lc], sinT[:, :, lc], ALU.mult, r=[gsc, sinT], w=[(hl, 1)])
            p.V("dve", "tensor_tensor", hl[:, 2, :], gsc[:, 0, :, lc], sinT[:, :, lc], ALU.mult, r=[gsc, sinT], w=[(hl, 2)])
            p.V("dve", "tensor_tensor", hl[:, 3, :], gsc[:, 1, :, lc], cosT[:, :, lc], ALU.mult, r=[gsc, cosT], w=[(hl, 3)])
            p.V("pool", "tensor_tensor", t_a[:, :, 0:nt], gsc[:, 0, :, 0:nt], sinT[:, :, 0:nt], ALU.mult, r=[gsc, sinT, (hbf, 0)], w=[rbuf])
            p.V("pool", "tensor_tensor", t_b[:, :, 0:nt], gsc[:, 1, :, 0:nt], cosT[:, :, 0:nt], ALU.mult, r=[gsc, cosT, (hbf, 0)], w=[rtmp])
            p.V("dve", "scalar_tensor_tensor", hbf[:, 1, :, 0:nt], t_a[:, :, 0:nt], -1.0, t_b[:, :, 0:nt], ALU.mult, ALU.subtract,
                r=[rbuf, rtmp], w=[(hbf, 1)])
            p.V("dve", "tensor_tensor", Hre[:], hl[:, 0, :], hl[:, 1, :], ALU.subtract, r=[hl], w=[Hre])
            p.V("dve", "tensor_tensor", Him[:], hl[:, 2, :], hl[:, 3, :], ALU.add, r=[hl], w=[Him])
            if tl["last"]:
                p.DM("sp", o_s5re[sq], Hre[:], r=[Hre], w=[T_out])
                p.DM("sp", o_s5im[sq], Him[:], r=[Him], w=[T_out])
            for ob in range(4):
                n = 0
                for b4 in range(4):
                    blk = ob * 4 + b4
                    for ri in range(2):
                        p.V("pe", "matmul", PS[1][:, ob * 128:ob * 128 + nt], cstt[:, ri, blk, :], hbf[:, ri, blk, 0:nt],
                            start=(n == 0), stop=(n == 7), r=[cstt, hbf], w=[PS[1]])
                        n += 1
            for ob in range(4):
                p.V("dve", "scalar_tensor_tensor", yA[:, ob, 0:nt], uTf[:, ob, 0:nt], dsk[:, ob:ob + 1], PS[1][:, ob * 128:ob * 128 + nt],
                    ALU.mult, ALU.add, r=[uTf, dsk, PS[1]], w=[yA])
            cg = math.sqrt(2.0 / math.pi)
            p.V("act", "activation", ysq[:, :, 0:nt], yA[:, :, 0:nt], AF.Square, r=[yA], w=[ysq])
            p.V("dve", "tensor_scalar", ysq[:, :, 0:nt], ysq[:, :, 0:nt], 2.0 * cg * 0.044715, 2.0 * cg, ALU.mult, ALU.add, r=[ysq], w=[ysq])
            p.V("dve", "tensor_tensor", ysq[:, :, 0:nt], ysq[:, :, 0:nt], yA[:, :, 0:nt], ALU.mult, r=[ysq, yA], w=[ysq])
            p.V("act", "activation", ysq[:, :, 0:nt], ysq[:, :, 0:nt], AF.Sigmoid, r=[ysq], w=[ysq])
            p.V("dve", "tensor_tensor", gaf[:, :, 0:nt], ysq[:, :, 0:nt], yA[:, :, 0:nt], ALU.mult, r=[ysq, yA], w=[gaf])
            p.V("act", "activation", gab[:, :, 0:nt], gaf[:, :, 0:nt], AF.Copy, r=[gaf], w=[gab])
            for ob in range(4):
                for kc in range(4):
                    p.V("pe", "matmul", PS[0][:, ob * 128:ob * 128 + nt], wglu[:, kc, ob * 128:(ob + 1) * 128], gab[:, kc, 0:nt],
                        start=(kc == 0), stop=(kc == 3), r=[wglu, gab], w=[PS[0]])
            for ob in range(4):
                p.V("act", "activation", ysq[:, ob, 0:nt], PS[0][:, ob * 128:ob * 128 + nt], AF.Sigmoid, bias=bgl[:, ob:ob + 1],
                    r=[PS[0], bgl], w=[ysq])
            p.V("dve", "tensor_tensor", mixT[:, 0:4, 0:nt], ysq[:, :, 0:nt], gaf[:, :, 0:nt], ALU.mult, r=[ysq, gaf], w=[(mixT, "a")])
            if CUT <= 3:
                continue
            for j in range(4):
                wj = wc[:, :, j:j + 1].to_broadcast([128, 12, nt])
                if j == 0:
                    p.V("dve", "tensor_tensor", cacc[:, :, 0:nt], cb[:, :, 0:nt], wj, ALU.mult, r=[cb, wc], w=[cacc])
                else:
                    p.V("pool", "tensor_tensor", ctmp[:, :, 0:nt], cb[:, :, j:j + nt], wj, ALU.mult, r=[cb, wc], w=[ctmp])
                    p.V("dve", "tensor_tensor", cacc[:, :, 0:nt], cacc[:, :, 0:nt], ctmp[:, :, 0:nt], ALU.add, r=[cacc, ctmp], w=[cacc])
            p.V("pool", "tensor_copy", ctx3[:, :, :], cb[:, :, nt:nt + 3], r=[cb], w=[ctx3])
            if tl["last"]:
                p.DM("sp", o_conv[sq], ctx3[:], r=[ctx3], w=[T_out])
            p.V("act", "activation", cacc[:, :, 0:nt], cacc[:, :, 0:nt], AF.Silu, r=[cacc], w=[cacc])
            p.V("act", "activation", sq8[:, :, 0:nt], cacc[:, 0:8, 0:nt], AF.Square, r=[cacc], w=[sq8])
            for hb in range(2):
                for b in range(4):
                    p.V("pe", "matmul", PS[2 + hb][:, b * 128:b * 128 + nt], ones_f[:, :], sq8[:, hb * 4 + b, 0:nt], start=True, stop=True,
                        r=[ones_f, sq8], w=[PS[2 + hb]])
            for hb in range(2):
                v = PS[2 + hb][:, :].rearrange("p (a b) -> p a b", a=4)[:, :, 0:nt]
                p.V("act", "activation", sq8[:, hb * 4:hb * 4 + 4, 0:nt], v, AF.Sqrt, bias=epsln[:, 1:2], r=[PS[2 + hb], epsln], w=[(sq8, hb)])
            p.V("dve", "reciprocal", sq8[:, :, 0:nt], sq8[:, :, 0:nt], r=[sq8], w=[sq8])
            p.V("dve", "scalar_tensor_tensor", qkn[:, 0:4, 0:nt], cacc[:, 0:4, 0:nt], 128.0 ** -0.5, sq8[:, 0:4, 0:nt], ALU.mult, ALU.mult,
                r=[cacc, sq8], w=[(qkn, "q")])
            p.V("dve", "tensor_tensor", qkn[:, 4:8, 0:nt], cacc[:, 4:8, 0:nt], sq8[:, 4:8, 0:nt], ALU.mult, r=[cacc, sq8], w=[(qkn, "k")])
            for b in range(4):
                p.V("pe", "transpose", PS[2][0:nt, b * 128:(b + 1) * 128], qkn[:, 4 + b, 0:nt], identf[:, :], r=[qkn, identf], w=[PS[2]])
                p.V("pe", "transpose", PS[3][0:nt, b * 128:(b + 1) * 128], cacc[:, 8 + b, 0:nt], identf[:, :], r=[cacc, identf], w=[PS[3]])
            evac(kvtok[0:nt, 0:4, :], PS[2][0:nt, :].rearrange("p (a b) -> p a b", a=4), r=[PS[2]], w=[kvtok])
            evac(kvtok[0:nt, 4:8, :], PS[3][0:nt, :].rearrange("p (a b) -> p a b", a=4), r=[PS[3]], w=[kvtok])
            if CUT2 <= 1:
                continue
            p.V("act", "activation", gd[0:nt, 0:4], ztok[0:nt, 0:4], AF.Sigmoid, r=[ztok], w=[(gd, "b")])
            p.V("dve", "tensor_scalar", gd[0:nt, 4:8], gd[0:nt, 0:4], -1.0, None, ALU.mult, r=[(gd, "b")], w=[(gd, "nb")])
            p.V("dve", "tensor_tensor", gd[0:nt, 8:12], ztok[0:nt, 4:8], dtb[0:nt, :], ALU.add, r=[ztok, dtb], w=[(gd, "g")])
            p.V("act", "activation", gd[0:nt, 8:12], gd[0:nt, 8:12], AF.Exp, r=[(gd, "g")], w=[(gd, "g")])
            p.V("act", "activation", gd[0:nt, 8:12], gd[0:nt, 8:12], AF.Ln, bias=1.0, r=[(gd, "g")], w=[(gd, "g")])
            p.V("dve", "scalar_tensor_tensor", gd[0:nt, 8:12], gd[0:nt, 8:12], -1.0, alog[0:nt, :], ALU.mult, ALU.mult, r=[(gd, "g"), alog], w=[(gd, "g")])
            p.V("pe", "matmul", PS[0][0:nt, 0:4], uincl[0:nt, 0:nt], gd[0:nt, 8:12], start=True, stop=True, r=[uincl, (gd, "g")], w=[PS[0]])
            p.V("dve", "tensor_copy", gd[0:nt, 12:16], PS[0][0:nt, 0:4], r=[PS[0]], w=[(gd, "G")])
            p.V("act", "activation", gd2[0:nt, 0:4], gd[0:nt, 12:16], AF.Exp, r=[(gd, "G")], w=[(gd2, "e")])
            p.V("dve", "tensor_tensor", gd2[0:nt, 4:8], gd2[0:nt, 0:4], gd[0:nt, 0:4], ALU.mult, r=[(gd2, "e"), (gd, "b")], w=[(gd2, "be")])
            if CUT2 <= 2:
                continue
            for hd in range(4):
                kT = qkn[:, 4 + hd, 0:nt]
                qT = qkn[:, hd, 0:nt]
                p.V("dve", "tensor_scalar", gbc[0:nt, :], ones_f[0:nt, :], gd[0:nt, 8 + hd:9 + hd], None, ALU.mult, r=[ones_f, (gd, "g")], w=[gbc])
                p.V("pe", "matmul", PS[0][:, 128:128 + nt], gbc[0:nt, :], uincl[0:nt, 0:nt], start=True, stop=True, r=[gbc, uincl], w=[PS[0]])
                p.V("pe", "matmul", PS[0][0:nt, 256:256 + nt], kT, kT, start=True, stop=True, r=[(qkn, "k")], w=[PS[0]])
                p.V("pe", "matmul", PS[0][0:nt, 384:384 + nt], qT, kT, start=True, stop=True, r=[(qkn, "q"), (qkn, "k")], w=[PS[0]])
                grow = PS[0][0:nt, 128:128 + nt]
                p.V("act", "activation", dec[0:nt, 0:nt], grow, AF.Exp, bias=gd[0:nt, 12 + hd:13 + hd], scale=-1.0, r=[PS[0], (gd, "G")], w=[dec])
                p.V("act", "activation", erow[:, 0:nt], PS[0][:, 128:128 + nt], AF.Exp, r=[PS[0]], w=[erow])
                p.V("pool", "affine_select", dec[0:nt, 0:nt], dec[0:nt, 0:nt], [[-1, nt]], ALU.is_ge, 0.0, base=0, channel_multiplier=1, r=[dec], w=[dec])
                p.V("dve", "scalar_tensor_tensor", Mm[0][0:nt, 0:nt], PS[0][0:nt, 256:256 + nt], gd[0:nt, 4 + hd:5 + hd], dec[0:nt, 0:nt], ALU.mult, ALU.mult,
                    r=[PS[0], (gd, "nb"), dec], w=[Mm[0]])
                p.V("pool", "affine_select", Mm[0][0:nt, 0:nt], Mm[0][0:nt, 0:nt], [[-1, nt]], ALU.is_gt, 0.0, base=0, channel_multiplier=1, r=[Mm[0]], w=[Mm[0]])
                p.V("dve", "tensor_tensor", attn[0:nt, 0:nt], PS[0][0:nt, 384:384 + nt], dec[0:nt, 0:nt], ALU.mult, r=[PS[0], dec], w=[attn])
                p.V("pe", "transpose", PS[1][0:nt, 0:nt], Mm[0][0:nt, 0:nt], identf[0:nt, 0:nt], r=[Mm[0], identf], w=[PS[1]])
                p.V("pe", "transpose", PS[1][0:nt, 128:128 + nt], attn[0:nt, 0:nt], identf[0:nt, 0:nt], r=[attn, identf], w=[PS[1]])
                p.V("act", "activation", MT[0][0:nt, 0:nt], PS[1][0:nt, 0:nt], AF.Copy, r=[PS[1]], w=[MT[0]])
                p.V("dve", "tensor_tensor", Yt[0:nt, 0:nt], PS[1][0:nt, 0:nt], identf[0:nt, 0:nt], ALU.add, r=[PS[1], identf], w=[Yt])
                p.V("act", "activation", attnT[0:nt, 0:nt], PS[1][0:nt, 128:128 + nt], AF.Copy, r=[PS[1]], w=[attnT])
                if CUT2 <= 3:
                    continue
                nlev = 6 if nt == 128 else 3
                for lv in range(nlev):
                    a, b_ = lv % 2, (lv + 1) % 2
                    bank = 6 + (lv % 2)
                    p.V("pe", "matmul", PS[bank][0:nt, 0:nt], MT[a][0:nt, 0:nt], Mm[a][0:nt, 0:nt], start=True, stop=True, r=[MT[a], Mm[a]], w=[PS[bank]])
                    if lv < nlev - 1:
                        p.V("pe", "matmul", PS[bank][0:nt, 128:128 + nt], Mm[a][0:nt, 0:nt], MT[a][0:nt, 0:nt], start=True, stop=True, r=[MT[a], Mm[a]], w=[PS[bank]])
                    p.V("act", "activation", Mm[b_][0:nt, 0:nt], PS[bank][0:nt, 0:nt], AF.Copy, r=[PS[bank]], w=[Mm[b_]])
                    if lv < nlev - 1:
                        p.V("dve", "tensor_copy", MT[b_][0:nt, 0:nt], PS[bank][0:nt, 128:128 + nt], r=[PS[bank]], w=[MT[b_]])
                    p.V("pe", "matmul", PS[bank][0:nt, 256:256 + nt], Mm[b_][0:nt, 0:nt], Yt[0:nt, 0:nt], start=True, stop=True, r=[Mm[b_], Yt], w=[PS[bank]])
                    p.V("dve", "tensor_tensor", Yt[0:nt, 0:nt], Yt[0:nt, 0:nt], PS[bank][0:nt, 256:256 + nt], ALU.add, r=[Yt, PS[bank]], w=[Yt])
                if CUT2 <= 4:
                    continue
                p.V("dve", "tensor_scalar", rv[0:nt, :], kvtok[0:nt, 4 + hd, :], gd[0:nt, hd:hd + 1], None, ALU.mult, r=[kvtok, (gd, "b")], w=[rv])
                p.V("dve", "tensor_scalar", rk_[0:nt, :], kvtok[0:nt, hd, :], gd2[0:nt, 4 + hd:5 + hd], None, ALU.mult, r=[kvtok, (gd2, "be")], w=[rk_])
                p.V("pe", "matmul", PS[1][:, 256:256 + nt], rk_[0:nt, :], Yt[0:nt, 0:nt], start=True, stop=True, r=[rk_, Yt], w=[PS[1]])
                p.V("dve", "tensor_scalar", nwT[:, 0:nt], PS[1][:, 256:256 + nt], -1.0, None, ALU.mult, r=[PS[1]], w=[nwT])
                p.V("pe", "matmul", PS[5][0:nt, 0:128], Yt[0:nt, 0:nt], rv[0:nt, :], start=True, stop=False, r=[Yt, rv], w=[PS[5]])
                p.V("pe", "matmul", PS[5][0:nt, 0:128], nwT[:, 0:nt], Sg[:, hd, :], start=False, stop=True, r=[nwT, Sg], w=[PS[5]])
                p.V("act", "activation", ub[0:nt, :], PS[5][0:nt, 0:128], AF.Copy, r=[PS[5]], w=[ub])
                p.V("dve", "tensor_tensor", qdT[:, 0:nt], qT, erow[:, 0:nt], ALU.mult, r=[(qkn, "q"), erow], w=[qdT])
                p.V("pe", "matmul", PS[5][0:nt, 128:256], qdT[:, 0:nt], Sg[:, hd, :], start=True, stop=False, r=[qdT, Sg], w=[PS[5]])
                p.V("pe", "matmul", PS[5][0:nt, 128:256], attnT[0:nt, 0:nt], ub[0:nt, :], start=False, stop=True, r=[attnT, ub], w=[PS[5]])
                if CUT2 <= 5:
                    continue
                p.V("dve", "tensor_copy", gd2[:, 12:13], PS[0][:, 128 + nt - 1:128 + nt], r=[PS[0]], w=[(gd2, "gl")])
                p.V("act", "activation", gd2[0:nt, 8 + hd:9 + hd], gd[0:nt, 12 + hd:13 + hd], AF.Exp, bias=gd2[0:nt, 12:13], scale=-1.0,
                    r=[(gd, "G"), (gd2, "gl")], w=[(gd2, ("kd", hd))])
                p.V("dve", "tensor_scalar", kd[0:nt, :], kvtok[0:nt, hd, :], gd2[0:nt, 8 + hd:9 + hd], None, ALU.mult, r=[kvtok, (gd2, ("kd", hd))], w=[kd])
                p.V("pe", "matmul", PS[5][:, 256:384], kd[0:nt, :], ub[0:nt, :], start=True, stop=True, r=[kd, ub], w=[PS[5]])
                p.V("act", "activation", gd2[:, 13:14], gd2[:, 12:13], AF.Exp, r=[(gd2, "gl")], w=[(gd2, "egl")])
                p.V("dve", "scalar_tensor_tensor", Sg[:, hd, :], Sg[:, hd, :], gd2[:, 13:14], PS[5][:, 256:384], ALU.mult, ALU.add,
                    r=[Sg, (gd2, "egl"), PS[5]], w=[Sg])
                if CUT2 <= 6:
                    continue
                p.V("act", "activation", osb[0:nt, :], PS[5][0:nt, 128:256], AF.Square, r=[PS[5]], w=[osb])
                p.V("dve", "reduce_sum", ssq[0:nt, hd:hd + 1], osb[0:nt, :], mybir.AxisListType.X, r=[osb], w=[(ssq, hd)])
                p.V("act", "activation", ssq[0:nt, hd:hd + 1], ssq[0:nt, hd:hd + 1], AF.Sqrt, bias=epsln[0:nt, 1:2], scale=1.0 / 128.0, r=[(ssq, hd), epsln], w=[(ssq, hd)])
                p.V("dve", "reciprocal", ssq[0:nt, hd:hd + 1], ssq[0:nt, hd:hd + 1], r=[(ssq, hd)], w=[(ssq, hd)])
                if CUT2 <= 7:
                    continue
                p.V("dve", "scalar_tensor_tensor", osb[0:nt, :], PS[5][0:nt, 128:256], ssq[0:nt, hd:hd + 1], nrmw[0:nt, :], ALU.mult, ALU.mult,
                    r=[PS[5], (ssq, hd), nrmw], w=[osb])
                p.V("dve", "tensor_tensor", yB[0:nt, hd * 128:(hd + 1) * 128], osb[0:nt, :], zs[0:nt, hd * 128:(hd + 1) * 128], ALU.mult, r=[osb, zs], w=[(yB, hd)])
            if tl["last"]:
                p.DM("sp", o_gdn[sq].rearrange("h k v -> k h v"), Sg[:], r=[Sg], w=[T_out])
            if CUT <= 4:
                continue
            for b in range(4):
                p.V("pe", "transpose", psbf(2, 1024)[:, b * 128:b * 128 + nt], yB[0:nt, b * 128:(b + 1) * 128], identb[0:nt, 0:nt], r=[yB, identb], w=[PS[2]])
            evac(mixT[:, 4:8, 0:nt], psbf(2, 1024).rearrange("p (a b) -> p a b", a=8)[:, 0:4, 0:nt], r=[PS[2]], w=[(mixT, "b")])
            for half in range(2):
                for kc in range(8):
                    p.V("pe", "matmul", PS[2 + half][0:nt, :], mixT[:, kc, 0:nt], wout[:, kc, half * 512:(half + 1) * 512], start=(kc == 0), stop=(kc == 7),
                        r=[mixT, wout], w=[PS[2 + half]])
            if CUT <= 5:
                continue
            phaseA_tail(0, tl, xt, (2, 3), lnw, rw, rb, work)
        p.release(m0)

    def run_streams(gens):
        gens = list(gens)
        while gens:
            for g in list(gens):
                try:
                    next(g)
                except StopIteration:
                    gens.remove(g)

    def phaseS0():
        m0 = p.mark()
        win_u = p.sb("win_u", [128, 8, 512], BF16)
        p.DM("pool", win_u[:], w_in_even[:, 0:512].rearrange("(kc q) n -> q kc n", q=128), r=[DR], w=[win_u])
        wglu = p.sb("wglu", [128, 4, 512], BF16)
        load_w_bf16(wglu, s5_wglu, 4)
        bst = p.sb("bst", [128, 2, 16, 128], BF16)
        cstt = p.sb("cstt", [128, 2, 16, 128], BF16)
        for ri in range(2):
            p.DM("pool", bst[:, ri, :, :], s5_bst[ri].rearrange("b k m -> k b m"), r=[DR], w=[(bst, ri)])
            p.DM("pool", cstt[:, ri, :, :], s5_cst[ri].rearrange("b k m -> k b m"), r=[DR], w=[(cstt, ri)])
        are = p.sb("are", [128, 16]); aim = p.sb("aim", [128, 16]); ldt = p.sb("ldt", [128, 16])
        for b, s_ in ((are, s5_are), (aim, s5_aim), (ldt, s5_ldt)):
            p.DM("sp", b[:], s_, r=[DR], w=[b])
        dsk = p.sb("dsk", [128, 4]); bgl = p.sb("bgl", [128, 4])
        p.DM("sp", dsk[:], s5_d, r=[DR], w=[dsk])
        p.DM("sp", bgl[:], s5_bglu, r=[DR], w=[bgl])
        tau = p.sb("tau", [128, 128])
        p.DM("sp", tau[:], cst["tau"], r=[DR], w=[tau])
        lam = p.sb("lam", [128, 16]); li_ = p.sb("li", [128, 16]); dtt = p.sb("dtt", [128, 16])
        p.V("act", "activation", dtt[:], ldt[:], AF.Exp, r=[ldt], w=[dtt])
        p.V("dve", "tensor_tensor", li_[:], aim[:], dtt[:], ALU.mult, r=[aim, dtt], w=[li_])
        p.V("dve", "tensor_tensor", lam[:], are[:], dtt[:], ALU.mult, r=[are, dtt], w=[lam])
        p.V("act", "activation", lam[:], lam[:], AF.Exp, r=[lam], w=[lam])
        cosT = p.sb("cosT", [128, 16, 128]); sinT = p.sb("sinT", [128, 16, 128])
        crT = p.sb("crT", [128, 16, 128]); ciT = p.sb("ciT", [128, 16, 128])
        ang = crT
        kq = ciT
        kif = p.sb("kif", [128, 16, 128])
        ki = alias(kif[:, :, :].bitcast(I32), kif, "ki")
        sc3 = p.sb("sc3", [128, 16, 128])
        TWO_PI = 2.0 * math.pi

        def sin_of(dst, shift):
            p.V("dve", "tensor_tensor", ang[:], li_[:, :].unsqueeze(2).to_broadcast([128, 16, 128]),
                tau[:, :].unsqueeze(1).to_broadcast([128, 16, 128]), ALU.mult, r=[li_, tau], w=[ang])
            if shift != 0.0:
                p.V("dve", "tensor_scalar", ang[:], ang[:], shift, None, ALU.add, r=[ang], w=[ang])
            p.V("dve", "tensor_scalar", kq[:], ang[:], 1.0 / TWO_PI, None, ALU.mult, r=[ang], w=[kq])
            p.V("dve", "tensor_copy", ki[:], kq[:], r=[kq], w=[ki])
            p.V("dve", "tensor_copy", kq[:], ki[:], r=[ki], w=[kq])
            p.V("dve", "scalar_tensor_tensor", ang[:], kq[:], -TWO_PI, ang[:], ALU.mult, ALU.add, r=[kq, ang], w=[ang])
            p.V("dve", "tensor_scalar", kq[:], ang[:], math.pi, TWO_PI, ALU.is_gt, ALU.mult, r=[ang], w=[kq])
            p.V("dve", "tensor_tensor", ang[:], ang[:], kq[:], ALU.subtract, r=[ang, kq], w=[ang])
            p.V("dve", "tensor_scalar", kq[:], ang[:], -math.pi, TWO_PI, ALU.is_lt, ALU.mult, r=[ang], w=[kq])
            p.V("dve", "tensor_tensor", ang[:], ang[:], kq[:], ALU.add, r=[ang, kq], w=[ang])
            p.V("dve", "tensor_scalar", ang[:], ang[:], math.pi, -math.pi, ALU.min, ALU.max, r=[ang], w=[ang])
            p.V("act", "activation", dst[:], ang[:], AF.Sin, r=[ang], w=[dst])

        sin_of(sinT, 0.0)
        sin_of(cosT, math.pi / 2)
        sm = p.sb("s5sm", [128, 8, 16])
        abre, abim, den, t1, t2, cfre, cfim, t3 = [sm[:, i, :] for i in range(8)]
        S = [sm]
        p.V("dve", "tensor_tensor", abre, lam[:], cosT[:, :, 0], ALU.mult, r=[lam, cosT], w=S)
        p.V("dve", "tensor_tensor", abim, lam[:], sinT[:, :, 0], ALU.mult, r=[lam, sinT], w=S)
        p.V("dve", "tensor_scalar", abre, abre, -1.0, None, ALU.add, r=S, w=S)
        p.V("dve", "tensor_tensor", t1, are[:], are[:], ALU.mult, r=[are], w=S)
        p.V("dve", "tensor_tensor", t2, aim[:], aim[:], ALU.mult, r=[aim], w=S)
        p.V("dve", "tensor_tensor", den, t1, t2, ALU.add, r=S, w=S)
        p.V("dve", "reciprocal", den, den, r=S, w=S)
        p.V("dve", "tensor_tensor", t1, abre, are[:], ALU.mult, r=S + [are], w=S)
        p.V("dve", "tensor_tensor", t2, abim, aim[:], ALU.mult, r=S + [aim], w=S)
        p.V("dve", "tensor_tensor", cfre, t1, t2, ALU.add, r=S, w=S)
        p.V("dve", "tensor_tensor", cfre, cfre, den, ALU.mult, r=S, w=S)
        p.V("dve", "tensor_tensor", t1, abim, are[:], ALU.mult, r=S + [are], w=S)
        p.V("dve", "tensor_tensor", t2, abre, aim[:], ALU.mult, r=S + [aim], w=S)
        p.V("dve", "tensor_tensor", cfim, t1, t2, ALU.subtract, r=S, w=S)
        p.V("dve", "tensor_tensor", cfim, cfim, den, ALU.mult, r=S, w=S)
        bc = lambda a: a.unsqueeze(2).to_broadcast([128, 16, 128])
        p.V("dve", "tensor_tensor", crT[:], cosT[:], bc(cfre), ALU.mult, r=[cosT] + S, w=[crT])
        p.V("dve", "tensor_tensor", sc3[:], sinT[:], bc(cfim), ALU.mult, r=[sinT] + S, w=[sc3])
        p.V("dve", "tensor_tensor", crT[:], crT[:], sc3[:], ALU.add, r=[crT, sc3], w=[crT])
        p.V("dve", "tensor_tensor", ciT[:], cosT[:], bc(cfim), ALU.mult, r=[cosT] + S, w=[ciT])
        p.V("dve", "tensor_tensor", sc3[:], sinT[:], bc(cfre), ALU.mult, r=[sinT] + S, w=[sc3])
        p.V("dve", "tensor_tensor", ciT[:], ciT[:], sc3[:], ALU.subtract, r=[ciT, sc3], w=[ciT])
        cg = math.sqrt(2.0 / math.pi)

        def stream(sx, tlist):
            B0, B1, B2, B3 = 4 * sx, 4 * sx + 1, 4 * sx + 2, 4 * sx + 3
            n_ = lambda s_: "%s_%d" % (s_, sx)
            xt = p.sb(n_("xt"), [128, D]); xb = p.sb(n_("xb"), [128, D], BF16); xT = p.sb(n_("xT"), [128, 8, 128], BF16)
            uTf = p.sb(n_("uTf"), [128, 4, 128]); uTb = p.sb(n_("uTb"), [128, 4, 128], BF16)
            rbuf = p.sb(n_("rbuf"), [128, 2, 4, 128]); rtmp = p.sb(n_("rtmp"), [128, 2, 4, 128]); gsc = p.sb(n_("gsc"), [128, 2, 4, 128])
            hbf = p.sb(n_("hbf"), [128, 2, 16, 128], BF16); hl = p.sb(n_("hl"), [128, 4, 16])
            yA = p.sb(n_("yA"), [128, 4, 128]); ysq = p.sb(n_("ysq"), [128, 4, 128]); gaf = p.sb(n_("gaf"), [128, 4, 128])
            gab = p.sb(n_("gab"), [128, 4, 128], BF16); yag = p.sb(n_("yag"), [128, 4, 128], BF16)
            Hre = p.sb(n_("Hre"), [128, 16]); Him = p.sb(n_("Him"), [128, 16])
            Hn = p.sb(n_("Hn"), [128, 2, 16])
            for tl in tlist:
                nt, ti, sq = tl["nt"], tl["ti"], tl["seq"]
                if nt < 128:
                    p.V("pool", "memset", xt[:], 0.0, w=[xt])
                p.DM("sp", xt[0:nt, :], xin[tl["row0"]:tl["row0"] + nt, :], r=[DR], w=[xt])
                if tl["first"]:
                    if sq < NPS:
                        p.V("pool", "memset", Hre[:], 0.0, w=[Hre]); p.V("pool", "memset", Him[:], 0.0, w=[Him])
                    else:
                        p.DM("sp", Hre[:], st_s5re, r=[DR], w=[Hre])
                        p.DM("sp", Him[:], st_s5im, r=[DR], w=[Him])
                yield
                p.V("act", "activation", xb[0:nt, :], xt[0:nt, :], AF.Copy, r=[xt], w=[xb])
                yield
                for b in range(8):
                    p.V("pe", "transpose", psbf(B0, 1024)[:, b * 128:b * 128 + nt], xb[0:nt, b * 128:(b + 1) * 128], identb[0:nt, 0:nt], r=[xb, identb], w=[PS[B0]])
                p.V("dve", "tensor_copy", xT[:, :, 0:nt], psbf(B0, 1024).rearrange("p (a b) -> p a b", a=8)[:, :, 0:nt], r=[PS[B0]], w=[xT])
                yield
                for ob in range(4):
                    for kc in range(8):
                        p.V("pe", "matmul", PS[B1][:, ob * 128:ob * 128 + nt], win_u[:, kc, ob * 128:(ob + 1) * 128], xT[:, kc, 0:nt],
                            start=(kc == 0), stop=(kc == 7), r=[win_u, xT], w=[PS[B1]])
                ps1v = PS[B1][:, :].rearrange("p (a b) -> p a b", a=4)[:, :, 0:nt]
                p.V("act", "activation", uTf[:, :, 0:nt], ps1v, AF.Copy, r=[PS[B1]], w=[uTf])
                p.V("act", "activation", uTb[:, :, 0:nt], uTf[:, :, 0:nt], AF.Copy, r=[uTf], w=[uTb])
                yield
                lc = nt - 1
                for g4 in range(4):
                    bs = slice(g4 * 4, g4 * 4 + 4)
                    for ri in range(2):
                        for b in range(4):
                            blk = g4 * 4 + b
                            p.V("pe", "matmul", PS[B2 + ri][:, b * 128:b * 128 + nt], bst[:, ri, blk, :], uTb[:, blk // 4, 0:nt], start=True, stop=True,
                                r=[bst, uTb], w=[PS[B2 + ri]])
                    bre = PS[B2][:, :].rearrange("p (a b) -> p a b", a=4)[:, :, 0:nt]
                    bim = PS[B3][:, :].rearrange("p (a b) -> p a b", a=4)[:, :, 0:nt]
                    p.V("dve", "tensor_tensor", rbuf[:, 0, :, 0:nt], bre, crT[:, bs, 0:nt], ALU.mult, r=[PS[B2], crT], w=[(rbuf, 0)])
                    p.V("dve", "tensor_tensor", rbuf[:, 1, :, 0:nt], bre, ciT[:, bs, 0:nt], ALU.mult, r=[PS[B2], ciT], w=[(rbuf, 1)])
                    yield
                    p.V("dve", "tensor_tensor", rtmp[:, 0, :, 0:nt], bim, ciT[:, bs, 0:nt], ALU.mult, r=[PS[B3], ciT], w=[(rtmp, 0)])
                    p.V("dve", "tensor_tensor", rtmp[:, 1, :, 0:nt], bim, crT[:, bs, 0:nt], ALU.mult, r=[PS[B3], crT], w=[(rtmp, 1)])
                    yield
                    p.V("pool", "tensor_tensor", rbuf[:, 0, :, 0:nt], rbuf[:, 0, :, 0:nt], rtmp[:, 0, :, 0:nt], ALU.subtract, r=[(rbuf, 0), (rtmp, 0)], w=[(rbuf, 0)])
                    p.V("pool", "tensor_tensor", rbuf[:, 1, :, 0:nt], rbuf[:, 1, :, 0:nt], rtmp[:, 1, :, 0:nt], ALU.add, r=[(rbuf, 1), (rtmp, 1)], w=[(rbuf, 1)])
                    yield
                    for b in range(4):
                        blk = g4 * 4 + b
                        p.V("dve", "tensor_tensor_scan", gsc[:, 0, b, 0:nt], lam[:, blk:blk + 1].to_broadcast([128, nt]), rbuf[:, 0, b, 0:nt],
                            Hre[:, blk:blk + 1], ALU.mult, ALU.add, r=[lam, (rbuf, 0), Hre], w=[(gsc, (0, b))])
                        p.V("dve", "tensor_tensor_scan", gsc[:, 1, b, 0:nt], lam[:, blk:blk + 1].to_broadcast([128, nt]), rbuf[:, 1, b, 0:nt],
                            Him[:, blk:blk + 1], ALU.mult, ALU.add, r=[lam, (rbuf, 1), Him], w=[(gsc, (1, b))])
                        yield
                    p.V("pool", "tensor_tensor", rbuf[:, 0, :, 0:nt], gsc[:, 0, :, 0:nt], cosT[:, bs, 0:nt], ALU.mult, r=[gsc, cosT], w=[(rbuf, 0)])
                    p.V("pool", "tensor_tensor", rbuf[:, 1, :, 0:nt], gsc[:, 1, :, 0:nt], sinT[:, bs, 0:nt], ALU.mult, r=[gsc, sinT], w=[(rbuf, 1)])
                    yield
                    p.V("pool", "tensor_tensor", rtmp[:, 0, :, 0:nt], gsc[:, 0, :, 0:nt], sinT[:, bs, 0:nt], ALU.mult, r=[gsc, sinT], w=[(rtmp, 0)])
                    p.V("pool", "tensor_tensor", rtmp[:, 1, :, 0:nt], gsc[:, 1, :, 0:nt], cosT[:, bs, 0:nt], ALU.mult, r=[gsc, cosT], w=[(rtmp, 1)])
                    yield
                    p.V("dve", "tensor_tensor", hbf[:, 0, bs, 0:nt], rbuf[:, 0, :, 0:nt], rbuf[:, 1, :, 0:nt], ALU.subtract, r=[rbuf], w=[(hbf, (0, g4))])
                    p.V("dve", "scalar_tensor_tensor", hbf[:, 1, bs, 0:nt], rtmp[:, 0, :, 0:nt], -1.0, rtmp[:, 1, :, 0:nt], ALU.mult, ALU.subtract,
                        r=[rtmp], w=[(hbf, (1, g4))])
                    yield
                    p.V("dve", "tensor_tensor", Hn[:, 0, bs], rbuf[:, 0, :, lc], rbuf[:, 1, :, lc], ALU.subtract, r=[rbuf], w=[(Hn, (0, g4))])
                    p.V("dve", "tensor_tensor", Hn[:, 1, bs], rtmp[:, 0, :, lc], rtmp[:, 1, :, lc], ALU.add, r=[rtmp], w=[(Hn, (1, g4))])
                    yield
                p.V("dve", "tensor_copy", Hre[:], Hn[:, 0, :], r=[Hn], w=[Hre])
                p.V("dve", "tensor_copy", Him[:], Hn[:, 1, :], r=[Hn], w=[Him])
                if tl["last"]:
                    p.DM("sp", o_s5re[sq], Hre[:], r=[Hre], w=[T_out])
                    p.DM("sp", o_s5im[sq], Him[:], r=[Him], w=[T_out])
                yield
                for ob in range(4):
                    n = 0
                    for b4 in range(4):
                        blk = ob * 4 + b4
                        for ri in range(2):
                            p.V("pe", "matmul", PS[B0][:, ob * 128:ob * 128 + nt], cstt[:, ri, blk, :], hbf[:, ri, blk, 0:nt],
                                start=(n == 0), stop=(n == 7), r=[cstt, hbf], w=[PS[B0]])
                            n += 1
                yield
                for ob in range(4):
                    p.V("dve", "scalar_tensor_tensor", yA[:, ob, 0:nt], uTf[:, ob, 0:nt], dsk[:, ob:ob + 1], PS[B0][:, ob * 128:ob * 128 + nt],
                        ALU.mult, ALU.add, r=[uTf, dsk, PS[B0]], w=[(yA, ob)])
                yield
                p.V("act", "activation", ysq[:, :, 0:nt], yA[:, :, 0:nt], AF.Square, r=[yA], w=[ysq])
                yield
                p.V("dve", "tensor_scalar", ysq[:, :, 0:nt], ysq[:, :, 0:nt], 2.0 * cg * 0.044715, 2.0 * cg, ALU.mult, ALU.add, r=[ysq], w=[ysq])
                yield
                p.V("pool", "tensor_tensor", ysq[:, :, 0:nt], ysq[:, :, 0:nt], yA[:, :, 0:nt], ALU.mult, r=[ysq, yA], w=[ysq])
                yield
                p.V("act", "activation", ysq[:, :, 0:nt], ysq[:, :, 0:nt], AF.Sigmoid, r=[ysq], w=[ysq])
                yield
                p.V("pool", "tensor_tensor", gaf[:, :, 0:nt], ysq[:, :, 0:nt], yA[:, :, 0:nt], ALU.mult, r=[ysq, yA], w=[gaf])
                yield
                p.V("act", "activation", gab[:, :, 0:nt], gaf[:, :, 0:nt], AF.Copy, r=[gaf], w=[gab])
                yield
                for ob in range(4):
                    for kc in range(4):
                        p.V("pe", "matmul", PS[B1][:, ob * 128:ob * 128 + nt], wglu[:, kc, ob * 128:(ob + 1) * 128], gab[:, kc, 0:nt],
                            start=(kc == 0), stop=(kc == 3), r=[wglu, gab], w=[PS[B1]])
                yield
                for ob in range(4):
                    p.V("act", "activation", ysq[:, ob, 0:nt], PS[B1][:, ob * 128:ob * 128 + nt], AF.Sigmoid, bias=bgl[:, ob:ob + 1],
                        r=[PS[B1], bgl], w=[ysq])
                yield
                p.V("pool", "tensor_tensor", yag[:, :, 0:nt], ysq[:, :, 0:nt], gaf[:, :, 0:nt], ALU.mult, r=[ysq, gaf], w=[yag])
                p.DM("sp", yas[ti, :, :, 0:nt], yag[:, :, 0:nt], r=[yag], w=[(T_yas, ti)])
                yield

        lists = [[], []]
        for tl in tiles:
            lists[tl["seq"] % 2].append(tl)
        run_streams([stream(0, lists[0]), stream(1, lists[1])])
        p.release(m0)

    def phaseG0():
        m0 = p.mark()
        NG = EVEN_IN - 512
        lmi = p.sb("lmi", [128, 128]); lms = p.sb("lms", [128, 128])
        p.DM("sp", lmi[:], cst["lmi"], r=[DR], w=[lmi])
        p.DM("sp", lms[:], cst["lms"], r=[DR], w=[lms])
        win = p.sb("win_g", [128, 8, NG], BF16)
        vsrc = w_in_even.rearrange("(kc q) n -> q kc n", q=128)
        p.DM("pool", win[:, :, 0:1024], vsrc[:, :, 512:1536], r=[DR], w=[(win, 0)])
        p.DM("pool", win[:, :, 1024:NG], vsrc[:, :, 1536:EVEN_IN], r=[DR], w=[(win, 1)])
        wout = p.sb("wout", [128, 8, D], BF16)
        load_w_bf16(wout, w_out_even, 8)
        lnw, rw, rb = load_ln_router(0)
        wc = p.sb("wc", [128, 12, 4])
        p.DM("sp", wc[:], gdn_convw, r=[DR], w=[wc])
        alog = p.sb("alog", [128, 4]); dtb = p.sb("dtb", [128, 4]); nrmw = p.sb("nrmw", [128, 128])
        bcast_load(alog, gdn_alog); bcast_load(dtb, gdn_dtb); bcast_load(nrmw, gdn_normw)
        p.V("act", "activation", alog[:], alog[:], AF.Exp, r=[alog], w=[alog])
        Sg = p.sb("Sg", [128, 4, 128])
        ctx3 = p.sb("ctx3", [128, 12, 3])
        xts = [p.sb("xt0", [128, D]), p.sb("xt1", [128, D])]
        xb = p.sb("xb", [128, D], BF16)
        xT = p.sb("xT", [128, 8, 128], BF16)
        cb = p.sb("cb", [128, 12, 131])
        cacc = p.sb("cacc", [128, 12, 128]); ctmp = p.sb("ctmp", [128, 12, 128])
        ztok = p.sb("ztok", [128, 8])
        mixT = p.sb("mixT", [128, 8, 128], BF16)
        qkn = p.sb("qkn", [128, 8, 128]); sq8 = p.sb("sq8", [128, 8, 128])
        kvtok = p.sb("kvtok", [128, 8, 128])
        gd = p.sb("gd", [128, 16]); gd2 = p.sb("gd2", [128, 4, 8])
        HT = []
        for hd in range(4):
            HT.append([p.sb("gt%d_%d" % (hd, i), [128, 128]) for i in range(17)])
        yB = p.sb("yB", [128, 512], BF16); ssq = p.sb("ssq", [128, 4])
        zs = p.sb("zs", [128, 512], BF16)
        work = alloc_tail_work()

        def load_x(tl, buf):
            if tl["nt"] < 128:
                p.V("pool", "memset", buf[:], 0.0, w=[buf])
            p.DM("sp", buf[0:tl["nt"], :], xin[tl["row0"]:tl["row0"] + tl["nt"], :], r=[DR], w=[buf])

        def head(hd, nt):
            A, B = 2 * hd, 2 * hd + 1
            gbc, dec, erow, attn, attnT, rv, rk_, nwT, ub, qdT, kd, Yt, M0, M1, T0, T1, osb = HT[hd]
            Mm = [M0, M1]; MT = [T0, T1]
            g2 = gd2[:, hd, :]
            kT = qkn[:, 4 + hd, 0:nt]
            qT = qkn[:, hd, 0:nt]
            p.V("dve", "tensor_scalar", gbc[0:nt, :], ones_f[0:nt, :], gd[0:nt, 8 + hd:9 + hd], None, ALU.mult, r=[ones_f, (gd, "g")], w=[gbc])
            yield
            p.V("pe", "matmul", PS[A][:, 0:nt], gbc[0:nt, :], uincl[0:nt, 0:nt], start=True, stop=True, r=[gbc, uincl], w=[PS[A]])
            p.V("pe", "matmul", PS[A][0:nt, 128:128 + nt], kT, kT, start=True, stop=True, r=[(qkn, "k")], w=[PS[A]])
            p.V("pe", "matmul", PS[A][0:nt, 256:256 + nt], qT, kT, start=True, stop=True, r=[(qkn, "q"), (qkn, "k")], w=[PS[A]])
            yield
            p.V("act", "activation", dec[0:nt, 0:nt], PS[A][0:nt, 0:nt], AF.Exp, bias=gd[0:nt, 12 + hd:13 + hd], scale=-1.0, r=[PS[A], (gd, "G")], w=[dec])
            yield
            p.V("act", "activation", erow[:, 0:nt], PS[A][:, 0:nt], AF.Exp, r=[PS[A]], w=[erow])
            yield
            p.V("dve", "tensor_copy", g2[:, 4:5], PS[A][:, nt - 1:nt], r=[PS[A]], w=[(gd2, (hd, "gl"))])
            yield
            p.V("dve", "scalar_tensor_tensor", dec[0:nt, 0:nt], dec[0:nt, 0:nt], 1.0, lmi[0:nt, 0:nt], ALU.min, ALU.mult, r=[dec, lmi], w=[dec])
            yield
            p.V("dve", "scalar_tensor_tensor", Mm[0][0:nt, 0:nt], PS[A][0:nt, 128:128 + nt], gd[0:nt, 4 + hd:5 + hd], dec[0:nt, 0:nt], ALU.mult, ALU.mult,
                r=[PS[A], (gd, "nb"), dec], w=[Mm[0]])
            yield
            p.V("dve", "tensor_tensor", Mm[0][0:nt, 0:nt], Mm[0][0:nt, 0:nt], lms[0:nt, 0:nt], ALU.mult, r=[Mm[0], lms], w=[Mm[0]])
            yield
            p.V("dve", "tensor_tensor", attn[0:nt, 0:nt], PS[A][0:nt, 256:256 + nt], dec[0:nt, 0:nt], ALU.mult, r=[PS[A], dec], w=[attn])
            yield
            p.V("pe", "transpose", PS[B][0:nt, 0:nt], Mm[0][0:nt, 0:nt], identf[0:nt, 0:nt], r=[Mm[0], identf], w=[PS[B]])
            p.V("pe", "transpose", PS[B][0:nt, 128:128 + nt], attn[0:nt, 0:nt], identf[0:nt, 0:nt], r=[attn, identf], w=[PS[B]])
            yield
            p.V("act", "activation", MT[0][0:nt, 0:nt], PS[B][0:nt, 0:nt], AF.Copy, r=[PS[B]], w=[MT[0]])
            yield
            p.V("dve", "tensor_tensor", Yt[0:nt, 0:nt], PS[B][0:nt, 0:nt], identf[0:nt, 0:nt], ALU.add, r=[PS[B], identf], w=[Yt])
            yield
            p.V("act", "activation", attnT[0:nt, 0:nt], PS[B][0:nt, 128:128 + nt], AF.Copy, r=[PS[B]], w=[attnT])
            yield
            nlev = 6 if nt == 128 else 3
            for lv in range(nlev):
                a, b_ = lv % 2, (lv + 1) % 2
                bank = A if lv % 2 == 0 else B
                p.V("pe", "matmul", PS[bank][0:nt, 0:nt], MT[a][0:nt, 0:nt], Mm[a][0:nt, 0:nt], start=True, stop=True, r=[MT[a], Mm[a]], w=[PS[bank]])
                if lv < nlev - 1:
                    p.V("pe", "matmul", PS[bank][0:nt, 128:128 + nt], Mm[a][0:nt, 0:nt], MT[a][0:nt, 0:nt], start=True, stop=True, r=[MT[a], Mm[a]], w=[PS[bank]])
                yield
                p.V("act", "activation", Mm[b_][0:nt, 0:nt], PS[bank][0:nt, 0:nt], AF.Copy, r=[PS[bank]], w=[Mm[b_]])
                yield
                if lv < nlev - 1:
                    p.V("dve", "tensor_copy", MT[b_][0:nt, 0:nt], PS[bank][0:nt, 128:128 + nt], r=[PS[bank]], w=[MT[b_]])
                    yield
                p.V("pe", "matmul", PS[bank][0:nt, 256:256 + nt], Mm[b_][0:nt, 0:nt], Yt[0:nt, 0:nt], start=True, stop=True, r=[Mm[b_], Yt], w=[PS[bank]])
                yield
                p.V("dve", "tensor_tensor", Yt[0:nt, 0:nt], Yt[0:nt, 0:nt], PS[bank][0:nt, 256:256 + nt], ALU.add, r=[Yt, PS[bank]], w=[Yt])
                yield
            p.V("dve", "tensor_scalar", rv[0:nt, :], kvtok[0:nt, 4 + hd, :], gd[0:nt, hd:hd + 1], None, ALU.mult, r=[(kvtok, "v"), (gd, "b")], w=[rv])
            p.V("pool", "tensor_scalar", rk_[0:nt, :], kvtok[0:nt, hd, :], gd[0:nt, 16 + hd:17 + hd] if False else g2[0:nt, 0:1], None, ALU.mult, r=[(kvtok, "k"), (gd2, (hd, "be"))], w=[rk_])
            yield
            p.V("pe", "matmul", PS[A][:, 0:nt], rk_[0:nt, :], Yt[0:nt, 0:nt], start=True, stop=True, r=[rk_, Yt], w=[PS[A]])
            yield
            p.V("dve", "tensor_scalar", nwT[:, 0:nt], PS[A][:, 0:nt], -1.0, None, ALU.mult, r=[PS[A]], w=[nwT])
            yield
            p.V("pe", "matmul", PS[B][0:nt, 0:128], Yt[0:nt, 0:nt], rv[0:nt, :], start=True, stop=False, r=[Yt, rv], w=[PS[B]])
            p.V("pe", "matmul", PS[B][0:nt, 0:128], nwT[:, 0:nt], Sg[:, hd, :], start=False, stop=True, r=[nwT, (Sg, hd)], w=[PS[B]])
            yield
            p.V("act", "activation", ub[0:nt, :], PS[B][0:nt, 0:128], AF.Copy, r=[PS[B]], w=[ub])
            yield
            p.V("pool", "tensor_tensor", qdT[:, 0:nt], qT, erow[:, 0:nt], ALU.mult, r=[(qkn, "q"), erow], w=[qdT])
            yield
            p.V("pe", "matmul", PS[A][0:nt, 128:256], qdT[:, 0:nt], Sg[:, hd, :], start=True, stop=False, r=[qdT, (Sg, hd)], w=[PS[A]])
            p.V("pe", "matmul", PS[A][0:nt, 128:256], attnT[0:nt, 0:nt], ub[0:nt, :], start=False, stop=True, r=[attnT, ub], w=[PS[A]])
            yield
            p.V("act", "activation", g2[0:nt, 1:2], gd[0:nt, 12 + hd:13 + hd], AF.Exp, bias=g2[0:nt, 4:5], scale=-1.0,
                r=[(gd, "G"), (gd2, (hd, "gl"))], w=[(gd2, (hd, "kd"))])
            yield
            p.V("dve", "tensor_scalar", kd[0:nt, :], kvtok[0:nt, hd, :], g2[0:nt, 1:2], None, ALU.mult, r=[(kvtok, "k"), (gd2, (hd, "kd"))], w=[kd])
            yield
            p.V("pe", "matmul", PS[B][:, 128:256], kd[0:nt, :], ub[0:nt, :], start=True, stop=True, r=[kd, ub], w=[PS[B]])
            yield
            p.V("act", "activation", g2[:, 5:6], g2[:, 4:5], AF.Exp, r=[(gd2, (hd, "gl"))], w=[(gd2, (hd, "egl"))])
            yield
            p.V("dve", "scalar_tensor_tensor", Sg[:, hd, :], Sg[:, hd, :], g2[:, 5:6], PS[B][:, 128:256], ALU.mult, ALU.add,
                r=[(Sg, hd), (gd2, (hd, "egl")), PS[B]], w=[(Sg, hd)])
            yield
            p.V("act", "activation", osb[0:nt, :], PS[A][0:nt, 128:256], AF.Square, r=[PS[A]], w=[osb])
            yield
            p.V("dve", "reduce_sum", ssq[0:nt, hd:hd + 1], osb[0:nt, :], mybir.AxisListType.X, r=[osb], w=[(ssq, hd)])
            yield
            p.V("act", "activation", ssq[0:nt, hd:hd + 1], ssq[0:nt, hd:hd + 1], AF.Sqrt, bias=epsln[0:nt, 1:2], scale=1.0 / 128.0, r=[(ssq, hd), epsln], w=[(ssq, hd)])
            yield
            p.V("dve", "reciprocal", ssq[0:nt, hd:hd + 1], ssq[0:nt, hd:hd + 1], r=[(ssq, hd)], w=[(ssq, hd)])
            yield
            p.V("dve", "scalar_tensor_tensor", osb[0:nt, :], PS[A][0:nt, 128:256], ssq[0:nt, hd:hd + 1], nrmw[0:nt, :], ALU.mult, ALU.mult,
                r=[PS[A], (ssq, hd), nrmw], w=[osb])
            yield
            p.V("pool", "tensor_tensor", yB[0:nt, hd * 128:(hd + 1) * 128], osb[0:nt, :], zs[0:nt, hd * 128:(hd + 1) * 128], ALU.mult, r=[osb, zs], w=[(yB, hd)])
            yield

        load_x(tiles[0], xts[0])
        for tl in tiles:
            nt, ti, sq = tl["nt"], tl["ti"], tl["seq"]
            xt = xts[ti % 2]
            if ti + 1 < NT:
                load_x(tiles[ti + 1], xts[(ti + 1) % 2])
            if tl["first"]:
                if sq < NPS:
                    p.V("pool", "memset", Sg[:], 0.0, w=[Sg]); p.V("pool", "memset", ctx3[:], 0.0, w=[ctx3])
                else:
                    p.DM("sp", Sg[:], st_gdn.rearrange("h k v -> k h v"), r=[DR], w=[Sg])
                    p.DM("sp", ctx3[:], st_conv, r=[DR], w=[ctx3])
            p.DM("sp", mixT[:, 0:4, 0:nt], yas[ti, :, :, 0:nt], r=[(T_yas, ti)], w=[(mixT, "a")])
            p.V("act", "activation", xb[0:nt, :], xt[0:nt, :], AF.Copy, r=[xt], w=[xb])
            transpose_to(xT, xb, nt, 8, 0)
            p.V("pool", "tensor_copy", cb[:, :, 0:3], ctx3[:, :, :], r=[ctx3], w=[(cb, "c")])
            for g4 in range(3):
                bank = 1 + g4
                for b in range(4):
                    blk = g4 * 4 + b
                    for kc in range(8):
                        p.V("pe", "matmul", PS[bank][:, b * 128:b * 128 + nt], win[:, kc, blk * 128:(blk + 1) * 128],
                            xT[:, kc, 0:nt], start=(kc == 0), stop=(kc == 7), r=[win, xT], w=[PS[bank]])
                evac(cb[:, g4 * 4:g4 * 4 + 4, 3:3 + nt], PS[bank][:, :].rearrange("p (a b) -> p a b", a=4)[:, :, 0:nt],
                     r=[PS[bank]], w=[(cb, g4)])
            for kc in range(8):
                p.V("pe", "matmul", PS[4][0:nt, 0:512], xT[:, kc, 0:nt], win[:, kc, 1536:2048], start=(kc == 0), stop=(kc == 7),
                    r=[xT, win], w=[PS[4]])
            for kc in range(8):
                p.V("pe", "matmul", PS[5][0:nt, 0:8], xT[:, kc, 0:nt], win[:, kc, 2048:2056], start=(kc == 0), stop=(kc == 7),
                    r=[xT, win], w=[PS[5]])
            p.V("act", "activation", zs[0:nt, :], PS[4][0:nt, 0:512], AF.Silu, r=[PS[4]], w=[zs])
            p.V("dve", "tensor_copy", ztok[0:nt, 0:8], PS[5][0:nt, 0:8], r=[PS[5]], w=[ztok])
            for j in range(4):
                wj = wc[:, :, j:j + 1].to_broadcast([128, 12, nt])
                if j == 0:
                    p.V("dve", "tensor_tensor", cacc[:, :, 0:nt], cb[:, :, 0:nt], wj, ALU.mult, r=[cb, wc], w=[cacc])
                else:
                    p.V("pool", "tensor_tensor", ctmp[:, :, 0:nt], cb[:, :, j:j + nt], wj, ALU.mult, r=[cb, wc], w=[ctmp])
                    p.V("dve", "tensor_tensor", cacc[:, :, 0:nt], cacc[:, :, 0:nt], ctmp[:, :, 0:nt], ALU.add, r=[cacc, ctmp], w=[cacc])
            p.V("pool", "tensor_copy", ctx3[:, :, :], cb[:, :, nt:nt + 3], r=[cb], w=[ctx3])
            if tl["last"]:
                p.DM("sp", o_conv[sq], ctx3[:], r=[ctx3], w=[T_out])
            p.V("act", "activation", cacc[:, :, 0:nt], cacc[:, :, 0:nt], AF.Silu, r=[cacc], w=[cacc])
            p.V("act", "activation", sq8[:, :, 0:nt], cacc[:, 0:8, 0:nt], AF.Square, r=[cacc], w=[sq8])
            for hb in range(2):
                for b in range(4):
                    p.V("pe", "matmul", PS[2 + hb][:, b * 128:b * 128 + nt], ones_f[:, :], sq8[:, hb * 4 + b, 0:nt], start=True, stop=True,
                        r=[ones_f, sq8], w=[PS[2 + hb]])
            for hb in range(2):
                v = PS[2 + hb][:, :].rearrange("p (a b) -> p a b", a=4)[:, :, 0:nt]
                p.V("act", "activation", sq8[:, hb * 4:hb * 4 + 4, 0:nt], v, AF.Sqrt, bias=epsln[:, 1:2], r=[PS[2 + hb], epsln], w=[sq8])
            p.V("dve", "reciprocal", sq8[:, :, 0:nt], sq8[:, :, 0:nt], r=[sq8], w=[sq8])
            p.V("dve", "scalar_tensor_tensor", qkn[:, 0:4, 0:nt], cacc[:, 0:4, 0:nt], 128.0 ** -0.5, sq8[:, 0:4, 0:nt], ALU.mult, ALU.mult,
                r=[cacc, sq8], w=[(qkn, "q")])
            p.V("pool", "tensor_tensor", qkn[:, 4:8, 0:nt], cacc[:, 4:8, 0:nt], sq8[:, 4:8, 0:nt], ALU.mult, r=[cacc, sq8], w=[(qkn, "k")])
            for b in range(4):
                p.V("pe", "transpose", PS[2][0:nt, b * 128:(b + 1) * 128], qkn[:, 4 + b, 0:nt], identf[:, :], r=[(qkn, "k"), identf], w=[PS[2]])
                p.V("pe", "transpose", PS[3][0:nt, b * 128:(b + 1) * 128], cacc[:, 8 + b, 0:nt], identf[:, :], r=[cacc, identf], w=[PS[3]])
            evac(kvtok[0:nt, 0:4, :], PS[2][0:nt, :].rearrange("p (a b) -> p a b", a=4), r=[PS[2]], w=[(kvtok, "k")])
            evac(kvtok[0:nt, 4:8, :], PS[3][0:nt, :].rearrange("p (a b) -> p a b", a=4), r=[PS[3]], w=[(kvtok, "v")])
            p.V("act", "activation", gd[0:nt, 0:4], ztok[0:nt, 0:4], AF.Sigmoid, r=[ztok], w=[(gd, "b")])
            p.V("dve", "tensor_scalar", gd[0:nt, 4:8], gd[0:nt, 0:4], -1.0, None, ALU.mult, r=[(gd, "b")], w=[(gd, "nb")])
            p.V("dve", "tensor_tensor", gd[0:nt, 8:12], ztok[0:nt, 4:8], dtb[0:nt, :], ALU.add, r=[ztok, dtb], w=[(gd, "g")])
            p.V("act", "activation", gd[0:nt, 8:12], gd[0:nt, 8:12], AF.Exp, r=[(gd, "g")], w=[(gd, "g")])
            p.V("act", "activation", gd[0:nt, 8:12], gd[0:nt, 8:12], AF.Ln, bias=1.0, r=[(gd, "g")], w=[(gd, "g")])
            p.V("dve", "scalar_tensor_tensor", gd[0:nt, 8:12], gd[0:nt, 8:12], -1.0, alog[0:nt, :], ALU.mult, ALU.mult, r=[(gd, "g"), alog], w=[(gd, "g")])
            p.V("pe", "matmul", PS[0][0:nt, 0:4], uincl[0:nt, 0:nt], gd[0:nt, 8:12], start=True, stop=True, r=[uincl, (gd, "g")], w=[PS[0]])
            p.V("dve", "tensor_copy", gd[0:nt, 12:16], PS[0][0:nt, 0:4], r=[PS[0]], w=[(gd, "G")])
            p.V("act", "activation", gd2[0:nt, :, 2], gd[0:nt, 12:16], AF.Exp, r=[(gd, "G")], w=[(gd2, "e")])
            p.V("dve", "tensor_tensor", gd2[0:nt, :, 0], gd2[0:nt, :, 2], gd[0:nt, 0:4], ALU.mult, r=[(gd2, "e"), (gd, "b")],
                w=[(gd2, (0, "be")), (gd2, (1, "be")), (gd2, (2, "be")), (gd2, (3, "be"))])
            run_streams([head(hd, nt) for hd in range(4)])
            if tl["last"]:
                p.DM("sp", o_gdn[sq].rearrange("h k v -> k h v"), Sg[:], r=[Sg], w=[T_out])
            for b in range(4):
                p.V("pe", "transpose", psbf(2, 1024)[:, b * 128:b * 128 + nt], yB[0:nt, b * 128:(b + 1) * 128], identb[0:nt, 0:nt], r=[yB, identb], w=[PS[2]])
            evac(mixT[:, 4:8, 0:nt], psbf(2, 1024).rearrange("p (a b) -> p a b", a=8)[:, 0:4, 0:nt], r=[PS[2]], w=[(mixT, "b")])
            for half in range(2):
                for kc in range(8):
                    p.V("pe", "matmul", PS[2 + half][0:nt, :], mixT[:, kc, 0:nt], wout[:, kc, half * 512:(half + 1) * 512], start=(kc == 0), stop=(kc == 7),
                        r=[mixT, wout], w=[PS[2 + half]])
            phaseA_tail(0, tl, xt, (2, 3), lnw, rw, rb, work)
        p.release(m0)

    def phaseA1():
        m0 = p.mark()
        win = p.sb("win1", [128, 8, ODD_IN], BF16)
        load_w_bf16(win, w_in_odd, 8)
        wout = p.sb("wout1", [128, 16, D], BF16)
        load_w_bf16(wout, w_out_odd, 16)
        lnw, rw, rb = load_ln_router(1)
        retmask = p.sb("retmask", [128, 4, 128]); retqs = p.sb("retqs", [128, 4, 128]); retks = p.sb("retks", [128, 8])
        for b, nm in ((retmask, "retmask"), (retqs, "retqs"), (retks, "retks")):
            p.DM("sp", b[:], cst[nm], r=[DR], w=[b])
        Rf = p.sb("Rf", [128, 4, 2, 512])
        xt_one = p.sb("xt0", [128, D])
        xts = [xt_one, xt_one]
        xb = p.sb("xb", [128, D], BF16)
        xT = p.sb("xT", [128, 8, 128], BF16)
        cs = p.sb("cs", [128, 2, 128])
        qkT = p.sb("qkT", [128, 16, 128], BF16)
        qsT = p.sb("qsT", [128, 2, 128])
        rt1 = p.sb("rt1", [128, 128]); rt2 = p.sb("rt2", [128, 128])
        ktok = p.sb("ktok", [128, 8, 128], BF16)
        vtok = p.sb("vtok", [128, 2048], BF16); gsil = p.sb("gsil", [128, 2048], BF16)
        sT = p.sb("sT", [128, 128], BF16)
        og = vtok
        ogT = p.sb("ogT", [128, 16, 128], BF16)
        onrm = p.sb("onrm", [128, 512]); st6 = p.sb("st6", [128, 6]); st2 = p.sb("st2", [128, 4])
        work = alloc_tail_work(h=xt_one)

        sT_h = [sT, p.sb("sT1", [128, 128], BF16)] * 2
        qsT_h = [qsT, p.sb("qsT1", [128, 2, 128])] * 2
        onrm_h = [onrm, p.sb("onrm1", [128, 512])] * 2
        st6_h = [st6, p.sb("st61", [128, 6])] * 2
        st2_h = [st2, p.sb("st21", [128, 4])] * 2
        print("A1 sbuf words", p.top)

        def rhead(h, nt, Lc):
            A, B = 2 * h, 2 * h + 1
            sT, qsT, onrm, st6, st2 = sT_h[h], qsT_h[h], onrm_h[h], st6_h[h], st2_h[h]
            for dc in range(2):
                p.V("pe", "matmul", PS[A][0:nt, 0:nt], qkT[:, 8 + 2 * h + dc, 0:nt], qkT[:, 2 * h + dc, 0:nt], start=(dc == 0), stop=(dc == 1),
                    r=[(qkT, 8 + 2 * h + dc), (qkT, 2 * h + dc)], w=[PS[A]])
            yield
            p.V("dve", "scalar_tensor_tensor", sT[0:nt, 0:nt], PS[A][0:nt, 0:nt], 256.0 ** -0.5, retmask[0:nt, h, 0:nt], ALU.mult, ALU.mult,
                r=[PS[A], retmask], w=[sT])
            yield
            for dc in range(2):
                p.V("pool", "tensor_tensor", qsT[:, dc, 0:nt], qkT[:, 2 * h + dc, 0:nt], retqs[:, h, 0:nt], ALU.mult, r=[(qkT, 2 * h + dc), retqs], w=[(qsT, dc)])
                yield
            p.V("pe", "matmul", PS[B][0:nt, :], sT[0:nt, 0:nt], vtok[0:nt, h * 512:(h + 1) * 512], start=True, stop=False, r=[sT, (vtok, h)], w=[PS[B]])
            for dc in range(2):
                p.V("pe", "matmul", PS[B][0:nt, :], qsT[:, dc, 0:nt], Rf[:, h, dc, :], start=False, stop=(dc == 1), r=[(qsT, dc), (Rf, (h, dc))], w=[PS[B]])
            yield
            cdec = cst_host["retcdec"][Lc][h]
            for dc in range(2):
                p.V("pe", "matmul", PS[A][:, :], ktok[0:nt, 2 * h + dc, :], vtok[0:nt, h * 512:(h + 1) * 512], start=True, stop=True,
                    r=[(ktok, h), (vtok, h)], w=[PS[A]])
                yield
                p.V("dve", "scalar_tensor_tensor", Rf[:, h, dc, :], Rf[:, h, dc, :], cdec, PS[A][:, :], ALU.mult, ALU.add, r=[(Rf, (h, dc)), PS[A]], w=[(Rf, (h, dc))])
                yield
            p.V("dve", "bn_stats", st6[0:nt, :], PS[B][0:nt, :], r=[PS[B]], w=[st6])
            yield
            p.V("dve", "bn_aggr", st2[0:nt, 0:2], st6[0:nt, :], r=[st6], w=[st2])
            yield
            p.V("act", "activation", st2[0:nt, 2:3], st2[0:nt, 1:2], AF.Sqrt, bias=epsln[0:nt, 0:1], r=[st2, epsln], w=[(st2, "s")])
            yield
            p.V("dve", "reciprocal", st2[0:nt, 3:4], st2[0:nt, 2:3], r=[(st2, "s")], w=[(st2, "r")])
            yield
            p.V("dve", "tensor_scalar", onrm[0:nt, :], PS[B][0:nt, :], st2[0:nt, 0:1], st2[0:nt, 3:4], ALU.subtract, ALU.mult, r=[PS[B], st2, (st2, "r")], w=[onrm])
            yield
            p.V("pool", "tensor_tensor", og[0:nt, h * 512:(h + 1) * 512], onrm[0:nt, :], gsil[0:nt, h * 512:(h + 1) * 512], ALU.mult, r=[onrm, (gsil, h)], w=[(vtok, h)])
            yield

        def load_x(tl, buf):
            if tl["nt"] < 128:
                p.V("pool", "memset", buf[:], 0.0, w=[buf])
            p.DM("sp", buf[0:tl["nt"], :], x3s[tl["ti"] * 128:tl["ti"] * 128 + tl["nt"], :], r=[(T_x3s, tl["ti"])], w=[buf])

        for tl in tiles:
            nt, ti, sq = tl["nt"], tl["ti"], tl["seq"]
            Lc = 0 if nt == 128 else 1
            xt = xts[ti % 2]
            load_x(tl, xt)
            if tl["first"]:
                if sq < NPS:
                    p.V("pool", "memset", Rf[:], 0.0, w=[Rf])
                else:
                    for h in range(4):
                        for par in range(2):
                            p.DM("sp", Rf[:, h, par, :], st_ret[h].rearrange("(q two) v -> q two v", two=2)[:, par, :], r=[DR], w=[(Rf, (h, par))])
            p.DM("sp", cs[:, 0, 0:nt], cst["rcos"][:, tl["pos0"]:tl["pos0"] + nt], r=[DR], w=[(cs, 0)])
            p.DM("sp", cs[:, 1, 0:nt], cst["rsin"][:, tl["pos0"]:tl["pos0"] + nt], r=[DR], w=[(cs, 1)])
            p.V("act", "activation", xb[0:nt, :], xt[0:nt, :], AF.Copy, r=[xt], w=[xb])
            transpose_to(xT, xb, nt, 8, 0)
            for qk in range(2):
                for h in range(4):
                    bank = 1 + ((qk * 4 + h) % 2)
                    for par in range(2):
                        col0 = qk * 1024 + h * 256 + par * 128
                        for kc in range(8):
                            p.V("pe", "matmul", PS[bank][:, par * 128:par * 128 + nt], win[:, kc, col0:col0 + 128], xT[:, kc, 0:nt],
                                start=(kc == 0), stop=(kc == 7), r=[win, xT], w=[PS[bank]])
                    x0 = PS[bank][:, 0:nt]
                    x1_ = PS[bank][:, 128:128 + nt]
                    blk = qk * 8 + h * 2
                    p.V("dve", "tensor_tensor", rt1[:, 0:nt], x0, cs[:, 0, 0:nt], ALU.mult, r=[PS[bank], cs], w=[rt1])
                    p.V("dve", "tensor_tensor", rt2[:, 0:nt], x1_, cs[:, 1, 0:nt], ALU.mult, r=[PS[bank], cs], w=[rt2])
                    p.V("pool", "tensor_tensor", qkT[:, blk, 0:nt], rt1[:, 0:nt], rt2[:, 0:nt], ALU.subtract, r=[rt1, rt2], w=[(qkT, blk)])
                    p.V("dve", "tensor_tensor", rt1[:, 0:nt], x0, cs[:, 1, 0:nt], ALU.mult, r=[PS[bank], cs, (qkT, blk)], w=[rt1])
                    p.V("dve", "tensor_tensor", rt2[:, 0:nt], x1_, cs[:, 0, 0:nt], ALU.mult, r=[PS[bank], cs, (qkT, blk)], w=[rt2])
                    p.V("pool", "tensor_tensor", qkT[:, blk + 1, 0:nt], rt1[:, 0:nt], rt2[:, 0:nt], ALU.add, r=[rt1, rt2], w=[(qkT, blk + 1)])
            for cg in range(8):
                bank = 3 + (cg % 2)
                for kc in range(8):
                    p.V("pe", "matmul", PS[bank][0:nt, :], xT[:, kc, 0:nt], win[:, kc, 2048 + cg * 512:2048 + (cg + 1) * 512], start=(kc == 0), stop=(kc == 7),
                        r=[xT, win], w=[PS[bank]])
                if cg < 4:
                    p.V("dve", "tensor_copy", vtok[0:nt, cg * 512:(cg + 1) * 512], PS[bank][0:nt, :], r=[PS[bank]], w=[(vtok, cg)])
                else:
                    p.V("act", "activation", gsil[0:nt, (cg - 4) * 512:(cg - 3) * 512], PS[bank][0:nt, :], AF.Silu, r=[PS[bank]], w=[(gsil, cg - 4)])
            for b in range(8):
                p.V("pe", "transpose", psbf(5, 1024)[0:nt, b * 128:(b + 1) * 128], qkT[:, 8 + b, 0:nt], identb[:, :], r=[(qkT, 8 + b), identb], w=[PS[5]])
            for h in range(4):
                p.V("dve", "tensor_scalar", ktok[0:nt, 2 * h:2 * h + 2, :], psbf(5, 1024).rearrange("p (a b) -> p a b", a=8)[0:nt, 2 * h:2 * h + 2, :],
                    retks[0:nt, Lc * 4 + h:Lc * 4 + h + 1], None, ALU.mult, r=[PS[5], retks], w=[(ktok, h)])
            def pair(x):
                yield from rhead(x, nt, Lc)
                yield from rhead(x + 2, nt, Lc)

            run_streams([pair(0), pair(1)])
            if tl["last"]:
                for h in range(4):
                    for par in range(2):
                        p.DM("sp", o_ret[sq, h].rearrange("(q two) v -> q two v", two=2)[:, par, :], Rf[:, h, par, :], r=[Rf], w=[T_out])
            transpose_to(ogT, og, nt, 16, 5)
            for half in range(2):
                for kc in range(16):
                    p.V("pe", "matmul", PS[2 + half][0:nt, :], ogT[:, kc, 0:nt], wout[:, kc, half * 512:(half + 1) * 512], start=(kc == 0), stop=(kc == 15),
                        r=[ogT, wout], w=[PS[2 + half]])
            phaseA_tail(1, tl, xt, (2, 3), lnw, rw, rb, work)
        p.release(m0)

    def phaseM(li):
        m0 = p.mark()
        w1b = [p.sb("w1b0", [128, 8, 2 * D], BF16), p.sb("w1b1", [128, 8, 2 * D], BF16)]
        w2b = [p.sb("w2b0", [128, 8, D], BF16), p.sb("w2b1", [128, 8, D], BF16)]
        b1t = [p.sb("b1t0", [128, 16]), p.sb("b1t1", [128, 16])]
        b2f = [p.sb("b2f0", [1, D]), p.sb("b2f1", [1, D])]
        b2b = [p.sb("b2b0", [1, D], BF16), p.sb("b2b1", [1, D], BF16)]
        xg = [p.sb("xg0", [128, CT, D], BF16), p.sb("xg1", [128, CT, D], BF16)]
        xgT = [p.sb("xgT0", [128, 8, C], BF16), p.sb("xgT1", [128, 8, C], BF16)]
        glu2 = [p.sb("glu0", [128, C]), p.sb("glu1", [128, C])]
        sig2 = [p.sb("sig0", [128, C]), p.sb("sig1", [128, C])]
        lin2 = [p.sb("lin0", [128, C]), p.sb("lin1", [128, C])]
        actT = p.sb("actT", [128, 8, C], BF16)
        yo = [p.sb("yo0", [128, D]), p.sb("yo1", [128, D])]

        def load_w(e):
            s = e % 2
            load_w_bf16(w1b[s], moe_w1[li, e], 8)
            load_w_bf16(w2b[s], moe_w2[li, e], 8)
            p.DM("sp", b1t[s][:], moe_b1[li, e], r=[DR], w=[b1t[s]])
            p.DM("sp", b2f[s][:], moe_b2[li, e:e + 1, :], r=[DR], w=[b2f[s]])

        def load_xg(e):
            s = e % 2
            p.DM("sp", xg[s][:], xs[e * C:(e + 1) * C, :].rearrange("(ct q) d -> q ct d", q=128), r=[(T_xs, "*")], w=[xg[s], (T_xs, e)])

        def transposes(e):
            s = e % 2
            p.V("act", "activation", b2b[s][:], b2f[s][:], AF.Copy, r=[b2f[s]], w=[b2b[s]])
            for ct in range(CT):
                bank = 4 + (ct % 2)
                for kc in range(8):
                    p.V("pe", "transpose", psbf(bank, 1024)[:, kc * 128:(kc + 1) * 128], xg[s][:, ct, kc * 128:(kc + 1) * 128], identb[:, :],
                        r=[xg[s], identb], w=[PS[bank]])
                evac(xgT[s][:, :, ct * 128:(ct + 1) * 128], psbf(bank, 1024).rearrange("p (a b) -> p a b", a=8), r=[PS[bank]], w=[(xgT[s], ct)])

        load_w(0)
        load_xg(0)
        transposes(0)
        for e in range(NE):
            s = e % 2
            if e + 1 < NE:
                load_w(e + 1)
                load_xg(e + 1)
            for i in range(8):
                glu, sig, lin = glu2[i % 2], sig2[i % 2], lin2[i % 2]
                for part in range(2):
                    fc = i + part * 8
                    banks = [(0, 1), (2, 3)][(i * 2 + part) % 2]
                    for gi, (ca, cb_) in enumerate(cgs):
                        for kc in range(8):
                            p.V("pe", "matmul", PS[banks[gi]][:, 0:cb_ - ca], w1b[s][:, kc, fc * 128:(fc + 1) * 128], xgT[s][:, kc, ca:cb_],
                                start=(kc == 0), stop=(kc == 7), r=[w1b[s], xgT[s]], w=[PS[banks[gi]]])
                    for gi, (ca, cb_) in enumerate(cgs):
                        src = PS[banks[gi]][:, 0:cb_ - ca]
                        if part == 0:
                            p.V("dve", "tensor_scalar", glu[:, ca:cb_], src, b1t[s][:, fc:fc + 1], 7.0, ALU.add, ALU.min, r=[PS[banks[gi]], b1t[s]], w=[(glu, gi)])
                        else:
                            p.V("dve", "tensor_scalar", lin[:, ca:cb_], src, b1t[s][:, fc:fc + 1], 7.0, ALU.add, ALU.min, r=[PS[banks[gi]], b1t[s]], w=[(lin, gi)])
                    if part == 0:
                        p.V("act", "activation", sig[:, :], glu[:, :], AF.Sigmoid, scale=1.702, r=[glu], w=[sig])
                        p.V("pool", "tensor_tensor", glu[:, :], glu[:, :], sig[:, :], ALU.mult, r=[glu, sig], w=[glu])
                    else:
                        p.V("dve", "tensor_scalar", lin[:, :], lin[:, :], -7.0, 1.0, ALU.max, ALU.add, r=[lin], w=[lin])
                        p.V("pool", "tensor_tensor", actT[:, i, :], glu[:, :], lin[:, :], ALU.mult, r=[glu, lin], w=[(actT, i)])
            if e + 1 < NE:
                transposes(e + 1)
            for ct in range(CT):
                yb = yo[ct % 2]
                for half in range(2):
                    bank = 4 + (ct % 2) * 2 + half
                    for fc in range(8):
                        p.V("pe", "matmul", PS[bank][:, :], actT[:, fc, ct * 128:(ct + 1) * 128], w2b[s][:, fc, half * 512:(half + 1) * 512],
                            start=(fc == 0), stop=False, r=[actT, w2b[s]], w=[PS[bank]])
                    p.V("pe", "matmul", PS[bank][:, :], ones_b[0:1, :], b2b[s][0:1, half * 512:(half + 1) * 512], start=False, stop=True,
                        r=[ones_b, b2b[s]], w=[PS[bank]])
                    evac(yb[:, half * 512:(half + 1) * 512], PS[bank][:, :], r=[PS[bank]], w=[(yb, half)])
                p.DM("act", ys[e * C + ct * 128:e * C + (ct + 1) * 128, :], yb[:, :], r=[yb], w=[(T_ys, (e, ct))])
        p.release(m0)

    def phaseC(li):
        m0 = p.mark()
        wg = p.sb("wg", [128, 8, D], BF16)
        load_w_bf16(wg, ple_gw[li], 8)
        wp = p.sb("wp", [128, 2, D], BF16)
        load_w_bf16(wp, ple_w[li], 2)
        g2 = p.sb("ln2g", [128, D]); b2 = p.sb("ln2b", [128, D])
        bcast_load(g2, ln2_g[li:li + 1, :]); bcast_load(b2, ln2_b[li:li + 1, :])

        def stream(sx, tlist):
            n_ = lambda s_: "%s_%d" % (s_, sx)
            rows = [[p.sb(n_("row%d%d" % (b, k)), [128, D]) for k in range(4)] for b in range(2)]
            x1t = [p.sb(n_("x1t0"), [128, D]), p.sb(n_("x1t1"), [128, D])]
            pt = [p.sb(n_("pt0"), [128, 256]), p.sb(n_("pt1"), [128, 256])]
            ff = p.sb(n_("ff"), [128, D]); x2 = p.sb(n_("x2"), [128, D]); x2b = p.sb(n_("x2b"), [128, D], BF16)
            x2T = p.sb(n_("x2T"), [128, 8, 128], BF16)
            pb = p.sb(n_("pb"), [128, 256], BF16); pT = p.sb(n_("pT"), [128, 2, 128], BF16)
            gt = p.sb(n_("gt"), [128, D]); x3 = p.sb(n_("x3"), [128, D])
            scr6 = p.sb(n_("scr6"), [128, 2, 6]); scr2 = p.sb(n_("scr2"), [128, 4])
            B0 = 4 * sx

            def loads(j):
                tl = tlist[j]
                b = j % 2
                ti, nt = tl["ti"], tl["nt"]
                for k in range(4):
                    p.dma("pool", lambda e, k=k, ti=ti, b=b: e.indirect_dma_start(
                        out=rows[b][k][:, :], out_offset=None, in_=ys[:, :],
                        in_offset=bass.IndirectOffsetOnAxis(ap=slots_all[:, ti, k:k + 1], axis=0)),
                        r=[(T_ys, "*"), (slots_all, ti)], w=[rows[b][k]])
                p.DM("sp", x1t[b][0:nt, :], x1s[ti * 128:ti * 128 + nt, :], r=[(T_x1s, ti)], w=[x1t[b]])
                p.DM("sp", pt[b][0:nt, :], pin[li, tl["row0"]:tl["row0"] + nt, :], r=[DR], w=[pt[b]])

            if tlist:
                loads(0)
            for j, tl in enumerate(tlist):
                nt, ti = tl["nt"], tl["ti"]
                b = j % 2
                if j + 1 < len(tlist):
                    loads(j + 1)
                yield
                p.V("dve", "tensor_scalar", ff[0:nt, :], rows[b][0][0:nt, :], gates_all[0:nt, ti, 0:1], None, ALU.mult, r=[rows[b][0], (gates_all, ti)], w=[ff])
                yield
                for k in range(1, 4):
                    p.V("dve", "scalar_tensor_tensor", ff[0:nt, :], rows[b][k][0:nt, :], gates_all[0:nt, ti, k:k + 1], ff[0:nt, :], ALU.mult, ALU.add,
                        r=[rows[b][k], (gates_all, ti), ff], w=[ff])
                    yield
                p.V("dve", "scalar_tensor_tensor", ff[0:nt, :], x1t[b][0:nt, :], ALPHA, ff[0:nt, :], ALU.mult, ALU.add, r=[x1t[b], ff], w=[ff])
                yield
                layernorm("dve", ff, nt, g2, b2, x2, scr6, scr2)
                yield
                p.V("act", "activation", x2b[0:nt, :], x2[0:nt, :], AF.Copy, r=[x2], w=[x2b])
                yield
                transpose_to(x2T, x2b, nt, 8, B0)
                yield
                p.V("act", "activation", pb[0:nt, :], pt[b][0:nt, :], AF.Copy, r=[pt[b]], w=[pb])
                transpose_to(pT, pb, nt, 2, B0 + 1)
                yield
                for half in range(2):
                    for kc in range(2):
                        p.V("pe", "matmul", PS[B0 + 2 + half][0:nt, :], pT[:, kc, 0:nt], wp[:, kc, half * 512:(half + 1) * 512], start=(kc == 0), stop=(kc == 1),
                            r=[pT, wp], w=[PS[B0 + 2 + half]])
                yield
                for half in range(2):
                    for kc in range(8):
                        p.V("pe", "matmul", PS[B0 + half][0:nt, :], x2T[:, kc, 0:nt], wg[:, kc, half * 512:(half + 1) * 512], start=(kc == 0), stop=(kc == 7),
                            r=[x2T, wg], w=[PS[B0 + half]])
                    yield
                    hs = slice(half * 512, (half + 1) * 512)
                    p.V("act", "activation", gt[0:nt, hs], PS[B0 + half][0:nt, :], AF.Sigmoid, r=[PS[B0 + half]], w=[(gt, half)])
                    yield
                    p.V("dve", "tensor_tensor", gt[0:nt, hs], gt[0:nt, hs], PS[B0 + 2 + half][0:nt, :], ALU.mult, r=[(gt, half), PS[B0 + 2 + half]], w=[(gt, half)])
                    yield
                    p.V("pool", "tensor_tensor", x3[0:nt, hs], gt[0:nt, hs], x2[0:nt, hs], ALU.add, r=[(gt, half), x2], w=[(x3, half)])
                    yield
                if li == 0:
                    p.DM("sp", x3s[ti * 128:ti * 128 + nt, :], x3[0:nt, :], r=[x3], w=[(T_x3s, ti)])
                    if "x3_0" in dbg_out:
                        p.DM("sp", dbg_out["x3_0"][tl["row0"]:tl["row0"] + nt, :], x3[0:nt, :], r=[x3], w=[T_out])
                else:
                    p.DM("sp", y_out[tl["row0"]:tl["row0"] + nt, :], x3[0:nt, :], r=[x3], w=[T_out])
                yield

        run_streams([stream(0, tiles[0::2]), stream(1, tiles[1::2])])
        p.release(m0)

    cst_host = host_consts()
    for st in stages:
        if st == "A0":
            if os.environ.get("KOLDA0"):
                phaseA0()
            else:
                phaseS0()
                phaseG0()
        elif st == "A1":
            phaseA1()
        elif st[0] == "M":
            phaseM(int(st[1]))
        elif st[0] == "C":
            phaseC(int(st[1]))
    p.finalize()
    return nc


def prep_shared(inp):
    f = lambda a: np.ascontiguousarray(np.asarray(a, dtype=np.float32))
    sh = {}
    sh["w_in_even"] = f(inp["w_in_even"][0])
    qb = lambda v, nb: np.ascontiguousarray(np.asarray(v, dtype=np.float32).reshape(nb, 128).T)
    sh["s5_are"] = qb(inp["s5_a_re"][0].reshape(-1), 16)
    sh["s5_aim"] = qb(inp["s5_a_im"][0].reshape(-1), 16)
    sh["s5_ldt"] = qb(np.repeat(np.asarray(inp["s5_log_dt"][0]), 64), 16)
    bst = np.zeros((2, 16, 128, 128), np.float32)
    cstt = np.zeros((2, 16, 128, 128), np.float32)
    for ri, (bsrc, csrc) in enumerate(((inp["s5_b_re"][0], inp["s5_c_re"][0]), (inp["s5_b_im"][0], inp["s5_c_im"][0]))):
        bsrc = np.asarray(bsrc)
        csrc = np.asarray(csrc)
        for g in range(32):
            blk = g // 2
            m0 = (g % 2) * 64
            k0 = (g % 8) * 16
            bst[ri, blk, k0:k0 + 16, m0:m0 + 64] = bsrc[g].T
            cstt[ri, blk, m0:m0 + 64, k0:k0 + 16] = csrc[g].T
    sh["s5_bst"] = bst
    sh["s5_cst"] = cstt
    sh["s5_d"] = qb(inp["s5_d"][0], 4)
    sh["s5_wglu"] = f(inp["s5_w_glu"][0])
    sh["s5_bglu"] = qb(inp["s5_b_glu"][0], 4)
    sh["gdn_convw"] = np.ascontiguousarray(np.asarray(inp["gdn_conv_w"][0], dtype=np.float32).reshape(4, 12, 128).transpose(2, 1, 0))
    sh["gdn_alog"] = f(inp["gdn_a_log"][0].reshape(1, 4))
    sh["gdn_dtb"] = f(inp["gdn_dt_bias"][0].reshape(1, 4))
    sh["gdn_normw"] = f(inp["gdn_norm_w"][0].reshape(1, 128))
    sh["w_out_even"] = f(inp["w_out_even"][0])
    wio = np.asarray(inp["w_in_odd"][0], dtype=np.float32)
    perm = np.arange(ODD_IN)
    for qk in range(2):
        for h in range(4):
            base = qk * 1024 + h * 256
            perm[base:base + 128] = base + np.arange(0, 256, 2)
            perm[base + 128:base + 256] = base + np.arange(1, 256, 2)
    sh["w_in_odd"] = np.ascontiguousarray(wio[:, perm])
    sh["w_out_odd"] = f(inp["w_out_odd"][0])
    for k in ("ln1_g", "ln1_b", "ln2_g", "ln2_b", "router_w", "router_b", "moe_w1", "moe_w2", "moe_b2", "ple_w"):
        sh[k] = f(inp[k])
    sh["moe_b1"] = np.ascontiguousarray(np.asarray(inp["moe_b1"], dtype=np.float32).reshape(2, NE, 16, 128).transpose(0, 1, 3, 2))
    sh["ple_gw"] = f(inp["ple_gate_w"])
    for k, v in host_consts().items():
        if k in CONST_SHAPES:
            sh["c_" + k] = np.ascontiguousarray(v.astype(np.float32)).reshape(CONST_SHAPES[k])
    return sh


def core_inputs(inp, sh, prompt_ids, sample_id, L):
    m = dict(sh)
    xp = [np.asarray(inp["x_prompt"][i][:L], dtype=np.float32) for i in prompt_ids]
    m["xin"] = np.ascontiguousarray(np.concatenate(xp + [np.asarray(inp["x_sample"][sample_id], dtype=np.float32)], axis=0))
    pp = [np.asarray(inp["p_prompt"][:, i, :L], dtype=np.float32) for i in prompt_ids]
    m["pin"] = np.ascontiguousarray(np.concatenate(pp + [np.asarray(inp["p_sample"][:, sample_id], dtype=np.float32)], axis=1))
    m["st_s5re"] = np.ascontiguousarray(np.asarray(inp["state_s5_re"][0, sample_id], dtype=np.float32).reshape(16, 128).T)
    m["st_s5im"] = np.ascontiguousarray(np.asarray(inp["state_s5_im"][0, sample_id], dtype=np.float32).reshape(16, 128).T)
    m["st_gdn"] = np.ascontiguousarray(np.asarray(inp["state_gdn"][0, sample_id], dtype=np.float32))
    m["st_conv"] = np.ascontiguousarray(np.asarray(inp["state_gdn_conv"][0, sample_id], dtype=np.float32).reshape(3, 12, 128).transpose(2, 1, 0))
    m["st_ret"] = np.ascontiguousarray(np.asarray(inp["state_ret"][0, sample_id], dtype=np.float32))
    return m


def unperm(k, a):
    a = np.asarray(a)
    if k in ("o_s5re", "o_s5im"):
        return a.reshape(128, 16).T.reshape(32, 64)
    if k == "o_conv":
        return a.reshape(128, 12, 3).transpose(2, 1, 0).reshape(3, 1536)
    return a


_CACHE = {}


def kernel(**inputs):
    NPS, L, C = 2, 2048, 768
    key = (NPS, L, C)
    if key not in _CACHE:
        _CACHE[key] = build(NPS, L, C)
    nc = _CACHE[key]
    sh = prep_shared(inputs)
    in_maps = [core_inputs(inputs, sh, [2 * c, 2 * c + 1], c, L) for c in range(8)]
    res = run_bass_kernel_spmd(nc, in_maps, core_ids=list(range(8)))
    R = res.results
    B, DB = 16, 8
    y_p = np.zeros((B, L, D), np.float32)
    y_s = np.zeros((DB, 16, D), np.float32)
    outs = {k: (np.zeros((1, B) + shp, np.float32), np.zeros((1, DB) + shp, np.float32))
            for k, shp in (("o_s5re", (32, 64)), ("o_s5im", (32, 64)), ("o_gdn", (4, 128, 128)), ("o_conv", (3, 1536)), ("o_ret", (4, 256, 512)))}
    for c in range(8):
        r = R[c]
        y = r["y_out"]
        for j in range(NPS):
            y_p[2 * c + j] = y[j * L:(j + 1) * L]
        y_s[c] = y[NPS * L:NPS * L + 16]
        for k, (po, so) in outs.items():
            a = r[k]
            for j in range(NPS):
                po[0, 2 * c + j] = unperm(k, a[j]).reshape(po.shape[2:])
            so[0, c] = unperm(k, a[NPS]).reshape(so.shape[2:])
    return (y_p, y_s, outs["o_s5re"][0], outs["o_s5im"][0], outs["o_gdn"][0], outs["o_conv"][0], outs["o_ret"][0],
            outs["o_s5re"][1], outs["o_s5im"][1], outs["o_gdn"][1], outs["o_conv"][1], outs["o_ret"][1])
```
